# Optimizing a Trainium2 kernel written in Bass

```python
import jax
import jax.numpy as jnp
from jax import lax
import numpy as np

D_MODEL = 2048
BATCH = 4
SEQ = 4096
DEPTH = 2

GRID_W = 64
CTX_LEN = 256
EPS = 1e-6
NEG_INF = -1e30

CONV_CH = D_MODEL // 4
CONV_K = 31
NA_HEADS = 16
NA_HD = D_MODEL // 32
NA_DIM = NA_HEADS * NA_HD
NA_KH_MAX = 8
NA_KW = 16
NA_QB = 16
NA_KB = 32
HG_HEADS = 4
HG_DK = D_MODEL // 16
HG_DV = HG_DK
HG_DIM = HG_HEADS * HG_DK
HG_CHUNK = 64
MIX_DIM = CONV_CH + NA_DIM + HG_DIM
OFF_NA = 2 * CONV_CH
OFF_HG = OFF_NA + 3 * NA_DIM
IN_DIM = OFF_HG + 5 * HG_DIM
N_GROUPS = 4
EXP_PER_GROUP = 8
N_EXPERTS = N_GROUPS * EXP_PER_GROUP
TOP_K = 2
D_EXPERT = D_MODEL // 4
MOE_BLOCK = 128

kernel_name = "hybrid_conv_natten_hgrn2_hmoe_dit"


def _rmsnorm(x, g):
    xf = x.astype(jnp.float32)
    y = xf * lax.rsqrt(jnp.mean(xf * xf, axis=-1, keepdims=True) + EPS)
    return (y * g.astype(jnp.float32)).astype(x.dtype)


def _layernorm(x, g, b):
    xf = x.astype(jnp.float32)
    mu = jnp.mean(xf, axis=-1, keepdims=True)
    var = jnp.mean(jnp.square(xf - mu), axis=-1, keepdims=True)
    y = (xf - mu) * lax.rsqrt(var + EPS) * g.astype(jnp.float32) + b.astype(jnp.float32)
    return y.astype(x.dtype)


def _conv_module(u, w_dw, b_dw, ln_g, ln_b):
    a, gate = jnp.split(u, 2, axis=-1)
    h = a * jax.nn.sigmoid(gate)
    h = lax.conv_general_dilated(
        h, w_dw[:, None, :].astype(h.dtype), window_strides=(1,),
        padding=[(CONV_K // 2, CONV_K // 2)],
        dimension_numbers=("NWC", "WIO", "NWC"), feature_group_count=CONV_CH)
    h = h + b_dw.astype(h.dtype)
    return jax.nn.silu(_layernorm(h, ln_g, ln_b))


def _na_col_tables():
    nb = GRID_W // NA_QB
    qcol = np.arange(GRID_W).reshape(nb, NA_QB)
    kstart = np.clip(np.arange(nb) * NA_QB - NA_KW // 2, 0, GRID_W - NA_KB)
    kcol = kstart[:, None] + np.arange(NA_KB)[None, :]
    wstart = np.clip(qcol - NA_KW // 2, 0, GRID_W - NA_KW)
    dcol = kcol[:, None, :] - qcol[:, :, None]
    ok = (kcol[:, None, :] >= wstart[:, :, None]) & (kcol[:, None, :] < wstart[:, :, None] + NA_KW)
    dcol_idx = np.clip(dcol + NA_KW - 1, 0, 2 * NA_KW - 2)
    return kcol, dcol_idx, ok


def _na_latent(q, k, v, kc, vc, rpb):
    B, T, H, hd = q.shape
    rows = T // GRID_W
    kh = min(NA_KH_MAX, rows)
    nb = GRID_W // NA_QB
    kcol, dcol_idx, ok = _na_col_tables()
    scale = hd ** -0.5
    col_bias = jnp.where(ok, rpb.astype(jnp.float32)[:, :, dcol_idx], NEG_INF)
    qg = jnp.moveaxis(q.reshape(B, rows, nb, NA_QB, H, hd), 1, 0)
    kg = k.reshape(B, rows, GRID_W, H, hd)
    vg = v.reshape(B, rows, GRID_W, H, hd)

    def row_block(args):
        r, q_r = args
        sr = jnp.clip(r - kh // 2, 0, rows - kh)
        k_b = lax.dynamic_slice_in_dim(kg, sr, kh, axis=1)[:, :, kcol]
        v_b = lax.dynamic_slice_in_dim(vg, sr, kh, axis=1)[:, :, kcol]
        drow = sr + jnp.arange(kh) - r
        bias = jnp.take(col_bias, drow + NA_KH_MAX - 1, axis=1).transpose(0, 2, 3, 1, 4)
        s_win = jnp.einsum("bjqhd,brjkhd->bhjqrk", q_r, k_b).astype(jnp.float32) * scale + bias[None]
        s_ctx = jnp.einsum("bjqhd,bchd->bhjqc", q_r, kc).astype(jnp.float32) * scale
        s = jnp.concatenate([s_win.reshape(B, H, nb, NA_QB, kh * NA_KB), s_ctx], axis=-1)
        p = jax.nn.softmax(s, axis=-1).astype(v.dtype)
        p_win = p[..., : kh * NA_KB].reshape(B, H, nb, NA_QB, kh, NA_KB)
        p_ctx = p[..., kh * NA_KB:]
        return (jnp.einsum("bhjqrk,brjkhd->bjqhd", p_win, v_b)
                + jnp.einsum("bhjqc,bchd->bjqhd", p_ctx, vc))

    o = lax.map(row_block, (jnp.arange(rows), qg))
    return jnp.moveaxis(o, 0, 1).reshape(B, T, H * hd)


def _ctx_attn(q, k, v):
    B, L, H, hd = q.shape
    s = jnp.einsum("bqhd,bkhd->bhqk", q, k).astype(jnp.float32) * hd ** -0.5
    p = jax.nn.softmax(s, axis=-1).astype(v.dtype)
    return jnp.einsum("bhqk,bkhd->bqhd", p, v).reshape(B, L, H * hd)


def _heads(a):
    B, T, _ = a.shape
    return a.astype(jnp.float32).reshape(B, T, HG_HEADS, -1).transpose(0, 2, 1, 3)


def _hgrn_gates(z, lb):
    logf = jnp.logaddexp(jnp.log(lb), jnp.log1p(-lb) + jax.nn.log_sigmoid(z))
    return logf, -jnp.expm1(logf)


def _hgrn_scan(q, k, v, logf, s0):
    B, H, T, DK = q.shape
    DV = v.shape[-1]
    n = T // HG_CHUNK

    def chunks(a):
        return a.reshape(B, H, n, HG_CHUNK, a.shape[-1]).transpose(2, 0, 1, 3, 4)

    tri = jnp.tril(jnp.ones((HG_CHUNK, HG_CHUNK), dtype=bool))[:, :, None]

    def step(S, inp):
        qc, kc, vc, lf = inp
        b = jnp.cumsum(lf, axis=2)
        diff = b[:, :, :, None, :] - b[:, :, None, :, :]
        decay = jnp.exp(jnp.where(tri, diff, -jnp.inf))
        att = jnp.einsum("bhtk,bhsk,bhtsk->bhts", qc, kc, decay)
        o = (jnp.einsum("bhts,bhsv->bhtv", att, vc)
             + jnp.einsum("bhtk,bhkv->bhtv", qc * jnp.exp(b), S))
        b_last = b[:, :, -1, :]
        S = (jnp.exp(b_last)[..., None] * S
             + jnp.einsum("bhsk,bhsv->bhkv", kc * jnp.exp(b_last[:, :, None, :] - b), vc))
        return S, o

    S, o = lax.scan(step, s0, (chunks(q), chunks(k), chunks(v), chunks(logf)))
    return o.transpose(1, 2, 0, 3, 4).reshape(B, H, T, DV), S


def _hgrn_readout(o, g, norm_g):
    B, H, T, DV = o.shape
    o = o * lax.rsqrt(jnp.mean(o * o, axis=-1, keepdims=True) + EPS)
    o = o.transpose(0, 2, 1, 3).reshape(B, T, H * DV) * norm_g.astype(jnp.float32)
    return (o * jax.nn.silu(g.astype(jnp.float32))).astype(g.dtype)


def _hgrn_branch(ul, uc, lb, norm_g, with_ctx):
    def prep(u):
        q, i, zf, zb, g = jnp.split(u, 5, axis=-1)
        return jax.nn.silu(_heads(q)), _heads(i), _heads(zf), _heads(zb), g

    ql, il, zfl, zbl, gl = prep(ul)
    qc, ic, zfc, zbc, gc = prep(uc)
    lb = lb.astype(jnp.float32)
    lb_f = lb[0].reshape(HG_HEADS, 1, HG_DK)
    lb_b = lb[1].reshape(HG_HEADS, 1, HG_DK)
    s0 = jnp.zeros((ul.shape[0], HG_HEADS, HG_DK, HG_DV), jnp.float32)

    def flip(a):
        return a[:, :, ::-1]

    lf, kf = _hgrn_gates(zfc, lb_f)
    oc_f, sc_f = _hgrn_scan(qc, kf, ic, lf, s0)
    lf, kf = _hgrn_gates(zfl, lb_f)
    ol_f, _ = _hgrn_scan(ql, kf, il, lf, sc_f)
    lbk, kb = _hgrn_gates(flip(zbc), lb_b)
    oc_b, sc_b = _hgrn_scan(flip(qc), kb, flip(ic), lbk, s0)
    lbk, kb = _hgrn_gates(flip(zbl), lb_b)
    ol_b, _ = _hgrn_scan(flip(ql), kb, flip(il), lbk, sc_b)

    out_l = _hgrn_readout(ol_f + flip(ol_b), gl, norm_g)
    out_c = _hgrn_readout(oc_f + flip(oc_b), gc, norm_g) if with_ctx else None
    return out_l, out_c


def _hier_moe(h, w_rg, b_rg, w_re, b_re, w1, w3, w2):
    N, D = h.shape
    pg = jax.nn.softmax((h @ w_rg).astype(jnp.float32) + b_rg.astype(jnp.float32), axis=-1)
    p_grp, grp = lax.top_k(pg, 1)
    le = ((h @ w_re).astype(jnp.float32) + b_re.astype(jnp.float32)).reshape(N, N_GROUPS, EXP_PER_GROUP)
    le = jnp.take_along_axis(le, grp[:, :, None], axis=1)[:, 0]
    p_exp, idx = lax.top_k(jax.nn.softmax(le, axis=-1), TOP_K)
    p_exp = p_exp / jnp.sum(p_exp, axis=-1, keepdims=True)
    eid = (grp * EXP_PER_GROUP + idx).reshape(-1).astype(jnp.int32)
    wt = (p_grp * p_exp).reshape(-1)
    A = N * TOP_K
    tok = jnp.arange(A, dtype=jnp.int32) // TOP_K
    nblk = -(-A // MOE_BLOCK) + N_EXPERTS
    P = nblk * MOE_BLOCK
    cnt = jax.ops.segment_sum(jnp.ones((A,), jnp.int32), eid, num_segments=N_EXPERTS)
    pcnt = (cnt + MOE_BLOCK - 1) // MOE_BLOCK * MOE_BLOCK
    pend = jnp.cumsum(pcnt)
    pstart = pend - pcnt
    start = jnp.cumsum(cnt) - cnt
    order = jnp.argsort(eid)
    se = eid[order]
    dst = pstart[se] + (jnp.arange(A, dtype=jnp.int32) - start[se])
    slot_tok = jnp.full((P,), N, jnp.int32).at[dst].set(tok[order])
    slot_w = jnp.zeros((P,), jnp.float32).at[dst].set(wt[order])
    blk_e = jnp.minimum(jnp.searchsorted(pend, jnp.arange(nblk, dtype=jnp.int32) * MOE_BLOCK, side="right"),
                        N_EXPERTS - 1)
    hpad = jnp.concatenate([h, jnp.zeros((1, D), h.dtype)], axis=0)
    xs = hpad[slot_tok].reshape(nblk, MOE_BLOCK, D)

    def expert_block(args):
        e, xb = args
        return (jax.nn.silu(xb @ w1[e]) * (xb @ w3[e])) @ w2[e]

    ys = lax.map(expert_block, (blk_e, xs)).reshape(P, D)
    ys = ys * slot_w.astype(ys.dtype)[:, None]
    return jax.ops.segment_sum(ys, slot_tok, num_segments=N + 1)[:N]


def setup_inputs(seed: int = 0) -> dict:
    key = jax.random.key(seed)
    ks = jax.random.split(key, 26)
    f32 = jnp.float32
    D = D_MODEL
    inv = D ** -0.5

    def nrm(k, shape, s):
        return jax.random.normal(k, shape, f32) * s

    return {
        "x": nrm(ks[0], (BATCH, SEQ, D), 1.0),
        "c": nrm(ks[1], (BATCH, D), 1.0),
        "ctx": nrm(ks[2], (BATCH, CTX_LEN, D), 1.0),
        "c_ctx": nrm(ks[3], (D,), 1.0),
        "w_ada": nrm(ks[4], (DEPTH, D, 6 * D), 0.5 * inv),
        "b_ada": nrm(ks[5], (DEPTH, 6 * D), 0.02),
        "g_mix": 1.0 + nrm(ks[6], (DEPTH, D), 0.02),
        "g_ffn": 1.0 + nrm(ks[7], (DEPTH, D), 0.02),
        "w_in": nrm(ks[8], (DEPTH, D, IN_DIM), inv),
        "conv_w": nrm(ks[9], (DEPTH, CONV_K, CONV_CH), CONV_K ** -0.5),
        "conv_b": nrm(ks[10], (DEPTH, CONV_CH), 0.02),
        "conv_ln_g": 1.0 + nrm(ks[11], (DEPTH, CONV_CH), 0.02),
        "conv_ln_b": nrm(ks[12], (DEPTH, CONV_CH), 0.02),
        "na_rpb": nrm(ks[13], (DEPTH, NA_HEADS, 2 * NA_KH_MAX - 1, 2 * NA_KW - 1), 0.1),
        "hgrn_lb": nrm(ks[14], (DEPTH, 2, HG_DIM), 0.5),
        "hgrn_norm_g": 1.0 + nrm(ks[15], (DEPTH, HG_DIM), 0.02),
        "w_out": nrm(ks[16], (DEPTH, MIX_DIM, D), MIX_DIM ** -0.5),
        "w_router_group": nrm(ks[17], (DEPTH, D, N_GROUPS), inv),
        "b_router_group": nrm(ks[18], (DEPTH, N_GROUPS), 0.01),
        "w_router_expert": nrm(ks[19], (DEPTH, D, N_EXPERTS), inv),
        "b_router_expert": nrm(ks[20], (DEPTH, N_EXPERTS), 0.01),
        "w_exp_gate": nrm(ks[21], (DEPTH, N_EXPERTS, D, D_EXPERT), inv),
        "w_exp_up": nrm(ks[22], (DEPTH, N_EXPERTS, D, D_EXPERT), inv),
        "w_exp_down": nrm(ks[23], (DEPTH, N_EXPERTS, D_EXPERT, D), D_EXPERT ** -0.5),
        "g_final": 1.0 + nrm(ks[24], (D,), 0.02),
    }


def reference(x, c, ctx, c_ctx, w_ada, b_ada, g_mix, g_ffn, w_in, conv_w, conv_b, conv_ln_g,
              conv_ln_b, na_rpb, hgrn_lb, hgrn_norm_g, w_out, w_router_group, b_router_group,
              w_router_expert, b_router_expert, w_exp_gate, w_exp_up, w_exp_down, g_final):
    B, T, D = x.shape
    L = ctx.shape[1]
    lbs = jnp.cumsum(jax.nn.softmax(hgrn_lb.astype(jnp.float32), axis=0), axis=0)
    lbs = lbs - lbs[:1]
    sc_lat = jax.nn.silu(c)
    sc_ctx = jax.nn.silu(c_ctx)
    xl, xc = x, ctx
    for l in range(DEPTH):
        with_ctx = l < DEPTH - 1
        mod_l = (sc_lat @ w_ada[l] + b_ada[l])[:, None, :]
        mod_c = (sc_ctx @ w_ada[l] + b_ada[l])[None, None, :]
        sh1_l, s1_l, ga1_l, sh2_l, s2_l, ga2_l = jnp.split(mod_l, 6, axis=-1)
        sh1_c, s1_c, ga1_c, sh2_c, s2_c, ga2_c = jnp.split(mod_c, 6, axis=-1)

        hl = _rmsnorm(xl, g_mix[l]) * (1.0 + s1_l) + sh1_l
        hc = _rmsnorm(xc, g_mix[l]) * (1.0 + s1_c) + sh1_c
        ul = hl @ w_in[l]
        uc = hc @ w_in[l]

        conv_l = _conv_module(ul[..., :OFF_NA], conv_w[l], conv_b[l], conv_ln_g[l], conv_ln_b[l])

        qa, ka, va = [a.reshape(B, T, NA_HEADS, NA_HD) for a in jnp.split(ul[..., OFF_NA:OFF_HG], 3, axis=-1)]
        qcx, kcx, vcx = [a.reshape(B, L, NA_HEADS, NA_HD) for a in jnp.split(uc[..., OFF_NA:OFF_HG], 3, axis=-1)]
        na_l = _na_latent(qa, ka, va, kcx, vcx, na_rpb[l])

        hg_l, hg_c = _hgrn_branch(ul[..., OFF_HG:], uc[..., OFF_HG:], lbs[l], hgrn_norm_g[l], with_ctx)

        xl = xl + ga1_l * (jnp.concatenate([conv_l, na_l, hg_l], axis=-1) @ w_out[l])
        if with_ctx:
            conv_c = _conv_module(uc[..., :OFF_NA], conv_w[l], conv_b[l], conv_ln_g[l], conv_ln_b[l])
            na_c = _ctx_attn(qcx, kcx, vcx)
            xc = xc + ga1_c * (jnp.concatenate([conv_c, na_c, hg_c], axis=-1) @ w_out[l])

        hl = (_rmsnorm(xl, g_ffn[l]) * (1.0 + s2_l) + sh2_l).reshape(B * T, D)
        if with_ctx:
            hc = (_rmsnorm(xc, g_ffn[l]) * (1.0 + s2_c) + sh2_c).reshape(B * L, D)
            tokens = jnp.concatenate([hl, hc], axis=0)
        else:
            tokens = hl
        f = _hier_moe(tokens, w_router_group[l], b_router_group[l], w_router_expert[l],
                      b_router_expert[l], w_exp_gate[l], w_exp_up[l], w_exp_down[l])
        xl = xl + ga2_l * f[: B * T].reshape(B, T, D)
        if with_ctx:
            xc = xc + ga2_c * f[B * T:].reshape(B, L, D)
    return _rmsnorm(xl, g_final)
```

```python
import numpy as np
import concourse.bass as bass
import concourse.mybir as mybir
from concourse.bass_utils import run_bass_kernel_spmd

F32 = mybir.dt.float32
BF16 = mybir.dt.bfloat16
AF = mybir.ActivationFunctionType
ALU = mybir.AluOpType
AX = mybir.AxisListType

D = 2048
DEPTH = 2
NT = 20
NTOK = NT * 128
NOWN = 18
EPS = 1e-6
IN_DIM = 6656
NEXP = 32
DEXP = 512


class Sched:
    CE = ("pe", "act", "dve", "pool")

    def __init__(self, nc):
        self.nc = nc
        self.sem = {}
        self.val = {}
        self.engs = ("pe", "act", "dve", "pool", "sp")
        self.seen = {e: {} for e in self.engs}
        self.prog = {e: [] for e in self.engs}
        self.lastw = {}
        self.readers = {}
        self.free_dsems = []
        self.dkeys = {}
        self.nblocks = 0
        for e in self.CE:
            self._mk("c_" + e)

    def _mk(self, name):
        self.sem[name] = self.nc.alloc_semaphore(name=name)
        self.val[name] = 0

    def dsem(self, key):
        if key not in self.dkeys:
            if self.free_dsems:
                nm = self.free_dsems.pop()
            else:
                nm = "d%d" % len([k for k in self.sem if k.startswith("d")])
                self._mk(nm)
            self.dkeys[key] = nm
        return self.dkeys[key]

    def _deps(self, reads, writes):
        deps = {}

        def add(t):
            if t is None:
                return
            s, v = t
            if deps.get(s, 0) < v:
                deps[s] = v

        for r in reads:
            add(self.lastw.get(r))
        for w in writes:
            add(self.lastw.get(w))
            for s, v in self.readers.get(w, {}).items():
                add((s, v))
        return deps

    def _commit(self, tok, reads, writes):
        s, v = tok
        for r in reads:
            d = self.readers.setdefault(r, {})
            if d.get(s, 0) < v:
                d[s] = v
        for w in writes:
            self.lastw[w] = tok
            self.readers[w] = {}

    def _emit(self, eng, fn, semname, inc, reads, writes, n=1):
        deps = self._deps(reads, writes)
        seen = self.seen[eng]
        waits = []
        for s, v in deps.items():
            if seen.get(s, 0) < v:
                seen[s] = v
                waits.append((s, v))
        self.val[semname] += inc * n
        tok = (semname, self.val[semname])
        self.prog[eng].append((waits, fn, semname, inc))
        self._commit(tok, reads, writes)
        return tok

    def op(self, eng, fn, reads=(), writes=()):
        return self._emit(eng, fn, "c_" + eng, 1, reads, writes)

    def dma(self, q, fn, key, reads=(), writes=(), n=1):
        return self._emit(q, fn, self.dsem(key), 16, reads, writes, n)

    def _replay(self, name, eng):
        for waits, fn, semname, inc in self.prog[name]:
            for s, v in waits:
                eng.wait_ge(self.sem[s], v)
            r = fn(eng)
            if isinstance(r, (list, tuple)):
                for ins in r:
                    ins.then_inc(self.sem[semname], inc)
            else:
                r.then_inc(self.sem[semname], inc)

    def flush(self, name=None):
        nc = self.nc
        pend = []
        for key, nm in self.dkeys.items():
            v = self.val[nm]
            if self.seen["sp"].get(nm, 0) < v:
                pend.append((nm, v))
        for e in self.CE:
            nm = "c_" + e
            if self.seen["sp"].get(nm, 0) < self.val[nm]:
                pend.append((nm, self.val[nm]))
        prog = self.prog
        sp_tail = pend
        self.nblocks += 1
        with nc.Block("%s_b%d" % (name or "ph", self.nblocks)) as block:
            @block.tensor
            def _(e):
                self._replay("pe", e)

            @block.scalar
            def _(e):
                self._replay("act", e)

            @block.vector
            def _(e):
                self._replay("dve", e)

            @block.gpsimd
            def _(e):
                self._replay("pool", e)

            @block.sync
            def _(e):
                self._replay("sp", e)
                for s, v in sp_tail:
                    e.wait_ge(self.sem[s], v)
        self.prog = {e: [] for e in self.engs}
        for e in self.engs:
            for s, v in self.val.items():
                self.seen[e][s] = v
        self.lastw = {}
        self.readers = {}
        for key, nm in self.dkeys.items():
            self.free_dsems.append(nm)
        self.dkeys = {}


_UID = [0]


def _u(name):
    _UID[0] += 1
    return "%s_%d" % (name, _UID[0])


class Ring:
    def __init__(self, name, bufs):
        self.name = name
        self.bufs = bufs
        self.i = 0

    def next(self):
        k = self.i % len(self.bufs)
        self.i += 1
        return self.bufs[k], (self.name, k)


MOD_SH1, MOD_S1, MOD_GA1, MOD_SH2, MOD_S2, MOD_GA2 = range(6)

FEAT_COLS = ([0 + 128 * i for i in range(4)] + [512 + 128 * i for i in range(4)]
             + [1024 + 128 * i for i in range(8)] + [2048 + 128 * i for i in range(8)]
             + [4096 + 128 * i for i in range(4)])
FEAT32_COLS = [5120 + 128 * i for i in range(4)] + [5632 + 128 * i for i in range(4)]
TOK_COLS = [3072, 3584, 4608, 6144]
NFB = len(FEAT_COLS)
NF32 = len(FEAT32_COLS)
CH_A, CH_G, CH_Q, CH_K, CH_HQ = 0, 4, 8, 16, 24


class Ctx:
    pass


def bcast_row(ap_row, n):
    return bass.AP(tensor=ap_row.tensor, offset=ap_row.offset, ap=[[0, 128], [1, n]])


def emit_norm(S, nc, K, tiles, xsrc, AB, hT, es, want32=None, htok=None):
    sb = lambda name, shape, dt: es.enter_context(nc.sbuf_tensor(_u(name), shape, dt))
    xb = Ring("xb", [sb("nx%d" % i, [128, D], F32) for i in range(2)])
    tb = Ring("tb", [sb("nt%d" % i, [128, D], F32) for i in range(2)])
    hb = Ring("hb", [sb("nh%d" % i, [128, D], BF16) for i in range(2)])
    jb = sb("njunk", [128, D], BF16)
    st = Ring("st", [sb("nst%d" % i, [128, 2], F32) for i in range(3)])
    pT = Ring("pT", [es.enter_context(nc.psum_tensor(_u("npT%d" % i), [128, 1024], BF16)) for i in range(4)])
    ident = K.ident_bf
    for i, (col0, kind) in enumerate(tiles):
        A, B = AB[kind]
        x, xk = xb.next()
        src = xsrc(i)
        S.dma("sp", lambda e, x=x, src=src: e.dma_start(out=x[:], in_=src), ("nx", xk[1]), writes=[xk])
        s, sk = st.next()
        S.op("act", lambda e, x=x, s=s: e.activation(out=jb[:], in_=x[:], func=AF.Square, accum_out=s[:, 0:1]),
             reads=[xk], writes=[sk, "njunk"])
        S.op("dve", lambda e, s=s: e.tensor_scalar(out=s[:, 1:2], in0=s[:, 0:1], scalar1=1.0 / D, scalar2=EPS,
                                                   op0=ALU.mult, op1=ALU.add), reads=[sk], writes=[sk])
        S.op("act", lambda e, s=s: e.activation(out=s[:, 1:2], in_=s[:, 1:2], func=AF.Sqrt), reads=[sk], writes=[sk])
        S.op("dve", lambda e, s=s: e.reciprocal(out=s[:, 1:2], in_=s[:, 1:2]), reads=[sk], writes=[sk])
        t, tk = tb.next()
        S.op("dve", lambda e, t=t, x=x, s=s, A=A: e.scalar_tensor_tensor(
            out=t[:], in0=x[:], scalar=s[:, 1:2], in1=A[:], op0=ALU.mult, op1=ALU.mult),
            reads=[xk, sk], writes=[tk])
        if htok is None:
            h, hk = hb.next()
        else:
            h, hk = htok(i)
        if want32 is None:
            S.op("pool", lambda e, t=t, h=h, B=B: e.tensor_tensor(out=h[:], in0=t[:], in1=B[:], op=ALU.add),
                 reads=[tk], writes=[hk])
        else:
            S.op("pool", lambda e, t=t, B=B: e.tensor_tensor(out=t[:], in0=t[:], in1=B[:], op=ALU.add),
                 reads=[tk], writes=[tk])
            S.op("act", lambda e, t=t, h=h: e.copy(out=h[:], in_=t[:]), reads=[tk], writes=[hk])
            want32(i, t, tk)
        if hT is None:
            continue
        for half in range(2):
            p, pk = pT.next()

            def tr(e, p=p, h=h, half=half):
                r = None
                for k in range(8):
                    kk = half * 8 + k
                    r = e.transpose(out=p[:, k * 128:(k + 1) * 128], in_=h[:, kk * 128:(kk + 1) * 128],
                                    identity=ident[:])
                return r
            S.op("pe", tr, reads=[hk], writes=[pk])
            eng = "act" if half == 0 else "dve"
            dst = hT[:, half * 8:(half + 1) * 8, col0:col0 + 128]
            hkey = ("hT", col0 // 128, half)
            if eng == "act":
                S.op("act", lambda e, p=p, dst=dst: e.copy(out=dst, in_=p[:].rearrange("p (k n) -> p k n", k=8)),
                     reads=[pk], writes=[hkey])
            else:
                S.op("dve", lambda e, p=p, dst=dst: e.tensor_copy(out=dst, in_=p[:].rearrange("p (k n) -> p k n", k=8)),
                     reads=[pk], writes=[hkey])


def load_mod_AB(S, nc, T, l, which, gvec, es, tag):
    sb = lambda name, shape, dt: es.enter_context(nc.sbuf_tensor(_u(name), shape, dt))
    gm = sb(tag + "gm", [128, D], F32)
    S.dma("sp", lambda e: e.dma_start(out=gm[:], in_=bcast_row(gvec, D)), (tag, "gm"), writes=[tag + "gm"])
    AB = {}
    for kind in range(2):
        A = sb(tag + "A%d" % kind, [128, D], F32)
        B = sb(tag + "B%d" % kind, [128, D], F32)
        sh = T.mod[l * 2 + kind:l * 2 + kind + 1, which[0] * D:(which[0] + 1) * D]
        sc = T.mod[l * 2 + kind:l * 2 + kind + 1, which[1] * D:(which[1] + 1) * D]
        S.dma("sp", lambda e, B=B, sh=sh: e.dma_start(out=B[:], in_=bcast_row(sh, D)), (tag, "B", kind),
              writes=[(tag, "B", kind)])
        S.dma("sp", lambda e, A=A, sc=sc: e.dma_start(out=A[:], in_=bcast_row(sc, D)), (tag, "A", kind),
              writes=[(tag, "A", kind)])
        S.op("dve", lambda e, A=A: e.scalar_tensor_tensor(out=A[:], in0=A[:], scalar=1.0, in1=gm[:],
                                                          op0=ALU.add, op1=ALU.mult),
             reads=[(tag, "A", kind), tag + "gm"], writes=[(tag, "A", kind)])
        AB[kind] = (A, B)
    return AB


def tile_kind(t):
    return 1 if t < 2 else 0


def phase_A(S, nc, K, T, l, xs):
    from contextlib import ExitStack
    with ExitStack() as es:
        sb = lambda name, shape, dt: es.enter_context(nc.sbuf_tensor(_u(name), shape, dt))
        hT = sb("hT", [128, 16, NTOK], BF16)
        with ExitStack() as es2:
            AB = load_mod_AB(S, nc, T, l, (MOD_SH1, MOD_S1), T.g_mix[l:l + 1, :], es2, "m1")
            tiles = [(t * 128, tile_kind(t)) for t in range(NT)]
            emit_norm(S, nc, K, tiles, lambda i: xs[i * 128:(i + 1) * 128, :], AB, hT, es2)
            S.flush("A1_%d" % l)
        hk_all = [("hT", t, half) for t in range(NT) for half in range(2)]
        wf = Ring("wf", [sb("wf%d" % i, [128, 16, 128], BF16) for i in range(3)])
        stg = Ring("stg", [sb("stg%d" % i, [128, NTOK], BF16) for i in range(2)])
        stg32 = Ring("stg32", [sb("stg32_%d" % i, [128, NTOK], F32) for i in range(2)])
        pp = Ring("pp", [es.enter_context(nc.psum_tensor(_u("app%d" % i), [128, 512], F32)) for i in range(4)])
        nev = 0
        for c in range(NFB + NF32):
            is32 = c >= NFB
            w, wk = wf.next()
            src = T.w_feat[(l * 36 + c) * 128:(l * 36 + c + 1) * 128, :]
            S.dma("pool", lambda e, w=w, src=src: e.dma_start(out=w[:].rearrange("p k n -> p (k n)"), in_=src),
                  ("wf", wk[1]), writes=[wk])
            so, sk = (stg32 if is32 else stg).next()
            for tg in range(NT // 4):
                p, pk = pp.next()

                def mm(e, p=p, w=w, tg=tg):
                    r = None
                    for k in range(16):
                        r = e.matmul(p[:], lhsT=w[:, k, :], rhs=hT[:, k, tg * 512:(tg + 1) * 512],
                                     start=(k == 0), stop=(k == 15))
                    return r
                S.op("pe", mm, reads=[wk] + hk_all[tg * 8:(tg + 1) * 8], writes=[pk])
                dst = so[:, tg * 512:(tg + 1) * 512]
                if nev % 2 == 0:
                    S.op("act", lambda e, p=p, dst=dst: e.copy(out=dst, in_=p[:]), reads=[pk], writes=[(sk, tg)])
                else:
                    S.op("dve", lambda e, p=p, dst=dst: e.tensor_copy(out=dst, in_=p[:]), reads=[pk],
                         writes=[(sk, tg)])
                nev += 1
            if is32:
                dstd = T.uT32[(c - NFB) * 128:(c - NFB + 1) * 128, :]
            else:
                dstd = T.uT[c * 128:(c + 1) * 128, :]
            S.dma("sp", lambda e, so=so, dstd=dstd: e.dma_start(out=dstd, in_=so[:]), ("stgo", is32, sk[1]),
                  reads=[(sk, tg) for tg in range(NT // 4)], writes=[("uT", c)])
            for tg in range(NT // 4):
                S.readers.setdefault((sk, tg), {})
                S.readers[(sk, tg)][S.dsem(("stgo", is32, sk[1]))] = S.val[S.dsem(("stgo", is32, sk[1]))]
        wt = Ring("wt", [sb("wt%d" % i, [128, 16, 512], BF16) for i in range(2)])
        so_t = Ring("sot", [sb("sot%d" % i, [128, 512], BF16) for i in range(4)])
        for g in range(4):
            w, wk = wt.next()
            src = T.w_tokm[(l * 4 + g) * 128:(l * 4 + g + 1) * 128, :]
            S.dma("pool", lambda e, w=w, src=src: e.dma_start(out=w[:].rearrange("p k n -> p (k n)"), in_=src),
                  ("wt", wk[1]), writes=[wk])
            for t in range(NT):
                p, pk = pp.next()

                def mm(e, p=p, w=w, t=t):
                    r = None
                    for k in range(16):
                        r = e.matmul(p[:], lhsT=hT[:, k, t * 128:(t + 1) * 128], rhs=w[:, k, :],
                                     start=(k == 0), stop=(k == 15))
                    return r
                S.op("pe", mm, reads=[wk] + hk_all[t * 2:(t + 1) * 2], writes=[pk])
                so, sk = so_t.next()
                if nev % 2 == 0:
                    S.op("act", lambda e, p=p, so=so: e.copy(out=so[:], in_=p[:]), reads=[pk], writes=[sk])
                else:
                    S.op("dve", lambda e, p=p, so=so: e.tensor_copy(out=so[:], in_=p[:]), reads=[pk], writes=[sk])
                nev += 1
                dstd = T.utok[t * 128:(t + 1) * 128, g * 512:(g + 1) * 512]
                S.dma("sp", lambda e, so=so, dstd=dstd: e.dma_start(out=dstd, in_=so[:]), ("soto", sk[1]),
                      reads=[sk], writes=[("utok", t, g)])
                S.readers.setdefault(sk, {})
                S.readers[sk][S.dsem(("soto", sk[1]))] = S.val[S.dsem(("soto", sk[1]))]
        S.flush("A2_%d" % l)


def host_consts():
    c = np.zeros((128, 10, 128), np.float32)
    c[:, 0] = np.eye(128)
    c[:, 1] = np.eye(128)[::-1]
    t = np.arange(128)
    same = (t[:, None] // 32) == (t[None, :] // 32)
    c[:, 2] = (same & (t[None, :] >= t[:, None]))
    c[:, 3] = (same & (t[None, :] <= t[:, None]))
    c[:, 4] = (t[None, :] % 32 != 0).astype(np.float32)
    c[:, 5] = 1.0 / 512.0
    c[:, 6] = 1.0
    c[:, 7, 0:4] = (t[:, None] // 32) == np.arange(4)[None, :]
    c[:, 8] = (t[:, None] < t[None, :])
    c[:, 9, 0:18] = 128.0 * np.arange(18)[None, :]
    c[:, 9, 32:32 + 80] = np.arange(80)[None, :]
    c[:, 9, 127] = np.arange(128)
    return c.reshape(128, 10 * 128)


def load_consts(S, nc, T, es):
    K = Ctx()
    sb = lambda name, shape, dt: es.enter_context(nc.sbuf_tensor(_u(name), shape, dt))
    K.c32 = sb("c32", [128, 10, 128], F32)
    K.cbf = sb("cbf", [128, 10, 128], BF16)
    S.dma("sp", lambda e: e.dma_start(out=K.c32[:].rearrange("p a b -> p (a b)"), in_=T.consts[:, :]), "c32",
          writes=["c32"])
    S.op("dve", lambda e: e.tensor_copy(out=K.cbf[:], in_=K.c32[:]), reads=["c32"], writes=["cbf"])
    K.ident_bf = K.cbf[:, 0, :]
    K.ident32 = K.c32[:, 0, :]
    K.anti32 = K.c32[:, 1, :]
    K.mask_f = K.c32[:, 2, :]
    K.mask_b = K.c32[:, 3, :]
    K.reset = K.c32[:, 4, :]
    K.avg32 = K.c32[:, 5, :]
    K.ones32 = K.c32[:, 6, :]
    K.ones_bf = K.cbf[:, 6, :]
    K.rowmask = K.c32[:, 7, 0:4]
    S.flush("consts")
    return K


def build(cfg):
    from contextlib import ExitStack
    nc = bass.Bass("TRN2", target_bir_lowering=False)
    T = Ctx()
    dbg = cfg.get("debug", ())
    nexp = cfg.get("nexp", NEXP)

    def din(name, shape, dt=F32):
        return nc.dram_tensor(name, list(shape), dt, kind="ExternalInput").ap()

    def dscr(name, shape, dt=F32):
        if name in dbg:
            return nc.dram_tensor(name, list(shape), dt, kind="ExternalOutput").ap()
        return nc.dram_tensor(name, list(shape), dt).ap()

    T.x_in = din("x_in", [NTOK, D])
    T.consts = din("consts", [128, 10 * 128])
    T.g_mix = din("g_mix", [DEPTH, D])
    T.w_feat = din("w_feat", [DEPTH * 36 * 128, 16 * 128])
    T.w_tokm = din("w_tokm", [DEPTH * 4 * 128, 16 * 512])
    if cfg.get("mod_input"):
        T.mod = din("mod", [DEPTH * 2, 6 * D])
    else:
        T.mod = dscr("mod", [DEPTH * 2, 6 * D])
        T.cT = din("cT", [128, 80])
        T.w_adam = din("w_adam", [DEPTH * 128, 16 * 1536])
        T.b_ada = din("b_ada", [1, DEPTH * 1536])
        T.bsel = din("bsel", [5, 2])
        T.msend = dscr("msend", [5, DEPTH * 1536])
        T.mrecv = dscr("mrecv", [40, DEPTH * 1536])
    T.pflag = din("pflag", [128, 2])
    T.uT = dscr("uT", [NFB * 128, NTOK], BF16)
    T.uT32 = dscr("uT32", [NF32 * 128, NTOK], F32)
    T.utok = dscr("utok", [NTOK, 2048], BF16)
    T.mixT = dscr("mixT", [16 * 128, NOWN * 128], BF16)
    T.conv_wT = din("conv_wT", [DEPTH * 512, 31])
    T.conv_vec = din("conv_vec", [DEPTH * 128, 12])
    T.na_bias = din("na_bias", [DEPTH * 16 * 10 * 128, 256])
    T.hlb = din("hlb", [128, 16])
    T.hnorm_g = din("hnorm_g", [DEPTH, 512])
    T.hsend = dscr("hsend", [512, 128])
    T.hrecv = dscr("hrecv", [512, 128])
    T.hrecv2 = dscr("hrecv2", [1024, 128])
    T.hostore = dscr("hostore", [NOWN * 128, 512])
    T.w_outm = din("w_outm", [DEPTH * 4 * 128, 16 * 512])
    T.g_ffn = din("g_ffn", [DEPTH, D])
    T.g_final = din("g_final", [1, D])
    T.w_r = din("w_r", [DEPTH * 128, 16 * 36])
    T.b_r = din("b_r", [DEPTH, 36])
    if cfg.get("sparse", True):
        _NE[0] = cfg.get("ne_eff", NEXP)
        T.w1m = din("w1m", [4 * RWv(), 2048])
        T.w3m = din("w3m", [4 * RWv(), 2048])
        T.w2m = din("w2m", [4 * RWv(), 2048])
    else:
        T.w1m = din("w1m", [DEPTH * nexp * 128, 16 * 512])
        T.w3m = din("w3m", [DEPTH * nexp * 128, 16 * 512])
        T.w2m = din("w2m", [DEPTH * nexp * 128, 4 * D])
    T.x1 = dscr("x1", [NOWN * 128, D])
    T.h2T = dscr("h2T", [16 * 128, NOWN * 128], BF16)
    T.wt = dscr("wt", [128, NOWN * 32])
    T.facc = dscr("facc", [NOWN * 128, D])
    sparse = cfg.get("sparse", True)
    if sparse:
        T.xsort = dscr("xsort", [68 * 128, D], BF16)
        T.ysd = dscr("ysd", [68 * 128, D])
        T.blku = dscr("blku", [128, 68], U32)
        T.slotsu = dscr("slotsu", [128, NOWN * 2], U32)
        T.aall = dscr("aall", [128, NOWN * 2])
    T.xs2 = dscr("xs2", [NTOK, D])
    T.xsend = dscr("xsend", [256, D])
    T.xrecv = dscr("xrecv", [512, D])
    T.out = nc.dram_tensor("out", [16 * 128, D], F32, kind="ExternalOutput").ap()

    S = Sched(nc)
    with ExitStack() as es:
        K = load_consts(S, nc, T, es)
        nlayers = cfg.get("layers", DEPTH)
        stop = cfg.get("stop", "")
        noex = cfg.get("no_exchange", False)
        if not cfg.get("mod_input"):
            phase_mod(S, nc, K, T)
        for l in range(nlayers):
            xs = T.x_in if l == 0 else T.xs2
            with_ctx = l < DEPTH - 1
            last = l == DEPTH - 1
            phase_A(S, nc, K, T, l, xs)
            if stop == "A":
                break
            phase_hg1(S, nc, K, T, l, with_ctx)
            if stop == "hg1":
                break
            if not noex:
                phase_xchg_h(S, nc, K, T)
            phase_conv(S, nc, K, T, l, with_ctx)
            if stop == "conv":
                break
            phase_na(S, nc, K, T, l, with_ctx)
            if stop == "na":
                break
            phase_hg2(S, nc, K, T, l, with_ctx, None if noex else T.hrecv)
            if stop == "hg":
                break
            phase_C(S, nc, K, T, l, with_ctx, xs)
            if sparse:
                phase_D1s(S, nc, K, T, l, with_ctx)
                if stop == "D1":
                    break
                phase_D2s(S, nc, K, T, l, with_ctx)
            else:
                phase_D1(S, nc, K, T, l, with_ctx)
                if stop == "D1":
                    break
                phase_D2(S, nc, K, T, l, with_ctx, nexp)
            phase_D3(S, nc, K, T, l, with_ctx, last)
            if stop == "D3":
                break
            if not last and not noex:
                phase_xchg_x(S, nc, K, T)
            if not last and noex:
                S.dma("sp", lambda e: e.dma_start(out=T.xs2[NOWN * 128:NTOK, :], in_=T.x_in[NOWN * 128:NTOK, :]), "dbgx")
                S.flush("dbgx")
    return nc


def core_tokens(x_b, ctx_b, odd):
    if odd:
        lat = x_b[::-1]
        cx = ctx_b[::-1]
    else:
        lat = x_b
        cx = ctx_b
    return np.ascontiguousarray(np.concatenate([cx, lat[:2304]], axis=0))


def prep_w_in(w_in, odd):
    fcols = list(FEAT_COLS) + (FEAT32_COLS[4:] + FEAT32_COLS[:4] if odd else list(FEAT32_COLS))
    wf = np.empty((DEPTH, 36, 128, 16 * 128), np.float32)
    wt = np.empty((DEPTH, 4, 128, 16 * 512), np.float32)
    for l in range(DEPTH):
        for i, c0 in enumerate(fcols):
            wf[l, i] = w_in[l][:, c0:c0 + 128].reshape(16, 128, 128).transpose(1, 0, 2).reshape(128, -1)
        for i, c0 in enumerate(TOK_COLS):
            wt[l, i] = w_in[l][:, c0:c0 + 512].reshape(16, 128, 512).transpose(1, 0, 2).reshape(128, -1)
    return wf.reshape(-1, 16 * 128), wt.reshape(-1, 16 * 512)


def prep_conv(conv_w, conv_b, ln_g, ln_b, odd):
    cw = conv_w[:, ::-1, :] if odd else conv_w
    cwT = np.ascontiguousarray(cw.transpose(0, 2, 1)).reshape(DEPTH * 512, 31)
    vec = np.stack([conv_b, ln_g, ln_b], axis=1)
    vec = vec.reshape(DEPTH, 3, 4, 128).transpose(0, 3, 1, 2).reshape(DEPTH * 128, 12)
    return cwT.astype(np.float32), np.ascontiguousarray(vec).astype(np.float32)


def phase_conv(S, nc, K, T, l, with_ctx):
    from contextlib import ExitStack
    with ExitStack() as es:
        sb = lambda name, shape, dt: es.enter_context(nc.sbuf_tensor(_u(name), shape, dt))
        pst = lambda name, shape, dt: es.enter_context(nc.psum_tensor(_u(name), shape, dt))
        cw = sb("cw", [128, 4, 31], F32)
        cv = sb("cv", [128, 12], F32)
        wd = sb("wd", [128, 4, 31, 128], BF16)
        hg = sb("hglu", [128, 4, 2078], BF16)
        hgc = sb("hgluc", [128, 4, 286], BF16)
        S.dma("sp", lambda e: e.dma_start(out=cw[:], in_=T.conv_wT[l * 512:(l + 1) * 512, :].rearrange(
            "(c p) k -> p c k", p=128)), "cw", writes=["cw"])
        S.dma("sp", lambda e: e.dma_start(out=cv[:], in_=T.conv_vec[l * 128:(l + 1) * 128, :]), "cv", writes=["cv"])
        for c in range(4):
            for k in range(31):
                eng = "dve" if (k % 2 == 0) else "pool"
                S.op(eng, lambda e, c=c, k=k: e.tensor_scalar(out=wd[:, c, k, :], in0=K.ident32, scalar1=cw[:, c, k:k + 1],
                                                              scalar2=None, op0=ALU.mult),
                     reads=["cw"], writes=[("wd", c, k)])
        S.op("pool", lambda e: e.memset(hg[:, :, 0:15], 0.0), writes=[("hgpad")])
        S.op("pool", lambda e: e.memset(hgc[:], 0.0), writes=[("hgc", c) for c in range(4)])
        ab = Ring("cab", [sb("cab%d" % i, [128, NTOK], BF16) for i in range(2)])
        gb = Ring("cgb", [sb("cgb%d" % i, [128, NTOK], BF16) for i in range(2)])
        sgb = Ring("csg", [sb("csg%d" % i, [128, NTOK], BF16) for i in range(2)])
        for c in range(4):
            a, ak = ab.next()
            g, gk = gb.next()
            sg, sgk = sgb.next()
            S.dma("sp", lambda e, a=a, c=c: e.dma_start(out=a[:], in_=T.uT[(CH_A + c) * 128:(CH_A + c + 1) * 128, :]),
                  ("cab", ak[1]), writes=[ak])
            S.dma("sp", lambda e, g=g, c=c: e.dma_start(out=g[:], in_=T.uT[(CH_G + c) * 128:(CH_G + c + 1) * 128, :]),
                  ("cgb", gk[1]), writes=[gk])
            S.op("act", lambda e, g=g, sg=sg: e.activation(out=sg[:], in_=g[:], func=AF.Sigmoid), reads=[gk], writes=[sgk])
            S.op("dve", lambda e, a=a, sg=sg, c=c: e.tensor_tensor(out=hg[:, c, 15:15 + 2063], in0=a[:, 256:256 + 2063],
                                                                 in1=sg[:, 256:256 + 2063], op=ALU.mult),
                 reads=[ak, sgk], writes=[("hg", c)])
            if with_ctx:
                S.op("dve", lambda e, a=a, sg=sg, c=c: e.tensor_tensor(out=hgc[:, c, 15:15 + 256], in0=a[:, 0:256],
                                                                     in1=sg[:, 0:256], op=ALU.mult),
                     reads=[ak, sgk, ("hgc", c)], writes=[("hgc", c)])
        groups = [(hg, "hg", tg * 512, 512, 256 + tg * 512) for tg in range(4)]
        if with_ctx:
            groups.append((hgc, "hgc", 0, 256, 0))
        pc = Ring("pc", [pst("pc%d" % i, [128, 512], F32) for i in range(3)])
        pmu = Ring("pmu", [pst("pmu%d" % i, [128, 512], F32) for i in range(2)])
        pm2 = Ring("pm2", [pst("pm2%d" % i, [128, 512], F32) for i in range(2)])
        y32 = Ring("y32", [sb("y32_%d" % i, [128, 4, 512], F32) for i in range(2)])
        ysq = Ring("ysq", [sb("ysq_%d" % i, [128, 4, 512], F32) for i in range(2)])
        mub = Ring("mub", [sb("mub%d" % i, [128, 512], F32) for i in range(2)])
        rsb = Ring("rsb", [sb("rsb%d" % i, [128, 512], F32) for i in range(2)])
        db = Ring("db", [sb("db%d" % i, [128, 512], F32) for i in range(3)])
        ob = Ring("ob", [sb("ob%d" % i, [128, 4, 512], BF16) for i in range(2)])
        for (src, sname, off, n, col0) in groups:
            y, yk = y32.next()
            q, qk = ysq.next()
            for c in range(4):
                p, pk = pc.next()

                def mm(e, p=p, c=c, src=src, off=off, n=n):
                    r = None
                    for k in range(31):
                        r = e.matmul(p[:, 0:n], lhsT=wd[:, c, k, :], rhs=src[:, c, off + k:off + k + n],
                                     start=(k == 0), stop=(k == 30))
                    return r
                S.op("pe", mm, reads=[("wd", c, k) for k in range(31)] + [(sname, c), "hgpad"], writes=[pk])
                S.op("act", lambda e, p=p, y=y, c=c, n=n: e.activation(out=y[:, c, 0:n], in_=p[:, 0:n], func=AF.Identity,
                                                                      bias=cv[:, c:c + 1]),
                     reads=[pk, "cv"], writes=[(yk, c)])
                S.op("act", lambda e, p=p, q=q, c=c, n=n: e.activation(out=q[:, c, 0:n], in_=p[:, 0:n], func=AF.Square,
                                                                      bias=cv[:, c:c + 1]),
                     reads=[pk, "cv"], writes=[(qk, c)])
            m, mk = pmu.next()
            m2, m2k = pm2.next()

            def st(e, m=m, m2=m2, y=y, q=q, n=n):
                r = None
                for c in range(4):
                    e.matmul(m[:, 0:n], lhsT=K.avg32, rhs=y[:, c, 0:n], start=(c == 0), stop=(c == 3))
                for c in range(4):
                    r = e.matmul(m2[:, 0:n], lhsT=K.avg32, rhs=q[:, c, 0:n], start=(c == 0), stop=(c == 3))
                return r
            S.op("pe", st, reads=[(yk, c) for c in range(4)] + [(qk, c) for c in range(4)], writes=[mk, m2k])
            mu, muk = mub.next()
            rs, rsk = rsb.next()
            S.op("act", lambda e, mu=mu, m=m, n=n: e.copy(out=mu[:, 0:n], in_=m[:, 0:n]), reads=[mk], writes=[muk])
            S.op("dve", lambda e, mu=mu, rs=rs, n=n: e.tensor_tensor(out=rs[:, 0:n], in0=mu[:, 0:n], in1=mu[:, 0:n],
                                                                    op=ALU.mult), reads=[muk], writes=[rsk])
            S.op("dve", lambda e, rs=rs, m2=m2, n=n: e.tensor_tensor(out=rs[:, 0:n], in0=m2[:, 0:n], in1=rs[:, 0:n],
                                                                    op=ALU.subtract), reads=[m2k, rsk], writes=[rsk])
            S.op("dve", lambda e, rs=rs, n=n: e.tensor_scalar(out=rs[:, 0:n], in0=rs[:, 0:n], scalar1=EPS, scalar2=None,
                                                              op0=ALU.add), reads=[rsk], writes=[rsk])
            S.op("act", lambda e, rs=rs, n=n: e.activation(out=rs[:, 0:n], in_=rs[:, 0:n], func=AF.Sqrt),
                 reads=[rsk], writes=[rsk])
            S.op("dve", lambda e, rs=rs, n=n: e.reciprocal(out=rs[:, 0:n], in_=rs[:, 0:n]), reads=[rsk], writes=[rsk])
            o, ok = ob.next()
            for c in range(4):
                d, dk = db.next()
                S.op("dve", lambda e, d=d, y=y, mu=mu, c=c, n=n: e.tensor_tensor(out=d[:, 0:n], in0=y[:, c, 0:n],
                                                                                in1=mu[:, 0:n], op=ALU.subtract),
                     reads=[(yk, c), muk], writes=[dk])
                S.op("pool", lambda e, d=d, rs=rs, n=n: e.tensor_tensor(out=d[:, 0:n], in0=d[:, 0:n], in1=rs[:, 0:n],
                                                                       op=ALU.mult), reads=[dk, rsk], writes=[dk])
                S.op("dve", lambda e, d=d, c=c, n=n: e.tensor_scalar(out=d[:, 0:n], in0=d[:, 0:n],
                                                                    scalar1=cv[:, 4 + c:5 + c], scalar2=cv[:, 8 + c:9 + c],
                                                                    op0=ALU.mult, op1=ALU.add),
                     reads=[dk, "cv"], writes=[dk])
                S.op("act", lambda e, d=d, o=o, c=c, n=n: e.activation(out=o[:, c, 0:n], in_=d[:, 0:n], func=AF.Silu),
                     reads=[dk], writes=[(ok, c)])
            dst = T.mixT[0:512, col0:col0 + n].rearrange("(c p) n -> p c n", p=128)
            S.dma("sp", lambda e, o=o, dst=dst, n=n: e.dma_start(out=dst, in_=o[:, :, 0:n]), ("ob", ok[1]),
                  reads=[(ok, c) for c in range(4)], writes=[("mixT", 0, col0)])
        S.flush("conv_%d" % l)


def na_groups(with_ctx):
    groups = []
    for g in range(8):
        if g == 0:
            chunks = [(2 + j, j) for j in range(4)]
        else:
            chunks = [(2 + 2 * g - 2 + c, 4 + c) for c in range(6)]
        chunks += [(0, None), (1, None)]
        groups.append((256 + 256 * g, chunks, (2 + 2 * g, 3 + 2 * g)))
    if with_ctx:
        groups.append((0, [(0, None), (1, None)], (0, 1)))
    return groups


def phase_na(S, nc, K, T, l, with_ctx):
    from contextlib import ExitStack
    with ExitStack() as es:
        sb = lambda name, shape, dt: es.enter_context(nc.sbuf_tensor(_u(name), shape, dt))
        pst = lambda name, shape, dt: es.enter_context(nc.psum_tensor(_u(name), shape, dt))
        na_tok = sb("na_tok", [128, NOWN, 1024], BF16)
        qb = Ring("naq", [sb("naq%d" % i, [128, NOWN * 128], BF16) for i in range(2)])
        kb = Ring("nak", [sb("nak%d" % i, [128, NTOK], BF16) for i in range(2)])
        vb = Ring("nav", [sb("nav%d" % i, [128, NT, 128], BF16) for i in range(2)])
        va = Ring("nava", [sb("nava%d" % i, [128, NT, 2, 65], BF16) for i in range(2)])
        bb = Ring("nab", [sb("nab%d" % i, [128, 10, 256], F32) for i in range(2)])
        pS = Ring("naps", [pst("naps%d" % i, [128, 256], F32) for i in range(3)])
        pO = Ring("napo", [pst("napo%d" % i, [128, 128], F32) for i in range(3)])
        tmpb = Ring("natmp", [sb("natmp%d" % i, [128, 256], F32) for i in range(3)])
        pTb = Ring("napT", [sb("napT%d" % i, [128, 8, 256], BF16) for i in range(2)])
        rcb = Ring("narc", [sb("narc%d" % i, [128, 1], F32) for i in range(4)])
        for i in range(2):
            S.op("pool", lambda e, i=i: e.memset(va.bufs[i][:], 1.0), writes=[("nava", i)])
        groups = na_groups(with_ctx)
        for hp in range(8):
            q, qk = qb.next()
            k, kk = kb.next()
            v, vk = vb.next()
            vau, vak = va.next()
            S.dma("sp", lambda e, q=q, hp=hp: e.dma_start(out=q[:], in_=T.uT[(CH_Q + hp) * 128:(CH_Q + hp + 1) * 128,
                                                                          0:NOWN * 128]), ("naq", qk[1]), writes=[qk])
            S.dma("sp", lambda e, k=k, hp=hp: e.dma_start(out=k[:], in_=T.uT[(CH_K + hp) * 128:(CH_K + hp + 1) * 128, :]),
                  ("nak", kk[1]), writes=[kk])
            S.dma("sp", lambda e, v=v, hp=hp: e.dma_start(out=v[:], in_=T.utok[:, hp * 128:(hp + 1) * 128].rearrange(
                "(t p) c -> p t c", p=128)), ("nav", vk[1]), writes=[vk])
            S.op("pool", lambda e, v=v, vau=vau: e.tensor_copy(out=vau[:, :, :, 0:64],
                                                             in_=v[:].rearrange("p t (h d) -> p t h d", h=2)),
                 reads=[vk], writes=[vak])
            for hl in range(2):
                h = 2 * hp + hl
                pb = 64 * hl
                b, bk = bb.next()
                S.dma("sp", lambda e, b=b, h=h: e.dma_start(
                    out=b[:], in_=T.na_bias[(l * 16 + h) * 1280:(l * 16 + h + 1) * 1280, :].rearrange(
                        "(b p) q -> p b q", p=128)), ("nab", bk[1]), writes=[bk])
                for (qc0, chunks, otiles) in groups:
                    pt, ptk = pTb.next()
                    for ci, (tile, bi) in enumerate(chunks):
                        p, pk = pS.next()
                        S.op("pe", lambda e, p=p, k=k, q=q, tile=tile, qc0=qc0, pb=pb: e.matmul(
                            p[:], lhsT=k[pb:pb + 64, tile * 128:(tile + 1) * 128], rhs=q[pb:pb + 64, qc0:qc0 + 256],
                            start=True, stop=True), reads=[kk, qk], writes=[pk])
                        if bi is not None:
                            tm, tmk = tmpb.next()
                            S.op("dve", lambda e, tm=tm, p=p, b=b, bi=bi: e.scalar_tensor_tensor(
                                out=tm[:], in0=p[:], scalar=0.125, in1=b[:, bi, :], op0=ALU.mult, op1=ALU.add),
                                reads=[pk, bk], writes=[tmk])
                            S.op("act", lambda e, tm=tm, pt=pt, ci=ci: e.activation(out=pt[:, ci, :], in_=tm[:], func=AF.Exp),
                                 reads=[tmk], writes=[(ptk, ci)])
                        else:
                            S.op("act", lambda e, p=p, pt=pt, ci=ci: e.activation(out=pt[:, ci, :], in_=p[:], func=AF.Exp,
                                                                                scale=0.125),
                                 reads=[pk], writes=[(ptk, ci)])
                    for qt in range(2):
                        o, ok = pO.next()

                        def pv(e, o=o, pt=pt, qt=qt, chunks=chunks, vau=vau, hl=hl):
                            r = None
                            n = len(chunks)
                            for ci, (tile, bi) in enumerate(chunks):
                                r = e.matmul(o[:, 0:65], lhsT=pt[:, ci, qt * 128:(qt + 1) * 128], rhs=vau[:, tile, hl, :],
                                             start=(ci == 0), stop=(ci == n - 1))
                            return r
                        S.op("pe", pv, reads=[(ptk, ci) for ci in range(len(chunks))] + [vak], writes=[ok])
                        rc, rck = rcb.next()
                        S.op("dve", lambda e, rc=rc, o=o: e.reciprocal(out=rc[:], in_=o[:, 64:65]), reads=[ok], writes=[rck])
                        ot = otiles[qt]
                        S.op("act", lambda e, o=o, rc=rc, ot=ot, h=h: e.activation(
                            out=na_tok[:, ot, h * 64:(h + 1) * 64], in_=o[:, 0:64], func=AF.Identity, scale=rc[:, 0:1]),
                            reads=[ok, rck], writes=[("natok", ot)])
        stT = sb("na_stT", [128, 8, NOWN * 128], BF16)
        pTr = Ring("naptr", [pst("naptr%d" % i, [128, 1024], BF16) for i in range(2)])
        t0 = 0 if with_ctx else 2
        for t in range(t0, NOWN):
            p, pk = pTr.next()

            def tr(e, p=p, t=t):
                r = None
                for c in range(8):
                    r = e.transpose(out=p[:, c * 128:(c + 1) * 128], in_=na_tok[:, t, c * 128:(c + 1) * 128],
                                    identity=K.ident_bf)
                return r
            S.op("pe", tr, reads=[("natok", t)], writes=[pk])
            dst = stT[:, :, t * 128:(t + 1) * 128]
            if t % 2 == 0:
                S.op("act", lambda e, p=p, dst=dst: e.copy(out=dst, in_=p[:].rearrange("p (c n) -> p c n", c=8)),
                     reads=[pk], writes=[("stT", t)])
            else:
                S.op("dve", lambda e, p=p, dst=dst: e.tensor_copy(out=dst, in_=p[:].rearrange("p (c n) -> p c n", c=8)),
                     reads=[pk], writes=[("stT", t)])
        c0 = t0 * 128
        for c in range(8):
            S.dma("sp", lambda e, c=c: e.dma_start(out=T.mixT[(4 + c) * 128:(5 + c) * 128, c0:NOWN * 128],
                                                   in_=stT[:, c, c0:NOWN * 128]), ("nast", c),
                  reads=[("stT", t) for t in range(t0, NOWN)], writes=[("mixT", 4 + c)])
        S.flush("na_%d" % l)


def prep_na_bias(rpb, odd):
    out = np.empty((DEPTH, 16, 10, 128, 256), np.float32)

    def tr(x):
        return 63 - x if odd else x
    kc = tr(np.arange(64))[None, :, None, None]
    qc = tr(np.arange(64))[None, None, None, :]
    pats = []
    for j in range(4):
        pats.append((np.arange(0, 4), np.arange(2 * j, 2 * j + 2)))
    for c in range(6):
        pats.append((np.arange(4, 8), np.arange(2 * c, 2 * c + 2)))
    for bi, (qrows, krows) in enumerate(pats):
        kr = tr(krows)[:, None, None, None]
        qr = tr(qrows)[None, None, :, None]
        sr = np.clip(qr - 4, 0, 56)
        ws = np.clip(qc - 8, 0, 48)
        valid = (kr >= sr) & (kr < sr + 8) & (kc >= ws) & (kc < ws + 16)
        valid = np.broadcast_to(valid, (2, 64, 4, 64))
        di = np.clip(np.broadcast_to(kr - qr + 7, (2, 64, 4, 64)), 0, 14)
        dj = np.clip(np.broadcast_to(kc - qc + 15, (2, 64, 4, 64)), 0, 30)
        vals = rpb[:, :, di, dj]
        vals = np.where(valid[None, None], vals, np.float32(-1e30))
        out[:, :, bi] = vals.reshape(DEPTH, 16, 128, 256)
    return out.reshape(-1, 256)


def _bc_last(ap3, n):
    a = [list(x) for x in ap3.ap]
    a[-1] = [0, n]
    return bass.AP(tensor=ap3.tensor, offset=ap3.offset, ap=a)


def _diag4(t):
    a = t[:, :, 0:32]
    ap = [list(x) for x in a.ap]
    ap[1][0] = ap[1][0] + 32
    return bass.AP(tensor=a.tensor, offset=a.offset, ap=ap)


import os
HG_DBG = int(os.environ.get('HG_DBG', '99'))
HG_SKIP = os.environ.get('HG_SKIP', '')
HG_KT = os.environ.get('HG_KT', 'dve')


class HgEnv:
    def __init__(self, S, nc, K, T, l, es):
        sb = lambda name, shape, dt: es.enter_context(nc.sbuf_tensor(_u(name), shape, dt))
        pst = lambda name, shape, dt: es.enter_context(nc.psum_tensor(_u(name), shape, dt))
        self.S, self.nc, self.K, self.T, self.l = S, nc, K, T, l
        self.zb = Ring("hgz", [sb("hgz%d" % i, [128, NOWN * 128], F32) for i in range(4)])
        self.qb = Ring("hgq", [sb("hgq%d" % i, [128, NOWN * 128], BF16) for i in range(4)])
        self.vb = Ring("hgv", [sb("hgv%d" % i, [128, NOWN, 128], BF16) for i in range(4)])
        f = lambda nm, n: Ring(nm, [sb("%s%d" % (nm, i), [128, 128], F32) for i in range(n)])
        self.sg, self.f, self.lf, self.kT, self.bT = f("hsg", 2), f("hf", 2), f("hlf", 2), f("hkT", 2), f("hbT", 2)
        self.tmp, self.b2, self.Eb, self.Enb, self.sq = f("htmp", 2), f("hb2", 2), f("hEb", 3), f("hEnb", 2), f("hsq", 2)
        self.e2, self.E2 = f("he2", 2), f("hE2", 2)
        self.Qtz = Ring("hQtz", [sb("hQtz%d" % i, [128, 4, 128], BF16) for i in range(4)])
        self.Kt = Ring("hKt", [sb("hKt%d" % i, [128, 128], BF16) for i in range(4)])
        self.KhT = Ring("hKhT", [sb("hKhT%d" % i, [128, 128], BF16) for i in range(4)])
        self.Khs = Ring("hKhs", [sb("hKhs%d" % i, [128, 128], BF16) for i in range(4)])
        self.Khz = Ring("hKhz", [sb("hKhz%d" % i, [128, 4, 128], BF16) for i in range(4)])
        self.attm = Ring("hattm", [sb("hattm%d" % i, [128, 128], BF16) for i in range(4)])
        self.Sbh = [Ring("hSb%d" % h, [sb("hSb%d_%d" % (h, i), [128, 128], BF16) for i in range(6)]) for h in range(4)]
        self.S32 = Ring("hS32", [sb("hS32_%d" % i, [128, 128], F32) for i in range(4)])
        self.pkh = Ring("hpkh", [pst("hpkh%d" % i, [128, 128], BF16) for i in range(1)])
        self.patt = Ring("hpatt", [pst("hpatt%d" % i, [128, 128], F32) for i in range(2)])
        self.pu = Ring("hpu", [pst("hpu%d" % i, [128, 128], F32) for i in range(2)])
        self.po = Ring("hpo", [pst("hpo%d" % i, [128, 128], F32) for i in range(2)])
        self.ostore = sb("hostore_sb", [128, NOWN, 512], F32)
        self.lbt = sb("hlbt", [128, 2, 8], F32)
        for i in range(4):
            S.op("pool", lambda e, i=i: e.memset(self.Qtz.bufs[i][:], 0.0), writes=[("hQtz", i)])
        if l == 0:
            S.op("pool", lambda e: e.memset(self.lbt[:, 0, :], 0.0), writes=["hlbt"])
            S.op("pool", lambda e: e.memset(self.lbt[:, 1, :], 1.0), reads=["hlbt"], writes=["hlbt"])
        else:
            raw = sb("hlbraw", [128, 16], F32)
            S.dma("sp", lambda e: e.dma_start(out=raw[:], in_=T.hlb[:, :]), "hlbraw", writes=["hlbraw"])
            S.op("dve", lambda e: e.tensor_tensor(out=self.lbt[:, 0, :], in0=raw[:, 8:16], in1=raw[:, 0:8], op=ALU.subtract),
                 reads=["hlbraw"], writes=["hlbt"])
            S.op("act", lambda e: e.activation(out=self.lbt[:, 0, :], in_=self.lbt[:, 0, :], func=AF.Sigmoid),
                 reads=["hlbt"], writes=["hlbt"])
            S.op("dve", lambda e: e.tensor_scalar(out=self.lbt[:, 1, :], in0=self.lbt[:, 0, :], scalar1=-1.0, scalar2=1.0,
                                                  op0=ALU.mult, op1=ALU.add), reads=["hlbt"], writes=["hlbt"])

    def load_head(self, d, h):
        S, T = self.S, self.T
        z, zk = self.zb.next()
        q, qk = self.qb.next()
        v, vk = self.vb.next()
        c = d * 4 + h
        S.dma("sp", lambda e: e.dma_start(out=z[:], in_=T.uT32[c * 128:(c + 1) * 128, 0:NOWN * 128]), ("hgz", zk[1]),
              writes=[zk])
        S.dma("sp", lambda e: e.dma_start(out=q[:], in_=T.uT[(CH_HQ + h) * 128:(CH_HQ + h + 1) * 128, 0:NOWN * 128]),
              ("hgq", qk[1]), writes=[qk])
        S.dma("sp", lambda e: e.dma_start(out=v[:], in_=T.utok[0:NOWN * 128, 1024 + h * 128:1024 + (h + 1) * 128].rearrange(
            "(t p) c -> p t c", p=128)), ("hgv", vk[1]), writes=[vk])
        return (z, zk, q, qk, v, vk)

    def chain_init(self, src=None, h=0):
        S = self.S
        s32, sk = self.S32.next()
        sbf, sbk = self.Sbh[h].next()
        if src is None:
            S.op("dve", lambda e: e.memset(s32[:], 0.0), writes=[sk])
            S.op("dve", lambda e: e.memset(sbf[:], 0.0), writes=[sbk])
        else:
            S.dma("sp", lambda e: e.dma_start(out=s32[:], in_=src), ("hS32", sk[1]), writes=[sk])
            S.op("act", lambda e: e.copy(out=sbf[:], in_=s32[:]), reads=[sk], writes=[sbk])
        return dict(s32=s32, sk=sk, sbf=sbf, sbk=sbk, h=h)

    def tile(self, d, h, t, hd, st, accumulate):
        S, K = self.S, self.K
        z, zk, q, qk, v, vk = hd
        tc0 = t * 128
        dh = d * 4 + h
        lb = self.lbt[:, 0, dh:dh + 1]
        oml = self.lbt[:, 1, dh:dh + 1]
        if HG_DBG <= 0:
            return
        sg, sgk = self.sg.next()
        S.op("act", lambda e: e.activation(out=sg[:], in_=z[:, tc0:tc0 + 128], func=AF.Sigmoid), reads=[zk], writes=[sgk])
        f, fk = self.f.next()
        S.op("dve", lambda e: e.tensor_scalar(out=f[:], in0=sg[:], scalar1=oml, scalar2=lb, op0=ALU.mult, op1=ALU.add),
             reads=[sgk, "hlbt"], writes=[fk])
        lf, lfk = self.lf.next()
        S.op("act", lambda e: e.activation(out=lf[:], in_=f[:], func=AF.Ln), reads=[fk], writes=[lfk])
        if HG_DBG <= 1:
            return
        kT, kTk = self.kT.next()
        if "kT" not in HG_SKIP:
            S.op(HG_KT, lambda e: e.tensor_scalar(out=kT[:], in0=f[:], scalar1=-1.0, scalar2=1.0, op0=ALU.mult, op1=ALU.add),
                 reads=[fk], writes=[kTk])
        if "scan" in HG_SKIP:
            return
        bT, bTk = self.bT.next()
        S.op("dve", lambda e: e.tensor_tensor_scan(out=bT[:], data0=K.reset, data1=lf[:], initial=0.0, op0=ALU.mult,
                                                   op1=ALU.add), reads=[lfk], writes=[bTk])
        b3 = bT[:].rearrange("p (c n) -> p c n", c=4)
        if d == 0:
            b, bk = bT, bTk
            tcb = _bc_last(b3[:, :, 31:32], 32)
        else:
            tm, tmk = self.tmp.next()
            S.op("dve", lambda e: e.tensor_tensor(out=tm[:], in0=lf[:], in1=bT[:], op=ALU.subtract), reads=[lfk, bTk],
                 writes=[tmk])
            b, bk = self.b2.next()
            S.op("dve", lambda e: e.tensor_tensor(out=b[:].rearrange("p (c n) -> p c n", c=4),
                                                  in0=tm[:].rearrange("p (c n) -> p c n", c=4),
                                                  in1=_bc_last(b3[:, :, 31:32], 32), op=ALU.add),
                 reads=[tmk, bTk], writes=[bk])
            tcb = _bc_last(b[:].rearrange("p (c n) -> p c n", c=4)[:, :, 0:1], 32)
        if HG_DBG <= 2:
            return
        Eb, Ebk = self.Eb.next()
        S.op("act", lambda e: e.activation(out=Eb[:], in_=b[:], func=AF.Exp), reads=[bk], writes=[Ebk])
        Enb, Enbk = self.Enb.next()
        S.op("act", lambda e: e.activation(out=Enb[:], in_=b[:], func=AF.Exp, scale=-1.0), reads=[bk], writes=[Enbk])
        sq, sqk = self.sq.next()
        S.op("act", lambda e: e.activation(out=sq[:], in_=q[:, tc0:tc0 + 128], func=AF.Silu), reads=[qk], writes=[sqk])
        Qtz, Qk = self.Qtz.next()
        qd = _diag4(Qtz)
        S.op("dve", lambda e: e.tensor_tensor(out=qd, in0=sq[:].rearrange("p (c n) -> p c n", c=4),
                                              in1=Eb[:].rearrange("p (c n) -> p c n", c=4), op=ALU.mult),
             reads=[sqk, Ebk], writes=[Qk])
        Kt, Ktk = self.Kt.next()
        S.op("dve", lambda e: e.tensor_tensor(out=Kt[:], in0=kT[:], in1=Enb[:], op=ALU.mult), reads=[kTk, Enbk],
             writes=[Ktk])
        if HG_DBG <= 3:
            return
        e2, e2k = self.e2.next()
        S.op("dve", lambda e: e.tensor_tensor(out=e2[:].rearrange("p (c n) -> p c n", c=4), in0=tcb,
                                              in1=b[:].rearrange("p (c n) -> p c n", c=4), op=ALU.subtract),
             reads=[bk], writes=[e2k])
        E2, E2k = self.E2.next()
        S.op("act", lambda e: e.activation(out=E2[:], in_=e2[:], func=AF.Exp), reads=[e2k], writes=[E2k])
        KhT, KhTk = self.KhT.next()
        S.op("dve", lambda e: e.tensor_tensor(out=KhT[:], in0=kT[:], in1=E2[:], op=ALU.mult), reads=[kTk, E2k],
             writes=[KhTk])
        if HG_DBG <= 4:
            return
        pkh, pkhk = self.pkh.next()
        S.op("pe", lambda e: e.transpose(out=pkh[:], in_=KhT[:], identity=K.ident_bf), reads=[KhTk], writes=[pkhk])
        Khz, Khzk = self.Khz.next()
        Khs, Khsk = self.Khs.next()
        S.op("act", lambda e: e.copy(out=Khs[:], in_=pkh[:]), reads=[pkhk], writes=[Khsk])
        khb = Khs[:]
        in0 = bass.AP(tensor=khb.tensor, offset=khb.offset, ap=[list(khb.ap[0]), [0, 4], [1, 128]])
        rm = K.rowmask
        in1 = bass.AP(tensor=rm.tensor, offset=rm.offset, ap=[list(rm.ap[0]), [1, 4], [0, 128]])
        S.op("dve", lambda e: e.tensor_tensor(out=Khz[:], in0=in0, in1=in1, op=ALU.mult), reads=[Khsk],
             writes=[(Khzk, c) for c in range(4)])
        if HG_DBG <= 5:
            return
        patt, pattk = self.patt.next()
        S.op("pe", lambda e: e.matmul(patt[:], lhsT=Kt[:], rhs=qd, start=True, stop=True), reads=[Ktk, Qk], writes=[pattk])
        attm, attmk = self.attm.next()
        mask = K.mask_f if d == 0 else K.mask_b
        S.op("dve", lambda e: e.tensor_tensor(out=attm[:], in0=patt[:], in1=mask, op=ALU.mult), reads=[pattk],
             writes=[attmk])
        if HG_DBG <= 6:
            return
        order = range(4) if d == 0 else range(3, -1, -1)
        snaps = {}
        for c in order:
            snaps[c] = (st["sbf"], st["sbk"])
            pu, puk = self.pu.next()
            S.op("pe", lambda e, c=c, pu=pu: e.matmul(pu[:], lhsT=Khz[:, c, :], rhs=v[:, t, :], start=True, stop=True),
                 reads=[(Khzk, c), vk], writes=[puk])
            col = (32 * c + 31) if d == 0 else (32 * c)
            s32, sk = st["s32"], st["sk"]
            S.op("dve", lambda e, pu=pu, s32=s32, col=col: e.scalar_tensor_tensor(
                out=s32[:], in0=s32[:], scalar=Eb[:, col:col + 1], in1=pu[:], op0=ALU.mult, op1=ALU.add),
                reads=[sk, Ebk, puk], writes=[sk])
            nb, nbk = self.Sbh[st["h"]].next()
            S.op("act", lambda e, nb=nb, s32=s32: e.copy(out=nb[:], in_=s32[:]), reads=[sk], writes=[nbk])
            st["sbf"], st["sbk"] = nb, nbk
        if HG_DBG <= 7:
            return
        po, pok = self.po.next()

        def om(e):
            e.matmul(po[:], lhsT=attm[:], rhs=v[:, t, :], start=True, stop=False)
            r = None
            for c in range(4):
                r = e.matmul(po[:], lhsT=Qtz[:, c, :], rhs=snaps[c][0][:], start=False, stop=(c == 3))
            return r
        S.op("pe", om, reads=[attmk, vk, Qk] + [snaps[c][1] for c in range(4)], writes=[pok])
        dst = self.ostore[:, t, h * 128:(h + 1) * 128]
        ok = ("ostore", t, h)
        if not accumulate:
            S.op("act", lambda e: e.copy(out=dst, in_=po[:]), reads=[pok], writes=[ok])
        else:
            S.op("dve", lambda e: e.tensor_tensor(out=dst, in0=po[:], in1=dst, op=ALU.add), reads=[pok, ok], writes=[ok])


def phase_hg1(S, nc, K, T, l, with_ctx):
    from contextlib import ExitStack
    with ExitStack() as es:
        E = HgEnv(S, nc, K, T, l, es)
        hds = [E.load_head(0, h) for h in range(4)]
        sts = [E.chain_init(None, h) for h in range(4)]
        for t in range(NOWN):
            for h in range(4):
                E.tile(0, h, t, hds[h], sts[h], accumulate=False)
        for h in range(4):
            s32 = sts[h]["s32"]
            S.dma("sp", lambda e, s32=s32, h=h: e.dma_start(out=T.hsend[h * 128:(h + 1) * 128, :], in_=s32[:]),
                  ("hsend", h), reads=[sts[h]["sk"]], writes=[("hsend", h)])
        if with_ctx and "bwd" not in HG_SKIP:
            hds = [E.load_head(1, h) for h in range(4)]
            sts = [E.chain_init(None, h) for h in range(4)]
            for t in (1, 0):
                for h in range(4):
                    E.tile(1, h, t, hds[h], sts[h], accumulate=True)
        for t in range(NOWN):
            S.dma("sp", lambda e, t=t: e.dma_start(out=T.hostore[t * 128:(t + 1) * 128, :], in_=E.ostore[:, t, :]),
                  ("hos", t % 4), reads=[("ostore", t, h) for h in range(4)], writes=[("hostore", t)])
        S.flush("hg1_%d" % l)


def phase_hg2(S, nc, K, T, l, with_ctx, state_src):
    from contextlib import ExitStack
    with ExitStack() as es:
        sb = lambda name, shape, dt: es.enter_context(nc.sbuf_tensor(_u(name), shape, dt))
        pst = lambda name, shape, dt: es.enter_context(nc.psum_tensor(_u(name), shape, dt))
        E = HgEnv(S, nc, K, T, l, es)
        for t in range(NOWN):
            S.dma("sp", lambda e, t=t: e.dma_start(out=E.ostore[:, t, :], in_=T.hostore[t * 128:(t + 1) * 128, :]),
                  ("hos", t % 4), writes=[("ostore", t, h) for h in range(4)])
        hds = [E.load_head(1, h) for h in range(4)]
        sts = [E.chain_init(None if state_src is None else state_src[h * 128:(h + 1) * 128, :], h) for h in range(4)]
        for t in range(NOWN - 1, 1, -1):
            for h in range(4):
                E.tile(1, h, t, hds[h], sts[h], accumulate=True)
        gt = sb("hgt", [128, NOWN, 512], BF16)
        ngb = sb("hngb", [128, 512], F32)
        stH = sb("hstH", [128, 4, NOWN * 128], BF16)
        S.dma("sp", lambda e: e.dma_start(out=gt[:], in_=T.utok[0:NOWN * 128, 1536:2048].rearrange("(t p) c -> p t c", p=128)),
              "hgt", writes=["hgt"])
        S.dma("sp", lambda e: e.dma_start(out=ngb[:], in_=bcast_row(T.hnorm_g[l:l + 1, :], 512)), "hngb", writes=["hngb"])
        ssb = Ring("hss", [sb("hss%d" % i, [128, 4], F32) for i in range(2)])
        junk = sb("hjunk", [128, 128], F32)
        sgb = Ring("hsgt", [sb("hsgt%d" % i, [128, 512], F32) for i in range(2)])
        onb = Ring("hon", [sb("hon%d" % i, [128, 512], F32) for i in range(2)])
        hgb = Ring("hhg", [sb("hhg%d" % i, [128, 512], BF16) for i in range(2)])
        pT = Ring("hpT", [pst("hpT%d" % i, [128, 512], BF16) for i in range(1)])
        t0 = 0 if with_ctx else 2
        for t in range(t0, NOWN):
            ss, ssk = ssb.next()
            for h in range(4):
                S.op("act", lambda e, h=h, ss=ss, t=t: e.activation(out=junk[:], in_=E.ostore[:, t, h * 128:(h + 1) * 128],
                                                                    func=AF.Square, accum_out=ss[:, h:h + 1]),
                     reads=[("ostore", t, h)], writes=[(ssk, h), "hjunk"])
            allss = [(ssk, h) for h in range(4)]
            S.op("dve", lambda e, ss=ss: e.tensor_scalar(out=ss[:], in0=ss[:], scalar1=1.0 / 128, scalar2=EPS, op0=ALU.mult,
                                                         op1=ALU.add), reads=allss, writes=allss)
            S.op("act", lambda e, ss=ss: e.activation(out=ss[:], in_=ss[:], func=AF.Sqrt), reads=allss, writes=allss)
            S.op("dve", lambda e, ss=ss: e.reciprocal(out=ss[:], in_=ss[:]), reads=allss, writes=allss)
            sgt, sgk = sgb.next()
            S.op("act", lambda e, sgt=sgt, t=t: e.activation(out=sgt[:], in_=gt[:, t, :], func=AF.Silu), reads=["hgt"],
                 writes=[sgk])
            on, onk = onb.next()
            S.op("dve", lambda e, on=on, ss=ss, t=t: e.tensor_tensor(
                out=on[:].rearrange("p (h n) -> p h n", h=4), in0=E.ostore[:, t, :].rearrange("p (h n) -> p h n", h=4),
                in1=_bc_last(ss[:].rearrange("p (h o) -> p h o", o=1), 128), op=ALU.mult),
                reads=allss + [("ostore", t, h) for h in range(4)], writes=[onk])
            S.op("dve", lambda e, on=on: e.tensor_tensor(out=on[:], in0=on[:], in1=ngb[:], op=ALU.mult), reads=[onk, "hngb"],
                 writes=[onk])
            hg, hgk = hgb.next()
            S.op("dve", lambda e, on=on, sgt=sgt, hg=hg: e.tensor_tensor(out=hg[:], in0=on[:], in1=sgt[:], op=ALU.mult),
                 reads=[onk, sgk], writes=[hgk])
            p, pk = pT.next()

            def tr(e, p=p, hg=hg):
                r = None
                for c in range(4):
                    r = e.transpose(out=p[:, c * 128:(c + 1) * 128], in_=hg[:, c * 128:(c + 1) * 128], identity=K.ident_bf)
                return r
            S.op("pe", tr, reads=[hgk], writes=[pk])
            S.op("act", lambda e, p=p, t=t: e.copy(out=stH[:, :, t * 128:(t + 1) * 128],
                                                   in_=p[:].rearrange("p (c n) -> p c n", c=4)), reads=[pk],
                 writes=[("stH", t)])
        c0 = t0 * 128
        for c in range(4):
            S.dma("sp", lambda e, c=c: e.dma_start(out=T.mixT[(12 + c) * 128:(13 + c) * 128, c0:NOWN * 128],
                                                   in_=stH[:, c, c0:NOWN * 128]), ("hst", c),
                  reads=[("stH", t) for t in range(t0, NOWN)], writes=[("mixT", 12 + c)])
        S.flush("hg2_%d" % l)


def prep_hgrn(hgrn_lb, hgrn_norm_g, odd):
    lb = hgrn_lb[:, ::-1] if odd else hgrn_lb
    hlb = lb.reshape(DEPTH, 2, 4, 128).transpose(3, 0, 1, 2).reshape(128, DEPTH * 8)
    return np.ascontiguousarray(hlb).astype(np.float32), np.ascontiguousarray(hgrn_norm_g).astype(np.float32)


def load_bc(S, nc, T, l, idx, es, tag):
    sb = lambda name, shape, dt: es.enter_context(nc.sbuf_tensor(_u(name), shape, dt))
    out = {}
    for kind in range(2):
        g = sb(tag + "%d" % kind, [128, D], F32)
        src = T.mod[l * 2 + kind:l * 2 + kind + 1, idx * D:(idx + 1) * D]
        S.dma("sp", lambda e, g=g, src=src: e.dma_start(out=g[:], in_=bcast_row(src, D)), (tag, kind), writes=[(tag, kind)])
        out[kind] = g
    return out


def phase_C(S, nc, K, T, l, with_ctx, xs):
    from contextlib import ExitStack
    with ExitStack() as es:
        sb = lambda name, shape, dt: es.enter_context(nc.sbuf_tensor(_u(name), shape, dt))
        pst = lambda name, shape, dt: es.enter_context(nc.psum_tensor(_u(name), shape, dt))
        mix = sb("mixall", [128, 16, NOWN * 128], BF16)
        for c in range(16):
            S.dma("sp", lambda e, c=c: e.dma_start(out=mix[:, c, :], in_=T.mixT[c * 128:(c + 1) * 128, :]), ("mixl", c % 4),
                  writes=[("mix", c)])
        ga = load_bc(S, nc, T, l, MOD_GA1, es, "ga1")
        wo = Ring("wo", [sb("wo%d" % i, [128, 16, 512], BF16) for i in range(2)])
        xi = Ring("cxi", [sb("cxi%d" % i, [128, 512], F32) for i in range(3)])
        yt = Ring("cyt", [sb("cyt%d" % i, [128, 512], F32) for i in range(3)])
        pp = Ring("cpp", [pst("cpp%d" % i, [128, 512], F32) for i in range(4)])
        t0 = 0 if with_ctx else 2
        for ng in range(4):
            w, wk = wo.next()
            src = T.w_outm[(l * 4 + ng) * 128:(l * 4 + ng + 1) * 128, :]
            S.dma("pool", lambda e, w=w, src=src: e.dma_start(out=w[:].rearrange("p k n -> p (k n)"), in_=src), ("wo", wk[1]),
                  writes=[wk])
            for t in range(t0, NOWN):
                p, pk = pp.next()

                def mm(e, p=p, w=w, t=t):
                    r = None
                    for k in range(16):
                        r = e.matmul(p[:], lhsT=mix[:, k, t * 128:(t + 1) * 128], rhs=w[:, k, :], start=(k == 0), stop=(k == 15))
                    return r
                S.op("pe", mm, reads=[wk] + [("mix", c) for c in range(16)], writes=[pk])
                x, xk = xi.next()
                S.dma("sp", lambda e, x=x, t=t, ng=ng: e.dma_start(out=x[:], in_=xs[t * 128:(t + 1) * 128, ng * 512:(ng + 1) * 512]),
                      ("cxi", xk[1]), writes=[xk])
                y, yk = yt.next()
                g = ga[tile_kind(t)]
                S.op("dve", lambda e, y=y, p=p, g=g, ng=ng: e.tensor_tensor(out=y[:], in0=p[:], in1=g[:, ng * 512:(ng + 1) * 512],
                                                                           op=ALU.mult),
                     reads=[pk, ("ga1", tile_kind(t))], writes=[yk])
                S.op("pool", lambda e, y=y, x=x: e.tensor_tensor(out=y[:], in0=y[:], in1=x[:], op=ALU.add), reads=[yk, xk],
                     writes=[yk])
                S.dma("sp", lambda e, y=y, t=t, ng=ng: e.dma_start(out=T.x1[t * 128:(t + 1) * 128, ng * 512:(ng + 1) * 512], in_=y[:]),
                      ("cyo", yk[1]), reads=[yk], writes=[("x1", t, ng)])
        S.flush("C_%d" % l)


BIG = 1.0e30


def phase_D1(S, nc, K, T, l, with_ctx):
    from contextlib import ExitStack
    with ExitStack() as es:
        sb = lambda name, shape, dt: es.enter_context(nc.sbuf_tensor(_u(name), shape, dt))
        pst = lambda name, shape, dt: es.enter_context(nc.psum_tensor(_u(name), shape, dt))
        h2T = sb("h2T", [128, 16, NOWN * 128], BF16)
        wtall = sb("wtall", [128, NOWN, 32], F32)
        wr = sb("wr", [128, 16, 36], F32)
        br = sb("brb", [128, 36], F32)
        S.dma("sp", lambda e: e.dma_start(out=wr[:].rearrange("p k n -> p (k n)"), in_=T.w_r[l * 128:(l + 1) * 128, :]), "wr",
              writes=["wr"])
        S.dma("sp", lambda e: e.dma_start(out=br[:], in_=bcast_row(T.b_r[l:l + 1, :], 36)), "brb", writes=["brb"])
        AB = load_mod_AB(S, nc, T, l, (MOD_SH2, MOD_S2), T.g_ffn[l:l + 1, :], es, "m2")
        t0 = 0 if with_ctx else 2
        tl = list(range(t0, NOWN))
        tiles = [(t * 128, tile_kind(t)) for t in tl]
        ptr = Ring("rptr", [pst("rptr%d" % i, [128, 512], F32) for i in range(2)])
        pr = Ring("rpr", [pst("rpr%d" % i, [128, 36], F32) for i in range(1)])
        hT32 = Ring("rhT32", [sb("rhT32_%d" % i, [128, 16, 128], F32) for i in range(2)])
        sm = lambda nm, w: Ring(nm, [sb("%s%d" % (nm, i), [128, w], F32) for i in range(2)])
        lgb, st1, gmb, egb, pen, lem, o1b, lem2, o2b = (sm("rlg", 36), sm("rst", 8), sm("rgm", 4), sm("reg", 4), sm("rpen", 4),
                                                        sm("rlem", 32), sm("ro1", 32), sm("rlem2", 32), sm("ro2", 32))

        def router(i, h32, h32k):
            t = tl[i]
            hT, hTk = hT32.next()
            for g in range(4):
                p, pk = ptr.next()

                def tr(e, p=p, g=g):
                    r = None
                    for j in range(4):
                        kk = g * 4 + j
                        r = e.transpose(out=p[:, j * 128:(j + 1) * 128], in_=h32[:, kk * 128:(kk + 1) * 128], identity=K.ident32)
                    return r
                S.op("pe", tr, reads=[h32k], writes=[pk])
                S.op("act", lambda e, p=p, g=g: e.copy(out=hT[:, g * 4:(g + 1) * 4, :], in_=p[:].rearrange("p (j n) -> p j n", j=4)),
                     reads=[pk], writes=[(hTk, g)])
            p, pk = pr.next()

            def mm(e, p=p):
                r = None
                for k in range(16):
                    r = e.matmul(p[:], lhsT=hT[:, k, :], rhs=wr[:, k, :], start=(k == 0), stop=(k == 15))
                return r
            S.op("pe", mm, reads=[(hTk, g) for g in range(4)] + ["wr"], writes=[pk])
            lg, lgk = lgb.next()
            S.op("dve", lambda e: e.tensor_tensor(out=lg[:], in0=p[:], in1=br[:], op=ALU.add), reads=[pk, "brb"], writes=[lgk])
            s, sk = st1.next()
            S.op("dve", lambda e: e.reduce_max(out=s[:, 0:1], in_=lg[:, 0:4], axis=AX.X), reads=[lgk], writes=[sk])
            S.op("dve", lambda e: e.tensor_scalar(out=s[:, 1:2], in0=s[:, 0:1], scalar1=-1.0, scalar2=None, op0=ALU.mult),
                 reads=[sk], writes=[sk])
            gm, gmk = gmb.next()
            S.op("dve", lambda e: e.tensor_scalar(out=gm[:], in0=lg[:, 0:4], scalar1=s[:, 0:1], scalar2=None, op0=ALU.is_equal),
                 reads=[lgk, sk], writes=[gmk])
            eg, egk = egb.next()
            S.op("act", lambda e: e.activation(out=eg[:], in_=lg[:, 0:4], func=AF.Exp, bias=s[:, 1:2], accum_out=s[:, 2:3]),
                 reads=[lgk, sk], writes=[egk, sk])
            pn, pnk = pen.next()
            S.op("dve", lambda e: e.tensor_scalar(out=pn[:], in0=gm[:], scalar1=BIG, scalar2=-BIG, op0=ALU.mult, op1=ALU.add),
                 reads=[gmk], writes=[pnk])
            le, lek = lem.next()
            S.op("dve", lambda e: e.tensor_tensor(out=le[:].rearrange("p (g n) -> p g n", g=4),
                                                  in0=lg[:, 4:36].rearrange("p (g n) -> p g n", g=4),
                                                  in1=_bc_last(pn[:].rearrange("p (g o) -> p g o", o=1), 8), op=ALU.add),
                 reads=[lgk, pnk], writes=[lek])
            S.op("dve", lambda e: e.reduce_max(out=s[:, 3:4], in_=le[:], axis=AX.X), reads=[lek, sk], writes=[sk])
            o1, o1k = o1b.next()
            S.op("dve", lambda e: e.tensor_scalar(out=o1[:], in0=le[:], scalar1=s[:, 3:4], scalar2=None, op0=ALU.is_equal),
                 reads=[lek, sk], writes=[o1k])
            l2, l2k = lem2.next()
            S.op("dve", lambda e: e.scalar_tensor_tensor(out=l2[:], in0=o1[:], scalar=-BIG, in1=le[:], op0=ALU.mult, op1=ALU.add),
                 reads=[o1k, lek], writes=[l2k])
            S.op("dve", lambda e: e.reduce_max(out=s[:, 4:5], in_=l2[:], axis=AX.X), reads=[l2k, sk], writes=[sk])
            o2, o2k = o2b.next()
            S.op("dve", lambda e: e.tensor_scalar(out=o2[:], in0=l2[:], scalar1=s[:, 4:5], scalar2=None, op0=ALU.is_equal),
                 reads=[l2k, sk], writes=[o2k])
            S.op("dve", lambda e: e.tensor_tensor(out=s[:, 5:6], in0=s[:, 4:5], in1=s[:, 3:4], op=ALU.subtract), reads=[sk],
                 writes=[sk])
            S.op("act", lambda e: e.activation(out=s[:, 6:7], in_=s[:, 5:6], func=AF.Exp), reads=[sk], writes=[sk])
            S.op("dve", lambda e: e.scalar_tensor_tensor(out=s[:, 7:8], in0=s[:, 6:7], scalar=1.0, in1=s[:, 2:3], op0=ALU.add,
                                                         op1=ALU.mult), reads=[sk], writes=[sk])
            S.op("dve", lambda e: e.reciprocal(out=s[:, 7:8], in_=s[:, 7:8]), reads=[sk], writes=[sk])
            S.op("dve", lambda e: e.tensor_tensor(out=s[:, 6:7], in0=s[:, 6:7], in1=s[:, 7:8], op=ALU.mult), reads=[sk], writes=[sk])
            S.op("dve", lambda e: e.tensor_scalar(out=wtall[:, t, :], in0=o1[:], scalar1=s[:, 7:8], scalar2=None, op0=ALU.mult),
                 reads=[o1k, sk], writes=[("wt", t)])
            S.op("dve", lambda e: e.scalar_tensor_tensor(out=wtall[:, t, :], in0=o2[:], scalar=s[:, 6:7], in1=wtall[:, t, :],
                                                         op0=ALU.mult, op1=ALU.add), reads=[o2k, sk, ("wt", t)], writes=[("wt", t)])

        emit_norm(S, nc, K, tiles, lambda i: T.x1[tl[i] * 128:(tl[i] + 1) * 128, :], AB, h2T, es, want32=router)
        for c in range(16):
            S.dma("sp", lambda e, c=c: e.dma_start(out=T.h2T[c * 128:(c + 1) * 128, :], in_=h2T[:, c, :]), ("h2o", c % 4),
                  reads=[("hT", t, half) for t in tl for half in range(2)], writes=[("h2Td", c)])
        S.dma("sp", lambda e: e.dma_start(out=T.wt[:, :], in_=wtall[:].rearrange("p t n -> p (t n)")), "wto",
              reads=[("wt", t) for t in tl], writes=["wtd"])
        S.flush("D1_%d" % l)


def prep_misc(w_out, w_rg, b_rg, w_re, b_re):
    wo = np.empty((DEPTH, 4, 128, 16 * 512), np.float32)
    for l in range(DEPTH):
        for g in range(4):
            wo[l, g] = w_out[l][:, g * 512:(g + 1) * 512].reshape(16, 128, 512).transpose(1, 0, 2).reshape(128, -1)
    wr = np.concatenate([w_rg, w_re], axis=-1)
    wr = wr.reshape(DEPTH, 16, 128, 36).transpose(0, 2, 1, 3).reshape(DEPTH * 128, 16 * 36)
    br = np.concatenate([b_rg, b_re], axis=-1)
    return wo.reshape(-1, 16 * 512), np.ascontiguousarray(wr).astype(np.float32), np.ascontiguousarray(br).astype(np.float32)


def phase_D2(S, nc, K, T, l, with_ctx, nexp=NEXP):
    from contextlib import ExitStack
    t0 = 0 if with_ctx else 2
    tl = list(range(t0, NOWN))
    parts = [tl[i:i + 6] for i in range(0, len(tl), 6)]
    with ExitStack() as es:
        sb = lambda name, shape, dt: es.enter_context(nc.sbuf_tensor(_u(name), shape, dt))
        pst = lambda name, shape, dt: es.enter_context(nc.psum_tensor(_u(name), shape, dt))
        wt = sb("wtl", [128, NOWN, 32], F32)
        S.dma("sp", lambda e: e.dma_start(out=wt[:].rearrange("p t n -> p (t n)"), in_=T.wt[:, :]), "wtl", writes=["wtl"])
        h2 = sb("h2p", [128, 16, 768], BF16)
        acc = sb("facc", [128, 6, D], F32)
        w1r = Ring("w1", [sb("w1_%d" % i, [128, 16, 512], BF16) for i in range(2)])
        w3r = Ring("w3", [sb("w3_%d" % i, [128, 16, 512], BF16) for i in range(2)])
        w2r = Ring("w2", [sb("w2_%d" % i, [128, 4, D], BF16) for i in range(2)])
        aTr = Ring("aT", [sb("aT_%d" % i, [128, 4, 512], BF16) for i in range(2)])
        sgr = Ring("sgm", [sb("sgm_%d" % i, [128, 512], F32) for i in range(2)])
        pg = Ring("pg", [pst("pg%d" % i, [128, 512], F32) for i in range(2)])
        pu = Ring("pu", [pst("pu%d" % i, [128, 512], F32) for i in range(2)])
        po = Ring("po", [pst("po%d" % i, [128, 512], F32) for i in range(3)])
        for pi, part in enumerate(parts):
            ntp = len(part)
            c0 = part[0] * 128
            S.dma("sp", lambda e, c0=c0, ntp=ntp: e.dma_start(
                out=h2[:, :, 0:ntp * 128], in_=T.h2T[:, c0:c0 + ntp * 128].rearrange("(c p) n -> p c n", p=128)),
                "h2p", writes=["h2p"])
            S.op("dve", lambda e: e.memset(acc[:], 0.0), writes=[("acc", i) for i in range(6)])
            groups = [(0, min(4, ntp))] + ([(4, ntp - 4)] if ntp > 4 else [])
            for ex in range(nexp):
                w1, w1k = w1r.next()
                w3, w3k = w3r.next()
                w2, w2k = w2r.next()
                r0 = (l * nexp + ex) * 128
                S.dma("pool", lambda e, w1=w1, r0=r0: e.dma_start(out=w1[:].rearrange("p k n -> p (k n)"),
                                                                in_=T.w1m[r0:r0 + 128, :]), ("w1", w1k[1]), writes=[w1k])
                S.dma("pool", lambda e, w3=w3, r0=r0: e.dma_start(out=w3[:].rearrange("p k n -> p (k n)"),
                                                                in_=T.w3m[r0:r0 + 128, :]), ("w3", w3k[1]), writes=[w3k])
                S.dma("pool", lambda e, w2=w2, r0=r0: e.dma_start(out=w2[:].rearrange("p k n -> p (k n)"),
                                                                in_=T.w2m[r0:r0 + 128, :]), ("w2", w2k[1]), writes=[w2k])
                for (g0, gn) in groups:
                    n = gn * 128
                    cs = g0 * 128
                    aT, aTk = aTr.next()
                    for j in range(4):
                        p1, p1k = pg.next()
                        p3, p3k = pu.next()

                        def mm(e, p=p1, w=w1, j=j, cs=cs, n=n):
                            r = None
                            for k in range(16):
                                r = e.matmul(p[:, 0:n], lhsT=w[:, k, j * 128:(j + 1) * 128], rhs=h2[:, k, cs:cs + n],
                                             start=(k == 0), stop=(k == 15))
                            return r
                        S.op("pe", mm, reads=[w1k, "h2p"], writes=[p1k])

                        def mm3(e, p=p3, w=w3, j=j, cs=cs, n=n):
                            r = None
                            for k in range(16):
                                r = e.matmul(p[:, 0:n], lhsT=w[:, k, j * 128:(j + 1) * 128], rhs=h2[:, k, cs:cs + n],
                                             start=(k == 0), stop=(k == 15))
                            return r
                        S.op("pe", mm3, reads=[w3k, "h2p"], writes=[p3k])
                        sg, sgk = sgr.next()
                        S.op("act", lambda e, sg=sg, p1=p1, n=n: e.activation(out=sg[:, 0:n], in_=p1[:, 0:n], func=AF.Silu),
                             reads=[p1k], writes=[sgk])
                        S.op("dve", lambda e, aT=aT, sg=sg, p3=p3, j=j, n=n: e.tensor_tensor(
                            out=aT[:, j, 0:n], in0=p3[:, 0:n], in1=sg[:, 0:n], op=ALU.mult), reads=[p3k, sgk], writes=[(aTk, j)])
                    for ti in range(gn):
                        tix = g0 + ti
                        t = part[tix]
                        for nn in range(4):
                            p, pk = po.next()

                            def mmo(e, p=p, aT=aT, w2=w2, ti=ti, nn=nn):
                                r = None
                                for j in range(4):
                                    r = e.matmul(p[:], lhsT=aT[:, j, ti * 128:(ti + 1) * 128], rhs=w2[:, j, nn * 512:(nn + 1) * 512],
                                                 start=(j == 0), stop=(j == 3))
                                return r
                            S.op("pe", mmo, reads=[(aTk, j) for j in range(4)] + [w2k], writes=[pk])
                            dst = acc[:, tix, nn * 512:(nn + 1) * 512]
                            S.op("dve", lambda e, p=p, dst=dst, t=t, ex=ex: e.scalar_tensor_tensor(
                                out=dst, in0=p[:], scalar=wt[:, t, ex:ex + 1], in1=dst, op0=ALU.mult, op1=ALU.add),
                                reads=[pk, "wtl", ("acc", tix)], writes=[("acc", tix)])
            for tix, t in enumerate(part):
                S.dma("sp", lambda e, tix=tix, t=t: e.dma_start(out=T.facc[t * 128:(t + 1) * 128, :], in_=acc[:, tix, :]),
                      ("fao", tix), reads=[("acc", tix)], writes=[("faccd", t)])
        S.flush("D2_%d" % l)


def phase_D3(S, nc, K, T, l, with_ctx, last):
    from contextlib import ExitStack
    t0 = 0 if with_ctx else 2
    with ExitStack() as es:
        sb = lambda name, shape, dt: es.enter_context(nc.sbuf_tensor(_u(name), shape, dt))
        ga = load_bc(S, nc, T, l, MOD_GA2, es, "ga2")
        if last:
            gf = sb("gfin", [128, D], F32)
            S.dma("sp", lambda e: e.dma_start(out=gf[:], in_=bcast_row(T.g_final[0:1, :], D)), "gfin", writes=["gfin"])
            jb = sb("d3junk", [128, D], BF16)
            st = Ring("d3st", [sb("d3st%d" % i, [128, 2], F32) for i in range(2)])
        xb = Ring("d3x", [sb("d3x%d" % i, [128, D], F32) for i in range(2)])
        fb = Ring("d3f", [sb("d3f%d" % i, [128, D], F32) for i in range(2)])
        for t in range(t0, NOWN):
            x, xk = xb.next()
            f, fk = fb.next()
            S.dma("sp", lambda e, x=x, t=t: e.dma_start(out=x[:], in_=T.x1[t * 128:(t + 1) * 128, :]), ("d3x", xk[1]), writes=[xk])
            S.dma("sp", lambda e, f=f, t=t: e.dma_start(out=f[:], in_=T.facc[t * 128:(t + 1) * 128, :]), ("d3f", fk[1]), writes=[fk])
            g = ga[tile_kind(t)]
            S.op("dve", lambda e, f=f, g=g: e.tensor_tensor(out=f[:], in0=f[:], in1=g[:], op=ALU.mult),
                 reads=[fk, ("ga2", tile_kind(t))], writes=[fk])
            S.op("pool", lambda e, f=f, x=x: e.tensor_tensor(out=f[:], in0=f[:], in1=x[:], op=ALU.add), reads=[fk, xk], writes=[fk])
            if not last:
                S.dma("sp", lambda e, f=f, t=t: e.dma_start(out=T.xs2[t * 128:(t + 1) * 128, :], in_=f[:]), ("d3o", fk[1]),
                      reads=[fk], writes=[("xs2", t)])
                if t >= 16:
                    S.dma("sp", lambda e, f=f, t=t: e.dma_start(out=T.xsend[(t - 16) * 128:(t - 15) * 128, :], in_=f[:]),
                          ("d3o2", fk[1]), reads=[fk], writes=[("xsend", t)])
            else:
                s, sk = st.next()
                S.op("act", lambda e, f=f, s=s: e.activation(out=jb[:], in_=f[:], func=AF.Square, accum_out=s[:, 0:1]),
                     reads=[fk], writes=[sk, "d3junk"])
                S.op("dve", lambda e, s=s: e.tensor_scalar(out=s[:, 1:2], in0=s[:, 0:1], scalar1=1.0 / D, scalar2=EPS,
                                                           op0=ALU.mult, op1=ALU.add), reads=[sk], writes=[sk])
                S.op("act", lambda e, s=s: e.activation(out=s[:, 1:2], in_=s[:, 1:2], func=AF.Sqrt), reads=[sk], writes=[sk])
                S.op("dve", lambda e, s=s: e.reciprocal(out=s[:, 1:2], in_=s[:, 1:2]), reads=[sk], writes=[sk])
                S.op("dve", lambda e, f=f, s=s: e.scalar_tensor_tensor(out=f[:], in0=f[:], scalar=s[:, 1:2], in1=gf[:],
                                                                       op0=ALU.mult, op1=ALU.mult),
                     reads=[fk, sk, "gfin"], writes=[fk])
                S.dma("sp", lambda e, f=f, t=t: e.dma_start(out=T.out[(t - 2) * 128:(t - 1) * 128, :], in_=f[:]), ("d3o", fk[1]),
                      reads=[fk], writes=[("out", t)])
        S.flush("D3_%d" % l)


PAIRS = [[0, 1], [2, 3], [4, 5], [6, 7]]


def coll(S, fn, key, reads, writes):
    return S._emit("pool", fn, S.dsem(key), 1, reads, writes)


def phase_xchg_x(S, nc, K, T):
    from contextlib import ExitStack
    with ExitStack() as es:
        sb = lambda name, shape, dt: es.enter_context(nc.sbuf_tensor(_u(name), shape, dt))
        pst = lambda name, shape, dt: es.enter_context(nc.psum_tensor(_u(name), shape, dt))
        coll(S, lambda e: e.collective_compute("AllGather", ALU.bypass, replica_groups=PAIRS, ins=[T.xsend.opt()],
                                               outs=[T.xrecv.opt()]), "ccx", [], ["xrecv"])
        pf = sb("pflag", [128, 2], F32)
        S.dma("sp", lambda e: e.dma_start(out=pf[:], in_=T.pflag[:, :]), "pflag", writes=["pflag"])
        s0r = Ring("xs0", [sb("xs0_%d" % i, [128, D], F32) for i in range(2)])
        s1r = Ring("xs1", [sb("xs1_%d" % i, [128, D], F32) for i in range(2)])
        pp = Ring("xpp", [pst("xpp%d" % i, [128, 512], F32) for i in range(4)])
        for (ht, pr0) in ((18, 128), (19, 0)):
            a, ak = s0r.next()
            b, bk = s1r.next()
            S.dma("sp", lambda e, a=a, pr0=pr0: e.dma_start(out=a[:], in_=T.xrecv[pr0:pr0 + 128, :]), ("xs0", ak[1]),
                  reads=["xrecv"], writes=[ak])
            S.dma("sp", lambda e, b=b, pr0=pr0: e.dma_start(out=b[:], in_=T.xrecv[256 + pr0:256 + pr0 + 128, :]), ("xs1", bk[1]),
                  reads=["xrecv"], writes=[bk])
            S.op("dve", lambda e, a=a: e.tensor_scalar(out=a[:], in0=a[:], scalar1=pf[:, 0:1], scalar2=None, op0=ALU.mult),
                 reads=[ak, "pflag"], writes=[ak])
            S.op("dve", lambda e, a=a, b=b: e.scalar_tensor_tensor(out=a[:], in0=b[:], scalar=pf[:, 1:2], in1=a[:], op0=ALU.mult,
                                                                   op1=ALU.add), reads=[ak, bk, "pflag"], writes=[ak])
            for nn in range(4):
                p, pk = pp.next()
                S.op("pe", lambda e, p=p, a=a, nn=nn: e.matmul(p[:], lhsT=K.anti32, rhs=a[:, nn * 512:(nn + 1) * 512], start=True,
                                                              stop=True), reads=[ak], writes=[pk])
                S.op("act", lambda e, p=p, b=b, nn=nn: e.copy(out=b[:, nn * 512:(nn + 1) * 512], in_=p[:]), reads=[pk, bk],
                     writes=[(bk, nn)])
            S.dma("sp", lambda e, b=b, ht=ht: e.dma_start(out=T.xs2[ht * 128:(ht + 1) * 128, :], in_=b[:]), ("xso", bk[1]),
                  reads=[(bk, nn) for nn in range(4)], writes=[("xs2", ht)])
        S.flush("xchg_x")


def phase_xchg_h(S, nc, K, T):
    from contextlib import ExitStack
    with ExitStack() as es:
        sb = lambda name, shape, dt: es.enter_context(nc.sbuf_tensor(_u(name), shape, dt))
        coll(S, lambda e: e.collective_compute("AllGather", ALU.bypass, replica_groups=PAIRS, ins=[T.hsend.opt()],
                                               outs=[T.hrecv2.opt()]), "cch", [], ["hrecv2"])
        pf = sb("pflagh", [128, 2], F32)
        S.dma("sp", lambda e: e.dma_start(out=pf[:], in_=T.pflag[:, :]), "pflagh", writes=["pflagh"])
        a = sb("hxa", [128, 4, 128], F32)
        b = sb("hxb", [128, 4, 128], F32)
        S.dma("sp", lambda e: e.dma_start(out=a[:], in_=T.hrecv2[0:512, :].rearrange("(h p) n -> p h n", p=128)), "hxa",
              reads=["hrecv2"], writes=["hxa"])
        S.dma("sp", lambda e: e.dma_start(out=b[:], in_=T.hrecv2[512:1024, :].rearrange("(h p) n -> p h n", p=128)), "hxb",
              reads=["hrecv2"], writes=["hxb"])
        S.op("dve", lambda e: e.tensor_scalar(out=a[:], in0=a[:], scalar1=pf[:, 0:1], scalar2=None, op0=ALU.mult),
             reads=["hxa", "pflagh"], writes=["hxa"])
        S.op("dve", lambda e: e.scalar_tensor_tensor(out=a[:], in0=b[:], scalar=pf[:, 1:2], in1=a[:], op0=ALU.mult, op1=ALU.add),
             reads=["hxa", "hxb", "pflagh"], writes=["hxa"])
        S.dma("sp", lambda e: e.dma_start(out=T.hrecv[:, :].rearrange("(h p) n -> p h n", p=128), in_=a[:]), "hxo",
              reads=["hxa"], writes=["hrecv"])
        S.flush("xchg_h")


def phase_mod(S, nc, K, T):
    from contextlib import ExitStack
    with ExitStack() as es:
        sb = lambda name, shape, dt: es.enter_context(nc.sbuf_tensor(_u(name), shape, dt))
        pst = lambda name, shape, dt: es.enter_context(nc.psum_tensor(_u(name), shape, dt))
        cT = sb("cT", [128, 16, 5], F32)
        S.dma("sp", lambda e: e.dma_start(out=cT[:].rearrange("p k r -> p (k r)"), in_=T.cT[:, :]), "cT", writes=["cT"])
        S.op("act", lambda e: e.activation(out=cT[:], in_=cT[:], func=AF.Silu), reads=["cT"], writes=["cT"])
        ba = sb("bada", [1, DEPTH * 1536], F32)
        S.dma("sp", lambda e: e.dma_start(out=ba[:], in_=T.b_ada[0:1, :]), "bada", writes=["bada"])
        wa = sb("wada", [128, 16, 1536], F32)
        ml = sb("modloc", [5, DEPTH * 1536], F32)
        pp = Ring("mpp", [pst("mpp%d" % i, [128, 512], F32) for i in range(3)])
        for l in range(DEPTH):
            S.dma("sp", lambda e, l=l: e.dma_start(out=wa[:].rearrange("p k n -> p (k n)"), in_=T.w_adam[l * 128:(l + 1) * 128, :]),
                  "wada", writes=["wada"])
            for nn in range(3):
                p, pk = pp.next()

                def mm(e, p=p, nn=nn, l=l):
                    for k in range(16):
                        e.matmul(p[0:5, :], lhsT=cT[:, k, :], rhs=wa[:, k, nn * 512:(nn + 1) * 512], start=(k == 0), stop=False)
                    return e.matmul(p[0:5, :], lhsT=K.ones32[0:1, 0:5], rhs=ba[0:1, l * 1536 + nn * 512:l * 1536 + (nn + 1) * 512],
                                    start=False, stop=True)
                S.op("pe", mm, reads=["cT", "wada", "bada"], writes=[pk])
                S.op("act", lambda e, p=p, nn=nn, l=l: e.copy(out=ml[:, l * 1536 + nn * 512:l * 1536 + (nn + 1) * 512], in_=p[0:5, :]),
                     reads=[pk], writes=[("ml", l, nn)])
        allml = [("ml", l, nn) for l in range(DEPTH) for nn in range(3)]
        S.dma("sp", lambda e: e.dma_start(out=T.msend[:, :], in_=ml[:]), "mso", reads=allml, writes=["msend"])
        coll(S, lambda e: e.collective_compute("AllGather", ALU.bypass, replica_groups=[list(range(8))], ins=[T.msend.opt()],
                                               outs=[T.mrecv.opt()]), "ccm", ["msend"], ["mrecv"])
        S.flush("mod1")
    with ExitStack() as es:
        sb = lambda name, shape, dt: es.enter_context(nc.sbuf_tensor(_u(name), shape, dt))
        pst = lambda name, shape, dt: es.enter_context(nc.psum_tensor(_u(name), shape, dt))
        pp = Ring("mpp2", [pst("mpp2_%d" % i, [128, 512], F32) for i in range(3)])
        G = sb("modG", [5, 8, DEPTH * 1536], F32)
        S.dma("sp", lambda e: e.dma_start(out=G[:], in_=T.mrecv[:, :].rearrange("(r f) n -> f r n", f=5)), "modG",
              reads=["mrecv"], writes=["modG"])
        bs = sb("bsel", [5, 2], F32)
        S.dma("sp", lambda e: e.dma_start(out=bs[:], in_=T.bsel[:, :]), "bsel", writes=["bsel"])
        ms = sb("modsel", [2, DEPTH, 8 * 1536], F32)
        for l in range(DEPTH):
            for r in range(8):
                for nn in range(3):
                    p, pk = pp.next()
                    S.op("pe", lambda e, p=p, l=l, r=r, nn=nn: e.matmul(
                        p[0:2, :], lhsT=bs[:, :], rhs=G[:, r, l * 1536 + nn * 512:l * 1536 + (nn + 1) * 512], start=True, stop=True),
                        reads=["modG", "bsel"], writes=[pk])
                    dst = ms[:, l, r * 1536 + nn * 512:r * 1536 + (nn + 1) * 512]
                    if (r + nn) % 2 == 0:
                        S.op("act", lambda e, p=p, dst=dst: e.copy(out=dst, in_=p[0:2, :]), reads=[pk], writes=[("ms", l, r, nn)])
                    else:
                        S.op("dve", lambda e, p=p, dst=dst: e.tensor_copy(out=dst, in_=p[0:2, :]), reads=[pk],
                             writes=[("ms", l, r, nn)])
        allms = [("ms", l, r, nn) for l in range(DEPTH) for r in range(8) for nn in range(3)]
        S.dma("sp", lambda e: e.dma_start(out=T.mod[:, :].rearrange("(l k) n -> k l n", k=2), in_=ms[:]), "mso2", reads=allms,
              writes=["mod"])
        S.flush("mod")


def prep_experts(wg, wu, wd):
    ne = wg.shape[1]
    w1 = np.ascontiguousarray(wg.reshape(DEPTH * ne, 16, 128, 512).transpose(0, 2, 1, 3)).reshape(-1, 16 * 512)
    w3 = np.ascontiguousarray(wu.reshape(DEPTH * ne, 16, 128, 512).transpose(0, 2, 1, 3)).reshape(-1, 16 * 512)
    w2 = np.ascontiguousarray(wd.reshape(DEPTH * ne, 4, 128, D).transpose(0, 2, 1, 3)).reshape(-1, 4 * D)
    return w1, w3, w2


_NC_CACHE = {}


def make_in_maps(x, c, ctx, c_ctx, w_ada, b_ada, g_mix, g_ffn, w_in, conv_w, conv_b, conv_ln_g, conv_ln_b, na_rpb,
                 hgrn_lb, hgrn_norm_g, w_out, w_router_group, b_router_group, w_router_expert, b_router_expert,
                 w_exp_gate, w_exp_up, w_exp_down, g_final):
    f = lambda a: np.ascontiguousarray(np.asarray(a, dtype=np.float32))
    x, c, ctx, c_ctx, w_ada, b_ada = f(x), f(c), f(ctx), f(c_ctx), f(w_ada), f(b_ada)
    w_in = f(w_in)
    consts = host_consts()
    shared = {}
    for odd in (False, True):
        wf, wt = prep_w_in(w_in, odd)
        cwT, cvec = prep_conv(f(conv_w), f(conv_b), f(conv_ln_g), f(conv_ln_b), odd)
        nab = prep_na_bias(f(na_rpb), odd)
        hlb, hng = prep_hgrn(f(hgrn_lb), f(hgrn_norm_g), odd)
        shared[odd] = dict(w_feat=wf, w_tokm=wt, conv_wT=cwT, conv_vec=cvec, na_bias=nab, hlb=hlb, hnorm_g=hng)
    wom, wrm, brm = prep_misc(f(w_out), f(w_router_group), f(b_router_group), f(w_router_expert), f(b_router_expert))
    w1m, w3m, w2m = prep_experts_sparse(f(w_exp_gate), f(w_exp_up), f(w_exp_down))
    C = np.concatenate([c, c_ctx[None, :]], axis=0)
    cT = np.ascontiguousarray(C.reshape(5, 16, 128).transpose(2, 1, 0)).reshape(128, 80)
    in_maps = []
    for core in range(8):
        b, s = core // 2, core % 2
        odd = s == 1
        m = dict(shared[odd])
        m["x_in"] = core_tokens(x[b], ctx[b], odd)
        m["consts"] = consts
        m["g_mix"] = f(g_mix)
        m["g_ffn"] = f(g_ffn)
        m["g_final"] = f(g_final).reshape(1, D)
        m["w_outm"] = wom
        m["w_r"] = wrm
        m["b_r"] = brm
        m["w1m"], m["w3m"], m["w2m"] = w1m, w3m, w2m
        m["cT"] = cT
        sl = slice(core * 1536, (core + 1) * 1536)
        wa = np.empty((DEPTH, 128, 16 * 1536), np.float32)
        for l in range(DEPTH):
            wa[l] = w_ada[l][:, sl].reshape(16, 128, 1536).transpose(1, 0, 2).reshape(128, -1)
        m["w_adam"] = wa.reshape(DEPTH * 128, 16 * 1536)
        m["b_ada"] = np.ascontiguousarray(np.concatenate([b_ada[l][sl] for l in range(DEPTH)])[None, :])
        bs = np.zeros((5, 2), np.float32)
        bs[b, 0] = 1.0
        bs[4, 1] = 1.0
        m["bsel"] = bs
        pf = np.zeros((128, 2), np.float32)
        pf[:, 0 if odd else 1] = 1.0
        m["pflag"] = pf
        in_maps.append(m)
    return in_maps


def kernel(**inputs):
    in_maps = make_in_maps(**inputs)
    if "nc" not in _NC_CACHE:
        _NC_CACHE["nc"] = build({})
    nc = _NC_CACHE["nc"]
    res = run_bass_kernel_spmd(nc, in_maps, core_ids=list(range(8)))
    out = np.empty((4, 4096, D), np.float32)
    for core in range(8):
        b, s = core // 2, core % 2
        o = np.asarray(res.results[core]["out"], dtype=np.float32)
        if s == 0:
            out[b, 0:2048] = o
        else:
            out[b, 2048:4096] = o[::-1]
    return out


U32 = mybir.dt.uint32
_NE = [NEXP]


def RWv():
    return DEPTH * _NE[0] * 128
OOB = 1.0e6


def nblocks(with_ctx):
    ntok = (NOWN if with_ctx else 16) * 128
    return (2 * ntok) // 128 + NEXP


def phase_D1s(S, nc, K, T, l, with_ctx):
    from contextlib import ExitStack
    NB = nblocks(with_ctx)
    with ExitStack() as es:
        sb = lambda name, shape, dt: es.enter_context(nc.sbuf_tensor(_u(name), shape, dt))
        pst = lambda name, shape, dt: es.enter_context(nc.psum_tensor(_u(name), shape, dt))
        h2 = sb("h2tok", [128, NOWN, D], BF16)
        o1all = sb("o1all", [128, NOWN, 32], F32)
        o2all = sb("o2all", [128, NOWN, 32], F32)
        aall = sb("aall", [128, NOWN, 2], F32)
        wr = sb("wr", [128, 16, 36], F32)
        br = sb("brb", [128, 36], F32)
        S.dma("sp", lambda e: e.dma_start(out=wr[:].rearrange("p k n -> p (k n)"), in_=T.w_r[l * 128:(l + 1) * 128, :]), "wr",
              writes=["wr"])
        S.dma("sp", lambda e: e.dma_start(out=br[:], in_=bcast_row(T.b_r[l:l + 1, :], 36)), "brb", writes=["brb"])
        t0 = 0 if with_ctx else 2
        tl = list(range(t0, NOWN))
        nt = len(tl)
        with ExitStack() as es2:
            sb2 = lambda name, shape, dt: es2.enter_context(nc.sbuf_tensor(_u(name), shape, dt))
            pst2 = lambda name, shape, dt: es2.enter_context(nc.psum_tensor(_u(name), shape, dt))
            AB = load_mod_AB(S, nc, T, l, (MOD_SH2, MOD_S2), T.g_ffn[l:l + 1, :], es2, "m2")
            ptr = Ring("rptr", [pst2("rptr%d" % i, [128, 512], F32) for i in range(2)])
            pr = Ring("rpr", [pst2("rpr%d" % i, [128, 36], F32) for i in range(1)])
            hT32 = Ring("rhT32", [sb2("rhT32_%d" % i, [128, 16, 128], F32) for i in range(2)])
            sm = lambda nm, w: Ring(nm, [sb2("%s%d" % (nm, i), [128, w], F32) for i in range(2)])
            lgb, st1, gmb, egb, pen, lem, lem2 = (sm("rlg", 36), sm("rst", 8), sm("rgm", 4), sm("reg", 4), sm("rpen", 4),
                                                  sm("rlem", 32), sm("rlem2", 32))

            def router(i, h32, h32k):
                t = tl[i]
                hT, hTk = hT32.next()
                for g in range(4):
                    p, pk = ptr.next()

                    def tr(e, p=p, g=g):
                        r = None
                        for j in range(4):
                            kk = g * 4 + j
                            r = e.transpose(out=p[:, j * 128:(j + 1) * 128], in_=h32[:, kk * 128:(kk + 1) * 128],
                                            identity=K.ident32)
                        return r
                    S.op("pe", tr, reads=[h32k], writes=[pk])
                    S.op("act", lambda e, p=p, g=g: e.copy(out=hT[:, g * 4:(g + 1) * 4, :],
                                                           in_=p[:].rearrange("p (j n) -> p j n", j=4)),
                         reads=[pk], writes=[(hTk, g)])
                p, pk = pr.next()

                def mm(e, p=p):
                    r = None
                    for k in range(16):
                        r = e.matmul(p[:], lhsT=hT[:, k, :], rhs=wr[:, k, :], start=(k == 0), stop=(k == 15))
                    return r
                S.op("pe", mm, reads=[(hTk, g) for g in range(4)] + ["wr"], writes=[pk])
                lg, lgk = lgb.next()
                S.op("dve", lambda e: e.tensor_tensor(out=lg[:], in0=p[:], in1=br[:], op=ALU.add), reads=[pk, "brb"], writes=[lgk])
                s, sk = st1.next()
                S.op("dve", lambda e: e.reduce_max(out=s[:, 0:1], in_=lg[:, 0:4], axis=AX.X), reads=[lgk], writes=[sk])
                S.op("dve", lambda e: e.tensor_scalar(out=s[:, 1:2], in0=s[:, 0:1], scalar1=-1.0, scalar2=None, op0=ALU.mult),
                     reads=[sk], writes=[sk])
                gm, gmk = gmb.next()
                S.op("dve", lambda e: e.tensor_scalar(out=gm[:], in0=lg[:, 0:4], scalar1=s[:, 0:1], scalar2=None,
                                                      op0=ALU.is_equal), reads=[lgk, sk], writes=[gmk])
                eg, egk = egb.next()
                S.op("act", lambda e: e.activation(out=eg[:], in_=lg[:, 0:4], func=AF.Exp, bias=s[:, 1:2], accum_out=s[:, 2:3]),
                     reads=[lgk, sk], writes=[egk, sk])
                pn, pnk = pen.next()
                S.op("dve", lambda e: e.tensor_scalar(out=pn[:], in0=gm[:], scalar1=BIG, scalar2=-BIG, op0=ALU.mult, op1=ALU.add),
                     reads=[gmk], writes=[pnk])
                le, lek = lem.next()
                S.op("dve", lambda e: e.tensor_tensor(out=le[:].rearrange("p (g n) -> p g n", g=4),
                                                      in0=lg[:, 4:36].rearrange("p (g n) -> p g n", g=4),
                                                      in1=_bc_last(pn[:].rearrange("p (g o) -> p g o", o=1), 8), op=ALU.add),
                     reads=[lgk, pnk], writes=[lek])
                S.op("dve", lambda e: e.reduce_max(out=s[:, 3:4], in_=le[:], axis=AX.X), reads=[lek, sk], writes=[sk])
                o1 = o1all[:, t, :]
                o2 = o2all[:, t, :]
                S.op("dve", lambda e: e.tensor_scalar(out=o1, in0=le[:], scalar1=s[:, 3:4], scalar2=None, op0=ALU.is_equal),
                     reads=[lek, sk], writes=[("o1", t)])
                l2, l2k = lem2.next()
                S.op("dve", lambda e: e.scalar_tensor_tensor(out=l2[:], in0=o1, scalar=-BIG, in1=le[:], op0=ALU.mult, op1=ALU.add),
                     reads=[("o1", t), lek], writes=[l2k])
                S.op("dve", lambda e: e.reduce_max(out=s[:, 4:5], in_=l2[:], axis=AX.X), reads=[l2k, sk], writes=[sk])
                S.op("dve", lambda e: e.tensor_scalar(out=o2, in0=l2[:], scalar1=s[:, 4:5], scalar2=None, op0=ALU.is_equal),
                     reads=[l2k, sk], writes=[("o2", t)])
                S.op("dve", lambda e: e.tensor_tensor(out=s[:, 5:6], in0=s[:, 4:5], in1=s[:, 3:4], op=ALU.subtract), reads=[sk],
                     writes=[sk])
                S.op("act", lambda e: e.activation(out=s[:, 6:7], in_=s[:, 5:6], func=AF.Exp), reads=[sk], writes=[sk])
                S.op("dve", lambda e: e.scalar_tensor_tensor(out=s[:, 7:8], in0=s[:, 6:7], scalar=1.0, in1=s[:, 2:3], op0=ALU.add,
                                                             op1=ALU.mult), reads=[sk], writes=[sk])
                S.op("dve", lambda e: e.reciprocal(out=aall[:, t, 0:1], in_=s[:, 7:8]), reads=[sk], writes=[("aa", t)])
                S.op("dve", lambda e: e.tensor_tensor(out=aall[:, t, 1:2], in0=s[:, 6:7], in1=aall[:, t, 0:1], op=ALU.mult),
                     reads=[sk, ("aa", t)], writes=[("aa", t)])

            tiles = [(t * D, tile_kind(t)) for t in tl]
            emit_norm(S, nc, K, tiles, lambda i: T.x1[tl[i] * 128:(tl[i] + 1) * 128, :], AB, None, es2, want32=router,
                      htok=lambda i: (h2[:, tl[i], :], ("h2tok", tl[i])))
            S.flush("D1s_a%d" % l)
        oe = sb("oeall", [128, NOWN, 32], BF16)
        allo = [("o1", t) for t in tl] + [("o2", t) for t in tl]
        lo, hi = tl[0], tl[-1] + 1
        S.op("dve", lambda e: e.tensor_tensor(out=oe[:, lo:hi, :], in0=o1all[:, lo:hi, :], in1=o2all[:, lo:hi, :], op=ALU.add),
             writes=["oe"])
        rall = sb("rall", [128, NOWN, 32], F32)
        pR = Ring("pR", [pst("pR%d" % i, [128, 32], F32) for i in range(3)])
        Lbf = K.cbf[:, 8, :]
        for i, t in enumerate(tl):
            p, pk = pR.next()

            def mm(e, p=p, i=i, t=t):
                r = e.matmul(p[:], lhsT=Lbf, rhs=oe[:, t, :], start=True, stop=(i == 0))
                for jj in range(i):
                    r = e.matmul(p[:], lhsT=K.ones_bf, rhs=oe[:, tl[jj], :], start=False, stop=(jj == i - 1))
                return r
            S.op("pe", mm, reads=["oe"], writes=[pk])
            S.op("act", lambda e, p=p, t=t: e.copy(out=rall[:, t, :], in_=p[:]), reads=[pk], writes=[("rall", t)])
        p, pk = pR.next()

        def mmc(e, p=p):
            r = None
            for jj in range(nt):
                r = e.matmul(p[:], lhsT=K.ones_bf, rhs=oe[:, tl[jj], :], start=(jj == 0), stop=(jj == nt - 1))
            return r
        S.op("pe", mmc, reads=["oe"], writes=[pk])
        cnt = sb("cnt", [128, 32], F32)
        S.op("act", lambda e: e.copy(out=cnt[:], in_=p[:]), reads=[pk], writes=["cnt"])
        cmp1 = sb("cmp1", [128, 32, 18], F32)
        thr = K.c32[:, 9, 0:18]
        thr_bc = bass.AP(tensor=thr.tensor, offset=thr.offset, ap=[list(thr.ap[0]), [0, 32], [1, 18]])
        S.op("dve", lambda e: e.tensor_tensor(out=cmp1[:], in0=_bc_last(cnt[:].rearrange("p (a o) -> p a o", o=1), 18), in1=thr_bc,
                                              op=ALU.is_gt), reads=["cnt"], writes=["cmp1"])
        nb = sb("nbk", [128, 32], F32)
        S.op("dve", lambda e: e.reduce_sum(out=nb[:], in_=cmp1[:], axis=AX.X), reads=["cmp1"], writes=["nbk"])
        pend = sb("pend", [128, 32], F32)
        S.op("dve", lambda e: e.tensor_tensor_scan(out=pend[:], data0=K.ones32[:, 0:32], data1=nb[:], initial=0.0, op0=ALU.mult,
                                                   op1=ALU.add), reads=["nbk"], writes=["pend"])
        pst128 = sb("pst128", [128, 32], F32)
        S.op("dve", lambda e: e.tensor_tensor(out=pst128[:], in0=pend[:], in1=nb[:], op=ALU.subtract), reads=["pend", "nbk"],
             writes=["pst128"])
        S.op("dve", lambda e: e.tensor_scalar(out=pst128[:], in0=pst128[:], scalar1=128.0, scalar2=None, op0=ALU.mult),
             reads=["pst128"], writes=["pst128"])
        slots_f = sb("slotsf", [128, NOWN, 2], F32)
        slots_u = sb("slotsu", [128, NOWN, 2], U32)
        S.op("dve", lambda e: e.memset(slots_f[:], 0.0), writes=[("slf", t) for t in range(NOWN)])
        basr = Ring("basr", [sb("basr%d" % i, [128, 32], F32) for i in range(2)])
        junkr = Ring("sjunk", [sb("sjunk%d" % i, [128, 32], F32) for i in range(2)])
        for t in tl:
            ba, bak = basr.next()
            S.op("dve", lambda e, ba=ba, t=t: e.tensor_tensor(out=ba[:], in0=rall[:, t, :], in1=pst128[:], op=ALU.add),
                 reads=[("rall", t), "pst128"], writes=[bak])
            for kk, oall in enumerate((o1all, o2all)):
                jk, jkk = junkr.next()
                S.op("dve", lambda e, ba=ba, t=t, oall=oall, jk=jk: e.tensor_tensor(out=jk[:], in0=oall[:, t, :], in1=ba[:], op=ALU.mult),
                     reads=[bak], writes=[jkk])
                S.op("dve", lambda e, t=t, kk=kk, jk=jk: e.reduce_sum(out=slots_f[:, t, kk:kk + 1], in_=jk[:], axis=AX.X),
                     reads=[jkk, ("slf", t)], writes=[("slf", t)])
        S.op("dve", lambda e: e.tensor_copy(out=slots_u[:], in_=slots_f[:]), reads=[("slf", t) for t in range(NOWN)],
             writes=["slu"])
        for t in tl:
            for kk in range(2):
                S.dma("pool", lambda e, t=t, kk=kk: e.indirect_dma_start(
                    out=T.xsort[:, :], out_offset=bass.IndirectOffsetOnAxis(ap=slots_u[:, t, kk:kk + 1], axis=0),
                    in_=h2[:, t, :], in_offset=None), ("xsc", (2 * t + kk) % 4), reads=["slu", ("h2tok", t)], writes=[("xsort", t, kk)])
        cmp2 = sb("cmp2", [128, NB, 32], F32)
        jv = K.c32[:, 9, 32:32 + NB]
        S.op("dve", lambda e: e.tensor_tensor(
            out=cmp2[:], in0=bass.AP(tensor=pend[:].tensor, offset=pend[:].offset, ap=[list(pend[:].ap[0]), [0, NB], [1, 32]]),
            in1=_bc_last(jv.rearrange("p (a o) -> p a o", o=1), 32), op=ALU.is_le), reads=["pend"], writes=["cmp2"])
        blk = sb("blk", [128, NB], F32)
        S.op("dve", lambda e: e.reduce_sum(out=blk[:], in_=cmp2[:], axis=AX.X), reads=["cmp2"], writes=["blk"])
        S.op("dve", lambda e: e.tensor_scalar(out=blk[:], in0=blk[:], scalar1=float(_NE[0] - 1), scalar2=128.0, op0=ALU.min, op1=ALU.mult),
             reads=["blk"], writes=["blk"])
        S.op("dve", lambda e: e.tensor_scalar(out=blk[:], in0=blk[:], scalar1=K.c32[:, 9, 127:128], scalar2=float(l * _NE[0] * 128),
                                              op0=ALU.add, op1=ALU.add), reads=["blk"], writes=["blk"])
        blku = sb("blku", [128, NB], U32)
        S.op("dve", lambda e: e.tensor_copy(out=blku[:], in_=blk[:]), reads=["blk"], writes=["blku"])
        S.dma("sp", lambda e: e.dma_start(out=T.blku[:, 0:NB], in_=blku[:]), "blkuo", reads=["blku"], writes=["blkud"])
        S.dma("sp", lambda e: e.dma_start(out=T.slotsu[:, :], in_=slots_u[:].rearrange("p t k -> p (t k)")), "sluo", reads=["slu"],
              writes=["slud"])
        S.dma("sp", lambda e: e.dma_start(out=T.aall[:, :], in_=aall[:].rearrange("p t k -> p (t k)")), "aao",
              reads=[("aa", t) for t in tl], writes=["aad"])
        S.flush("D1s_b%d" % l)


def phase_D2s(S, nc, K, T, l, with_ctx):
    from contextlib import ExitStack
    NB = nblocks(with_ctx)
    t0 = 0 if with_ctx else 2
    tl = list(range(t0, NOWN))
    with ExitStack() as es:
        sb = lambda name, shape, dt: es.enter_context(nc.sbuf_tensor(_u(name), shape, dt))
        pst = lambda name, shape, dt: es.enter_context(nc.psum_tensor(_u(name), shape, dt))
        blku = sb("blkul", [128, NB], U32)
        S.dma("sp", lambda e: e.dma_start(out=blku[:], in_=T.blku[:, 0:NB]), "blkul", writes=["blkul"])
        w1r = Ring("sw1", [sb("sw1_%d" % i, [128, 16, 512], BF16) for i in range(2)])
        w3r = Ring("sw3", [sb("sw3_%d" % i, [128, 16, 512], BF16) for i in range(2)])
        w2r = Ring("sw2", [sb("sw2_%d" % i, [128, 4, D], BF16) for i in range(2)])
        xtr = Ring("sxt", [sb("sxt%d" % i, [128, D], BF16) for i in range(2)])
        xTr = Ring("sxT", [sb("sxT%d" % i, [128, 16, 128], BF16) for i in range(2)])
        sgr = Ring("ssg", [sb("ssg%d" % i, [128, 512], F32) for i in range(2)])
        aTr = Ring("saT", [sb("saT%d" % i, [128, 4, 128], BF16) for i in range(2)])
        ysr = Ring("sys", [sb("sys%d" % i, [128, D], F32) for i in range(2)])
        pT = Ring("spT", [pst("spT%d" % i, [128, 1024], BF16) for i in range(2)])
        pg = Ring("spg", [pst("spg%d" % i, [128, 512], F32) for i in range(2)])
        pu = Ring("spu", [pst("spu%d" % i, [128, 512], F32) for i in range(2)])
        po = Ring("spo", [pst("spo%d" % i, [128, 512], F32) for i in range(2)])
        for j in range(NB):
            w1, w1k = w1r.next()
            w3, w3k = w3r.next()
            w2, w2k = w2r.next()
            for (w, wk, src, nm) in ((w1, w1k, T.w1m, "sw1"), (w3, w3k, T.w3m, "sw3"), (w2, w2k, T.w2m, "sw2")):
                wf = w[:].rearrange("p k n -> p (k n)")

                def ld(e, wf=wf, src=src, j=j):
                    r = []
                    for c in range(4):
                        r.append(e.indirect_dma_start(out=wf[:, c * 2048:(c + 1) * 2048], out_offset=None, in_=src[:, :],
                                                      in_offset=bass.IndirectOffsetOnAxis(ap=blku[:, j:j + 1], axis=0),
                                                      element_offset=c * RWv() * 2048))
                    return r
                S.dma("pool", ld, (nm, wk[1]), reads=["blkul"], writes=[wk], n=4)
            xt, xtk = xtr.next()
            S.dma("sp", lambda e, xt=xt, j=j: e.dma_start(out=xt[:], in_=T.xsort[j * 128:(j + 1) * 128, :]), ("sxt", xtk[1]),
                  writes=[xtk])
            xT, xTk = xTr.next()
            for half in range(2):
                p, pk = pT.next()

                def tr(e, p=p, xt=xt, half=half):
                    r = None
                    for k in range(8):
                        kk = half * 8 + k
                        r = e.transpose(out=p[:, k * 128:(k + 1) * 128], in_=xt[:, kk * 128:(kk + 1) * 128], identity=K.ident_bf)
                    return r
                S.op("pe", tr, reads=[xtk], writes=[pk])
                if half == 0:
                    S.op("act", lambda e, p=p, xT=xT: e.copy(out=xT[:, 0:8, :], in_=p[:].rearrange("p (k n) -> p k n", k=8)),
                         reads=[pk], writes=[(xTk, 0)])
                else:
                    S.op("dve", lambda e, p=p, xT=xT: e.tensor_copy(out=xT[:, 8:16, :], in_=p[:].rearrange("p (k n) -> p k n", k=8)),
                         reads=[pk], writes=[(xTk, 1)])
            p1, p1k = pg.next()
            p3, p3k = pu.next()

            def mg(e, p=p1, w=w1, xT=xT):
                r = None
                for jd in range(4):
                    for k in range(16):
                        r = e.matmul(p[:, jd * 128:(jd + 1) * 128], lhsT=w[:, k, jd * 128:(jd + 1) * 128], rhs=xT[:, k, :],
                                     start=(k == 0), stop=(k == 15))
                return r
            S.op("pe", mg, reads=[w1k, (xTk, 0), (xTk, 1)], writes=[p1k])

            def mu(e, p=p3, w=w3, xT=xT):
                r = None
                for jd in range(4):
                    for k in range(16):
                        r = e.matmul(p[:, jd * 128:(jd + 1) * 128], lhsT=w[:, k, jd * 128:(jd + 1) * 128], rhs=xT[:, k, :],
                                     start=(k == 0), stop=(k == 15))
                return r
            S.op("pe", mu, reads=[w3k, (xTk, 0), (xTk, 1)], writes=[p3k])
            sg, sgk = sgr.next()
            S.op("act", lambda e, sg=sg, p1=p1: e.activation(out=sg[:], in_=p1[:], func=AF.Silu), reads=[p1k], writes=[sgk])
            aT, aTk = aTr.next()
            S.op("dve", lambda e, aT=aT, sg=sg, p3=p3: e.tensor_tensor(out=aT[:].rearrange("p j n -> p (j n)"), in0=p3[:], in1=sg[:],
                                                                       op=ALU.mult), reads=[p3k, sgk], writes=[aTk])
            ys, ysk = ysr.next()
            for nn in range(4):
                p, pk = po.next()

                def mo(e, p=p, aT=aT, w2=w2, nn=nn):
                    r = None
                    for jd in range(4):
                        r = e.matmul(p[:], lhsT=aT[:, jd, :], rhs=w2[:, jd, nn * 512:(nn + 1) * 512], start=(jd == 0), stop=(jd == 3))
                    return r
                S.op("pe", mo, reads=[aTk, w2k], writes=[pk])
                if nn % 2 == 0:
                    S.op("act", lambda e, p=p, ys=ys, nn=nn: e.copy(out=ys[:, nn * 512:(nn + 1) * 512], in_=p[:]), reads=[pk],
                         writes=[(ysk, nn)])
                else:
                    S.op("dve", lambda e, p=p, ys=ys, nn=nn: e.tensor_copy(out=ys[:, nn * 512:(nn + 1) * 512], in_=p[:]), reads=[pk],
                         writes=[(ysk, nn)])
            S.dma("sp", lambda e, ys=ys, j=j: e.dma_start(out=T.ysd[j * 128:(j + 1) * 128, :], in_=ys[:]), ("syo", ysk[1]),
                  reads=[(ysk, nn) for nn in range(4)], writes=[("ysd", j)])
        S.flush("D2s_%d" % l)
    with ExitStack() as es:
        sb = lambda name, shape, dt: es.enter_context(nc.sbuf_tensor(_u(name), shape, dt))
        slu = sb("slul", [128, NOWN, 2], U32)
        aa = sb("aal", [128, NOWN, 2], F32)
        S.dma("sp", lambda e: e.dma_start(out=slu[:].rearrange("p t k -> p (t k)"), in_=T.slotsu[:, :]), "slul", writes=["slul"])
        S.dma("sp", lambda e: e.dma_start(out=aa[:].rearrange("p t k -> p (t k)"), in_=T.aall[:, :]), "aal", writes=["aal"])
        y1r = Ring("cy1", [sb("cy1_%d" % i, [128, D], F32) for i in range(2)])
        y2r = Ring("cy2", [sb("cy2_%d" % i, [128, D], F32) for i in range(2)])
        for t in tl:
            y1, y1k = y1r.next()
            y2, y2k = y2r.next()
            S.dma("pool", lambda e, y1=y1, t=t: e.indirect_dma_start(out=y1[:], out_offset=None, in_=T.ysd[:, :],
                                                                    in_offset=bass.IndirectOffsetOnAxis(ap=slu[:, t, 0:1], axis=0)),
                  ("cy1", y1k[1]), reads=["slul"], writes=[y1k])
            S.dma("pool", lambda e, y2=y2, t=t: e.indirect_dma_start(out=y2[:], out_offset=None, in_=T.ysd[:, :],
                                                                    in_offset=bass.IndirectOffsetOnAxis(ap=slu[:, t, 1:2], axis=0)),
                  ("cy2", y2k[1]), reads=["slul"], writes=[y2k])
            S.op("dve", lambda e, y1=y1, t=t: e.tensor_scalar(out=y1[:], in0=y1[:], scalar1=aa[:, t, 0:1], scalar2=None, op0=ALU.mult),
                 reads=[y1k, "aal"], writes=[y1k])
            S.op("dve", lambda e, y1=y1, y2=y2, t=t: e.scalar_tensor_tensor(out=y1[:], in0=y2[:], scalar=aa[:, t, 1:2], in1=y1[:],
                                                                            op0=ALU.mult, op1=ALU.add),
                 reads=[y1k, y2k, "aal"], writes=[y1k])
            S.dma("sp", lambda e, y1=y1, t=t: e.dma_start(out=T.facc[t * 128:(t + 1) * 128, :], in_=y1[:]), ("cfo", y1k[1]),
                  reads=[y1k], writes=[("faccd", t)])
        S.flush("D2c_%d" % l)


def prep_experts_sparse(wg, wu, wd):
    ne = wg.shape[1]
    rw = DEPTH * ne * 128
    w1 = np.ascontiguousarray(wg.reshape(DEPTH * ne, 4, 4, 128, 512).transpose(1, 0, 3, 2, 4)).reshape(4 * rw, 2048)
    w3 = np.ascontiguousarray(wu.reshape(DEPTH * ne, 4, 4, 128, 512).transpose(1, 0, 3, 2, 4)).reshape(4 * rw, 2048)
    w2 = np.ascontiguousarray(wd.reshape(DEPTH * ne, 4, 128, D).transpose(1, 0, 2, 3)).reshape(4 * rw, 2048)
    return w1, w3, w2
```

```python
import numpy as np
import concourse.bass as bass
import concourse.mybir as mybir
from concourse.bass_utils import run_bass_kernel_spmd

F32 = mybir.dt.float32
BF16 = mybir.dt.bfloat16
AF = mybir.ActivationFunctionType
ALU = mybir.AluOpType
AX = mybir.AxisListType

D = 2048
DEPTH = 2
NT = 20
NTOK = NT * 128
NOWN = 18
EPS = 1e-6
IN_DIM = 6656
NEXP = 32
DEXP = 512


class Sched:
    CE = ("pe", "act", "dve", "pool")

    def __init__(self, nc):
        self.nc = nc
        self.sem = {}
        self.val = {}
        self.engs = ("pe", "act", "dve", "pool", "sp")
        self.seen = {e: {} for e in self.engs}
        self.prog = {e: [] for e in self.engs}
        self.lastw = {}
        self.readers = {}
        self.free_dsems = []
        self.dkeys = {}
        self.nblocks = 0
        for e in self.CE:
            self._mk("c_" + e)

    def _mk(self, name):
        self.sem[name] = self.nc.alloc_semaphore(name=name)
        self.val[name] = 0

    def dsem(self, key):
        if key not in self.dkeys:
            if self.free_dsems:
                nm = self.free_dsems.pop()
            else:
                nm = "d%d" % len([k for k in self.sem if k.startswith("d")])
                self._mk(nm)
            self.dkeys[key] = nm
        return self.dkeys[key]

    def _deps(self, reads, writes):
        deps = {}

        def add(t):
            if t is None:
                return
            s, v = t
            if deps.get(s, 0) < v:
                deps[s] = v

        for r in reads:
            add(self.lastw.get(r))
        for w in writes:
            add(self.lastw.get(w))
            for s, v in self.readers.get(w, {}).items():
                add((s, v))
        return deps

    def _commit(self, tok, reads, writes):
        s, v = tok
        for r in reads:
            d = self.readers.setdefault(r, {})
            if d.get(s, 0) < v:
                d[s] = v
        for w in writes:
            self.lastw[w] = tok
            self.readers[w] = {}

    def _emit(self, eng, fn, semname, inc, reads, writes, n=1):
        deps = self._deps(reads, writes)
        seen = self.seen[eng]
        waits = []
        for s, v in deps.items():
            if seen.get(s, 0) < v:
                seen[s] = v
                waits.append((s, v))
        self.val[semname] += inc * n
        tok = (semname, self.val[semname])
        self.prog[eng].append((waits, fn, semname, inc))
        self._commit(tok, reads, writes)
        return tok

    def op(self, eng, fn, reads=(), writes=()):
        return self._emit(eng, fn, "c_" + eng, 1, reads, writes)

    def dma(self, q, fn, key, reads=(), writes=(), n=1):
        return self._emit(q, fn, self.dsem(key), 16, reads, writes, n)

    def _replay(self, name, eng):
        for waits, fn, semname, inc in self.prog[name]:
            for s, v in waits:
                eng.wait_ge(self.sem[s], v)
            r = fn(eng)
            if isinstance(r, (list, tuple)):
                for ins in r:
                    ins.then_inc(self.sem[semname], inc)
            else:
                r.then_inc(self.sem[semname], inc)

    def flush(self, name=None):
        nc = self.nc
        pend = []
        for key, nm in self.dkeys.items():
            v = self.val[nm]
            if self.seen["sp"].get(nm, 0) < v:
                pend.append((nm, v))
        for e in self.CE:
            nm = "c_" + e
            if self.seen["sp"].get(nm, 0) < self.val[nm]:
                pend.append((nm, self.val[nm]))
        prog = self.prog
        sp_tail = pend
        self.nblocks += 1
        with nc.Block("%s_b%d" % (name or "ph", self.nblocks)) as block:
            @block.tensor
            def _(e):
                self._replay("pe", e)

            @block.scalar
            def _(e):
                self._replay("act", e)

            @block.vector
            def _(e):
                self._replay("dve", e)

            @block.gpsimd
            def _(e):
                self._replay("pool", e)

            @block.sync
            def _(e):
                self._replay("sp", e)
                for s, v in sp_tail:
                    e.wait_ge(self.sem[s], v)
        self.prog = {e: [] for e in self.engs}
        for e in self.engs:
            for s, v in self.val.items():
                self.seen[e][s] = v
        self.lastw = {}
        self.readers = {}
        for key, nm in self.dkeys.items():
            self.free_dsems.append(nm)
        self.dkeys = {}


_UID = [0]


def _u(name):
    _UID[0] += 1
    return "%s_%d" % (name, _UID[0])


class Ring:
    def __init__(self, name, bufs):
        self.name = name
        self.bufs = bufs
        self.i = 0

    def next(self):
        k = self.i % len(self.bufs)
        self.i += 1
        return self.bufs[k], (self.name, k)


MOD_SH1, MOD_S1, MOD_GA1, MOD_SH2, MOD_S2, MOD_GA2 = range(6)

FEAT_COLS = ([0 + 128 * i for i in range(4)] + [512 + 128 * i for i in range(4)]
             + [1024 + 128 * i for i in range(8)] + [2048 + 128 * i for i in range(8)]
             + [4096 + 128 * i for i in range(4)])
FEAT32_COLS = [5120 + 128 * i for i in range(4)] + [5632 + 128 * i for i in range(4)]
TOK_COLS = [3072, 3584, 4608, 6144]
NFB = len(FEAT_COLS)
NF32 = len(FEAT32_COLS)
CH_A, CH_G, CH_Q, CH_K, CH_HQ = 0, 4, 8, 16, 24


class Ctx:
    pass


def bcast_row(ap_row, n):
    return bass.AP(tensor=ap_row.tensor, offset=ap_row.offset, ap=[[0, 128], [1, n]])


def emit_norm(S, nc, K, tiles, xsrc, AB, hT, es, want32=None, htok=None):
    sb = lambda name, shape, dt: es.enter_context(nc.sbuf_tensor(_u(name), shape, dt))
    xb = Ring("xb", [sb("nx%d" % i, [128, D], F32) for i in range(2)])
    tb = Ring("tb", [sb("nt%d" % i, [128, D], F32) for i in range(2)])
    hb = Ring("hb", [sb("nh%d" % i, [128, D], BF16) for i in range(2)])
    jb = sb("njunk", [128, D], BF16)
    st = Ring("st", [sb("nst%d" % i, [128, 2], F32) for i in range(3)])
    pT = Ring("pT", [es.enter_context(nc.psum_tensor(_u("npT%d" % i), [128, 1024], BF16)) for i in range(4)])
    ident = K.ident_bf
    for i, (col0, kind) in enumerate(tiles):
        A, B = AB[kind]
        x, xk = xb.next()
        src = xsrc(i)
        S.dma("sp", lambda e, x=x, src=src: e.dma_start(out=x[:], in_=src), ("nx", xk[1]), writes=[xk])
        s, sk = st.next()
        S.op("act", lambda e, x=x, s=s: e.activation(out=jb[:], in_=x[:], func=AF.Square, accum_out=s[:, 0:1]),
             reads=[xk], writes=[sk, "njunk"])
        S.op("dve", lambda e, s=s: e.tensor_scalar(out=s[:, 1:2], in0=s[:, 0:1], scalar1=1.0 / D, scalar2=EPS,
                                                   op0=ALU.mult, op1=ALU.add), reads=[sk], writes=[sk])
        S.op("act", lambda e, s=s: e.activation(out=s[:, 1:2], in_=s[:, 1:2], func=AF.Sqrt), reads=[sk], writes=[sk])
        S.op("dve", lambda e, s=s: e.reciprocal(out=s[:, 1:2], in_=s[:, 1:2]), reads=[sk], writes=[sk])
        t, tk = tb.next()
        S.op("dve", lambda e, t=t, x=x, s=s, A=A: e.scalar_tensor_tensor(
            out=t[:], in0=x[:], scalar=s[:, 1:2], in1=A[:], op0=ALU.mult, op1=ALU.mult),
            reads=[xk, sk], writes=[tk])
        if htok is None:
            h, hk = hb.next()
        else:
            h, hk = htok(i)
        if want32 is None:
            S.op("pool", lambda e, t=t, h=h, B=B: e.tensor_tensor(out=h[:], in0=t[:], in1=B[:], op=ALU.add),
                 reads=[tk], writes=[hk])
        else:
            S.op("pool", lambda e, t=t, B=B: e.tensor_tensor(out=t[:], in0=t[:], in1=B[:], op=ALU.add),
                 reads=[tk], writes=[tk])
            S.op("act", lambda e, t=t, h=h: e.copy(out=h[:], in_=t[:]), reads=[tk], writes=[hk])
            want32(i, t, tk)
        if hT is None:
            continue
        for half in range(2):
            p, pk = pT.next()

            def tr(e, p=p, h=h, half=half):
                r = None
                for k in range(8):
                    kk = half * 8 + k
                    r = e.transpose(out=p[:, k * 128:(k + 1) * 128], in_=h[:, kk * 128:(kk + 1) * 128],
                                    identity=ident[:])
                return r
            S.op("pe", tr, reads=[hk], writes=[pk])
            eng = "act" if half == 0 else "dve"
            dst = hT[:, half * 8:(half + 1) * 8, col0:col0 + 128]
            hkey = ("hT", col0 // 128, half)
            if eng == "act":
                S.op("act", lambda e, p=p, dst=dst: e.copy(out=dst, in_=p[:].rearrange("p (k n) -> p k n", k=8)),
                     reads=[pk], writes=[hkey])
            else:
                S.op("dve", lambda e, p=p, dst=dst: e.tensor_copy(out=dst, in_=p[:].rearrange("p (k n) -> p k n", k=8)),
                     reads=[pk], writes=[hkey])


def load_mod_AB(S, nc, T, l, which, gvec, es, tag):
    sb = lambda name, shape, dt: es.enter_context(nc.sbuf_tensor(_u(name), shape, dt))
    gm = sb(tag + "gm", [128, D], F32)
    S.dma("sp", lambda e: e.dma_start(out=gm[:], in_=bcast_row(gvec, D)), (tag, "gm"), writes=[tag + "gm"])
    AB = {}
    for kind in range(2):
        A = sb(tag + "A%d" % kind, [128, D], F32)
        B = sb(tag + "B%d" % kind, [128, D], F32)
        sh = T.mod[l * 2 + kind:l * 2 + kind + 1, which[0] * D:(which[0] + 1) * D]
        sc = T.mod[l * 2 + kind:l * 2 + kind + 1, which[1] * D:(which[1] + 1) * D]
        S.dma("sp", lambda e, B=B, sh=sh: e.dma_start(out=B[:], in_=bcast_row(sh, D)), (tag, "B", kind),
              writes=[(tag, "B", kind)])
        S.dma("sp", lambda e, A=A, sc=sc: e.dma_start(out=A[:], in_=bcast_row(sc, D)), (tag, "A", kind),
              writes=[(tag, "A", kind)])
        S.op("dve", lambda e, A=A: e.scalar_tensor_tensor(out=A[:], in0=A[:], scalar=1.0, in1=gm[:],
                                                          op0=ALU.add, op1=ALU.mult),
             reads=[(tag, "A", kind), tag + "gm"], writes=[(tag, "A", kind)])
        AB[kind] = (A, B)
    return AB


def tile_kind(t):
    return 1 if t < 2 else 0


def phase_A(S, nc, K, T, l, xs):
    from contextlib import ExitStack
    with ExitStack() as es:
        sb = lambda name, shape, dt: es.enter_context(nc.sbuf_tensor(_u(name), shape, dt))
        hT = sb("hT", [128, 16, NTOK], BF16)
        with ExitStack() as es2:
            AB = load_mod_AB(S, nc, T, l, (MOD_SH1, MOD_S1), T.g_mix[l:l + 1, :], es2, "m1")
            tiles = [(t * 128, tile_kind(t)) for t in range(NT)]
            emit_norm(S, nc, K, tiles, lambda i: xs[i * 128:(i + 1) * 128, :], AB, hT, es2)
            S.flush("A1_%d" % l)
        hk_all = [("hT", t, half) for t in range(NT) for half in range(2)]
        wf = Ring("wf", [sb("wf%d" % i, [128, 16, 128], BF16) for i in range(3)])
        stg = Ring("stg", [sb("stg%d" % i, [128, NTOK], BF16) for i in range(2)])
        stg32 = Ring("stg32", [sb("stg32_%d" % i, [128, NTOK], F32) for i in range(2)])
        pp = Ring("pp", [es.enter_context(nc.psum_tensor(_u("app%d" % i), [128, 512], F32)) for i in range(4)])
        nev = 0
        for c in range(NFB + NF32):
            is32 = c >= NFB
            w, wk = wf.next()
            src = T.w_feat[(l * 36 + c) * 128:(l * 36 + c + 1) * 128, :]
            S.dma("pool", lambda e, w=w, src=src: e.dma_start(out=w[:].rearrange("p k n -> p (k n)"), in_=src),
                  ("wf", wk[1]), writes=[wk])
            so, sk = (stg32 if is32 else stg).next()
            for tg in range(NT // 4):
                p, pk = pp.next()

                def mm(e, p=p, w=w, tg=tg):
                    r = None
                    for k in range(16):
                        r = e.matmul(p[:], lhsT=w[:, k, :], rhs=hT[:, k, tg * 512:(tg + 1) * 512],
                                     start=(k == 0), stop=(k == 15))
                    return r
                S.op("pe", mm, reads=[wk] + hk_all[tg * 8:(tg + 1) * 8], writes=[pk])
                dst = so[:, tg * 512:(tg + 1) * 512]
                if nev % 2 == 0:
                    S.op("act", lambda e, p=p, dst=dst: e.copy(out=dst, in_=p[:]), reads=[pk], writes=[(sk, tg)])
                else:
                    S.op("dve", lambda e, p=p, dst=dst: e.tensor_copy(out=dst, in_=p[:]), reads=[pk],
                         writes=[(sk, tg)])
                nev += 1
            if is32:
                dstd = T.uT32[(c - NFB) * 128:(c - NFB + 1) * 128, :]
            else:
                dstd = T.uT[c * 128:(c + 1) * 128, :]
            S.dma("sp", lambda e, so=so, dstd=dstd: e.dma_start(out=dstd, in_=so[:]), ("stgo", is32, sk[1]),
                  reads=[(sk, tg) for tg in range(NT // 4)], writes=[("uT", c)])
            for tg in range(NT // 4):
                S.readers.setdefault((sk, tg), {})
                S.readers[(sk, tg)][S.dsem(("stgo", is32, sk[1]))] = S.val[S.dsem(("stgo", is32, sk[1]))]
        wt = Ring("wt", [sb("wt%d" % i, [128, 16, 512], BF16) for i in range(2)])
        so_t = Ring("sot", [sb("sot%d" % i, [128, 512], BF16) for i in range(4)])
        for g in range(4):
            w, wk = wt.next()
            src = T.w_tokm[(l * 4 + g) * 128:(l * 4 + g + 1) * 128, :]
            S.dma("pool", lambda e, w=w, src=src: e.dma_start(out=w[:].rearrange("p k n -> p (k n)"), in_=src),
                  ("wt", wk[1]), writes=[wk])
            for t in range(NT):
                p, pk = pp.next()

                def mm(e, p=p, w=w, t=t):
                    r = None
                    for k in range(16):
                        r = e.matmul(p[:], lhsT=hT[:, k, t * 128:(t + 1) * 128], rhs=w[:, k, :],
                                     start=(k == 0), stop=(k == 15))
                    return r
                S.op("pe", mm, reads=[wk] + hk_all[t * 2:(t + 1) * 2], writes=[pk])
                so, sk = so_t.next()
                if nev % 2 == 0:
                    S.op("act", lambda e, p=p, so=so: e.copy(out=so[:], in_=p[:]), reads=[pk], writes=[sk])
                else:
                    S.op("dve", lambda e, p=p, so=so: e.tensor_copy(out=so[:], in_=p[:]), reads=[pk], writes=[sk])
                nev += 1
                dstd = T.utok[t * 128:(t + 1) * 128, g * 512:(g + 1) * 512]
                S.dma("sp", lambda e, so=so, dstd=dstd: e.dma_start(out=dstd, in_=so[:]), ("soto", sk[1]),
                      reads=[sk], writes=[("utok", t, g)])
                S.readers.setdefault(sk, {})
                S.readers[sk][S.dsem(("soto", sk[1]))] = S.val[S.dsem(("soto", sk[1]))]
        S.flush("A2_%d" % l)


def host_consts():
    c = np.zeros((128, 14, 128), np.float32)
    c[:, 0] = np.eye(128)
    c[:, 1] = np.eye(128)[::-1]
    t = np.arange(128)
    same = (t[:, None] // 32) == (t[None, :] // 32)
    c[:, 2] = (same & (t[None, :] >= t[:, None]))
    c[:, 3] = (same & (t[None, :] <= t[:, None]))
    c[:, 4] = (t[None, :] % 32 != 0).astype(np.float32)
    c[:, 5] = 1.0 / 512.0
    c[:, 6] = 1.0
    c[:, 7, 0:4] = (t[:, None] // 32) == np.arange(4)[None, :]
    c[:, 8] = (t[:, None] < t[None, :])
    c[:, 9, 0:18] = 128.0 * np.arange(18)[None, :]
    c[:, 9, 32:32 + 80] = np.arange(80)[None, :]
    c[:, 9, 127] = np.arange(128)
    c[:, 10:14] = c[:, 4:5]
    return c.reshape(128, 14 * 128)


def load_consts(S, nc, T, es):
    K = Ctx()
    sb = lambda name, shape, dt: es.enter_context(nc.sbuf_tensor(_u(name), shape, dt))
    K.c32 = sb("c32", [128, 14, 128], F32)
    K.cbf = sb("cbf", [128, 14, 128], BF16)
    S.dma("sp", lambda e: e.dma_start(out=K.c32[:].rearrange("p a b -> p (a b)"), in_=T.consts[:, :]), "c32",
          writes=["c32"])
    S.op("dve", lambda e: e.tensor_copy(out=K.cbf[:], in_=K.c32[:]), reads=["c32"], writes=["cbf"])
    K.ident_bf = K.cbf[:, 0, :]
    K.ident32 = K.c32[:, 0, :]
    K.anti32 = K.c32[:, 1, :]
    K.mask_f = K.c32[:, 2, :]
    K.mask_b = K.c32[:, 3, :]
    K.reset = K.c32[:, 4, :]
    K.avg32 = K.c32[:, 5, :]
    K.ones32 = K.c32[:, 6, :]
    K.ones_bf = K.cbf[:, 6, :]
    K.rowmask = K.c32[:, 7, 0:4]
    K.reset4 = K.c32[:, 10:14, :].rearrange("p a b -> p (a b)")
    S.flush("consts")
    return K


def build(cfg):
    from contextlib import ExitStack
    nc = bass.Bass("TRN2", target_bir_lowering=False)
    T = Ctx()
    dbg = cfg.get("debug", ())
    nexp = cfg.get("nexp", NEXP)

    def din(name, shape, dt=F32):
        return nc.dram_tensor(name, list(shape), dt, kind="ExternalInput").ap()

    def dscr(name, shape, dt=F32):
        if name in dbg:
            return nc.dram_tensor(name, list(shape), dt, kind="ExternalOutput").ap()
        return nc.dram_tensor(name, list(shape), dt).ap()

    T.x_in = din("x_in", [NTOK, D])
    T.consts = din("consts", [128, 14 * 128])
    T.g_mix = din("g_mix", [DEPTH, D])
    T.w_feat = din("w_feat", [DEPTH * 36 * 128, 16 * 128])
    T.w_tokm = din("w_tokm", [DEPTH * 4 * 128, 16 * 512])
    if cfg.get("mod_input"):
        T.mod = din("mod", [DEPTH * 2, 6 * D])
    else:
        T.mod = dscr("mod", [DEPTH * 2, 6 * D])
        T.cT = din("cT", [128, 80])
        T.w_adam = din("w_adam", [DEPTH * 128, 16 * 1536])
        T.b_ada = din("b_ada", [1, DEPTH * 1536])
        T.bsel = din("bsel", [5, 2])
        T.msend = dscr("msend", [5, DEPTH * 1536])
        T.mrecv = dscr("mrecv", [40, DEPTH * 1536])
    T.pflag = din("pflag", [128, 2])
    T.uT = dscr("uT", [NFB * 128, NTOK], BF16)
    T.uT32 = dscr("uT32", [NF32 * 128, NTOK], F32)
    T.utok = dscr("utok", [NTOK, 2048], BF16)
    T.mixT = dscr("mixT", [16 * 128, NOWN * 128], BF16)
    T.conv_wT = din("conv_wT", [DEPTH * 512, 31])
    T.conv_vec = din("conv_vec", [DEPTH * 128, 12])
    T.na_bias = din("na_bias", [DEPTH * 16 * 10 * 128, 256])
    T.hlb = din("hlb", [128, 16])
    T.hnorm_g = din("hnorm_g", [DEPTH, 512])
    T.hsend = dscr("hsend", [512, 128])
    T.hrecv = dscr("hrecv", [512, 128])
    T.hrecv2 = dscr("hrecv2", [1024, 128])
    T.hostore = dscr("hostore", [NOWN * 128, 512])
    T.w_outm = din("w_outm", [DEPTH * 4 * 128, 16 * 512])
    T.g_ffn = din("g_ffn", [DEPTH, D])
    T.g_final = din("g_final", [1, D])
    T.w_r = din("w_r", [DEPTH * 128, 16 * 36])
    T.b_r = din("b_r", [DEPTH, 36])
    if cfg.get("sparse", True):
        _NE[0] = cfg.get("ne_eff", NEXP)
        T.w1m = din("w1m", [4 * RWv(), 2048])
        T.w3m = din("w3m", [4 * RWv(), 2048])
        T.w2m = din("w2m", [4 * RWv(), 2048])
    else:
        T.w1m = din("w1m", [DEPTH * nexp * 128, 16 * 512])
        T.w3m = din("w3m", [DEPTH * nexp * 128, 16 * 512])
        T.w2m = din("w2m", [DEPTH * nexp * 128, 4 * D])
    T.x1 = dscr("x1", [NOWN * 128, D])
    T.h2T = dscr("h2T", [16 * 128, NOWN * 128], BF16)
    T.wt = dscr("wt", [128, NOWN * 32])
    T.facc = dscr("facc", [NOWN * 128, D])
    sparse = cfg.get("sparse", True)
    if sparse:
        T.xsort = dscr("xsort", [50 * 256, D], BF16)
        T.ysd = dscr("ysd", [50 * 256, D])
        T.blku = dscr("blku", [128, 256], U32)
        T.slotsu = dscr("slotsu", [128, NOWN * 2], U32)
        T.aall = dscr("aall", [128, NOWN * 2])
    T.xs2 = dscr("xs2", [NTOK, D])
    T.xsend = dscr("xsend", [256, D])
    T.xrecv = dscr("xrecv", [512, D])
    T.out = nc.dram_tensor("out", [16 * 128, D], F32, kind="ExternalOutput").ap()

    S = Sched(nc)
    with ExitStack() as es:
        K = load_consts(S, nc, T, es)
        nlayers = cfg.get("layers", DEPTH)
        stop = cfg.get("stop", "")
        noex = cfg.get("no_exchange", False)
        if not cfg.get("mod_input"):
            phase_mod(S, nc, K, T)
        for l in range(nlayers):
            xs = T.x_in if l == 0 else T.xs2
            with_ctx = l < DEPTH - 1
            last = l == DEPTH - 1
            phase_A(S, nc, K, T, l, xs)
            if stop == "A":
                break
            phase_hg1(S, nc, K, T, l, with_ctx)
            if stop == "hg1":
                break
            if not noex:
                phase_xchg_h(S, nc, K, T)
            phase_conv(S, nc, K, T, l, with_ctx)
            if stop == "conv":
                break
            phase_na(S, nc, K, T, l, with_ctx)
            if stop == "na":
                break
            phase_hg2(S, nc, K, T, l, with_ctx, None if noex else T.hrecv)
            if stop == "hg":
                break
            phase_C(S, nc, K, T, l, with_ctx, xs)
            if sparse:
                phase_D1s(S, nc, K, T, l, with_ctx)
                if stop == "D1":
                    break
                phase_D2s(S, nc, K, T, l, with_ctx)
            else:
                phase_D1(S, nc, K, T, l, with_ctx)
                if stop == "D1":
                    break
                phase_D2(S, nc, K, T, l, with_ctx, nexp)
            phase_D3(S, nc, K, T, l, with_ctx, last)
            if stop == "D3":
                break
            if not last and not noex:
                phase_xchg_x(S, nc, K, T)
            if not last and noex:
                S.dma("sp", lambda e: e.dma_start(out=T.xs2[NOWN * 128:NTOK, :], in_=T.x_in[NOWN * 128:NTOK, :]), "dbgx")
                S.flush("dbgx")
    return nc


def core_tokens(x_b, ctx_b, odd):
    if odd:
        lat = x_b[::-1]
        cx = ctx_b[::-1]
    else:
        lat = x_b
        cx = ctx_b
    return np.ascontiguousarray(np.concatenate([cx, lat[:2304]], axis=0))


def prep_w_in(w_in, odd):
    fcols = list(FEAT_COLS) + (FEAT32_COLS[4:] + FEAT32_COLS[:4] if odd else list(FEAT32_COLS))
    wf = np.empty((DEPTH, 36, 128, 16 * 128), np.float32)
    wt = np.empty((DEPTH, 4, 128, 16 * 512), np.float32)
    for l in range(DEPTH):
        for i, c0 in enumerate(fcols):
            wf[l, i] = w_in[l][:, c0:c0 + 128].reshape(16, 128, 128).transpose(1, 0, 2).reshape(128, -1)
        for i, c0 in enumerate(TOK_COLS):
            wt[l, i] = w_in[l][:, c0:c0 + 512].reshape(16, 128, 512).transpose(1, 0, 2).reshape(128, -1)
    return wf.reshape(-1, 16 * 128), wt.reshape(-1, 16 * 512)


def prep_conv(conv_w, conv_b, ln_g, ln_b, odd):
    cw = conv_w[:, ::-1, :] if odd else conv_w
    cwT = np.ascontiguousarray(cw.transpose(0, 2, 1)).reshape(DEPTH * 512, 31)
    vec = np.stack([conv_b, ln_g, ln_b], axis=1)
    vec = vec.reshape(DEPTH, 3, 4, 128).transpose(0, 3, 1, 2).reshape(DEPTH * 128, 12)
    return cwT.astype(np.float32), np.ascontiguousarray(vec).astype(np.float32)


def phase_conv(S, nc, K, T, l, with_ctx):
    from contextlib import ExitStack
    with ExitStack() as es:
        sb = lambda name, shape, dt: es.enter_context(nc.sbuf_tensor(_u(name), shape, dt))
        pst = lambda name, shape, dt: es.enter_context(nc.psum_tensor(_u(name), shape, dt))
        cw = sb("cw", [128, 4, 31], F32)
        cv = sb("cv", [128, 12], F32)
        wd = sb("wd", [128, 4, 31, 128], BF16)
        hg = sb("hglu", [128, 4, 2078], BF16)
        hgc = sb("hgluc", [128, 4, 286], BF16)
        S.dma("sp", lambda e: e.dma_start(out=cw[:], in_=T.conv_wT[l * 512:(l + 1) * 512, :].rearrange(
            "(c p) k -> p c k", p=128)), "cw", writes=["cw"])
        S.dma("sp", lambda e: e.dma_start(out=cv[:], in_=T.conv_vec[l * 128:(l + 1) * 128, :]), "cv", writes=["cv"])
        for c in range(4):
            for k in range(31):
                eng = "dve" if (k % 2 == 0) else "pool"
                S.op(eng, lambda e, c=c, k=k: e.tensor_scalar(out=wd[:, c, k, :], in0=K.ident32, scalar1=cw[:, c, k:k + 1],
                                                              scalar2=None, op0=ALU.mult),
                     reads=["cw"], writes=[("wd", c, k)])
        S.op("pool", lambda e: e.memset(hg[:, :, 0:15], 0.0), writes=[("hgpad")])
        S.op("pool", lambda e: e.memset(hgc[:], 0.0), writes=[("hgc", c) for c in range(4)])
        ab = Ring("cab", [sb("cab%d" % i, [128, NTOK], BF16) for i in range(2)])
        gb = Ring("cgb", [sb("cgb%d" % i, [128, NTOK], BF16) for i in range(2)])
        sgb = Ring("csg", [sb("csg%d" % i, [128, NTOK], BF16) for i in range(2)])
        for c in range(4):
            a, ak = ab.next()
            g, gk = gb.next()
            sg, sgk = sgb.next()
            S.dma("sp", lambda e, a=a, c=c: e.dma_start(out=a[:], in_=T.uT[(CH_A + c) * 128:(CH_A + c + 1) * 128, :]),
                  ("cab", ak[1]), writes=[ak])
            S.dma("sp", lambda e, g=g, c=c: e.dma_start(out=g[:], in_=T.uT[(CH_G + c) * 128:(CH_G + c + 1) * 128, :]),
                  ("cgb", gk[1]), writes=[gk])
            S.op("act", lambda e, g=g, sg=sg: e.activation(out=sg[:], in_=g[:], func=AF.Sigmoid), reads=[gk], writes=[sgk])
            S.op("dve", lambda e, a=a, sg=sg, c=c: e.tensor_tensor(out=hg[:, c, 15:15 + 2063], in0=a[:, 256:256 + 2063],
                                                                 in1=sg[:, 256:256 + 2063], op=ALU.mult),
                 reads=[ak, sgk], writes=[("hg", c)])
            if with_ctx:
                S.op("dve", lambda e, a=a, sg=sg, c=c: e.tensor_tensor(out=hgc[:, c, 15:15 + 256], in0=a[:, 0:256],
                                                                     in1=sg[:, 0:256], op=ALU.mult),
                     reads=[ak, sgk, ("hgc", c)], writes=[("hgc", c)])
        groups = [(hg, "hg", tg * 512, 512, 256 + tg * 512) for tg in range(4)]
        if with_ctx:
            groups.append((hgc, "hgc", 0, 256, 0))
        pc = Ring("pc", [pst("pc%d" % i, [128, 512], F32) for i in range(3)])
        pmu = Ring("pmu", [pst("pmu%d" % i, [128, 512], F32) for i in range(2)])
        pm2 = Ring("pm2", [pst("pm2%d" % i, [128, 512], F32) for i in range(2)])
        y32 = Ring("y32", [sb("y32_%d" % i, [128, 4, 512], F32) for i in range(2)])
        ysq = Ring("ysq", [sb("ysq_%d" % i, [128, 4, 512], F32) for i in range(2)])
        mub = Ring("mub", [sb("mub%d" % i, [128, 512], F32) for i in range(2)])
        rsb = Ring("rsb", [sb("rsb%d" % i, [128, 512], F32) for i in range(2)])
        db = Ring("db", [sb("db%d" % i, [128, 512], F32) for i in range(3)])
        ob = Ring("ob", [sb("ob%d" % i, [128, 4, 512], BF16) for i in range(2)])
        for (src, sname, off, n, col0) in groups:
            y, yk = y32.next()
            q, qk = ysq.next()
            for c in range(4):
                p, pk = pc.next()

                def mm(e, p=p, c=c, src=src, off=off, n=n):
                    r = None
                    for k in range(31):
                        r = e.matmul(p[:, 0:n], lhsT=wd[:, c, k, :], rhs=src[:, c, off + k:off + k + n],
                                     start=(k == 0), stop=(k == 30))
                    return r
                S.op("pe", mm, reads=[("wd", c, k) for k in range(31)] + [(sname, c), "hgpad"], writes=[pk])
                S.op("act", lambda e, p=p, y=y, c=c, n=n: e.activation(out=y[:, c, 0:n], in_=p[:, 0:n], func=AF.Identity,
                                                                      bias=cv[:, c:c + 1]),
                     reads=[pk, "cv"], writes=[(yk, c)])
                S.op("act", lambda e, p=p, q=q, c=c, n=n: e.activation(out=q[:, c, 0:n], in_=p[:, 0:n], func=AF.Square,
                                                                      bias=cv[:, c:c + 1]),
                     reads=[pk, "cv"], writes=[(qk, c)])
            m, mk = pmu.next()
            m2, m2k = pm2.next()

            def st(e, m=m, m2=m2, y=y, q=q, n=n):
                r = None
                for c in range(4):
                    e.matmul(m[:, 0:n], lhsT=K.avg32, rhs=y[:, c, 0:n], start=(c == 0), stop=(c == 3))
                for c in range(4):
                    r = e.matmul(m2[:, 0:n], lhsT=K.avg32, rhs=q[:, c, 0:n], start=(c == 0), stop=(c == 3))
                return r
            S.op("pe", st, reads=[(yk, c) for c in range(4)] + [(qk, c) for c in range(4)], writes=[mk, m2k])
            mu, muk = mub.next()
            rs, rsk = rsb.next()
            S.op("act", lambda e, mu=mu, m=m, n=n: e.copy(out=mu[:, 0:n], in_=m[:, 0:n]), reads=[mk], writes=[muk])
            S.op("dve", lambda e, mu=mu, rs=rs, n=n: e.tensor_tensor(out=rs[:, 0:n], in0=mu[:, 0:n], in1=mu[:, 0:n],
                                                                    op=ALU.mult), reads=[muk], writes=[rsk])
            S.op("dve", lambda e, rs=rs, m2=m2, n=n: e.tensor_tensor(out=rs[:, 0:n], in0=m2[:, 0:n], in1=rs[:, 0:n],
                                                                    op=ALU.subtract), reads=[m2k, rsk], writes=[rsk])
            S.op("dve", lambda e, rs=rs, n=n: e.tensor_scalar(out=rs[:, 0:n], in0=rs[:, 0:n], scalar1=EPS, scalar2=None,
                                                              op0=ALU.add), reads=[rsk], writes=[rsk])
            S.op("act", lambda e, rs=rs, n=n: e.activation(out=rs[:, 0:n], in_=rs[:, 0:n], func=AF.Sqrt),
                 reads=[rsk], writes=[rsk])
            S.op("dve", lambda e, rs=rs, n=n: e.reciprocal(out=rs[:, 0:n], in_=rs[:, 0:n]), reads=[rsk], writes=[rsk])
            o, ok = ob.next()
            for c in range(4):
                d, dk = db.next()
                S.op("dve", lambda e, d=d, y=y, mu=mu, c=c, n=n: e.tensor_tensor(out=d[:, 0:n], in0=y[:, c, 0:n],
                                                                                in1=mu[:, 0:n], op=ALU.subtract),
                     reads=[(yk, c), muk], writes=[dk])
                S.op("pool", lambda e, d=d, rs=rs, n=n: e.tensor_tensor(out=d[:, 0:n], in0=d[:, 0:n], in1=rs[:, 0:n],
                                                                       op=ALU.mult), reads=[dk, rsk], writes=[dk])
                S.op("dve", lambda e, d=d, c=c, n=n: e.tensor_scalar(out=d[:, 0:n], in0=d[:, 0:n],
                                                                    scalar1=cv[:, 4 + c:5 + c], scalar2=cv[:, 8 + c:9 + c],
                                                                    op0=ALU.mult, op1=ALU.add),
                     reads=[dk, "cv"], writes=[dk])
                S.op("act", lambda e, d=d, o=o, c=c, n=n: e.activation(out=o[:, c, 0:n], in_=d[:, 0:n], func=AF.Silu),
                     reads=[dk], writes=[(ok, c)])
            dst = T.mixT[0:512, col0:col0 + n].rearrange("(c p) n -> p c n", p=128)
            S.dma("sp", lambda e, o=o, dst=dst, n=n: e.dma_start(out=dst, in_=o[:, :, 0:n]), ("ob", ok[1]),
                  reads=[(ok, c) for c in range(4)], writes=[("mixT", 0, col0)])
        S.flush("conv_%d" % l)


def na_groups(with_ctx):
    groups = []
    for g in range(8):
        if g == 0:
            chunks = [(2 + j, j) for j in range(4)]
        else:
            chunks = [(2 + 2 * g - 2 + c, 4 + c) for c in range(6)]
        chunks += [(0, None), (1, None)]
        groups.append((256 + 256 * g, chunks, (2 + 2 * g, 3 + 2 * g)))
    if with_ctx:
        groups.append((0, [(0, None), (1, None)], (0, 1)))
    return groups


def phase_na(S, nc, K, T, l, with_ctx):
    from contextlib import ExitStack
    with ExitStack() as es:
        sb = lambda name, shape, dt: es.enter_context(nc.sbuf_tensor(_u(name), shape, dt))
        pst = lambda name, shape, dt: es.enter_context(nc.psum_tensor(_u(name), shape, dt))
        na_tok = sb("na_tok", [128, NOWN, 1024], BF16)
        qb = Ring("naq", [sb("naq%d" % i, [128, NOWN * 128], BF16) for i in range(2)])
        kb = Ring("nak", [sb("nak%d" % i, [128, NTOK], BF16) for i in range(2)])
        vb = Ring("nav", [sb("nav%d" % i, [128, NT, 128], BF16) for i in range(2)])
        va = Ring("nava", [sb("nava%d" % i, [128, NT, 2, 65], BF16) for i in range(2)])
        bb = Ring("nab", [sb("nab%d" % i, [128, 10, 256], F32) for i in range(2)])
        pS = Ring("naps", [pst("naps%d" % i, [128, 256], F32) for i in range(3)])
        pO = Ring("napo", [pst("napo%d" % i, [128, 128], F32) for i in range(3)])
        tmpb = Ring("natmp", [sb("natmp%d" % i, [128, 256], F32) for i in range(3)])
        pTb = Ring("napT", [sb("napT%d" % i, [128, 8, 256], BF16) for i in range(2)])
        rcb = Ring("narc", [sb("narc%d" % i, [128, 1], F32) for i in range(4)])
        for i in range(2):
            S.op("pool", lambda e, i=i: e.memset(va.bufs[i][:], 1.0), writes=[("nava", i)])
        groups = na_groups(with_ctx)
        for hp in range(8):
            q, qk = qb.next()
            k, kk = kb.next()
            v, vk = vb.next()
            vau, vak = va.next()
            S.dma("sp", lambda e, q=q, hp=hp: e.dma_start(out=q[:], in_=T.uT[(CH_Q + hp) * 128:(CH_Q + hp + 1) * 128,
                                                                          0:NOWN * 128]), ("naq", qk[1]), writes=[qk])
            S.dma("sp", lambda e, k=k, hp=hp: e.dma_start(out=k[:], in_=T.uT[(CH_K + hp) * 128:(CH_K + hp + 1) * 128, :]),
                  ("nak", kk[1]), writes=[kk])
            S.dma("sp", lambda e, v=v, hp=hp: e.dma_start(out=v[:], in_=T.utok[:, hp * 128:(hp + 1) * 128].rearrange(
                "(t p) c -> p t c", p=128)), ("nav", vk[1]), writes=[vk])
            S.op("pool", lambda e, v=v, vau=vau: e.tensor_copy(out=vau[:, :, :, 0:64],
                                                             in_=v[:].rearrange("p t (h d) -> p t h d", h=2)),
                 reads=[vk], writes=[vak])
            for hl in range(2):
                h = 2 * hp + hl
                pb = 64 * hl
                b, bk = bb.next()
                S.dma("sp", lambda e, b=b, h=h: e.dma_start(
                    out=b[:], in_=T.na_bias[(l * 16 + h) * 1280:(l * 16 + h + 1) * 1280, :].rearrange(
                        "(b p) q -> p b q", p=128)), ("nab", bk[1]), writes=[bk])
                for (qc0, chunks, otiles) in groups:
                    pt, ptk = pTb.next()
                    for ci, (tile, bi) in enumerate(chunks):
                        p, pk = pS.next()
                        S.op("pe", lambda e, p=p, k=k, q=q, tile=tile, qc0=qc0, pb=pb: e.matmul(
                            p[:], lhsT=k[pb:pb + 64, tile * 128:(tile + 1) * 128], rhs=q[pb:pb + 64, qc0:qc0 + 256],
                            start=True, stop=True), reads=[kk, qk], writes=[pk])
                        if bi is not None:
                            tm, tmk = tmpb.next()
                            S.op("dve", lambda e, tm=tm, p=p, b=b, bi=bi: e.scalar_tensor_tensor(
                                out=tm[:], in0=p[:], scalar=0.125, in1=b[:, bi, :], op0=ALU.mult, op1=ALU.add),
                                reads=[pk, bk], writes=[tmk])
                            S.op("act", lambda e, tm=tm, pt=pt, ci=ci: e.activation(out=pt[:, ci, :], in_=tm[:], func=AF.Exp),
                                 reads=[tmk], writes=[(ptk, ci)])
                        else:
                            S.op("act", lambda e, p=p, pt=pt, ci=ci: e.activation(out=pt[:, ci, :], in_=p[:], func=AF.Exp,
                                                                                scale=0.125),
                                 reads=[pk], writes=[(ptk, ci)])
                    for qt in range(2):
                        o, ok = pO.next()

                        def pv(e, o=o, pt=pt, qt=qt, chunks=chunks, vau=vau, hl=hl):
                            r = None
                            n = len(chunks)
                            for ci, (tile, bi) in enumerate(chunks):
                                r = e.matmul(o[:, 0:65], lhsT=pt[:, ci, qt * 128:(qt + 1) * 128], rhs=vau[:, tile, hl, :],
                                             start=(ci == 0), stop=(ci == n - 1))
                            return r
                        S.op("pe", pv, reads=[(ptk, ci) for ci in range(len(chunks))] + [vak], writes=[ok])
                        rc, rck = rcb.next()
                        S.op("dve", lambda e, rc=rc, o=o: e.reciprocal(out=rc[:], in_=o[:, 64:65]), reads=[ok], writes=[rck])
                        ot = otiles[qt]
                        S.op("act", lambda e, o=o, rc=rc, ot=ot, h=h: e.activation(
                            out=na_tok[:, ot, h * 64:(h + 1) * 64], in_=o[:, 0:64], func=AF.Identity, scale=rc[:, 0:1]),
                            reads=[ok, rck], writes=[("natok", ot)])
        stT = sb("na_stT", [128, 8, NOWN * 128], BF16)
        pTr = Ring("naptr", [pst("naptr%d" % i, [128, 1024], BF16) for i in range(2)])
        t0 = 0 if with_ctx else 2
        for t in range(t0, NOWN):
            p, pk = pTr.next()

            def tr(e, p=p, t=t):
                r = None
                for c in range(8):
                    r = e.transpose(out=p[:, c * 128:(c + 1) * 128], in_=na_tok[:, t, c * 128:(c + 1) * 128],
                                    identity=K.ident_bf)
                return r
            S.op("pe", tr, reads=[("natok", t)], writes=[pk])
            dst = stT[:, :, t * 128:(t + 1) * 128]
            if t % 2 == 0:
                S.op("act", lambda e, p=p, dst=dst: e.copy(out=dst, in_=p[:].rearrange("p (c n) -> p c n", c=8)),
                     reads=[pk], writes=[("stT", t)])
            else:
                S.op("dve", lambda e, p=p, dst=dst: e.tensor_copy(out=dst, in_=p[:].rearrange("p (c n) -> p c n", c=8)),
                     reads=[pk], writes=[("stT", t)])
        c0 = t0 * 128
        for c in range(8):
            S.dma("sp", lambda e, c=c: e.dma_start(out=T.mixT[(4 + c) * 128:(5 + c) * 128, c0:NOWN * 128],
                                                   in_=stT[:, c, c0:NOWN * 128]), ("nast", c),
                  reads=[("stT", t) for t in range(t0, NOWN)], writes=[("mixT", 4 + c)])
        S.flush("na_%d" % l)


def prep_na_bias(rpb, odd):
    out = np.empty((DEPTH, 16, 10, 128, 256), np.float32)

    def tr(x):
        return 63 - x if odd else x
    kc = tr(np.arange(64))[None, :, None, None]
    qc = tr(np.arange(64))[None, None, None, :]
    pats = []
    for j in range(4):
        pats.append((np.arange(0, 4), np.arange(2 * j, 2 * j + 2)))
    for c in range(6):
        pats.append((np.arange(4, 8), np.arange(2 * c, 2 * c + 2)))
    for bi, (qrows, krows) in enumerate(pats):
        kr = tr(krows)[:, None, None, None]
        qr = tr(qrows)[None, None, :, None]
        sr = np.clip(qr - 4, 0, 56)
        ws = np.clip(qc - 8, 0, 48)
        valid = (kr >= sr) & (kr < sr + 8) & (kc >= ws) & (kc < ws + 16)
        valid = np.broadcast_to(valid, (2, 64, 4, 64))
        di = np.clip(np.broadcast_to(kr - qr + 7, (2, 64, 4, 64)), 0, 14)
        dj = np.clip(np.broadcast_to(kc - qc + 15, (2, 64, 4, 64)), 0, 30)
        vals = rpb[:, :, di, dj]
        vals = np.where(valid[None, None], vals, np.float32(-1e30))
        out[:, :, bi] = vals.reshape(DEPTH, 16, 128, 256)
    return out.reshape(-1, 256)


def _bc_last(ap3, n):
    a = [list(x) for x in ap3.ap]
    a[-1] = [0, n]
    return bass.AP(tensor=ap3.tensor, offset=ap3.offset, ap=a)


def _diag4(t):
    a = t[:, :, 0:32]
    ap = [list(x) for x in a.ap]
    ap[1][0] = ap[1][0] + 32
    return bass.AP(tensor=a.tensor, offset=a.offset, ap=ap)


import os
HG_DBG = int(os.environ.get('HG_DBG', '99'))
HG_SKIP = os.environ.get('HG_SKIP', '')
HG_KT = os.environ.get('HG_KT', 'dve')


def _fap(t, off, dims):
    a = t[:]
    return bass.AP(tensor=a.tensor, offset=a.offset + off, ap=[list(a.ap[0])] + [list(d) for d in dims])


class HgEnv:
    def __init__(self, S, nc, K, T, l, es):
        sb = lambda name, shape, dt: es.enter_context(nc.sbuf_tensor(_u(name), shape, dt))
        pst = lambda name, shape, dt: es.enter_context(nc.psum_tensor(_u(name), shape, dt))
        self.S, self.nc, self.K, self.T, self.l = S, nc, K, T, l
        self.z = sb("hgz", [128, 4, NOWN * 128], F32)
        self.q = sb("hgq", [128, 4, NOWN * 128], BF16)
        self.v = sb("hgv", [128, NOWN, 512], BF16)
        f = lambda nm, n: Ring(nm, [sb("%s%d" % (nm, i), [128, 512], F32) for i in range(n)])
        self.sg, self.f, self.lf, self.kT, self.bT = f("hsg", 2), f("hf", 2), f("hlf", 2), f("hkT", 2), f("hbT", 2)
        self.tmp, self.b2, self.Eb, self.Enb, self.sq = f("htmp", 2), f("hb2", 2), f("hEb", 3), f("hEnb", 2), f("hsq", 2)
        self.e2, self.E2 = f("he2", 2), f("hE2", 2)
        self.Qtz = Ring("hQtz", [sb("hQtz%d" % i, [128, 16, 128], BF16) for i in range(2)])
        self.Kt = Ring("hKt", [sb("hKt%d" % i, [128, 512], BF16) for i in range(2)])
        self.KhT = Ring("hKhT", [sb("hKhT%d" % i, [128, 512], BF16) for i in range(2)])
        self.Khs = Ring("hKhs", [sb("hKhs%d" % i, [128, 512], BF16) for i in range(2)])
        self.Khz = Ring("hKhz", [sb("hKhz%d" % i, [128, 16, 128], BF16) for i in range(2)])
        self.attm = Ring("hattm", [sb("hattm%d" % i, [128, 512], BF16) for i in range(2)])
        self.Sb = Ring("hSb", [sb("hSb%d" % i, [128, 512], BF16) for i in range(6)])
        self.S32 = Ring("hS32", [sb("hS32_%d" % i, [128, 512], F32) for i in range(2)])
        self.pkh = Ring("hpkh", [pst("hpkh%d" % i, [128, 512], BF16) for i in range(1)])
        self.patt = Ring("hpatt", [pst("hpatt%d" % i, [128, 512], F32) for i in range(2)])
        self.pu = Ring("hpu", [pst("hpu%d" % i, [128, 512], F32) for i in range(2)])
        self.po = Ring("hpo", [pst("hpo%d" % i, [128, 512], F32) for i in range(2)])
        self.ostore = sb("hostore_sb", [128, NOWN, 512], F32)
        self.lbt = sb("hlbt", [128, 2, 8], F32)
        for i in range(2):
            S.op("pool", lambda e, i=i: e.memset(self.Qtz.bufs[i][:], 0.0), writes=[("hQtz", i)])
        if l == 0:
            S.op("pool", lambda e: e.memset(self.lbt[:, 0, :], 0.0), writes=["hlbt"])
            S.op("pool", lambda e: e.memset(self.lbt[:, 1, :], 1.0), reads=["hlbt"], writes=["hlbt"])
        else:
            raw = sb("hlbraw", [128, 16], F32)
            S.dma("sp", lambda e: e.dma_start(out=raw[:], in_=T.hlb[:, :]), "hlbraw", writes=["hlbraw"])
            S.op("dve", lambda e: e.tensor_tensor(out=self.lbt[:, 0, :], in0=raw[:, 8:16], in1=raw[:, 0:8], op=ALU.subtract),
                 reads=["hlbraw"], writes=["hlbt"])
            S.op("act", lambda e: e.activation(out=self.lbt[:, 0, :], in_=self.lbt[:, 0, :], func=AF.Sigmoid),
                 reads=["hlbt"], writes=["hlbt"])
            S.op("dve", lambda e: e.tensor_scalar(out=self.lbt[:, 1, :], in0=self.lbt[:, 0, :], scalar1=-1.0, scalar2=1.0,
                                                  op0=ALU.mult, op1=ALU.add), reads=["hlbt"], writes=["hlbt"])

    def load(self, d):
        S, T = self.S, self.T
        for h in range(4):
            c = d * 4 + h
            S.dma("sp", lambda e, c=c, h=h: e.dma_start(out=self.z[:, h, :], in_=T.uT32[c * 128:(c + 1) * 128, 0:NOWN * 128]),
                  ("hgz", h), writes=[("hgz", h)])
            S.dma("sp", lambda e, h=h: e.dma_start(out=self.q[:, h, :], in_=T.uT[(CH_HQ + h) * 128:(CH_HQ + h + 1) * 128,
                                                                                 0:NOWN * 128]), ("hgq", h), writes=[("hgq", h)])
        S.dma("sp", lambda e: e.dma_start(out=self.v[:], in_=T.utok[0:NOWN * 128, 1024:1536].rearrange("(t p) c -> p t c", p=128)),
              "hgv", writes=["hgv"])
        self.zk = [("hgz", h) for h in range(4)]
        self.qk = [("hgq", h) for h in range(4)]

    def chain_init(self, src=None):
        S = self.S
        s32, sk = self.S32.next()
        sbf, sbk = self.Sb.next()
        if src is None:
            S.op("dve", lambda e: e.memset(s32[:], 0.0), writes=[sk])
            S.op("dve", lambda e: e.memset(sbf[:], 0.0), writes=[sbk])
        else:
            S.dma("sp", lambda e: e.dma_start(out=s32[:].rearrange("p (h n) -> p h n", h=4),
                                              in_=src.rearrange("(h p) n -> p h n", p=128)), ("hS32", sk[1]), writes=[sk])
            S.op("act", lambda e: e.copy(out=sbf[:], in_=s32[:]), reads=[sk], writes=[sbk])
        return dict(s32=s32, sk=sk, sbf=sbf, sbk=sbk)

    def tile(self, d, t, st, accumulate):
        S, K = self.S, self.K
        tc0 = t * 128
        v3 = lambda x: x[:].rearrange("p (h n) -> p h n", h=4)
        c16 = lambda x: x[:].rearrange("p (c n) -> p c n", c=16)
        lb_bc = _fap(self.lbt, 0 * 8 + d * 4, [[1, 4], [0, 128]])
        oml_bc = _fap(self.lbt, 1 * 8 + d * 4, [[1, 4], [0, 128]])
        sg, sgk = self.sg.next()
        S.op("act", lambda e: e.activation(out=v3(sg), in_=self.z[:, :, tc0:tc0 + 128], func=AF.Sigmoid), reads=self.zk,
             writes=[sgk])
        f, fk = self.f.next()
        S.op("dve", lambda e: e.tensor_tensor(out=v3(f), in0=v3(sg), in1=oml_bc, op=ALU.mult), reads=[sgk, "hlbt"], writes=[fk])
        S.op("dve", lambda e: e.tensor_tensor(out=v3(f), in0=v3(f), in1=lb_bc, op=ALU.add), reads=[fk, "hlbt"], writes=[fk])
        lf, lfk = self.lf.next()
        S.op("act", lambda e: e.activation(out=lf[:], in_=f[:], func=AF.Ln), reads=[fk], writes=[lfk])
        kT, kTk = self.kT.next()
        S.op("dve", lambda e: e.tensor_scalar(out=kT[:], in0=f[:], scalar1=-1.0, scalar2=1.0, op0=ALU.mult, op1=ALU.add),
             reads=[fk], writes=[kTk])
        bT, bTk = self.bT.next()
        S.op("dve", lambda e: e.tensor_tensor_scan(out=bT[:], data0=K.reset4, data1=lf[:], initial=0.0, op0=ALU.mult,
                                                   op1=ALU.add), reads=[lfk], writes=[bTk])
        if d == 0:
            b, bk = bT, bTk
            tcb = _bc_last(c16(b)[:, :, 31:32], 32)
        else:
            tm, tmk = self.tmp.next()
            S.op("dve", lambda e: e.tensor_tensor(out=tm[:], in0=lf[:], in1=bT[:], op=ALU.subtract), reads=[lfk, bTk],
                 writes=[tmk])
            b, bk = self.b2.next()
            S.op("dve", lambda e: e.tensor_tensor(out=c16(b), in0=c16(tm), in1=_bc_last(c16(bT)[:, :, 31:32], 32), op=ALU.add),
                 reads=[tmk, bTk], writes=[bk])
            tcb = _bc_last(c16(b)[:, :, 0:1], 32)
        Eb, Ebk = self.Eb.next()
        S.op("act", lambda e: e.activation(out=Eb[:], in_=b[:], func=AF.Exp), reads=[bk], writes=[Ebk])
        Enb, Enbk = self.Enb.next()
        S.op("act", lambda e: e.activation(out=Enb[:], in_=b[:], func=AF.Exp, scale=-1.0), reads=[bk], writes=[Enbk])
        sq, sqk = self.sq.next()
        S.op("act", lambda e: e.activation(out=v3(sq), in_=self.q[:, :, tc0:tc0 + 128], func=AF.Silu), reads=self.qk,
             writes=[sqk])
        Qtz, Qk = self.Qtz.next()
        qd_all = _fap(Qtz, 0, [[512, 4], [160, 4], [1, 32]])
        S.op("dve", lambda e: e.tensor_tensor(out=qd_all, in0=sq[:].rearrange("p (h c n) -> p h c n", h=4, c=4),
                                              in1=Eb[:].rearrange("p (h c n) -> p h c n", h=4, c=4), op=ALU.mult),
             reads=[sqk, Ebk], writes=[Qk])
        Kt, Ktk = self.Kt.next()
        S.op("dve", lambda e: e.tensor_tensor(out=Kt[:], in0=kT[:], in1=Enb[:], op=ALU.mult), reads=[kTk, Enbk], writes=[Ktk])
        e2, e2k = self.e2.next()
        S.op("dve", lambda e: e.tensor_tensor(out=c16(e2), in0=tcb, in1=c16(b), op=ALU.subtract), reads=[bk], writes=[e2k])
        E2, E2k = self.E2.next()
        S.op("act", lambda e: e.activation(out=E2[:], in_=e2[:], func=AF.Exp), reads=[e2k], writes=[E2k])
        KhT, KhTk = self.KhT.next()
        S.op("dve", lambda e: e.tensor_tensor(out=KhT[:], in0=kT[:], in1=E2[:], op=ALU.mult), reads=[kTk, E2k], writes=[KhTk])
        pkh, pkhk = self.pkh.next()

        def trk(e):
            r = None
            for h in range(4):
                r = e.transpose(out=pkh[:, h * 128:(h + 1) * 128], in_=KhT[:, h * 128:(h + 1) * 128], identity=K.ident_bf)
            return r
        S.op("pe", trk, reads=[KhTk], writes=[pkhk])
        Khs, Khsk = self.Khs.next()
        S.op("act", lambda e: e.copy(out=Khs[:], in_=pkh[:]), reads=[pkhk], writes=[Khsk])
        Khz, Khzk = self.Khz.next()
        rm = K.rowmask
        S.op("dve", lambda e: e.tensor_tensor(
            out=Khz[:].rearrange("p (h c) n -> p h c n", h=4), in0=_fap(Khs, 0, [[128, 4], [0, 4], [1, 128]]),
            in1=bass.AP(tensor=rm.tensor, offset=rm.offset, ap=[list(rm.ap[0]), [0, 4], [1, 4], [0, 128]]), op=ALU.mult),
            reads=[Khsk], writes=[Khzk])
        patt, pattk = self.patt.next()

        def att(e):
            r = None
            for h in range(4):
                r = e.matmul(patt[:, h * 128:(h + 1) * 128], lhsT=Kt[:, h * 128:(h + 1) * 128],
                             rhs=_fap(Qtz, h * 512, [[160, 4], [1, 32]]), start=True, stop=True)
            return r
        S.op("pe", att, reads=[Ktk, Qk], writes=[pattk])
        attm, attmk = self.attm.next()
        mask = K.mask_f if d == 0 else K.mask_b
        S.op("dve", lambda e: e.tensor_tensor(out=v3(attm), in0=v3(patt),
                                              in1=bass.AP(tensor=mask.tensor, offset=mask.offset,
                                                          ap=[list(mask.ap[0]), [0, 4], [1, 128]]), op=ALU.mult),
             reads=[pattk], writes=[attmk])
        order = range(4) if d == 0 else range(3, -1, -1)
        snaps = {}
        vt = self.v
        for c in order:
            snaps[c] = (st["sbf"], st["sbk"])
            pu, puk = self.pu.next()

            def um(e, c=c, pu=pu):
                r = None
                for h in range(4):
                    r = e.matmul(pu[:, h * 128:(h + 1) * 128], lhsT=Khz[:, h * 4 + c, :], rhs=vt[:, t, h * 128:(h + 1) * 128],
                                 start=True, stop=True)
                return r
            S.op("pe", um, reads=[Khzk, "hgv"], writes=[puk])
            col = (32 * c + 31) if d == 0 else (32 * c)
            s32, sk = st["s32"], st["sk"]
            dbc = _fap(Eb, col, [[128, 4], [0, 128]])
            S.op("dve", lambda e, s32=s32, dbc=dbc: e.tensor_tensor(out=v3(s32), in0=v3(s32), in1=dbc, op=ALU.mult),
                 reads=[sk, Ebk], writes=[sk])
            S.op("dve", lambda e, s32=s32, pu=pu: e.tensor_tensor(out=s32[:], in0=s32[:], in1=pu[:], op=ALU.add),
                 reads=[sk, puk], writes=[sk])
            nb, nbk = self.Sb.next()
            S.op("act", lambda e, nb=nb, s32=s32: e.copy(out=nb[:], in_=s32[:]), reads=[sk], writes=[nbk])
            st["sbf"], st["sbk"] = nb, nbk
        po, pok = self.po.next()

        def om(e):
            r = None
            for h in range(4):
                e.matmul(po[:, h * 128:(h + 1) * 128], lhsT=attm[:, h * 128:(h + 1) * 128], rhs=vt[:, t, h * 128:(h + 1) * 128],
                         start=True, stop=False)
                for c in range(4):
                    r = e.matmul(po[:, h * 128:(h + 1) * 128], lhsT=Qtz[:, h * 4 + c, :], rhs=snaps[c][0][:, h * 128:(h + 1) * 128],
                                 start=False, stop=(c == 3))
            return r
        S.op("pe", om, reads=[attmk, "hgv", Qk] + [snaps[c][1] for c in range(4)], writes=[pok])
        dst = self.ostore[:, t, :]
        ok = ("ostore", t)
        if not accumulate:
            S.op("act", lambda e: e.copy(out=dst, in_=po[:]), reads=[pok], writes=[ok])
        else:
            S.op("dve", lambda e: e.tensor_tensor(out=dst, in0=po[:], in1=dst, op=ALU.add), reads=[pok, ok], writes=[ok])


def phase_hg1(S, nc, K, T, l, with_ctx):
    from contextlib import ExitStack
    with ExitStack() as es:
        E = HgEnv(S, nc, K, T, l, es)
        E.load(0)
        st = E.chain_init(None)
        for t in range(NOWN):
            E.tile(0, t, st, accumulate=False)
        s32 = st["s32"]
        S.dma("sp", lambda e: e.dma_start(out=T.hsend[:, :].rearrange("(h p) n -> p h n", p=128),
                                          in_=s32[:].rearrange("p (h n) -> p h n", h=4)), "hsend", reads=[st["sk"]],
              writes=["hsend"])
        if with_ctx:
            E.load(1)
            st = E.chain_init(None)
            for t in (1, 0):
                E.tile(1, t, st, accumulate=True)
        for t in range(NOWN):
            S.dma("sp", lambda e, t=t: e.dma_start(out=T.hostore[t * 128:(t + 1) * 128, :], in_=E.ostore[:, t, :]),
                  ("hos", t % 4), reads=[("ostore", t)], writes=[("hostore", t)])
        S.flush("hg1_%d" % l)


def phase_hg2(S, nc, K, T, l, with_ctx, state_src):
    from contextlib import ExitStack
    with ExitStack() as es:
        sb = lambda name, shape, dt: es.enter_context(nc.sbuf_tensor(_u(name), shape, dt))
        pst = lambda name, shape, dt: es.enter_context(nc.psum_tensor(_u(name), shape, dt))
        E = HgEnv(S, nc, K, T, l, es)
        for t in range(NOWN):
            S.dma("sp", lambda e, t=t: e.dma_start(out=E.ostore[:, t, :], in_=T.hostore[t * 128:(t + 1) * 128, :]),
                  ("hos", t % 4), writes=[("ostore", t)])
        E.load(1)
        st = E.chain_init(None if state_src is None else state_src)
        for t in range(NOWN - 1, 1, -1):
            E.tile(1, t, st, accumulate=True)
        for t in range(NOWN):
            S.dma("sp", lambda e, t=t: e.dma_start(out=T.hostore[t * 128:(t + 1) * 128, :], in_=E.ostore[:, t, :]),
                  ("hos2", t % 4), reads=[("ostore", t)], writes=[("hostore", t)])
        S.flush("hg2_%d" % l)
    with ExitStack() as es:
        sb = lambda name, shape, dt: es.enter_context(nc.sbuf_tensor(_u(name), shape, dt))
        pst = lambda name, shape, dt: es.enter_context(nc.psum_tensor(_u(name), shape, dt))
        orr = Ring("hor", [sb("hor%d" % i, [128, 512], F32) for i in range(3)])
        gt = sb("hgt", [128, NOWN, 512], BF16)
        ngb = sb("hngb", [128, 512], F32)
        stH = sb("hstH", [128, 4, NOWN * 128], BF16)
        S.dma("sp", lambda e: e.dma_start(out=gt[:], in_=T.utok[0:NOWN * 128, 1536:2048].rearrange("(t p) c -> p t c", p=128)),
              "hgt", writes=["hgt"])
        S.dma("sp", lambda e: e.dma_start(out=ngb[:], in_=bcast_row(T.hnorm_g[l:l + 1, :], 512)), "hngb", writes=["hngb"])
        ssb = Ring("hss", [sb("hss%d" % i, [128, 4], F32) for i in range(2)])
        junk = sb("hjunk", [128, 128], F32)
        sgb = Ring("hsgt", [sb("hsgt%d" % i, [128, 512], F32) for i in range(2)])
        onb = Ring("hon", [sb("hon%d" % i, [128, 512], F32) for i in range(2)])
        hgb = Ring("hhg", [sb("hhg%d" % i, [128, 512], BF16) for i in range(2)])
        pT = Ring("hpT", [pst("hpT%d" % i, [128, 512], BF16) for i in range(1)])
        t0 = 0 if with_ctx else 2
        for t in range(t0, NOWN):
            ot, otk = orr.next()
            S.dma("sp", lambda e, ot=ot, t=t: e.dma_start(out=ot[:], in_=T.hostore[t * 128:(t + 1) * 128, :]), ("hor", otk[1]),
                  writes=[otk])
            ss, ssk = ssb.next()
            for h in range(4):
                S.op("act", lambda e, h=h, ss=ss, ot=ot: e.activation(out=junk[:], in_=ot[:, h * 128:(h + 1) * 128],
                                                                    func=AF.Square, accum_out=ss[:, h:h + 1]),
                     reads=[otk], writes=[(ssk, h), "hjunk"])
            allss = [(ssk, h) for h in range(4)]
            S.op("dve", lambda e, ss=ss: e.tensor_scalar(out=ss[:], in0=ss[:], scalar1=1.0 / 128, scalar2=EPS, op0=ALU.mult,
                                                         op1=ALU.add), reads=allss, writes=allss)
            S.op("act", lambda e, ss=ss: e.activation(out=ss[:], in_=ss[:], func=AF.Sqrt), reads=allss, writes=allss)
            S.op("dve", lambda e, ss=ss: e.reciprocal(out=ss[:], in_=ss[:]), reads=allss, writes=allss)
            sgt, sgk = sgb.next()
            S.op("act", lambda e, sgt=sgt, t=t: e.activation(out=sgt[:], in_=gt[:, t, :], func=AF.Silu), reads=["hgt"],
                 writes=[sgk])
            on, onk = onb.next()
            S.op("dve", lambda e, on=on, ss=ss, ot=ot: e.tensor_tensor(
                out=on[:].rearrange("p (h n) -> p h n", h=4), in0=ot[:].rearrange("p (h n) -> p h n", h=4),
                in1=_bc_last(ss[:].rearrange("p (h o) -> p h o", o=1), 128), op=ALU.mult),
                reads=allss + [otk], writes=[onk])
            S.op("dve", lambda e, on=on: e.tensor_tensor(out=on[:], in0=on[:], in1=ngb[:], op=ALU.mult), reads=[onk, "hngb"],
                 writes=[onk])
            hg, hgk = hgb.next()
            S.op("dve", lambda e, on=on, sgt=sgt, hg=hg: e.tensor_tensor(out=hg[:], in0=on[:], in1=sgt[:], op=ALU.mult),
                 reads=[onk, sgk], writes=[hgk])
            p, pk = pT.next()

            def tr(e, p=p, hg=hg):
                r = None
                for c in range(4):
                    r = e.transpose(out=p[:, c * 128:(c + 1) * 128], in_=hg[:, c * 128:(c + 1) * 128], identity=K.ident_bf)
                return r
            S.op("pe", tr, reads=[hgk], writes=[pk])
            S.op("act", lambda e, p=p, t=t: e.copy(out=stH[:, :, t * 128:(t + 1) * 128],
                                                   in_=p[:].rearrange("p (c n) -> p c n", c=4)), reads=[pk],
                 writes=[("stH", t)])
        c0 = t0 * 128
        for c in range(4):
            S.dma("sp", lambda e, c=c: e.dma_start(out=T.mixT[(12 + c) * 128:(13 + c) * 128, c0:NOWN * 128],
                                                   in_=stH[:, c, c0:NOWN * 128]), ("hst", c),
                  reads=[("stH", t) for t in range(t0, NOWN)], writes=[("mixT", 12 + c)])
        S.flush("hg3_%d" % l)


def prep_hgrn(hgrn_lb, hgrn_norm_g, odd):
    lb = hgrn_lb[:, ::-1] if odd else hgrn_lb
    hlb = lb.reshape(DEPTH, 2, 4, 128).transpose(3, 0, 1, 2).reshape(128, DEPTH * 8)
    return np.ascontiguousarray(hlb).astype(np.float32), np.ascontiguousarray(hgrn_norm_g).astype(np.float32)


def load_bc(S, nc, T, l, idx, es, tag):
    sb = lambda name, shape, dt: es.enter_context(nc.sbuf_tensor(_u(name), shape, dt))
    out = {}
    for kind in range(2):
        g = sb(tag + "%d" % kind, [128, D], F32)
        src = T.mod[l * 2 + kind:l * 2 + kind + 1, idx * D:(idx + 1) * D]
        S.dma("sp", lambda e, g=g, src=src: e.dma_start(out=g[:], in_=bcast_row(src, D)), (tag, kind), writes=[(tag, kind)])
        out[kind] = g
    return out


def phase_C(S, nc, K, T, l, with_ctx, xs):
    from contextlib import ExitStack
    with ExitStack() as es:
        sb = lambda name, shape, dt: es.enter_context(nc.sbuf_tensor(_u(name), shape, dt))
        pst = lambda name, shape, dt: es.enter_context(nc.psum_tensor(_u(name), shape, dt))
        mix = sb("mixall", [128, 16, NOWN * 128], BF16)
        for c in range(16):
            S.dma("sp", lambda e, c=c: e.dma_start(out=mix[:, c, :], in_=T.mixT[c * 128:(c + 1) * 128, :]), ("mixl", c % 4),
                  writes=[("mix", c)])
        ga = load_bc(S, nc, T, l, MOD_GA1, es, "ga1")
        wo = Ring("wo", [sb("wo%d" % i, [128, 16, 512], BF16) for i in range(2)])
        xi = Ring("cxi", [sb("cxi%d" % i, [128, 512], F32) for i in range(3)])
        yt = Ring("cyt", [sb("cyt%d" % i, [128, 512], F32) for i in range(3)])
        pp = Ring("cpp", [pst("cpp%d" % i, [128, 512], F32) for i in range(4)])
        t0 = 0 if with_ctx else 2
        for ng in range(4):
            w, wk = wo.next()
            src = T.w_outm[(l * 4 + ng) * 128:(l * 4 + ng + 1) * 128, :]
            S.dma("pool", lambda e, w=w, src=src: e.dma_start(out=w[:].rearrange("p k n -> p (k n)"), in_=src), ("wo", wk[1]),
                  writes=[wk])
            for t in range(t0, NOWN):
                p, pk = pp.next()

                def mm(e, p=p, w=w, t=t):
                    r = None
                    for k in range(16):
                        r = e.matmul(p[:], lhsT=mix[:, k, t * 128:(t + 1) * 128], rhs=w[:, k, :], start=(k == 0), stop=(k == 15))
                    return r
                S.op("pe", mm, reads=[wk] + [("mix", c) for c in range(16)], writes=[pk])
                x, xk = xi.next()
                S.dma("sp", lambda e, x=x, t=t, ng=ng: e.dma_start(out=x[:], in_=xs[t * 128:(t + 1) * 128, ng * 512:(ng + 1) * 512]),
                      ("cxi", xk[1]), writes=[xk])
                y, yk = yt.next()
                g = ga[tile_kind(t)]
                S.op("dve", lambda e, y=y, p=p, g=g, ng=ng: e.tensor_tensor(out=y[:], in0=p[:], in1=g[:, ng * 512:(ng + 1) * 512],
                                                                           op=ALU.mult),
                     reads=[pk, ("ga1", tile_kind(t))], writes=[yk])
                S.op("pool", lambda e, y=y, x=x: e.tensor_tensor(out=y[:], in0=y[:], in1=x[:], op=ALU.add), reads=[yk, xk],
                     writes=[yk])
                S.dma("sp", lambda e, y=y, t=t, ng=ng: e.dma_start(out=T.x1[t * 128:(t + 1) * 128, ng * 512:(ng + 1) * 512], in_=y[:]),
                      ("cyo", yk[1]), reads=[yk], writes=[("x1", t, ng)])
        S.flush("C_%d" % l)


BIG = 1.0e30


def phase_D1(S, nc, K, T, l, with_ctx):
    from contextlib import ExitStack
    with ExitStack() as es:
        sb = lambda name, shape, dt: es.enter_context(nc.sbuf_tensor(_u(name), shape, dt))
        pst = lambda name, shape, dt: es.enter_context(nc.psum_tensor(_u(name), shape, dt))
        h2T = sb("h2T", [128, 16, NOWN * 128], BF16)
        wtall = sb("wtall", [128, NOWN, 32], F32)
        wr = sb("wr", [128, 16, 36], F32)
        br = sb("brb", [128, 36], F32)
        S.dma("sp", lambda e: e.dma_start(out=wr[:].rearrange("p k n -> p (k n)"), in_=T.w_r[l * 128:(l + 1) * 128, :]), "wr",
              writes=["wr"])
        S.dma("sp", lambda e: e.dma_start(out=br[:], in_=bcast_row(T.b_r[l:l + 1, :], 36)), "brb", writes=["brb"])
        AB = load_mod_AB(S, nc, T, l, (MOD_SH2, MOD_S2), T.g_ffn[l:l + 1, :], es, "m2")
        t0 = 0 if with_ctx else 2
        tl = list(range(t0, NOWN))
        tiles = [(t * 128, tile_kind(t)) for t in tl]
        ptr = Ring("rptr", [pst("rptr%d" % i, [128, 512], F32) for i in range(2)])
        pr = Ring("rpr", [pst("rpr%d" % i, [128, 36], F32) for i in range(1)])
        hT32 = Ring("rhT32", [sb("rhT32_%d" % i, [128, 16, 128], F32) for i in range(2)])
        sm = lambda nm, w: Ring(nm, [sb("%s%d" % (nm, i), [128, w], F32) for i in range(2)])
        lgb, st1, gmb, egb, pen, lem, o1b, lem2, o2b = (sm("rlg", 36), sm("rst", 8), sm("rgm", 4), sm("reg", 4), sm("rpen", 4),
                                                        sm("rlem", 32), sm("ro1", 32), sm("rlem2", 32), sm("ro2", 32))

        def router(i, h32, h32k):
            t = tl[i]
            hT, hTk = hT32.next()
            for g in range(4):
                p, pk = ptr.next()

                def tr(e, p=p, g=g):
                    r = None
                    for j in range(4):
                        kk = g * 4 + j
                        r = e.transpose(out=p[:, j * 128:(j + 1) * 128], in_=h32[:, kk * 128:(kk + 1) * 128], identity=K.ident32)
                    return r
                S.op("pe", tr, reads=[h32k], writes=[pk])
                S.op("act", lambda e, p=p, g=g: e.copy(out=hT[:, g * 4:(g + 1) * 4, :], in_=p[:].rearrange("p (j n) -> p j n", j=4)),
                     reads=[pk], writes=[(hTk, g)])
            p, pk = pr.next()

            def mm(e, p=p):
                r = None
                for k in range(16):
                    r = e.matmul(p[:], lhsT=hT[:, k, :], rhs=wr[:, k, :], start=(k == 0), stop=(k == 15))
                return r
            S.op("pe", mm, reads=[(hTk, g) for g in range(4)] + ["wr"], writes=[pk])
            lg, lgk = lgb.next()
            S.op("dve", lambda e: e.tensor_tensor(out=lg[:], in0=p[:], in1=br[:], op=ALU.add), reads=[pk, "brb"], writes=[lgk])
            s, sk = st1.next()
            S.op("dve", lambda e: e.reduce_max(out=s[:, 0:1], in_=lg[:, 0:4], axis=AX.X), reads=[lgk], writes=[sk])
            S.op("dve", lambda e: e.tensor_scalar(out=s[:, 1:2], in0=s[:, 0:1], scalar1=-1.0, scalar2=None, op0=ALU.mult),
                 reads=[sk], writes=[sk])
            gm, gmk = gmb.next()
            S.op("dve", lambda e: e.tensor_scalar(out=gm[:], in0=lg[:, 0:4], scalar1=s[:, 0:1], scalar2=None, op0=ALU.is_equal),
                 reads=[lgk, sk], writes=[gmk])
            eg, egk = egb.next()
            S.op("act", lambda e: e.activation(out=eg[:], in_=lg[:, 0:4], func=AF.Exp, bias=s[:, 1:2], accum_out=s[:, 2:3]),
                 reads=[lgk, sk], writes=[egk, sk])
            pn, pnk = pen.next()
            S.op("dve", lambda e: e.tensor_scalar(out=pn[:], in0=gm[:], scalar1=BIG, scalar2=-BIG, op0=ALU.mult, op1=ALU.add),
                 reads=[gmk], writes=[pnk])
            le, lek = lem.next()
            S.op("dve", lambda e: e.tensor_tensor(out=le[:].rearrange("p (g n) -> p g n", g=4),
                                                  in0=lg[:, 4:36].rearrange("p (g n) -> p g n", g=4),
                                                  in1=_bc_last(pn[:].rearrange("p (g o) -> p g o", o=1), 8), op=ALU.add),
                 reads=[lgk, pnk], writes=[lek])
            S.op("dve", lambda e: e.reduce_max(out=s[:, 3:4], in_=le[:], axis=AX.X), reads=[lek, sk], writes=[sk])
            o1, o1k = o1b.next()
            S.op("dve", lambda e: e.tensor_scalar(out=o1[:], in0=le[:], scalar1=s[:, 3:4], scalar2=None, op0=ALU.is_equal),
                 reads=[lek, sk], writes=[o1k])
            l2, l2k = lem2.next()
            S.op("dve", lambda e: e.scalar_tensor_tensor(out=l2[:], in0=o1[:], scalar=-BIG, in1=le[:], op0=ALU.mult, op1=ALU.add),
                 reads=[o1k, lek], writes=[l2k])
            S.op("dve", lambda e: e.reduce_max(out=s[:, 4:5], in_=l2[:], axis=AX.X), reads=[l2k, sk], writes=[sk])
            o2, o2k = o2b.next()
            S.op("dve", lambda e: e.tensor_scalar(out=o2[:], in0=l2[:], scalar1=s[:, 4:5], scalar2=None, op0=ALU.is_equal),
                 reads=[l2k, sk], writes=[o2k])
            S.op("dve", lambda e: e.tensor_tensor(out=s[:, 5:6], in0=s[:, 4:5], in1=s[:, 3:4], op=ALU.subtract), reads=[sk],
                 writes=[sk])
            S.op("act", lambda e: e.activation(out=s[:, 6:7], in_=s[:, 5:6], func=AF.Exp), reads=[sk], writes=[sk])
            S.op("dve", lambda e: e.scalar_tensor_tensor(out=s[:, 7:8], in0=s[:, 6:7], scalar=1.0, in1=s[:, 2:3], op0=ALU.add,
                                                         op1=ALU.mult), reads=[sk], writes=[sk])
            S.op("dve", lambda e: e.reciprocal(out=s[:, 7:8], in_=s[:, 7:8]), reads=[sk], writes=[sk])
            S.op("dve", lambda e: e.tensor_tensor(out=s[:, 6:7], in0=s[:, 6:7], in1=s[:, 7:8], op=ALU.mult), reads=[sk], writes=[sk])
            S.op("dve", lambda e: e.tensor_scalar(out=wtall[:, t, :], in0=o1[:], scalar1=s[:, 7:8], scalar2=None, op0=ALU.mult),
                 reads=[o1k, sk], writes=[("wt", t)])
            S.op("dve", lambda e: e.scalar_tensor_tensor(out=wtall[:, t, :], in0=o2[:], scalar=s[:, 6:7], in1=wtall[:, t, :],
                                                         op0=ALU.mult, op1=ALU.add), reads=[o2k, sk, ("wt", t)], writes=[("wt", t)])

        emit_norm(S, nc, K, tiles, lambda i: T.x1[tl[i] * 128:(tl[i] + 1) * 128, :], AB, h2T, es, want32=router)
        for c in range(16):
            S.dma("sp", lambda e, c=c: e.dma_start(out=T.h2T[c * 128:(c + 1) * 128, :], in_=h2T[:, c, :]), ("h2o", c % 4),
                  reads=[("hT", t, half) for t in tl for half in range(2)], writes=[("h2Td", c)])
        S.dma("sp", lambda e: e.dma_start(out=T.wt[:, :], in_=wtall[:].rearrange("p t n -> p (t n)")), "wto",
              reads=[("wt", t) for t in tl], writes=["wtd"])
        S.flush("D1_%d" % l)


def prep_misc(w_out, w_rg, b_rg, w_re, b_re):
    wo = np.empty((DEPTH, 4, 128, 16 * 512), np.float32)
    for l in range(DEPTH):
        for g in range(4):
            wo[l, g] = w_out[l][:, g * 512:(g + 1) * 512].reshape(16, 128, 512).transpose(1, 0, 2).reshape(128, -1)
    wr = np.concatenate([w_rg, w_re], axis=-1)
    wr = wr.reshape(DEPTH, 16, 128, 36).transpose(0, 2, 1, 3).reshape(DEPTH * 128, 16 * 36)
    br = np.concatenate([b_rg, b_re], axis=-1)
    return wo.reshape(-1, 16 * 512), np.ascontiguousarray(wr).astype(np.float32), np.ascontiguousarray(br).astype(np.float32)


def phase_D2(S, nc, K, T, l, with_ctx, nexp=NEXP):
    from contextlib import ExitStack
    t0 = 0 if with_ctx else 2
    tl = list(range(t0, NOWN))
    parts = [tl[i:i + 6] for i in range(0, len(tl), 6)]
    with ExitStack() as es:
        sb = lambda name, shape, dt: es.enter_context(nc.sbuf_tensor(_u(name), shape, dt))
        pst = lambda name, shape, dt: es.enter_context(nc.psum_tensor(_u(name), shape, dt))
        wt = sb("wtl", [128, NOWN, 32], F32)
        S.dma("sp", lambda e: e.dma_start(out=wt[:].rearrange("p t n -> p (t n)"), in_=T.wt[:, :]), "wtl", writes=["wtl"])
        h2 = sb("h2p", [128, 16, 768], BF16)
        acc = sb("facc", [128, 6, D], F32)
        w1r = Ring("w1", [sb("w1_%d" % i, [128, 16, 512], BF16) for i in range(2)])
        w3r = Ring("w3", [sb("w3_%d" % i, [128, 16, 512], BF16) for i in range(2)])
        w2r = Ring("w2", [sb("w2_%d" % i, [128, 4, D], BF16) for i in range(2)])
        aTr = Ring("aT", [sb("aT_%d" % i, [128, 4, 512], BF16) for i in range(2)])
        sgr = Ring("sgm", [sb("sgm_%d" % i, [128, 512], F32) for i in range(2)])
        pg = Ring("pg", [pst("pg%d" % i, [128, 512], F32) for i in range(2)])
        pu = Ring("pu", [pst("pu%d" % i, [128, 512], F32) for i in range(2)])
        po = Ring("po", [pst("po%d" % i, [128, 512], F32) for i in range(3)])
        for pi, part in enumerate(parts):
            ntp = len(part)
            c0 = part[0] * 128
            S.dma("sp", lambda e, c0=c0, ntp=ntp: e.dma_start(
                out=h2[:, :, 0:ntp * 128], in_=T.h2T[:, c0:c0 + ntp * 128].rearrange("(c p) n -> p c n", p=128)),
                "h2p", writes=["h2p"])
            S.op("dve", lambda e: e.memset(acc[:], 0.0), writes=[("acc", i) for i in range(6)])
            groups = [(0, min(4, ntp))] + ([(4, ntp - 4)] if ntp > 4 else [])
            for ex in range(nexp):
                w1, w1k = w1r.next()
                w3, w3k = w3r.next()
                w2, w2k = w2r.next()
                r0 = (l * nexp + ex) * 128
                S.dma("pool", lambda e, w1=w1, r0=r0: e.dma_start(out=w1[:].rearrange("p k n -> p (k n)"),
                                                                in_=T.w1m[r0:r0 + 128, :]), ("w1", w1k[1]), writes=[w1k])
                S.dma("pool", lambda e, w3=w3, r0=r0: e.dma_start(out=w3[:].rearrange("p k n -> p (k n)"),
                                                                in_=T.w3m[r0:r0 + 128, :]), ("w3", w3k[1]), writes=[w3k])
                S.dma("pool", lambda e, w2=w2, r0=r0: e.dma_start(out=w2[:].rearrange("p k n -> p (k n)"),
                                                                in_=T.w2m[r0:r0 + 128, :]), ("w2", w2k[1]), writes=[w2k])
                for (g0, gn) in groups:
                    n = gn * 128
                    cs = g0 * 128
                    aT, aTk = aTr.next()
                    for j in range(4):
                        p1, p1k = pg.next()
                        p3, p3k = pu.next()

                        def mm(e, p=p1, w=w1, j=j, cs=cs, n=n):
                            r = None
                            for k in range(16):
                                r = e.matmul(p[:, 0:n], lhsT=w[:, k, j * 128:(j + 1) * 128], rhs=h2[:, k, cs:cs + n],
                                             start=(k == 0), stop=(k == 15))
                            return r
                        S.op("pe", mm, reads=[w1k, "h2p"], writes=[p1k])

                        def mm3(e, p=p3, w=w3, j=j, cs=cs, n=n):
                            r = None
                            for k in range(16):
                                r = e.matmul(p[:, 0:n], lhsT=w[:, k, j * 128:(j + 1) * 128], rhs=h2[:, k, cs:cs + n],
                                             start=(k == 0), stop=(k == 15))
                            return r
                        S.op("pe", mm3, reads=[w3k, "h2p"], writes=[p3k])
                        sg, sgk = sgr.next()
                        S.op("act", lambda e, sg=sg, p1=p1, n=n: e.activation(out=sg[:, 0:n], in_=p1[:, 0:n], func=AF.Silu),
                             reads=[p1k], writes=[sgk])
                        S.op("dve", lambda e, aT=aT, sg=sg, p3=p3, j=j, n=n: e.tensor_tensor(
                            out=aT[:, j, 0:n], in0=p3[:, 0:n], in1=sg[:, 0:n], op=ALU.mult), reads=[p3k, sgk], writes=[(aTk, j)])
                    for ti in range(gn):
                        tix = g0 + ti
                        t = part[tix]
                        for nn in range(4):
                            p, pk = po.next()

                            def mmo(e, p=p, aT=aT, w2=w2, ti=ti, nn=nn):
                                r = None
                                for j in range(4):
                                    r = e.matmul(p[:], lhsT=aT[:, j, ti * 128:(ti + 1) * 128], rhs=w2[:, j, nn * 512:(nn + 1) * 512],
                                                 start=(j == 0), stop=(j == 3))
                                return r
                            S.op("pe", mmo, reads=[(aTk, j) for j in range(4)] + [w2k], writes=[pk])
                            dst = acc[:, tix, nn * 512:(nn + 1) * 512]
                            S.op("dve", lambda e, p=p, dst=dst, t=t, ex=ex: e.scalar_tensor_tensor(
                                out=dst, in0=p[:], scalar=wt[:, t, ex:ex + 1], in1=dst, op0=ALU.mult, op1=ALU.add),
                                reads=[pk, "wtl", ("acc", tix)], writes=[("acc", tix)])
            for tix, t in enumerate(part):
                S.dma("sp", lambda e, tix=tix, t=t: e.dma_start(out=T.facc[t * 128:(t + 1) * 128, :], in_=acc[:, tix, :]),
                      ("fao", tix), reads=[("acc", tix)], writes=[("faccd", t)])
        S.flush("D2_%d" % l)


def phase_D3(S, nc, K, T, l, with_ctx, last):
    from contextlib import ExitStack
    t0 = 0 if with_ctx else 2
    with ExitStack() as es:
        sb = lambda name, shape, dt: es.enter_context(nc.sbuf_tensor(_u(name), shape, dt))
        ga = load_bc(S, nc, T, l, MOD_GA2, es, "ga2")
        if last:
            gf = sb("gfin", [128, D], F32)
            S.dma("sp", lambda e: e.dma_start(out=gf[:], in_=bcast_row(T.g_final[0:1, :], D)), "gfin", writes=["gfin"])
            jb = sb("d3junk", [128, D], BF16)
            st = Ring("d3st", [sb("d3st%d" % i, [128, 2], F32) for i in range(2)])
        xb = Ring("d3x", [sb("d3x%d" % i, [128, D], F32) for i in range(2)])
        fb = Ring("d3f", [sb("d3f%d" % i, [128, D], F32) for i in range(2)])
        for t in range(t0, NOWN):
            x, xk = xb.next()
            f, fk = fb.next()
            S.dma("sp", lambda e, x=x, t=t: e.dma_start(out=x[:], in_=T.x1[t * 128:(t + 1) * 128, :]), ("d3x", xk[1]), writes=[xk])
            S.dma("sp", lambda e, f=f, t=t: e.dma_start(out=f[:], in_=T.facc[t * 128:(t + 1) * 128, :]), ("d3f", fk[1]), writes=[fk])
            g = ga[tile_kind(t)]
            S.op("dve", lambda e, f=f, g=g: e.tensor_tensor(out=f[:], in0=f[:], in1=g[:], op=ALU.mult),
                 reads=[fk, ("ga2", tile_kind(t))], writes=[fk])
            S.op("pool", lambda e, f=f, x=x: e.tensor_tensor(out=f[:], in0=f[:], in1=x[:], op=ALU.add), reads=[fk, xk], writes=[fk])
            if not last:
                S.dma("sp", lambda e, f=f, t=t: e.dma_start(out=T.xs2[t * 128:(t + 1) * 128, :], in_=f[:]), ("d3o", fk[1]),
                      reads=[fk], writes=[("xs2", t)])
                if t >= 16:
                    S.dma("sp", lambda e, f=f, t=t: e.dma_start(out=T.xsend[(t - 16) * 128:(t - 15) * 128, :], in_=f[:]),
                          ("d3o2", fk[1]), reads=[fk], writes=[("xsend", t)])
            else:
                s, sk = st.next()
                S.op("act", lambda e, f=f, s=s: e.activation(out=jb[:], in_=f[:], func=AF.Square, accum_out=s[:, 0:1]),
                     reads=[fk], writes=[sk, "d3junk"])
                S.op("dve", lambda e, s=s: e.tensor_scalar(out=s[:, 1:2], in0=s[:, 0:1], scalar1=1.0 / D, scalar2=EPS,
                                                           op0=ALU.mult, op1=ALU.add), reads=[sk], writes=[sk])
                S.op("act", lambda e, s=s: e.activation(out=s[:, 1:2], in_=s[:, 1:2], func=AF.Sqrt), reads=[sk], writes=[sk])
                S.op("dve", lambda e, s=s: e.reciprocal(out=s[:, 1:2], in_=s[:, 1:2]), reads=[sk], writes=[sk])
                S.op("dve", lambda e, f=f, s=s: e.scalar_tensor_tensor(out=f[:], in0=f[:], scalar=s[:, 1:2], in1=gf[:],
                                                                       op0=ALU.mult, op1=ALU.mult),
                     reads=[fk, sk, "gfin"], writes=[fk])
                S.dma("sp", lambda e, f=f, t=t: e.dma_start(out=T.out[(t - 2) * 128:(t - 1) * 128, :], in_=f[:]), ("d3o", fk[1]),
                      reads=[fk], writes=[("out", t)])
        S.flush("D3_%d" % l)


PAIRS = [[0, 1], [2, 3], [4, 5], [6, 7]]


def coll(S, fn, key, reads, writes):
    return S._emit("pool", fn, S.dsem(key), 1, reads, writes)


def phase_xchg_x(S, nc, K, T):
    from contextlib import ExitStack
    with ExitStack() as es:
        sb = lambda name, shape, dt: es.enter_context(nc.sbuf_tensor(_u(name), shape, dt))
        pst = lambda name, shape, dt: es.enter_context(nc.psum_tensor(_u(name), shape, dt))
        coll(S, lambda e: e.collective_compute("AllGather", ALU.bypass, replica_groups=PAIRS, ins=[T.xsend.opt()],
                                               outs=[T.xrecv.opt()]), "ccx", [], ["xrecv"])
        pf = sb("pflag", [128, 2], F32)
        S.dma("sp", lambda e: e.dma_start(out=pf[:], in_=T.pflag[:, :]), "pflag", writes=["pflag"])
        s0r = Ring("xs0", [sb("xs0_%d" % i, [128, D], F32) for i in range(2)])
        s1r = Ring("xs1", [sb("xs1_%d" % i, [128, D], F32) for i in range(2)])
        pp = Ring("xpp", [pst("xpp%d" % i, [128, 512], F32) for i in range(4)])
        for (ht, pr0) in ((18, 128), (19, 0)):
            a, ak = s0r.next()
            b, bk = s1r.next()
            S.dma("sp", lambda e, a=a, pr0=pr0: e.dma_start(out=a[:], in_=T.xrecv[pr0:pr0 + 128, :]), ("xs0", ak[1]),
                  reads=["xrecv"], writes=[ak])
            S.dma("sp", lambda e, b=b, pr0=pr0: e.dma_start(out=b[:], in_=T.xrecv[256 + pr0:256 + pr0 + 128, :]), ("xs1", bk[1]),
                  reads=["xrecv"], writes=[bk])
            S.op("dve", lambda e, a=a: e.tensor_scalar(out=a[:], in0=a[:], scalar1=pf[:, 0:1], scalar2=None, op0=ALU.mult),
                 reads=[ak, "pflag"], writes=[ak])
            S.op("dve", lambda e, a=a, b=b: e.scalar_tensor_tensor(out=a[:], in0=b[:], scalar=pf[:, 1:2], in1=a[:], op0=ALU.mult,
                                                                   op1=ALU.add), reads=[ak, bk, "pflag"], writes=[ak])
            for nn in range(4):
                p, pk = pp.next()
                S.op("pe", lambda e, p=p, a=a, nn=nn: e.matmul(p[:], lhsT=K.anti32, rhs=a[:, nn * 512:(nn + 1) * 512], start=True,
                                                              stop=True), reads=[ak], writes=[pk])
                S.op("act", lambda e, p=p, b=b, nn=nn: e.copy(out=b[:, nn * 512:(nn + 1) * 512], in_=p[:]), reads=[pk, bk],
                     writes=[(bk, nn)])
            S.dma("sp", lambda e, b=b, ht=ht: e.dma_start(out=T.xs2[ht * 128:(ht + 1) * 128, :], in_=b[:]), ("xso", bk[1]),
                  reads=[(bk, nn) for nn in range(4)], writes=[("xs2", ht)])
        S.flush("xchg_x")


def phase_xchg_h(S, nc, K, T):
    from contextlib import ExitStack
    with ExitStack() as es:
        sb = lambda name, shape, dt: es.enter_context(nc.sbuf_tensor(_u(name), shape, dt))
        coll(S, lambda e: e.collective_compute("AllGather", ALU.bypass, replica_groups=PAIRS, ins=[T.hsend.opt()],
                                               outs=[T.hrecv2.opt()]), "cch", [], ["hrecv2"])
        pf = sb("pflagh", [128, 2], F32)
        S.dma("sp", lambda e: e.dma_start(out=pf[:], in_=T.pflag[:, :]), "pflagh", writes=["pflagh"])
        a = sb("hxa", [128, 4, 128], F32)
        b = sb("hxb", [128, 4, 128], F32)
        S.dma("sp", lambda e: e.dma_start(out=a[:], in_=T.hrecv2[0:512, :].rearrange("(h p) n -> p h n", p=128)), "hxa",
              reads=["hrecv2"], writes=["hxa"])
        S.dma("sp", lambda e: e.dma_start(out=b[:], in_=T.hrecv2[512:1024, :].rearrange("(h p) n -> p h n", p=128)), "hxb",
              reads=["hrecv2"], writes=["hxb"])
        S.op("dve", lambda e: e.tensor_scalar(out=a[:], in0=a[:], scalar1=pf[:, 0:1], scalar2=None, op0=ALU.mult),
             reads=["hxa", "pflagh"], writes=["hxa"])
        S.op("dve", lambda e: e.scalar_tensor_tensor(out=a[:], in0=b[:], scalar=pf[:, 1:2], in1=a[:], op0=ALU.mult, op1=ALU.add),
             reads=["hxa", "hxb", "pflagh"], writes=["hxa"])
        S.dma("sp", lambda e: e.dma_start(out=T.hrecv[:, :].rearrange("(h p) n -> p h n", p=128), in_=a[:]), "hxo",
              reads=["hxa"], writes=["hrecv"])
        S.flush("xchg_h")


def phase_mod(S, nc, K, T):
    from contextlib import ExitStack
    with ExitStack() as es:
        sb = lambda name, shape, dt: es.enter_context(nc.sbuf_tensor(_u(name), shape, dt))
        pst = lambda name, shape, dt: es.enter_context(nc.psum_tensor(_u(name), shape, dt))
        cT = sb("cT", [128, 16, 5], F32)
        S.dma("sp", lambda e: e.dma_start(out=cT[:].rearrange("p k r -> p (k r)"), in_=T.cT[:, :]), "cT", writes=["cT"])
        S.op("act", lambda e: e.activation(out=cT[:], in_=cT[:], func=AF.Silu), reads=["cT"], writes=["cT"])
        ba = sb("bada", [1, DEPTH * 1536], F32)
        S.dma("sp", lambda e: e.dma_start(out=ba[:], in_=T.b_ada[0:1, :]), "bada", writes=["bada"])
        wa = sb("wada", [128, 16, 1536], F32)
        ml = sb("modloc", [5, DEPTH * 1536], F32)
        pp = Ring("mpp", [pst("mpp%d" % i, [128, 512], F32) for i in range(3)])
        for l in range(DEPTH):
            S.dma("sp", lambda e, l=l: e.dma_start(out=wa[:].rearrange("p k n -> p (k n)"), in_=T.w_adam[l * 128:(l + 1) * 128, :]),
                  "wada", writes=["wada"])
            for nn in range(3):
                p, pk = pp.next()

                def mm(e, p=p, nn=nn, l=l):
                    for k in range(16):
                        e.matmul(p[0:5, :], lhsT=cT[:, k, :], rhs=wa[:, k, nn * 512:(nn + 1) * 512], start=(k == 0), stop=False)
                    return e.matmul(p[0:5, :], lhsT=K.ones32[0:1, 0:5], rhs=ba[0:1, l * 1536 + nn * 512:l * 1536 + (nn + 1) * 512],
                                    start=False, stop=True)
                S.op("pe", mm, reads=["cT", "wada", "bada"], writes=[pk])
                S.op("act", lambda e, p=p, nn=nn, l=l: e.copy(out=ml[:, l * 1536 + nn * 512:l * 1536 + (nn + 1) * 512], in_=p[0:5, :]),
                     reads=[pk], writes=[("ml", l, nn)])
        allml = [("ml", l, nn) for l in range(DEPTH) for nn in range(3)]
        S.dma("sp", lambda e: e.dma_start(out=T.msend[:, :], in_=ml[:]), "mso", reads=allml, writes=["msend"])
        coll(S, lambda e: e.collective_compute("AllGather", ALU.bypass, replica_groups=[list(range(8))], ins=[T.msend.opt()],
                                               outs=[T.mrecv.opt()]), "ccm", ["msend"], ["mrecv"])
        S.flush("mod1")
    with ExitStack() as es:
        sb = lambda name, shape, dt: es.enter_context(nc.sbuf_tensor(_u(name), shape, dt))
        pst = lambda name, shape, dt: es.enter_context(nc.psum_tensor(_u(name), shape, dt))
        pp = Ring("mpp2", [pst("mpp2_%d" % i, [128, 512], F32) for i in range(3)])
        G = sb("modG", [5, 8, DEPTH * 1536], F32)
        S.dma("sp", lambda e: e.dma_start(out=G[:], in_=T.mrecv[:, :].rearrange("(r f) n -> f r n", f=5)), "modG",
              reads=["mrecv"], writes=["modG"])
        bs = sb("bsel", [5, 2], F32)
        S.dma("sp", lambda e: e.dma_start(out=bs[:], in_=T.bsel[:, :]), "bsel", writes=["bsel"])
        ms = sb("modsel", [2, DEPTH, 8 * 1536], F32)
        for l in range(DEPTH):
            for r in range(8):
                for nn in range(3):
                    p, pk = pp.next()
                    S.op("pe", lambda e, p=p, l=l, r=r, nn=nn: e.matmul(
                        p[0:2, :], lhsT=bs[:, :], rhs=G[:, r, l * 1536 + nn * 512:l * 1536 + (nn + 1) * 512], start=True, stop=True),
                        reads=["modG", "bsel"], writes=[pk])
                    dst = ms[:, l, r * 1536 + nn * 512:r * 1536 + (nn + 1) * 512]
                    if (r + nn) % 2 == 0:
                        S.op("act", lambda e, p=p, dst=dst: e.copy(out=dst, in_=p[0:2, :]), reads=[pk], writes=[("ms", l, r, nn)])
                    else:
                        S.op("dve", lambda e, p=p, dst=dst: e.tensor_copy(out=dst, in_=p[0:2, :]), reads=[pk],
                             writes=[("ms", l, r, nn)])
        allms = [("ms", l, r, nn) for l in range(DEPTH) for r in range(8) for nn in range(3)]
        S.dma("sp", lambda e: e.dma_start(out=T.mod[:, :].rearrange("(l k) n -> k l n", k=2), in_=ms[:]), "mso2", reads=allms,
              writes=["mod"])
        S.flush("mod")


def prep_experts(wg, wu, wd):
    ne = wg.shape[1]
    w1 = np.ascontiguousarray(wg.reshape(DEPTH * ne, 16, 128, 512).transpose(0, 2, 1, 3)).reshape(-1, 16 * 512)
    w3 = np.ascontiguousarray(wu.reshape(DEPTH * ne, 16, 128, 512).transpose(0, 2, 1, 3)).reshape(-1, 16 * 512)
    w2 = np.ascontiguousarray(wd.reshape(DEPTH * ne, 4, 128, D).transpose(0, 2, 1, 3)).reshape(-1, 4 * D)
    return w1, w3, w2


_NC_CACHE = {}


def make_in_maps(x, c, ctx, c_ctx, w_ada, b_ada, g_mix, g_ffn, w_in, conv_w, conv_b, conv_ln_g, conv_ln_b, na_rpb,
                 hgrn_lb, hgrn_norm_g, w_out, w_router_group, b_router_group, w_router_expert, b_router_expert,
                 w_exp_gate, w_exp_up, w_exp_down, g_final):
    f = lambda a: np.ascontiguousarray(np.asarray(a, dtype=np.float32))
    x, c, ctx, c_ctx, w_ada, b_ada = f(x), f(c), f(ctx), f(c_ctx), f(w_ada), f(b_ada)
    w_in = f(w_in)
    consts = host_consts()
    shared = {}
    for odd in (False, True):
        wf, wt = prep_w_in(w_in, odd)
        cwT, cvec = prep_conv(f(conv_w), f(conv_b), f(conv_ln_g), f(conv_ln_b), odd)
        nab = prep_na_bias(f(na_rpb), odd)
        hlb, hng = prep_hgrn(f(hgrn_lb), f(hgrn_norm_g), odd)
        shared[odd] = dict(w_feat=wf, w_tokm=wt, conv_wT=cwT, conv_vec=cvec, na_bias=nab, hlb=hlb, hnorm_g=hng)
    wom, wrm, brm = prep_misc(f(w_out), f(w_router_group), f(b_router_group), f(w_router_expert), f(b_router_expert))
    w1m, w3m, w2m = prep_experts_sparse(f(w_exp_gate), f(w_exp_up), f(w_exp_down))
    C = np.concatenate([c, c_ctx[None, :]], axis=0)
    cT = np.ascontiguousarray(C.reshape(5, 16, 128).transpose(2, 1, 0)).reshape(128, 80)
    in_maps = []
    for core in range(8):
        b, s = core // 2, core % 2
        odd = s == 1
        m = dict(shared[odd])
        m["x_in"] = core_tokens(x[b], ctx[b], odd)
        m["consts"] = consts
        m["g_mix"] = f(g_mix)
        m["g_ffn"] = f(g_ffn)
        m["g_final"] = f(g_final).reshape(1, D)
        m["w_outm"] = wom
        m["w_r"] = wrm
        m["b_r"] = brm
        m["w1m"], m["w3m"], m["w2m"] = w1m, w3m, w2m
        m["cT"] = cT
        sl = slice(core * 1536, (core + 1) * 1536)
        wa = np.empty((DEPTH, 128, 16 * 1536), np.float32)
        for l in range(DEPTH):
            wa[l] = w_ada[l][:, sl].reshape(16, 128, 1536).transpose(1, 0, 2).reshape(128, -1)
        m["w_adam"] = wa.reshape(DEPTH * 128, 16 * 1536)
        m["b_ada"] = np.ascontiguousarray(np.concatenate([b_ada[l][sl] for l in range(DEPTH)])[None, :])
        bs = np.zeros((5, 2), np.float32)
        bs[b, 0] = 1.0
        bs[4, 1] = 1.0
        m["bsel"] = bs
        pf = np.zeros((128, 2), np.float32)
        pf[:, 0 if odd else 1] = 1.0
        m["pflag"] = pf
        in_maps.append(m)
    return in_maps


def kernel(**inputs):
    in_maps = make_in_maps(**inputs)
    if "nc" not in _NC_CACHE:
        _NC_CACHE["nc"] = build({})
    nc = _NC_CACHE["nc"]
    res = run_bass_kernel_spmd(nc, in_maps, core_ids=list(range(8)))
    out = np.empty((4, 4096, D), np.float32)
    for core in range(8):
        b, s = core // 2, core % 2
        o = np.asarray(res.results[core]["out"], dtype=np.float32)
        if s == 0:
            out[b, 0:2048] = o
        else:
            out[b, 2048:4096] = o[::-1]
    return out


U32 = mybir.dt.uint32
_NE = [NEXP]


def RWv():
    return DEPTH * _NE[0] * 128
OOB = 1.0e6


BLK = 256


def nblocks(with_ctx):
    ntok = (NOWN if with_ctx else 16) * 128
    return (2 * ntok) // BLK + NEXP


def phase_D1s(S, nc, K, T, l, with_ctx):
    from contextlib import ExitStack
    NB = nblocks(with_ctx)
    with ExitStack() as es:
        sb = lambda name, shape, dt: es.enter_context(nc.sbuf_tensor(_u(name), shape, dt))
        pst = lambda name, shape, dt: es.enter_context(nc.psum_tensor(_u(name), shape, dt))
        h2 = sb("h2tok", [128, NOWN, D], BF16)
        o1all = sb("o1all", [128, NOWN, 32], F32)
        o2all = sb("o2all", [128, NOWN, 32], F32)
        aall = sb("aall", [128, NOWN, 2], F32)
        wr = sb("wr", [128, 16, 36], F32)
        br = sb("brb", [128, 36], F32)
        S.dma("sp", lambda e: e.dma_start(out=wr[:].rearrange("p k n -> p (k n)"), in_=T.w_r[l * 128:(l + 1) * 128, :]), "wr",
              writes=["wr"])
        S.dma("sp", lambda e: e.dma_start(out=br[:], in_=bcast_row(T.b_r[l:l + 1, :], 36)), "brb", writes=["brb"])
        t0 = 0 if with_ctx else 2
        tl = list(range(t0, NOWN))
        nt = len(tl)
        with ExitStack() as es2:
            sb2 = lambda name, shape, dt: es2.enter_context(nc.sbuf_tensor(_u(name), shape, dt))
            pst2 = lambda name, shape, dt: es2.enter_context(nc.psum_tensor(_u(name), shape, dt))
            AB = load_mod_AB(S, nc, T, l, (MOD_SH2, MOD_S2), T.g_ffn[l:l + 1, :], es2, "m2")
            ptr = Ring("rptr", [pst2("rptr%d" % i, [128, 512], F32) for i in range(2)])
            pr = Ring("rpr", [pst2("rpr%d" % i, [128, 36], F32) for i in range(1)])
            hT32 = Ring("rhT32", [sb2("rhT32_%d" % i, [128, 16, 128], F32) for i in range(2)])
            sm = lambda nm, w: Ring(nm, [sb2("%s%d" % (nm, i), [128, w], F32) for i in range(2)])
            lgb, st1, gmb, egb, pen, lem, lem2 = (sm("rlg", 36), sm("rst", 8), sm("rgm", 4), sm("reg", 4), sm("rpen", 4),
                                                  sm("rlem", 32), sm("rlem2", 32))

            def router(i, h32, h32k):
                t = tl[i]
                hT, hTk = hT32.next()
                for g in range(4):
                    p, pk = ptr.next()

                    def tr(e, p=p, g=g):
                        r = None
                        for j in range(4):
                            kk = g * 4 + j
                            r = e.transpose(out=p[:, j * 128:(j + 1) * 128], in_=h32[:, kk * 128:(kk + 1) * 128],
                                            identity=K.ident32)
                        return r
                    S.op("pe", tr, reads=[h32k], writes=[pk])
                    S.op("act", lambda e, p=p, g=g: e.copy(out=hT[:, g * 4:(g + 1) * 4, :],
                                                           in_=p[:].rearrange("p (j n) -> p j n", j=4)),
                         reads=[pk], writes=[(hTk, g)])
                p, pk = pr.next()

                def mm(e, p=p):
                    r = None
                    for k in range(16):
                        r = e.matmul(p[:], lhsT=hT[:, k, :], rhs=wr[:, k, :], start=(k == 0), stop=(k == 15))
                    return r
                S.op("pe", mm, reads=[(hTk, g) for g in range(4)] + ["wr"], writes=[pk])
                lg, lgk = lgb.next()
                S.op("dve", lambda e: e.tensor_tensor(out=lg[:], in0=p[:], in1=br[:], op=ALU.add), reads=[pk, "brb"], writes=[lgk])
                s, sk = st1.next()
                S.op("dve", lambda e: e.reduce_max(out=s[:, 0:1], in_=lg[:, 0:4], axis=AX.X), reads=[lgk], writes=[sk])
                S.op("dve", lambda e: e.tensor_scalar(out=s[:, 1:2], in0=s[:, 0:1], scalar1=-1.0, scalar2=None, op0=ALU.mult),
                     reads=[sk], writes=[sk])
                gm, gmk = gmb.next()
                S.op("dve", lambda e: e.tensor_scalar(out=gm[:], in0=lg[:, 0:4], scalar1=s[:, 0:1], scalar2=None,
                                                      op0=ALU.is_equal), reads=[lgk, sk], writes=[gmk])
                eg, egk = egb.next()
                S.op("act", lambda e: e.activation(out=eg[:], in_=lg[:, 0:4], func=AF.Exp, bias=s[:, 1:2], accum_out=s[:, 2:3]),
                     reads=[lgk, sk], writes=[egk, sk])
                pn, pnk = pen.next()
                S.op("dve", lambda e: e.tensor_scalar(out=pn[:], in0=gm[:], scalar1=BIG, scalar2=-BIG, op0=ALU.mult, op1=ALU.add),
                     reads=[gmk], writes=[pnk])
                le, lek = lem.next()
                S.op("dve", lambda e: e.tensor_tensor(out=le[:].rearrange("p (g n) -> p g n", g=4),
                                                      in0=lg[:, 4:36].rearrange("p (g n) -> p g n", g=4),
                                                      in1=_bc_last(pn[:].rearrange("p (g o) -> p g o", o=1), 8), op=ALU.add),
                     reads=[lgk, pnk], writes=[lek])
                S.op("dve", lambda e: e.reduce_max(out=s[:, 3:4], in_=le[:], axis=AX.X), reads=[lek, sk], writes=[sk])
                o1 = o1all[:, t, :]
                o2 = o2all[:, t, :]
                S.op("dve", lambda e: e.tensor_scalar(out=o1, in0=le[:], scalar1=s[:, 3:4], scalar2=None, op0=ALU.is_equal),
                     reads=[lek, sk], writes=[("o1", t)])
                l2, l2k = lem2.next()
                S.op("dve", lambda e: e.scalar_tensor_tensor(out=l2[:], in0=o1, scalar=-BIG, in1=le[:], op0=ALU.mult, op1=ALU.add),
                     reads=[("o1", t), lek], writes=[l2k])
                S.op("dve", lambda e: e.reduce_max(out=s[:, 4:5], in_=l2[:], axis=AX.X), reads=[l2k, sk], writes=[sk])
                S.op("dve", lambda e: e.tensor_scalar(out=o2, in0=l2[:], scalar1=s[:, 4:5], scalar2=None, op0=ALU.is_equal),
                     reads=[l2k, sk], writes=[("o2", t)])
                S.op("dve", lambda e: e.tensor_tensor(out=s[:, 5:6], in0=s[:, 4:5], in1=s[:, 3:4], op=ALU.subtract), reads=[sk],
                     writes=[sk])
                S.op("act", lambda e: e.activation(out=s[:, 6:7], in_=s[:, 5:6], func=AF.Exp), reads=[sk], writes=[sk])
                S.op("dve", lambda e: e.scalar_tensor_tensor(out=s[:, 7:8], in0=s[:, 6:7], scalar=1.0, in1=s[:, 2:3], op0=ALU.add,
                                                             op1=ALU.mult), reads=[sk], writes=[sk])
                S.op("dve", lambda e: e.reciprocal(out=aall[:, t, 0:1], in_=s[:, 7:8]), reads=[sk], writes=[("aa", t)])
                S.op("dve", lambda e: e.tensor_tensor(out=aall[:, t, 1:2], in0=s[:, 6:7], in1=aall[:, t, 0:1], op=ALU.mult),
                     reads=[sk, ("aa", t)], writes=[("aa", t)])

            tiles = [(t * D, tile_kind(t)) for t in tl]
            emit_norm(S, nc, K, tiles, lambda i: T.x1[tl[i] * 128:(tl[i] + 1) * 128, :], AB, None, es2, want32=router,
                      htok=lambda i: (h2[:, tl[i], :], ("h2tok", tl[i])))
            S.flush("D1s_a%d" % l)
        oe = sb("oeall", [128, NOWN, 32], BF16)
        allo = [("o1", t) for t in tl] + [("o2", t) for t in tl]
        lo, hi = tl[0], tl[-1] + 1
        S.op("dve", lambda e: e.tensor_tensor(out=oe[:, lo:hi, :], in0=o1all[:, lo:hi, :], in1=o2all[:, lo:hi, :], op=ALU.add),
             writes=["oe"])
        rall = sb("rall", [128, NOWN, 32], F32)
        pR = Ring("pR", [pst("pR%d" % i, [128, 32], F32) for i in range(3)])
        Lbf = K.cbf[:, 8, :]
        for i, t in enumerate(tl):
            p, pk = pR.next()

            def mm(e, p=p, i=i, t=t):
                r = e.matmul(p[:], lhsT=Lbf, rhs=oe[:, t, :], start=True, stop=(i == 0))
                for jj in range(i):
                    r = e.matmul(p[:], lhsT=K.ones_bf, rhs=oe[:, tl[jj], :], start=False, stop=(jj == i - 1))
                return r
            S.op("pe", mm, reads=["oe"], writes=[pk])
            S.op("act", lambda e, p=p, t=t: e.copy(out=rall[:, t, :], in_=p[:]), reads=[pk], writes=[("rall", t)])
        p, pk = pR.next()

        def mmc(e, p=p):
            r = None
            for jj in range(nt):
                r = e.matmul(p[:], lhsT=K.ones_bf, rhs=oe[:, tl[jj], :], start=(jj == 0), stop=(jj == nt - 1))
            return r
        S.op("pe", mmc, reads=["oe"], writes=[pk])
        cnt = sb("cnt", [128, 32], F32)
        S.op("act", lambda e: e.copy(out=cnt[:], in_=p[:]), reads=[pk], writes=["cnt"])
        cmp1 = sb("cmp1", [128, 32, 18], F32)
        thr = K.c32[:, 9, 0:18]
        thr_bc = bass.AP(tensor=thr.tensor, offset=thr.offset, ap=[list(thr.ap[0]), [0, 32], [1, 18]])
        S.op("dve", lambda e: e.tensor_scalar(out=cnt[:], in0=cnt[:], scalar1=128.0 / BLK, scalar2=None, op0=ALU.mult),
             reads=["cnt"], writes=["cnt"])
        S.op("dve", lambda e: e.tensor_tensor(out=cmp1[:], in0=_bc_last(cnt[:].rearrange("p (a o) -> p a o", o=1), 18), in1=thr_bc,
                                              op=ALU.is_gt), reads=["cnt"], writes=["cmp1"])
        nb = sb("nbk", [128, 32], F32)
        S.op("dve", lambda e: e.reduce_sum(out=nb[:], in_=cmp1[:], axis=AX.X), reads=["cmp1"], writes=["nbk"])
        pend = sb("pend", [128, 32], F32)
        S.op("dve", lambda e: e.tensor_tensor_scan(out=pend[:], data0=K.ones32[:, 0:32], data1=nb[:], initial=0.0, op0=ALU.mult,
                                                   op1=ALU.add), reads=["nbk"], writes=["pend"])
        pst128 = sb("pst128", [128, 32], F32)
        S.op("dve", lambda e: e.tensor_tensor(out=pst128[:], in0=pend[:], in1=nb[:], op=ALU.subtract), reads=["pend", "nbk"],
             writes=["pst128"])
        S.op("dve", lambda e: e.tensor_scalar(out=pst128[:], in0=pst128[:], scalar1=float(BLK), scalar2=None, op0=ALU.mult),
             reads=["pst128"], writes=["pst128"])
        slots_f = sb("slotsf", [128, NOWN, 2], F32)
        slots_u = sb("slotsu", [128, NOWN, 2], U32)
        S.op("dve", lambda e: e.memset(slots_f[:], 0.0), writes=[("slf", t) for t in range(NOWN)])
        basr = Ring("basr", [sb("basr%d" % i, [128, 32], F32) for i in range(2)])
        junkr = Ring("sjunk", [sb("sjunk%d" % i, [128, 32], F32) for i in range(2)])
        for t in tl:
            ba, bak = basr.next()
            S.op("dve", lambda e, ba=ba, t=t: e.tensor_tensor(out=ba[:], in0=rall[:, t, :], in1=pst128[:], op=ALU.add),
                 reads=[("rall", t), "pst128"], writes=[bak])
            for kk, oall in enumerate((o1all, o2all)):
                jk, jkk = junkr.next()
                S.op("dve", lambda e, ba=ba, t=t, oall=oall, jk=jk: e.tensor_tensor(out=jk[:], in0=oall[:, t, :], in1=ba[:], op=ALU.mult),
                     reads=[bak], writes=[jkk])
                S.op("dve", lambda e, t=t, kk=kk, jk=jk: e.reduce_sum(out=slots_f[:, t, kk:kk + 1], in_=jk[:], axis=AX.X),
                     reads=[jkk, ("slf", t)], writes=[("slf", t)])
        S.op("dve", lambda e: e.tensor_copy(out=slots_u[:], in_=slots_f[:]), reads=[("slf", t) for t in range(NOWN)],
             writes=["slu"])
        for t in tl:
            for kk in range(2):
                S.dma("pool", lambda e, t=t, kk=kk: e.indirect_dma_start(
                    out=T.xsort[:, :], out_offset=bass.IndirectOffsetOnAxis(ap=slots_u[:, t, kk:kk + 1], axis=0),
                    in_=h2[:, t, :], in_offset=None), ("xsc", (2 * t + kk) % 4), reads=["slu", ("h2tok", t)], writes=[("xsort", t, kk)])
        cmp2 = sb("cmp2", [128, NB, 32], F32)
        jv = K.c32[:, 9, 32:32 + NB]
        S.op("dve", lambda e: e.tensor_tensor(
            out=cmp2[:], in0=bass.AP(tensor=pend[:].tensor, offset=pend[:].offset, ap=[list(pend[:].ap[0]), [0, NB], [1, 32]]),
            in1=_bc_last(jv.rearrange("p (a o) -> p a o", o=1), 32), op=ALU.is_le), reads=["pend"], writes=["cmp2"])
        blk = sb("blk", [128, NB], F32)
        S.op("dve", lambda e: e.reduce_sum(out=blk[:], in_=cmp2[:], axis=AX.X), reads=["cmp2"], writes=["blk"])
        oobf = sb("oobf", [128, NB], F32)
        S.op("dve", lambda e: e.tensor_scalar(out=oobf[:], in0=blk[:], scalar1=float(_NE[0]) - 0.5, scalar2=OOB, op0=ALU.is_gt,
                                              op1=ALU.mult), reads=["blk"], writes=["oobf"])
        S.op("dve", lambda e: e.tensor_scalar(out=blk[:], in0=blk[:], scalar1=float(_NE[0] - 1), scalar2=128.0, op0=ALU.min, op1=ALU.mult),
             reads=["blk"], writes=["blk"])
        S.op("dve", lambda e: e.tensor_tensor(out=blk[:], in0=blk[:], in1=oobf[:], op=ALU.add), reads=["blk", "oobf"], writes=["blk"])
        S.op("dve", lambda e: e.tensor_scalar(out=blk[:], in0=blk[:], scalar1=K.c32[:, 9, 127:128], scalar2=float(l * _NE[0] * 128),
                                              op0=ALU.add, op1=ALU.add), reads=["blk"], writes=["blk"])
        blk4 = sb("blk4", [128, NB, 4], F32)
        for c in range(4):
            S.op("dve", lambda e, c=c: e.tensor_scalar(out=blk4[:, :, c], in0=blk[:], scalar1=float(c * RWv()), scalar2=None,
                                                       op0=ALU.add), reads=["blk"], writes=[("blk4", c)])
        blku = sb("blku", [128, NB, 4], U32)
        S.op("dve", lambda e: e.tensor_copy(out=blku[:], in_=blk4[:]), reads=[("blk4", c) for c in range(4)], writes=["blku"])
        S.dma("sp", lambda e: e.dma_start(out=T.blku[:, 0:NB * 4], in_=blku[:].rearrange("p j c -> p (j c)")), "blkuo",
              reads=["blku"], writes=["blkud"])
        S.dma("sp", lambda e: e.dma_start(out=T.slotsu[:, :], in_=slots_u[:].rearrange("p t k -> p (t k)")), "sluo", reads=["slu"],
              writes=["slud"])
        S.dma("sp", lambda e: e.dma_start(out=T.aall[:, :], in_=aall[:].rearrange("p t k -> p (t k)")), "aao",
              reads=[("aa", t) for t in tl], writes=["aad"])
        S.flush("D1s_b%d" % l)


def phase_D2s(S, nc, K, T, l, with_ctx):
    from contextlib import ExitStack
    NB = nblocks(with_ctx)
    NS = BLK // 128
    t0 = 0 if with_ctx else 2
    tl = list(range(t0, NOWN))
    with ExitStack() as es:
        sb = lambda name, shape, dt: es.enter_context(nc.sbuf_tensor(_u(name), shape, dt))
        pst = lambda name, shape, dt: es.enter_context(nc.psum_tensor(_u(name), shape, dt))
        blku = sb("blkul", [128, NB, 4], U32)
        S.dma("sp", lambda e: e.dma_start(out=blku[:].rearrange("p j c -> p (j c)"), in_=T.blku[:, 0:NB * 4]), "blkul",
              writes=["blkul"])
        w1r = Ring("sw1", [sb("sw1_%d" % i, [128, 16, 512], BF16) for i in range(2)])
        w3r = Ring("sw3", [sb("sw3_%d" % i, [128, 16, 512], BF16) for i in range(2)])
        w2r = Ring("sw2", [sb("sw2_%d" % i, [128, 4, D], BF16) for i in range(3)])
        for r_ in (w1r, w3r, w2r):
            for i in range(len(r_.bufs)):
                S.op("dve", lambda e, r_=r_, i=i: e.memset(r_.bufs[i][:], 0.0), writes=[(r_.name, i)])
        xtr = Ring("sxt", [sb("sxt%d" % i, [128, NS, D], BF16) for i in range(2)])
        xTr = Ring("sxT", [sb("sxT%d" % i, [128, 16, BLK], BF16) for i in range(2)])
        sgr = Ring("ssg", [sb("ssg%d" % i, [128, 4 * BLK], F32) for i in range(2)])
        aTr = Ring("saT", [sb("saT%d" % i, [128, 4, BLK], BF16) for i in range(3)])
        ysr = Ring("sys", [sb("sys%d" % i, [128, D], F32) for i in range(3)])
        pT = Ring("spT", [pst("spT%d" % i, [128, 1024], BF16) for i in range(2)])
        pg = Ring("spg", [pst("spg%d" % i, [128, 2 * BLK], F32) for i in range(2)])
        pu = Ring("spu", [pst("spu%d" % i, [128, 2 * BLK], F32) for i in range(2)])
        po = Ring("spo", [pst("spo%d" % i, [128, 512], F32) for i in range(2)])
        breg = {}
        HB = 2 * BLK
        state = {}

        def stage_a(j):
            w1, w1k = w1r.next()
            w3, w3k = w3r.next()
            w2, w2k = w2r.next()
            for (w, wk, src, nm) in ((w1, w1k, T.w1m, "sw1"), (w3, w3k, T.w3m, "sw3"), (w2, w2k, T.w2m, "sw2")):
                wf = w[:].rearrange("p k n -> p (k n)")

                def ld(e, wf=wf, src=src, j=j):
                    r = []
                    if "reg" not in breg:
                        breg["reg"] = e.to_reg(0 if os.environ.get("MOE_NOLOAD") else 4 * RWv() - 1)
                    for c in range(4):
                        r.append(e.indirect_dma_start(out=wf[:, c * 2048:(c + 1) * 2048], out_offset=None, in_=src[:, :],
                                                      in_offset=bass.IndirectOffsetOnAxis(ap=blku[:, j, c:c + 1], axis=0),
                                                      bounds_check=breg["reg"], oob_is_err=False))
                    return r
                S.dma("pool", ld, (nm, wk[1]), reads=["blkul"], writes=[wk], n=4)
            xt, xtk = xtr.next()
            S.dma("sp", lambda e, xt=xt, j=j: e.dma_start(out=xt[:], in_=T.xsort[j * BLK:(j + 1) * BLK, :].rearrange(
                "(s p) n -> p s n", p=128)), ("sxt", xtk[1]), writes=[xtk])
            xT, xTk = xTr.next()
            for si in range(NS):
                for half in range(2):
                    p, pk = pT.next()

                    def tr(e, p=p, xt=xt, half=half, si=si):
                        r = None
                        for k in range(8):
                            kk = half * 8 + k
                            r = e.transpose(out=p[:, k * 128:(k + 1) * 128], in_=xt[:, si, kk * 128:(kk + 1) * 128],
                                            identity=K.ident_bf)
                        return r
                    S.op("pe", tr, reads=[xtk], writes=[pk])
                    dst = xT[:, half * 8:(half + 1) * 8, si * 128:(si + 1) * 128]
                    if half == 0:
                        S.op("act", lambda e, p=p, dst=dst: e.copy(out=dst, in_=p[:].rearrange("p (k n) -> p k n", k=8)),
                             reads=[pk], writes=[(xTk, si, 0)])
                    else:
                        S.op("dve", lambda e, p=p, dst=dst: e.tensor_copy(out=dst, in_=p[:].rearrange("p (k n) -> p k n", k=8)),
                             reads=[pk], writes=[(xTk, si, 1)])
            xTkeys = [(xTk, si, hh) for si in range(NS) for hh in range(2)]
            aT, aTk = aTr.next()
            for hf in range(2):
                p1, p1k = pg.next()
                p3, p3k = pu.next()

                def mg(e, p=p1, w=w1, xT=xT, hf=hf):
                    r = None
                    for jj in range(2):
                        jd = hf * 2 + jj
                        for k in range(16):
                            r = e.matmul(p[:, jj * BLK:(jj + 1) * BLK], lhsT=w[:, k, jd * 128:(jd + 1) * 128], rhs=xT[:, k, :],
                                         start=(k == 0), stop=(k == 15))
                    return r
                S.op("pe", mg, reads=[w1k] + xTkeys, writes=[p1k])

                def mu(e, p=p3, w=w3, xT=xT, hf=hf):
                    r = None
                    for jj in range(2):
                        jd = hf * 2 + jj
                        for k in range(16):
                            r = e.matmul(p[:, jj * BLK:(jj + 1) * BLK], lhsT=w[:, k, jd * 128:(jd + 1) * 128], rhs=xT[:, k, :],
                                         start=(k == 0), stop=(k == 15))
                    return r
                S.op("pe", mu, reads=[w3k] + xTkeys, writes=[p3k])
                sg, sgk = sgr.next()
                S.op("act", lambda e, sg=sg, p1=p1: e.activation(out=sg[:, 0:HB], in_=p1[:], func=AF.Silu), reads=[p1k], writes=[sgk])
                S.op("dve", lambda e, aT=aT, sg=sg, p3=p3, hf=hf: e.tensor_tensor(
                    out=aT[:, hf * 2:hf * 2 + 2, :].rearrange("p j n -> p (j n)"), in0=p3[:], in1=sg[:, 0:HB], op=ALU.mult),
                    reads=[p3k, sgk], writes=[(aTk, hf)])
            state[j] = (aT, aTk, w2, w2k)

        def stage_b(j):
            aT, aTk, w2, w2k = state.pop(j)
            for si in range(NS):
                ys, ysk = ysr.next()
                for nn in range(4):
                    p, pk = po.next()

                    def mo(e, p=p, aT=aT, w2=w2, nn=nn, si=si):
                        r = None
                        for jd in range(4):
                            r = e.matmul(p[:], lhsT=aT[:, jd, si * 128:(si + 1) * 128], rhs=w2[:, jd, nn * 512:(nn + 1) * 512],
                                         start=(jd == 0), stop=(jd == 3))
                        return r
                    S.op("pe", mo, reads=[(aTk, 0), (aTk, 1), w2k], writes=[pk])
                    if nn % 2 == 0:
                        S.op("act", lambda e, p=p, ys=ys, nn=nn: e.copy(out=ys[:, nn * 512:(nn + 1) * 512], in_=p[:]), reads=[pk],
                             writes=[(ysk, nn)])
                    else:
                        S.op("dve", lambda e, p=p, ys=ys, nn=nn: e.tensor_copy(out=ys[:, nn * 512:(nn + 1) * 512], in_=p[:]),
                             reads=[pk], writes=[(ysk, nn)])
                r0 = j * BLK + si * 128
                S.dma("sp", lambda e, ys=ys, r0=r0: e.dma_start(out=T.ysd[r0:r0 + 128, :], in_=ys[:]), ("syo", ysk[1]),
                      reads=[(ysk, nn) for nn in range(4)], writes=[("ysd", j, si)])

        for j in range(NB + 1):
            if j < NB:
                stage_a(j)
            if j >= 1:
                stage_b(j - 1)
        S.flush("D2s_%d" % l)
    with ExitStack() as es:
        sb = lambda name, shape, dt: es.enter_context(nc.sbuf_tensor(_u(name), shape, dt))
        slu = sb("slul", [128, NOWN, 2], U32)
        aa = sb("aal", [128, NOWN, 2], F32)
        S.dma("sp", lambda e: e.dma_start(out=slu[:].rearrange("p t k -> p (t k)"), in_=T.slotsu[:, :]), "slul", writes=["slul"])
        S.dma("sp", lambda e: e.dma_start(out=aa[:].rearrange("p t k -> p (t k)"), in_=T.aall[:, :]), "aal", writes=["aal"])
        y1r = Ring("cy1", [sb("cy1_%d" % i, [128, D], F32) for i in range(2)])
        y2r = Ring("cy2", [sb("cy2_%d" % i, [128, D], F32) for i in range(2)])
        for t in tl:
            y1, y1k = y1r.next()
            y2, y2k = y2r.next()
            S.dma("pool", lambda e, y1=y1, t=t: e.indirect_dma_start(out=y1[:], out_offset=None, in_=T.ysd[:, :],
                                                                    in_offset=bass.IndirectOffsetOnAxis(ap=slu[:, t, 0:1], axis=0)),
                  ("cy1", y1k[1]), reads=["slul"], writes=[y1k])
            S.dma("pool", lambda e, y2=y2, t=t: e.indirect_dma_start(out=y2[:], out_offset=None, in_=T.ysd[:, :],
                                                                    in_offset=bass.IndirectOffsetOnAxis(ap=slu[:, t, 1:2], axis=0)),
                  ("cy2", y2k[1]), reads=["slul"], writes=[y2k])
            S.op("dve", lambda e, y1=y1, t=t: e.tensor_scalar(out=y1[:], in0=y1[:], scalar1=aa[:, t, 0:1], scalar2=None, op0=ALU.mult),
                 reads=[y1k, "aal"], writes=[y1k])
            S.op("dve", lambda e, y1=y1, y2=y2, t=t: e.scalar_tensor_tensor(out=y1[:], in0=y2[:], scalar=aa[:, t, 1:2], in1=y1[:],
                                                                            op0=ALU.mult, op1=ALU.add),
                 reads=[y1k, y2k, "aal"], writes=[y1k])
            S.dma("sp", lambda e, y1=y1, t=t: e.dma_start(out=T.facc[t * 128:(t + 1) * 128, :], in_=y1[:]), ("cfo", y1k[1]),
                  reads=[y1k], writes=[("faccd", t)])
        S.flush("D2c_%d" % l)


def prep_experts_sparse(wg, wu, wd):
    ne = wg.shape[1]
    rw = DEPTH * ne * 128
    w1 = np.ascontiguousarray(wg.reshape(DEPTH * ne, 4, 4, 128, 512).transpose(1, 0, 3, 2, 4)).reshape(4 * rw, 2048)
    w3 = np.ascontiguousarray(wu.reshape(DEPTH * ne, 4, 4, 128, 512).transpose(1, 0, 3, 2, 4)).reshape(4 * rw, 2048)
    w2 = np.ascontiguousarray(wd.reshape(DEPTH * ne, 4, 128, D).transpose(1, 0, 2, 3)).reshape(4 * rw, 2048)
    return w1, w3, w2
```

```python
import numpy as np
import concourse.bass as bass
import concourse.mybir as mybir
from concourse.bass_utils import run_bass_kernel_spmd

F32 = mybir.dt.float32
BF16 = mybir.dt.bfloat16
AF = mybir.ActivationFunctionType
ALU = mybir.AluOpType
AX = mybir.AxisListType

D = 2048
DEPTH = 2
NT = 20
NTOK = NT * 128
NOWN = 18
EPS = 1e-6
IN_DIM = 6656
NEXP = 32
DEXP = 512


class Sched:
    CE = ("pe", "act", "dve", "pool")

    def __init__(self, nc):
        self.nc = nc
        self.sem = {}
        self.val = {}
        self.engs = ("pe", "act", "dve", "pool", "sp")
        self.seen = {e: {} for e in self.engs}
        self.prog = {e: [] for e in self.engs}
        self.lastw = {}
        self.readers = {}
        self.free_dsems = []
        self.dkeys = {}
        self.nblocks = 0
        for e in self.CE:
            self._mk("c_" + e)

    def _mk(self, name):
        self.sem[name] = self.nc.alloc_semaphore(name=name)
        self.val[name] = 0

    def dsem(self, key):
        if key not in self.dkeys:
            if self.free_dsems:
                nm = self.free_dsems.pop()
            else:
                nm = "d%d" % len([k for k in self.sem if k.startswith("d")])
                self._mk(nm)
            self.dkeys[key] = nm
        return self.dkeys[key]

    def _deps(self, reads, writes):
        deps = {}

        def add(t):
            if t is None:
                return
            s, v = t
            if deps.get(s, 0) < v:
                deps[s] = v

        for r in reads:
            add(self.lastw.get(r))
        for w in writes:
            add(self.lastw.get(w))
            for s, v in self.readers.get(w, {}).items():
                add((s, v))
        return deps

    def _commit(self, tok, reads, writes):
        s, v = tok
        for r in reads:
            d = self.readers.setdefault(r, {})
            if d.get(s, 0) < v:
                d[s] = v
        for w in writes:
            self.lastw[w] = tok
            self.readers[w] = {}

    def _emit(self, eng, fn, semname, inc, reads, writes, n=1):
        deps = self._deps(reads, writes)
        seen = self.seen[eng]
        waits = []
        for s, v in deps.items():
            if seen.get(s, 0) < v:
                seen[s] = v
                waits.append((s, v))
        self.val[semname] += inc * n
        tok = (semname, self.val[semname])
        self.prog[eng].append((waits, fn, semname, inc))
        self._commit(tok, reads, writes)
        return tok

    def op(self, eng, fn, reads=(), writes=()):
        return self._emit(eng, fn, "c_" + eng, 1, reads, writes)

    def dma(self, q, fn, key, reads=(), writes=(), n=1):
        return self._emit(q, fn, self.dsem(key), 16, reads, writes, n)

    def _replay(self, name, eng):
        for waits, fn, semname, inc in self.prog[name]:
            for s, v in waits:
                eng.wait_ge(self.sem[s], v)
            r = fn(eng)
            if isinstance(r, (list, tuple)):
                for ins in r:
                    ins.then_inc(self.sem[semname], inc)
            else:
                r.then_inc(self.sem[semname], inc)

    def flush(self, name=None):
        nc = self.nc
        pend = []
        for key, nm in self.dkeys.items():
            v = self.val[nm]
            if self.seen["sp"].get(nm, 0) < v:
                pend.append((nm, v))
        for e in self.CE:
            nm = "c_" + e
            if self.seen["sp"].get(nm, 0) < self.val[nm]:
                pend.append((nm, self.val[nm]))
        prog = self.prog
        sp_tail = pend
        self.nblocks += 1
        with nc.Block("%s_b%d" % (name or "ph", self.nblocks)) as block:
            @block.tensor
            def _(e):
                self._replay("pe", e)

            @block.scalar
            def _(e):
                self._replay("act", e)

            @block.vector
            def _(e):
                self._replay("dve", e)

            @block.gpsimd
            def _(e):
                self._replay("pool", e)

            @block.sync
            def _(e):
                self._replay("sp", e)
                for s, v in sp_tail:
                    e.wait_ge(self.sem[s], v)
        self.prog = {e: [] for e in self.engs}
        for e in self.engs:
            for s, v in self.val.items():
                self.seen[e][s] = v
        self.lastw = {}
        self.readers = {}
        for key, nm in self.dkeys.items():
            self.free_dsems.append(nm)
        self.dkeys = {}


_UID = [0]


def _u(name):
    _UID[0] += 1
    return "%s_%d" % (name, _UID[0])


class Ring:
    def __init__(self, name, bufs):
        self.name = name
        self.bufs = bufs
        self.i = 0

    def next(self):
        k = self.i % len(self.bufs)
        self.i += 1
        return self.bufs[k], (self.name, k)


MOD_SH1, MOD_S1, MOD_GA1, MOD_SH2, MOD_S2, MOD_GA2 = range(6)

FEAT_COLS = ([0 + 128 * i for i in range(4)] + [512 + 128 * i for i in range(4)]
             + [1024 + 128 * i for i in range(8)] + [2048 + 128 * i for i in range(8)]
             + [4096 + 128 * i for i in range(4)])
FEAT32_COLS = [5120 + 128 * i for i in range(4)] + [5632 + 128 * i for i in range(4)]
TOK_COLS = [3072, 3584, 4608, 6144]
NFB = len(FEAT_COLS)
NF32 = len(FEAT32_COLS)
CH_A, CH_G, CH_Q, CH_K, CH_HQ = 0, 4, 8, 16, 24


class Ctx:
    pass


def bcast_row(ap_row, n):
    return bass.AP(tensor=ap_row.tensor, offset=ap_row.offset, ap=[[0, 128], [1, n]])


def emit_norm(S, nc, K, tiles, xsrc, AB, hT, es, want32=None, htok=None):
    sb = lambda name, shape, dt: es.enter_context(nc.sbuf_tensor(_u(name), shape, dt))
    xb = Ring("xb", [sb("nx%d" % i, [128, D], F32) for i in range(2)])
    tb = Ring("tb", [sb("nt%d" % i, [128, D], F32) for i in range(2)])
    hb = Ring("hb", [sb("nh%d" % i, [128, D], BF16) for i in range(2)])
    jb = sb("njunk", [128, D], BF16)
    st = Ring("st", [sb("nst%d" % i, [128, 2], F32) for i in range(3)])
    pT = Ring("pT", [es.enter_context(nc.psum_tensor(_u("npT%d" % i), [128, 1024], BF16)) for i in range(4)])
    ident = K.ident_bf
    for i, (col0, kind) in enumerate(tiles):
        A, B = AB[kind]
        x, xk = xb.next()
        src = xsrc(i)
        S.dma("sp", lambda e, x=x, src=src: e.dma_start(out=x[:], in_=src), ("nx", xk[1]), writes=[xk])
        s, sk = st.next()
        S.op("act", lambda e, x=x, s=s: e.activation(out=jb[:], in_=x[:], func=AF.Square, accum_out=s[:, 0:1]),
             reads=[xk], writes=[sk, "njunk"])
        S.op("dve", lambda e, s=s: e.tensor_scalar(out=s[:, 1:2], in0=s[:, 0:1], scalar1=1.0 / D, scalar2=EPS,
                                                   op0=ALU.mult, op1=ALU.add), reads=[sk], writes=[sk])
        S.op("act", lambda e, s=s: e.activation(out=s[:, 1:2], in_=s[:, 1:2], func=AF.Sqrt), reads=[sk], writes=[sk])
        S.op("dve", lambda e, s=s: e.reciprocal(out=s[:, 1:2], in_=s[:, 1:2]), reads=[sk], writes=[sk])
        t, tk = tb.next()
        S.op("dve", lambda e, t=t, x=x, s=s, A=A: e.scalar_tensor_tensor(
            out=t[:], in0=x[:], scalar=s[:, 1:2], in1=A[:], op0=ALU.mult, op1=ALU.mult),
            reads=[xk, sk], writes=[tk])
        if htok is None:
            h, hk = hb.next()
        else:
            h, hk = htok(i)
        if want32 is None:
            S.op("pool", lambda e, t=t, h=h, B=B: e.tensor_tensor(out=h[:], in0=t[:], in1=B[:], op=ALU.add),
                 reads=[tk], writes=[hk])
        else:
            S.op("pool", lambda e, t=t, B=B: e.tensor_tensor(out=t[:], in0=t[:], in1=B[:], op=ALU.add),
                 reads=[tk], writes=[tk])
            S.op("act", lambda e, t=t, h=h: e.copy(out=h[:], in_=t[:]), reads=[tk], writes=[hk])
            want32(i, t, tk)
        if hT is None:
            continue
        for half in range(2):
            p, pk = pT.next()

            def tr(e, p=p, h=h, half=half):
                r = None
                for k in range(8):
                    kk = half * 8 + k
                    r = e.transpose(out=p[:, k * 128:(k + 1) * 128], in_=h[:, kk * 128:(kk + 1) * 128],
                                    identity=ident[:])
                return r
            S.op("pe", tr, reads=[hk], writes=[pk])
            eng = "act" if half == 0 else "dve"
            dst = hT[:, half * 8:(half + 1) * 8, col0:col0 + 128]
            hkey = ("hT", col0 // 128, half)
            if eng == "act":
                S.op("act", lambda e, p=p, dst=dst: e.copy(out=dst, in_=p[:].rearrange("p (k n) -> p k n", k=8)),
                     reads=[pk], writes=[hkey])
            else:
                S.op("dve", lambda e, p=p, dst=dst: e.tensor_copy(out=dst, in_=p[:].rearrange("p (k n) -> p k n", k=8)),
                     reads=[pk], writes=[hkey])


def load_mod_AB(S, nc, T, l, which, gvec, es, tag):
    sb = lambda name, shape, dt: es.enter_context(nc.sbuf_tensor(_u(name), shape, dt))
    gm = sb(tag + "gm", [128, D], F32)
    S.dma("sp", lambda e: e.dma_start(out=gm[:], in_=bcast_row(gvec, D)), (tag, "gm"), writes=[tag + "gm"])
    AB = {}
    for kind in range(2):
        A = sb(tag + "A%d" % kind, [128, D], F32)
        B = sb(tag + "B%d" % kind, [128, D], F32)
        sh = T.mod[l * 2 + kind:l * 2 + kind + 1, which[0] * D:(which[0] + 1) * D]
        sc = T.mod[l * 2 + kind:l * 2 + kind + 1, which[1] * D:(which[1] + 1) * D]
        S.dma("sp", lambda e, B=B, sh=sh: e.dma_start(out=B[:], in_=bcast_row(sh, D)), (tag, "B", kind),
              writes=[(tag, "B", kind)])
        S.dma("sp", lambda e, A=A, sc=sc: e.dma_start(out=A[:], in_=bcast_row(sc, D)), (tag, "A", kind),
              writes=[(tag, "A", kind)])
        S.op("dve", lambda e, A=A: e.scalar_tensor_tensor(out=A[:], in0=A[:], scalar=1.0, in1=gm[:],
                                                          op0=ALU.add, op1=ALU.mult),
             reads=[(tag, "A", kind), tag + "gm"], writes=[(tag, "A", kind)])
        AB[kind] = (A, B)
    return AB


def tile_kind(t):
    return 1 if t < 2 else 0


def phase_A(S, nc, K, T, l, xs):
    from contextlib import ExitStack
    with ExitStack() as es:
        sb = lambda name, shape, dt: es.enter_context(nc.sbuf_tensor(_u(name), shape, dt))
        hT = sb("hT", [128, 16, NTOK], BF16)
        with ExitStack() as es2:
            AB = load_mod_AB(S, nc, T, l, (MOD_SH1, MOD_S1), T.g_mix[l:l + 1, :], es2, "m1")
            tiles = [(t * 128, tile_kind(t)) for t in range(NT)]
            emit_norm(S, nc, K, tiles, lambda i: xs[i * 128:(i + 1) * 128, :], AB, hT, es2)
            S.flush("A1_%d" % l)
        hk_all = [("hT", t, half) for t in range(NT) for half in range(2)]
        wf = Ring("wf", [sb("wf%d" % i, [128, 16, 128], BF16) for i in range(3)])
        stg = Ring("stg", [sb("stg%d" % i, [128, NTOK], BF16) for i in range(2)])
        stg32 = Ring("stg32", [sb("stg32_%d" % i, [128, NTOK], F32) for i in range(2)])
        pp = Ring("pp", [es.enter_context(nc.psum_tensor(_u("app%d" % i), [128, 512], F32)) for i in range(4)])
        nev = 0
        for c in range(NFB + NF32):
            is32 = c >= NFB
            w, wk = wf.next()
            src = T.w_feat[(l * 36 + c) * 128:(l * 36 + c + 1) * 128, :]
            S.dma("pool", lambda e, w=w, src=src: e.dma_start(out=w[:].rearrange("p k n -> p (k n)"), in_=src),
                  ("wf", wk[1]), writes=[wk])
            so, sk = (stg32 if is32 else stg).next()
            for tg in range(NT // 4):
                p, pk = pp.next()

                def mm(e, p=p, w=w, tg=tg):
                    r = None
                    for k in range(16):
                        r = e.matmul(p[:], lhsT=w[:, k, :], rhs=hT[:, k, tg * 512:(tg + 1) * 512],
                                     start=(k == 0), stop=(k == 15))
                    return r
                S.op("pe", mm, reads=[wk] + hk_all[tg * 8:(tg + 1) * 8], writes=[pk])
                dst = so[:, tg * 512:(tg + 1) * 512]
                if nev % 2 == 0:
                    S.op("act", lambda e, p=p, dst=dst: e.copy(out=dst, in_=p[:]), reads=[pk], writes=[(sk, tg)])
                else:
                    S.op("dve", lambda e, p=p, dst=dst: e.tensor_copy(out=dst, in_=p[:]), reads=[pk],
                         writes=[(sk, tg)])
                nev += 1
            if is32:
                dstd = T.uT32[(c - NFB) * 128:(c - NFB + 1) * 128, :]
            else:
                dstd = T.uT[c * 128:(c + 1) * 128, :]
            S.dma("sp", lambda e, so=so, dstd=dstd: e.dma_start(out=dstd, in_=so[:]), ("stgo", is32, sk[1]),
                  reads=[(sk, tg) for tg in range(NT // 4)], writes=[("uT", c)])
            for tg in range(NT // 4):
                S.readers.setdefault((sk, tg), {})
                S.readers[(sk, tg)][S.dsem(("stgo", is32, sk[1]))] = S.val[S.dsem(("stgo", is32, sk[1]))]
        wt = Ring("wt", [sb("wt%d" % i, [128, 16, 512], BF16) for i in range(2)])
        so_t = Ring("sot", [sb("sot%d" % i, [128, 512], BF16) for i in range(4)])
        for g in range(4):
            w, wk = wt.next()
            src = T.w_tokm[(l * 4 + g) * 128:(l * 4 + g + 1) * 128, :]
            S.dma("pool", lambda e, w=w, src=src: e.dma_start(out=w[:].rearrange("p k n -> p (k n)"), in_=src),
                  ("wt", wk[1]), writes=[wk])
            for t in range(NT):
                p, pk = pp.next()

                def mm(e, p=p, w=w, t=t):
                    r = None
                    for k in range(16):
                        r = e.matmul(p[:], lhsT=hT[:, k, t * 128:(t + 1) * 128], rhs=w[:, k, :],
                                     start=(k == 0), stop=(k == 15))
                    return r
                S.op("pe", mm, reads=[wk] + hk_all[t * 2:(t + 1) * 2], writes=[pk])
                so, sk = so_t.next()
                if nev % 2 == 0:
                    S.op("act", lambda e, p=p, so=so: e.copy(out=so[:], in_=p[:]), reads=[pk], writes=[sk])
                else:
                    S.op("dve", lambda e, p=p, so=so: e.tensor_copy(out=so[:], in_=p[:]), reads=[pk], writes=[sk])
                nev += 1
                dstd = T.utok[t * 128:(t + 1) * 128, g * 512:(g + 1) * 512]
                S.dma("sp", lambda e, so=so, dstd=dstd: e.dma_start(out=dstd, in_=so[:]), ("soto", sk[1]),
                      reads=[sk], writes=[("utok", t, g)])
                S.readers.setdefault(sk, {})
                S.readers[sk][S.dsem(("soto", sk[1]))] = S.val[S.dsem(("soto", sk[1]))]
        S.flush("A2_%d" % l)


def host_consts():
    c = np.zeros((128, 14, 128), np.float32)
    c[:, 0] = np.eye(128)
    c[:, 1] = np.eye(128)[::-1]
    t = np.arange(128)
    same = (t[:, None] // 32) == (t[None, :] // 32)
    c[:, 2] = (same & (t[None, :] >= t[:, None]))
    c[:, 3] = (same & (t[None, :] <= t[:, None]))
    c[:, 4] = (t[None, :] % 32 != 0).astype(np.float32)
    c[:, 5] = 1.0 / 512.0
    c[:, 6] = 1.0
    c[:, 7, 0:4] = (t[:, None] // 32) == np.arange(4)[None, :]
    c[:, 8] = (t[:, None] < t[None, :])
    c[:, 9, 0:18] = 128.0 * np.arange(18)[None, :]
    c[:, 9, 32:32 + 80] = np.arange(80)[None, :]
    c[:, 9, 127] = np.arange(128)
    c[:, 10:14] = c[:, 4:5]
    return c.reshape(128, 14 * 128)


def load_consts(S, nc, T, es):
    K = Ctx()
    sb = lambda name, shape, dt: es.enter_context(nc.sbuf_tensor(_u(name), shape, dt))
    K.c32 = sb("c32", [128, 14, 128], F32)
    K.cbf = sb("cbf", [128, 14, 128], BF16)
    S.dma("sp", lambda e: e.dma_start(out=K.c32[:].rearrange("p a b -> p (a b)"), in_=T.consts[:, :]), "c32",
          writes=["c32"])
    S.op("dve", lambda e: e.tensor_copy(out=K.cbf[:], in_=K.c32[:]), reads=["c32"], writes=["cbf"])
    K.ident_bf = K.cbf[:, 0, :]
    K.ident32 = K.c32[:, 0, :]
    K.anti32 = K.c32[:, 1, :]
    K.mask_f = K.c32[:, 2, :]
    K.mask_b = K.c32[:, 3, :]
    K.reset = K.c32[:, 4, :]
    K.avg32 = K.c32[:, 5, :]
    K.ones32 = K.c32[:, 6, :]
    K.ones_bf = K.cbf[:, 6, :]
    K.rowmask = K.c32[:, 7, 0:4]
    K.reset4 = K.c32[:, 10:14, :].rearrange("p a b -> p (a b)")
    S.flush("consts")
    return K


def build(cfg):
    from contextlib import ExitStack
    nc = bass.Bass("TRN2", target_bir_lowering=False)
    T = Ctx()
    dbg = cfg.get("debug", ())
    nexp = cfg.get("nexp", NEXP)

    def din(name, shape, dt=F32):
        return nc.dram_tensor(name, list(shape), dt, kind="ExternalInput").ap()

    def dscr(name, shape, dt=F32):
        if name in dbg:
            return nc.dram_tensor(name, list(shape), dt, kind="ExternalOutput").ap()
        return nc.dram_tensor(name, list(shape), dt).ap()

    T.x_in = din("x_in", [NTOK, D])
    T.consts = din("consts", [128, 14 * 128])
    T.g_mix = din("g_mix", [DEPTH, D])
    T.w_feat = din("w_feat", [DEPTH * 36 * 128, 16 * 128])
    T.w_tokm = din("w_tokm", [DEPTH * 4 * 128, 16 * 512])
    if cfg.get("mod_input"):
        T.mod = din("mod", [DEPTH * 2, 6 * D])
    else:
        T.mod = dscr("mod", [DEPTH * 2, 6 * D])
        T.cT = din("cT", [128, 80])
        T.w_adam = din("w_adam", [DEPTH * 128, 16 * 1536])
        T.b_ada = din("b_ada", [1, DEPTH * 1536])
        T.bsel = din("bsel", [5, 2])
        T.msend = dscr("msend", [5, DEPTH * 1536])
        T.mrecv = dscr("mrecv", [40, DEPTH * 1536])
    T.pflag = din("pflag", [128, 2])
    T.uT = dscr("uT", [NFB * 128, NTOK], BF16)
    T.uT32 = dscr("uT32", [NF32 * 128, NTOK], F32)
    T.utok = dscr("utok", [NTOK, 2048], BF16)
    T.mixT = dscr("mixT", [16 * 128, NOWN * 128], BF16)
    T.conv_wT = din("conv_wT", [DEPTH * 512, 31])
    T.conv_vec = din("conv_vec", [DEPTH * 128, 12])
    T.na_bias = din("na_bias", [DEPTH * 16 * 10 * 128, 256])
    T.hlb = din("hlb", [128, 16])
    T.hnorm_g = din("hnorm_g", [DEPTH, 512])
    T.hsend = dscr("hsend", [512, 128])
    T.hrecv = dscr("hrecv", [512, 128])
    T.hrecv2 = dscr("hrecv2", [1024, 128])
    T.hostore = dscr("hostore", [NOWN * 128, 512])
    T.w_outm = din("w_outm", [DEPTH * 4 * 128, 16 * 512])
    T.g_ffn = din("g_ffn", [DEPTH, D])
    T.g_final = din("g_final", [1, D])
    T.w_r = din("w_r", [DEPTH * 128, 16 * 36])
    T.b_r = din("b_r", [DEPTH, 36])
    if cfg.get("sparse", True):
        _NE[0] = cfg.get("ne_eff", NEXP)
        T.w1m = din("w1m", [4 * RWv(), 2048])
        T.w3m = din("w3m", [4 * RWv(), 2048])
        T.w2m = din("w2m", [4 * RWv(), 2048])
    else:
        T.w1m = din("w1m", [DEPTH * nexp * 128, 16 * 512])
        T.w3m = din("w3m", [DEPTH * nexp * 128, 16 * 512])
        T.w2m = din("w2m", [DEPTH * nexp * 128, 4 * D])
    T.x1 = dscr("x1", [NOWN * 128, D])
    T.h2T = dscr("h2T", [16 * 128, NOWN * 128], BF16)
    T.wt = dscr("wt", [128, NOWN * 32])
    T.facc = dscr("facc", [NOWN * 128, D])
    sparse = cfg.get("sparse", True)
    if sparse:
        T.xsort = dscr("xsort", [50 * 256, D], BF16)
        T.ysd = dscr("ysd", [50 * 256, D])
        T.blku = dscr("blku", [128, 256], U32)
        T.slotsu = dscr("slotsu", [128, NOWN * 2], U32)
        T.aall = dscr("aall", [128, NOWN * 2])
    T.xs2 = dscr("xs2", [NTOK, D])
    T.xsend = dscr("xsend", [256, D])
    T.xrecv = dscr("xrecv", [512, D])
    T.out = nc.dram_tensor("out", [16 * 128, D], F32, kind="ExternalOutput").ap()

    S = Sched(nc)
    with ExitStack() as es:
        K = load_consts(S, nc, T, es)
        nlayers = cfg.get("layers", DEPTH)
        stop = cfg.get("stop", "")
        noex = cfg.get("no_exchange", False)
        if not cfg.get("mod_input"):
            phase_mod(S, nc, K, T)
        for l in range(nlayers):
            xs = T.x_in if l == 0 else T.xs2
            with_ctx = l < DEPTH - 1
            last = l == DEPTH - 1
            phase_A(S, nc, K, T, l, xs)
            if stop == "A":
                break
            phase_hg1(S, nc, K, T, l, with_ctx)
            if stop == "hg1":
                break
            if not noex:
                phase_xchg_h(S, nc, K, T)
            phase_conv(S, nc, K, T, l, with_ctx)
            if stop == "conv":
                break
            phase_na(S, nc, K, T, l, with_ctx)
            if stop == "na":
                break
            phase_hg2(S, nc, K, T, l, with_ctx, None if noex else T.hrecv)
            if stop == "hg":
                break
            phase_C(S, nc, K, T, l, with_ctx, xs)
            if sparse:
                phase_D1s(S, nc, K, T, l, with_ctx)
                if stop == "D1":
                    break
                phase_D2s(S, nc, K, T, l, with_ctx)
            else:
                phase_D1(S, nc, K, T, l, with_ctx)
                if stop == "D1":
                    break
                phase_D2(S, nc, K, T, l, with_ctx, nexp)
            phase_D3(S, nc, K, T, l, with_ctx, last, sparse)
            if stop == "D3":
                break
            if not last and not noex:
                phase_xchg_x(S, nc, K, T)
            if not last and noex:
                S.dma("sp", lambda e: e.dma_start(out=T.xs2[NOWN * 128:NTOK, :], in_=T.x_in[NOWN * 128:NTOK, :]), "dbgx")
                S.flush("dbgx")
    return nc


def core_tokens(x_b, ctx_b, odd):
    if odd:
        lat = x_b[::-1]
        cx = ctx_b[::-1]
    else:
        lat = x_b
        cx = ctx_b
    return np.ascontiguousarray(np.concatenate([cx, lat[:2304]], axis=0))


def prep_w_in(w_in, odd):
    fcols = list(FEAT_COLS) + (FEAT32_COLS[4:] + FEAT32_COLS[:4] if odd else list(FEAT32_COLS))
    wf = np.empty((DEPTH, 36, 128, 16 * 128), np.float32)
    wt = np.empty((DEPTH, 4, 128, 16 * 512), np.float32)
    for l in range(DEPTH):
        for i, c0 in enumerate(fcols):
            wf[l, i] = w_in[l][:, c0:c0 + 128].reshape(16, 128, 128).transpose(1, 0, 2).reshape(128, -1)
        for i, c0 in enumerate(TOK_COLS):
            wt[l, i] = w_in[l][:, c0:c0 + 512].reshape(16, 128, 512).transpose(1, 0, 2).reshape(128, -1)
    return wf.reshape(-1, 16 * 128), wt.reshape(-1, 16 * 512)


def prep_conv(conv_w, conv_b, ln_g, ln_b, odd):
    cw = conv_w[:, ::-1, :] if odd else conv_w
    cwT = np.ascontiguousarray(cw.transpose(0, 2, 1)).reshape(DEPTH * 512, 31)
    vec = np.stack([conv_b, ln_g, ln_b], axis=1)
    vec = vec.reshape(DEPTH, 3, 4, 128).transpose(0, 3, 1, 2).reshape(DEPTH * 128, 12)
    return cwT.astype(np.float32), np.ascontiguousarray(vec).astype(np.float32)


def phase_conv(S, nc, K, T, l, with_ctx):
    from contextlib import ExitStack
    with ExitStack() as es:
        sb = lambda name, shape, dt: es.enter_context(nc.sbuf_tensor(_u(name), shape, dt))
        pst = lambda name, shape, dt: es.enter_context(nc.psum_tensor(_u(name), shape, dt))
        cw = sb("cw", [128, 4, 31], F32)
        cv = sb("cv", [128, 12], F32)
        wd = sb("wd", [128, 4, 31, 128], BF16)
        hg = sb("hglu", [128, 4, 2078], BF16)
        hgc = sb("hgluc", [128, 4, 286], BF16)
        S.dma("sp", lambda e: e.dma_start(out=cw[:], in_=T.conv_wT[l * 512:(l + 1) * 512, :].rearrange(
            "(c p) k -> p c k", p=128)), "cw", writes=["cw"])
        S.dma("sp", lambda e: e.dma_start(out=cv[:], in_=T.conv_vec[l * 128:(l + 1) * 128, :]), "cv", writes=["cv"])
        for c in range(4):
            for k in range(31):
                eng = "dve" if (k % 2 == 0) else "pool"
                S.op(eng, lambda e, c=c, k=k: e.tensor_scalar(out=wd[:, c, k, :], in0=K.ident32, scalar1=cw[:, c, k:k + 1],
                                                              scalar2=None, op0=ALU.mult),
                     reads=["cw"], writes=[("wd", c, k)])
        S.op("pool", lambda e: e.memset(hg[:, :, 0:15], 0.0), writes=[("hgpad")])
        S.op("pool", lambda e: e.memset(hgc[:], 0.0), writes=[("hgc", c) for c in range(4)])
        ab = Ring("cab", [sb("cab%d" % i, [128, NTOK], BF16) for i in range(2)])
        gb = Ring("cgb", [sb("cgb%d" % i, [128, NTOK], BF16) for i in range(2)])
        sgb = Ring("csg", [sb("csg%d" % i, [128, NTOK], BF16) for i in range(2)])
        for c in range(4):
            a, ak = ab.next()
            g, gk = gb.next()
            sg, sgk = sgb.next()
            S.dma("sp", lambda e, a=a, c=c: e.dma_start(out=a[:], in_=T.uT[(CH_A + c) * 128:(CH_A + c + 1) * 128, :]),
                  ("cab", ak[1]), writes=[ak])
            S.dma("sp", lambda e, g=g, c=c: e.dma_start(out=g[:], in_=T.uT[(CH_G + c) * 128:(CH_G + c + 1) * 128, :]),
                  ("cgb", gk[1]), writes=[gk])
            S.op("act", lambda e, g=g, sg=sg: e.activation(out=sg[:], in_=g[:], func=AF.Sigmoid), reads=[gk], writes=[sgk])
            S.op("dve", lambda e, a=a, sg=sg, c=c: e.tensor_tensor(out=hg[:, c, 15:15 + 2063], in0=a[:, 256:256 + 2063],
                                                                 in1=sg[:, 256:256 + 2063], op=ALU.mult),
                 reads=[ak, sgk], writes=[("hg", c)])
            if with_ctx:
                S.op("dve", lambda e, a=a, sg=sg, c=c: e.tensor_tensor(out=hgc[:, c, 15:15 + 256], in0=a[:, 0:256],
                                                                     in1=sg[:, 0:256], op=ALU.mult),
                     reads=[ak, sgk, ("hgc", c)], writes=[("hgc", c)])
        groups = [(hg, "hg", tg * 512, 512, 256 + tg * 512) for tg in range(4)]
        if with_ctx:
            groups.append((hgc, "hgc", 0, 256, 0))
        pc = Ring("pc", [pst("pc%d" % i, [128, 512], F32) for i in range(3)])
        pmu = Ring("pmu", [pst("pmu%d" % i, [128, 512], F32) for i in range(2)])
        pm2 = Ring("pm2", [pst("pm2%d" % i, [128, 512], F32) for i in range(2)])
        y32 = Ring("y32", [sb("y32_%d" % i, [128, 4, 512], F32) for i in range(2)])
        ysq = Ring("ysq", [sb("ysq_%d" % i, [128, 4, 512], F32) for i in range(2)])
        mub = Ring("mub", [sb("mub%d" % i, [128, 512], F32) for i in range(2)])
        rsb = Ring("rsb", [sb("rsb%d" % i, [128, 512], F32) for i in range(2)])
        db = Ring("db", [sb("db%d" % i, [128, 512], F32) for i in range(3)])
        ob = Ring("ob", [sb("ob%d" % i, [128, 4, 512], BF16) for i in range(2)])
        for (src, sname, off, n, col0) in groups:
            y, yk = y32.next()
            q, qk = ysq.next()
            for c in range(4):
                p, pk = pc.next()

                def mm(e, p=p, c=c, src=src, off=off, n=n):
                    r = None
                    for k in range(31):
                        r = e.matmul(p[:, 0:n], lhsT=wd[:, c, k, :], rhs=src[:, c, off + k:off + k + n],
                                     start=(k == 0), stop=(k == 30))
                    return r
                S.op("pe", mm, reads=[("wd", c, k) for k in range(31)] + [(sname, c), "hgpad"], writes=[pk])
                S.op("act", lambda e, p=p, y=y, c=c, n=n: e.activation(out=y[:, c, 0:n], in_=p[:, 0:n], func=AF.Identity,
                                                                      bias=cv[:, c:c + 1]),
                     reads=[pk, "cv"], writes=[(yk, c)])
                S.op("act", lambda e, p=p, q=q, c=c, n=n: e.activation(out=q[:, c, 0:n], in_=p[:, 0:n], func=AF.Square,
                                                                      bias=cv[:, c:c + 1]),
                     reads=[pk, "cv"], writes=[(qk, c)])
            m, mk = pmu.next()
            m2, m2k = pm2.next()

            def st(e, m=m, m2=m2, y=y, q=q, n=n):
                r = None
                for c in range(4):
                    e.matmul(m[:, 0:n], lhsT=K.avg32, rhs=y[:, c, 0:n], start=(c == 0), stop=(c == 3))
                for c in range(4):
                    r = e.matmul(m2[:, 0:n], lhsT=K.avg32, rhs=q[:, c, 0:n], start=(c == 0), stop=(c == 3))
                return r
            S.op("pe", st, reads=[(yk, c) for c in range(4)] + [(qk, c) for c in range(4)], writes=[mk, m2k])
            mu, muk = mub.next()
            rs, rsk = rsb.next()
            S.op("act", lambda e, mu=mu, m=m, n=n: e.copy(out=mu[:, 0:n], in_=m[:, 0:n]), reads=[mk], writes=[muk])
            S.op("dve", lambda e, mu=mu, rs=rs, n=n: e.tensor_tensor(out=rs[:, 0:n], in0=mu[:, 0:n], in1=mu[:, 0:n],
                                                                    op=ALU.mult), reads=[muk], writes=[rsk])
            S.op("dve", lambda e, rs=rs, m2=m2, n=n: e.tensor_tensor(out=rs[:, 0:n], in0=m2[:, 0:n], in1=rs[:, 0:n],
                                                                    op=ALU.subtract), reads=[m2k, rsk], writes=[rsk])
            S.op("dve", lambda e, rs=rs, n=n: e.tensor_scalar(out=rs[:, 0:n], in0=rs[:, 0:n], scalar1=EPS, scalar2=None,
                                                              op0=ALU.add), reads=[rsk], writes=[rsk])
            S.op("act", lambda e, rs=rs, n=n: e.activation(out=rs[:, 0:n], in_=rs[:, 0:n], func=AF.Sqrt),
                 reads=[rsk], writes=[rsk])
            S.op("dve", lambda e, rs=rs, n=n: e.reciprocal(out=rs[:, 0:n], in_=rs[:, 0:n]), reads=[rsk], writes=[rsk])
            o, ok = ob.next()
            for c in range(4):
                d, dk = db.next()
                S.op("dve", lambda e, d=d, y=y, mu=mu, c=c, n=n: e.tensor_tensor(out=d[:, 0:n], in0=y[:, c, 0:n],
                                                                                in1=mu[:, 0:n], op=ALU.subtract),
                     reads=[(yk, c), muk], writes=[dk])
                S.op("pool", lambda e, d=d, rs=rs, n=n: e.tensor_tensor(out=d[:, 0:n], in0=d[:, 0:n], in1=rs[:, 0:n],
                                                                       op=ALU.mult), reads=[dk, rsk], writes=[dk])
                S.op("dve", lambda e, d=d, c=c, n=n: e.tensor_scalar(out=d[:, 0:n], in0=d[:, 0:n],
                                                                    scalar1=cv[:, 4 + c:5 + c], scalar2=cv[:, 8 + c:9 + c],
                                                                    op0=ALU.mult, op1=ALU.add),
                     reads=[dk, "cv"], writes=[dk])
                S.op("act", lambda e, d=d, o=o, c=c, n=n: e.activation(out=o[:, c, 0:n], in_=d[:, 0:n], func=AF.Silu),
                     reads=[dk], writes=[(ok, c)])
            dst = T.mixT[0:512, col0:col0 + n].rearrange("(c p) n -> p c n", p=128)
            S.dma("sp", lambda e, o=o, dst=dst, n=n: e.dma_start(out=dst, in_=o[:, :, 0:n]), ("ob", ok[1]),
                  reads=[(ok, c) for c in range(4)], writes=[("mixT", 0, col0)])
        S.flush("conv_%d" % l)


def na_groups(with_ctx):
    groups = []
    for g in range(8):
        if g == 0:
            chunks = [(2 + j, j) for j in range(4)]
        else:
            chunks = [(2 + 2 * g - 2 + c, 4 + c) for c in range(6)]
        chunks += [(0, None), (1, None)]
        groups.append((256 + 256 * g, chunks, (2 + 2 * g, 3 + 2 * g)))
    if with_ctx:
        groups.append((0, [(0, None), (1, None)], (0, 1)))
    return groups


def phase_na(S, nc, K, T, l, with_ctx):
    from contextlib import ExitStack
    with ExitStack() as es:
        sb = lambda name, shape, dt: es.enter_context(nc.sbuf_tensor(_u(name), shape, dt))
        pst = lambda name, shape, dt: es.enter_context(nc.psum_tensor(_u(name), shape, dt))
        na_tok = sb("na_tok", [128, NOWN, 1024], BF16)
        qb = Ring("naq", [sb("naq%d" % i, [128, NOWN * 128], BF16) for i in range(2)])
        kb = Ring("nak", [sb("nak%d" % i, [128, NTOK], BF16) for i in range(2)])
        vb = Ring("nav", [sb("nav%d" % i, [128, NT, 128], BF16) for i in range(2)])
        va = Ring("nava", [sb("nava%d" % i, [128, NT, 2, 65], BF16) for i in range(2)])
        bb = Ring("nab", [sb("nab%d" % i, [128, 10, 256], F32) for i in range(2)])
        pS = Ring("naps", [pst("naps%d" % i, [128, 256], F32) for i in range(3)])
        pO = Ring("napo", [pst("napo%d" % i, [128, 128], F32) for i in range(3)])
        tmpb = Ring("natmp", [sb("natmp%d" % i, [128, 256], F32) for i in range(3)])
        pTb = Ring("napT", [sb("napT%d" % i, [128, 8, 256], BF16) for i in range(2)])
        rcb = Ring("narc", [sb("narc%d" % i, [128, 1], F32) for i in range(4)])
        for i in range(2):
            S.op("pool", lambda e, i=i: e.memset(va.bufs[i][:], 1.0), writes=[("nava", i)])
        groups = na_groups(with_ctx)
        for hp in range(8):
            q, qk = qb.next()
            k, kk = kb.next()
            v, vk = vb.next()
            vau, vak = va.next()
            S.dma("sp", lambda e, q=q, hp=hp: e.dma_start(out=q[:], in_=T.uT[(CH_Q + hp) * 128:(CH_Q + hp + 1) * 128,
                                                                          0:NOWN * 128]), ("naq", qk[1]), writes=[qk])
            S.dma("sp", lambda e, k=k, hp=hp: e.dma_start(out=k[:], in_=T.uT[(CH_K + hp) * 128:(CH_K + hp + 1) * 128, :]),
                  ("nak", kk[1]), writes=[kk])
            S.dma("sp", lambda e, v=v, hp=hp: e.dma_start(out=v[:], in_=T.utok[:, hp * 128:(hp + 1) * 128].rearrange(
                "(t p) c -> p t c", p=128)), ("nav", vk[1]), writes=[vk])
            S.op("pool", lambda e, v=v, vau=vau: e.tensor_copy(out=vau[:, :, :, 0:64],
                                                             in_=v[:].rearrange("p t (h d) -> p t h d", h=2)),
                 reads=[vk], writes=[vak])
            for hl in range(2):
                h = 2 * hp + hl
                pb = 64 * hl
                b, bk = bb.next()
                S.dma("sp", lambda e, b=b, h=h: e.dma_start(
                    out=b[:], in_=T.na_bias[(l * 16 + h) * 1280:(l * 16 + h + 1) * 1280, :].rearrange(
                        "(b p) q -> p b q", p=128)), ("nab", bk[1]), writes=[bk])
                for (qc0, chunks, otiles) in groups:
                    pt, ptk = pTb.next()
                    for ci, (tile, bi) in enumerate(chunks):
                        p, pk = pS.next()
                        S.op("pe", lambda e, p=p, k=k, q=q, tile=tile, qc0=qc0, pb=pb: e.matmul(
                            p[:], lhsT=k[pb:pb + 64, tile * 128:(tile + 1) * 128], rhs=q[pb:pb + 64, qc0:qc0 + 256],
                            start=True, stop=True), reads=[kk, qk], writes=[pk])
                        if bi is not None:
                            tm, tmk = tmpb.next()
                            S.op("dve", lambda e, tm=tm, p=p, b=b, bi=bi: e.scalar_tensor_tensor(
                                out=tm[:], in0=p[:], scalar=0.125, in1=b[:, bi, :], op0=ALU.mult, op1=ALU.add),
                                reads=[pk, bk], writes=[tmk])
                            S.op("act", lambda e, tm=tm, pt=pt, ci=ci: e.activation(out=pt[:, ci, :], in_=tm[:], func=AF.Exp),
                                 reads=[tmk], writes=[(ptk, ci)])
                        else:
                            S.op("act", lambda e, p=p, pt=pt, ci=ci: e.activation(out=pt[:, ci, :], in_=p[:], func=AF.Exp,
                                                                                scale=0.125),
                                 reads=[pk], writes=[(ptk, ci)])
                    for qt in range(2):
                        o, ok = pO.next()

                        def pv(e, o=o, pt=pt, qt=qt, chunks=chunks, vau=vau, hl=hl):
                            r = None
                            n = len(chunks)
                            for ci, (tile, bi) in enumerate(chunks):
                                r = e.matmul(o[:, 0:65], lhsT=pt[:, ci, qt * 128:(qt + 1) * 128], rhs=vau[:, tile, hl, :],
                                             start=(ci == 0), stop=(ci == n - 1))
                            return r
                        S.op("pe", pv, reads=[(ptk, ci) for ci in range(len(chunks))] + [vak], writes=[ok])
                        rc, rck = rcb.next()
                        S.op("dve", lambda e, rc=rc, o=o: e.reciprocal(out=rc[:], in_=o[:, 64:65]), reads=[ok], writes=[rck])
                        ot = otiles[qt]
                        S.op("act", lambda e, o=o, rc=rc, ot=ot, h=h: e.activation(
                            out=na_tok[:, ot, h * 64:(h + 1) * 64], in_=o[:, 0:64], func=AF.Identity, scale=rc[:, 0:1]),
                            reads=[ok, rck], writes=[("natok", ot)])
        stT = sb("na_stT", [128, 8, NOWN * 128], BF16)
        pTr = Ring("naptr", [pst("naptr%d" % i, [128, 1024], BF16) for i in range(2)])
        t0 = 0 if with_ctx else 2
        for t in range(t0, NOWN):
            p, pk = pTr.next()

            def tr(e, p=p, t=t):
                r = None
                for c in range(8):
                    r = e.transpose(out=p[:, c * 128:(c + 1) * 128], in_=na_tok[:, t, c * 128:(c + 1) * 128],
                                    identity=K.ident_bf)
                return r
            S.op("pe", tr, reads=[("natok", t)], writes=[pk])
            dst = stT[:, :, t * 128:(t + 1) * 128]
            if t % 2 == 0:
                S.op("act", lambda e, p=p, dst=dst: e.copy(out=dst, in_=p[:].rearrange("p (c n) -> p c n", c=8)),
                     reads=[pk], writes=[("stT", t)])
            else:
                S.op("dve", lambda e, p=p, dst=dst: e.tensor_copy(out=dst, in_=p[:].rearrange("p (c n) -> p c n", c=8)),
                     reads=[pk], writes=[("stT", t)])
        c0 = t0 * 128
        for c in range(8):
            S.dma("sp", lambda e, c=c: e.dma_start(out=T.mixT[(4 + c) * 128:(5 + c) * 128, c0:NOWN * 128],
                                                   in_=stT[:, c, c0:NOWN * 128]), ("nast", c),
                  reads=[("stT", t) for t in range(t0, NOWN)], writes=[("mixT", 4 + c)])
        S.flush("na_%d" % l)


def prep_na_bias(rpb, odd):
    out = np.empty((DEPTH, 16, 10, 128, 256), np.float32)

    def tr(x):
        return 63 - x if odd else x
    kc = tr(np.arange(64))[None, :, None, None]
    qc = tr(np.arange(64))[None, None, None, :]
    pats = []
    for j in range(4):
        pats.append((np.arange(0, 4), np.arange(2 * j, 2 * j + 2)))
    for c in range(6):
        pats.append((np.arange(4, 8), np.arange(2 * c, 2 * c + 2)))
    for bi, (qrows, krows) in enumerate(pats):
        kr = tr(krows)[:, None, None, None]
        qr = tr(qrows)[None, None, :, None]
        sr = np.clip(qr - 4, 0, 56)
        ws = np.clip(qc - 8, 0, 48)
        valid = (kr >= sr) & (kr < sr + 8) & (kc >= ws) & (kc < ws + 16)
        valid = np.broadcast_to(valid, (2, 64, 4, 64))
        di = np.clip(np.broadcast_to(kr - qr + 7, (2, 64, 4, 64)), 0, 14)
        dj = np.clip(np.broadcast_to(kc - qc + 15, (2, 64, 4, 64)), 0, 30)
        vals = rpb[:, :, di, dj]
        vals = np.where(valid[None, None], vals, np.float32(-1e30))
        out[:, :, bi] = vals.reshape(DEPTH, 16, 128, 256)
    return out.reshape(-1, 256)


def _bc_last(ap3, n):
    a = [list(x) for x in ap3.ap]
    a[-1] = [0, n]
    return bass.AP(tensor=ap3.tensor, offset=ap3.offset, ap=a)


def _diag4(t):
    a = t[:, :, 0:32]
    ap = [list(x) for x in a.ap]
    ap[1][0] = ap[1][0] + 32
    return bass.AP(tensor=a.tensor, offset=a.offset, ap=ap)


import os
HG_DBG = int(os.environ.get('HG_DBG', '99'))
HG_SKIP = os.environ.get('HG_SKIP', '')
HG_KT = os.environ.get('HG_KT', 'dve')


def _fap(t, off, dims):
    a = t[:]
    return bass.AP(tensor=a.tensor, offset=a.offset + off, ap=[list(a.ap[0])] + [list(d) for d in dims])


class HgEnv:
    def __init__(self, S, nc, K, T, l, es):
        sb = lambda name, shape, dt: es.enter_context(nc.sbuf_tensor(_u(name), shape, dt))
        pst = lambda name, shape, dt: es.enter_context(nc.psum_tensor(_u(name), shape, dt))
        self.S, self.nc, self.K, self.T, self.l = S, nc, K, T, l
        self.z = sb("hgz", [128, 4, NOWN * 128], F32)
        self.q = sb("hgq", [128, 4, NOWN * 128], BF16)
        self.v = sb("hgv", [128, NOWN, 512], BF16)
        f = lambda nm, n: Ring(nm, [sb("%s%d" % (nm, i), [128, 512], F32) for i in range(n)])
        self.sg, self.f, self.lf, self.kT, self.bT = f("hsg", 2), f("hf", 2), f("hlf", 2), f("hkT", 2), f("hbT", 2)
        self.tmp, self.b2, self.Eb, self.Enb, self.sq = f("htmp", 2), f("hb2", 2), f("hEb", 3), f("hEnb", 2), f("hsq", 2)
        self.e2, self.E2 = f("he2", 2), f("hE2", 2)
        self.Qtz = Ring("hQtz", [sb("hQtz%d" % i, [128, 16, 128], BF16) for i in range(2)])
        self.Kt = Ring("hKt", [sb("hKt%d" % i, [128, 512], BF16) for i in range(2)])
        self.KhT = Ring("hKhT", [sb("hKhT%d" % i, [128, 512], BF16) for i in range(2)])
        self.Khs = Ring("hKhs", [sb("hKhs%d" % i, [128, 512], BF16) for i in range(2)])
        self.Khz = Ring("hKhz", [sb("hKhz%d" % i, [128, 16, 128], BF16) for i in range(2)])
        self.attm = Ring("hattm", [sb("hattm%d" % i, [128, 512], BF16) for i in range(2)])
        self.Sb = Ring("hSb", [sb("hSb%d" % i, [128, 512], BF16) for i in range(6)])
        self.S32 = Ring("hS32", [sb("hS32_%d" % i, [128, 512], F32) for i in range(2)])
        self.pkh = Ring("hpkh", [pst("hpkh%d" % i, [128, 512], BF16) for i in range(1)])
        self.patt = Ring("hpatt", [pst("hpatt%d" % i, [128, 512], F32) for i in range(2)])
        self.pu = Ring("hpu", [pst("hpu%d" % i, [128, 512], F32) for i in range(2)])
        self.po = Ring("hpo", [pst("hpo%d" % i, [128, 512], F32) for i in range(2)])
        self.ostore = sb("hostore_sb", [128, NOWN, 512], F32)
        self.lbt = sb("hlbt", [128, 2, 8], F32)
        for i in range(2):
            S.op("pool", lambda e, i=i: e.memset(self.Qtz.bufs[i][:], 0.0), writes=[("hQtz", i)])
        if l == 0:
            S.op("pool", lambda e: e.memset(self.lbt[:, 0, :], 0.0), writes=["hlbt"])
            S.op("pool", lambda e: e.memset(self.lbt[:, 1, :], 1.0), reads=["hlbt"], writes=["hlbt"])
        else:
            raw = sb("hlbraw", [128, 16], F32)
            S.dma("sp", lambda e: e.dma_start(out=raw[:], in_=T.hlb[:, :]), "hlbraw", writes=["hlbraw"])
            S.op("dve", lambda e: e.tensor_tensor(out=self.lbt[:, 0, :], in0=raw[:, 8:16], in1=raw[:, 0:8], op=ALU.subtract),
                 reads=["hlbraw"], writes=["hlbt"])
            S.op("act", lambda e: e.activation(out=self.lbt[:, 0, :], in_=self.lbt[:, 0, :], func=AF.Sigmoid),
                 reads=["hlbt"], writes=["hlbt"])
            S.op("dve", lambda e: e.tensor_scalar(out=self.lbt[:, 1, :], in0=self.lbt[:, 0, :], scalar1=-1.0, scalar2=1.0,
                                                  op0=ALU.mult, op1=ALU.add), reads=["hlbt"], writes=["hlbt"])

    def load(self, d):
        S, T = self.S, self.T
        for h in range(4):
            c = d * 4 + h
            S.dma("sp", lambda e, c=c, h=h: e.dma_start(out=self.z[:, h, :], in_=T.uT32[c * 128:(c + 1) * 128, 0:NOWN * 128]),
                  ("hgz", h), writes=[("hgz", h)])
            S.dma("sp", lambda e, h=h: e.dma_start(out=self.q[:, h, :], in_=T.uT[(CH_HQ + h) * 128:(CH_HQ + h + 1) * 128,
                                                                                 0:NOWN * 128]), ("hgq", h), writes=[("hgq", h)])
        S.dma("sp", lambda e: e.dma_start(out=self.v[:], in_=T.utok[0:NOWN * 128, 1024:1536].rearrange("(t p) c -> p t c", p=128)),
              "hgv", writes=["hgv"])
        self.zk = [("hgz", h) for h in range(4)]
        self.qk = [("hgq", h) for h in range(4)]

    def chain_init(self, src=None):
        S = self.S
        s32, sk = self.S32.next()
        sbf, sbk = self.Sb.next()
        if src is None:
            S.op("dve", lambda e: e.memset(s32[:], 0.0), writes=[sk])
            S.op("dve", lambda e: e.memset(sbf[:], 0.0), writes=[sbk])
        else:
            S.dma("sp", lambda e: e.dma_start(out=s32[:].rearrange("p (h n) -> p h n", h=4),
                                              in_=src.rearrange("(h p) n -> p h n", p=128)), ("hS32", sk[1]), writes=[sk])
            S.op("act", lambda e: e.copy(out=sbf[:], in_=s32[:]), reads=[sk], writes=[sbk])
        return dict(s32=s32, sk=sk, sbf=sbf, sbk=sbk)

    def tile(self, d, t, st, accumulate):
        S, K = self.S, self.K
        tc0 = t * 128
        v3 = lambda x: x[:].rearrange("p (h n) -> p h n", h=4)
        c16 = lambda x: x[:].rearrange("p (c n) -> p c n", c=16)
        lb_bc = _fap(self.lbt, 0 * 8 + d * 4, [[1, 4], [0, 128]])
        oml_bc = _fap(self.lbt, 1 * 8 + d * 4, [[1, 4], [0, 128]])
        sg, sgk = self.sg.next()
        S.op("act", lambda e: e.activation(out=v3(sg), in_=self.z[:, :, tc0:tc0 + 128], func=AF.Sigmoid), reads=self.zk,
             writes=[sgk])
        f, fk = self.f.next()
        S.op("dve", lambda e: e.tensor_tensor(out=v3(f), in0=v3(sg), in1=oml_bc, op=ALU.mult), reads=[sgk, "hlbt"], writes=[fk])
        S.op("dve", lambda e: e.tensor_tensor(out=v3(f), in0=v3(f), in1=lb_bc, op=ALU.add), reads=[fk, "hlbt"], writes=[fk])
        lf, lfk = self.lf.next()
        S.op("act", lambda e: e.activation(out=lf[:], in_=f[:], func=AF.Ln), reads=[fk], writes=[lfk])
        kT, kTk = self.kT.next()
        S.op("dve", lambda e: e.tensor_scalar(out=kT[:], in0=f[:], scalar1=-1.0, scalar2=1.0, op0=ALU.mult, op1=ALU.add),
             reads=[fk], writes=[kTk])
        bT, bTk = self.bT.next()
        S.op("dve", lambda e: e.tensor_tensor_scan(out=bT[:], data0=K.reset4, data1=lf[:], initial=0.0, op0=ALU.mult,
                                                   op1=ALU.add), reads=[lfk], writes=[bTk])
        if d == 0:
            b, bk = bT, bTk
            tcb = _bc_last(c16(b)[:, :, 31:32], 32)
        else:
            tm, tmk = self.tmp.next()
            S.op("dve", lambda e: e.tensor_tensor(out=tm[:], in0=lf[:], in1=bT[:], op=ALU.subtract), reads=[lfk, bTk],
                 writes=[tmk])
            b, bk = self.b2.next()
            S.op("dve", lambda e: e.tensor_tensor(out=c16(b), in0=c16(tm), in1=_bc_last(c16(bT)[:, :, 31:32], 32), op=ALU.add),
                 reads=[tmk, bTk], writes=[bk])
            tcb = _bc_last(c16(b)[:, :, 0:1], 32)
        Eb, Ebk = self.Eb.next()
        S.op("act", lambda e: e.activation(out=Eb[:], in_=b[:], func=AF.Exp), reads=[bk], writes=[Ebk])
        Enb, Enbk = self.Enb.next()
        S.op("act", lambda e: e.activation(out=Enb[:], in_=b[:], func=AF.Exp, scale=-1.0), reads=[bk], writes=[Enbk])
        sq, sqk = self.sq.next()
        S.op("act", lambda e: e.activation(out=v3(sq), in_=self.q[:, :, tc0:tc0 + 128], func=AF.Silu), reads=self.qk,
             writes=[sqk])
        Qtz, Qk = self.Qtz.next()
        qd_all = _fap(Qtz, 0, [[512, 4], [160, 4], [1, 32]])
        S.op("dve", lambda e: e.tensor_tensor(out=qd_all, in0=sq[:].rearrange("p (h c n) -> p h c n", h=4, c=4),
                                              in1=Eb[:].rearrange("p (h c n) -> p h c n", h=4, c=4), op=ALU.mult),
             reads=[sqk, Ebk], writes=[Qk])
        Kt, Ktk = self.Kt.next()
        S.op("dve", lambda e: e.tensor_tensor(out=Kt[:], in0=kT[:], in1=Enb[:], op=ALU.mult), reads=[kTk, Enbk], writes=[Ktk])
        e2, e2k = self.e2.next()
        S.op("dve", lambda e: e.tensor_tensor(out=c16(e2), in0=tcb, in1=c16(b), op=ALU.subtract), reads=[bk], writes=[e2k])
        E2, E2k = self.E2.next()
        S.op("act", lambda e: e.activation(out=E2[:], in_=e2[:], func=AF.Exp), reads=[e2k], writes=[E2k])
        KhT, KhTk = self.KhT.next()
        S.op("dve", lambda e: e.tensor_tensor(out=KhT[:], in0=kT[:], in1=E2[:], op=ALU.mult), reads=[kTk, E2k], writes=[KhTk])
        pkh, pkhk = self.pkh.next()

        def trk(e):
            r = None
            for h in range(4):
                r = e.transpose(out=pkh[:, h * 128:(h + 1) * 128], in_=KhT[:, h * 128:(h + 1) * 128], identity=K.ident_bf)
            return r
        S.op("pe", trk, reads=[KhTk], writes=[pkhk])
        Khs, Khsk = self.Khs.next()
        S.op("act", lambda e: e.copy(out=Khs[:], in_=pkh[:]), reads=[pkhk], writes=[Khsk])
        Khz, Khzk = self.Khz.next()
        rm = K.rowmask
        S.op("dve", lambda e: e.tensor_tensor(
            out=Khz[:].rearrange("p (h c) n -> p h c n", h=4), in0=_fap(Khs, 0, [[128, 4], [0, 4], [1, 128]]),
            in1=bass.AP(tensor=rm.tensor, offset=rm.offset, ap=[list(rm.ap[0]), [0, 4], [1, 4], [0, 128]]), op=ALU.mult),
            reads=[Khsk], writes=[Khzk])
        patt, pattk = self.patt.next()

        def att(e):
            r = None
            for h in range(4):
                r = e.matmul(patt[:, h * 128:(h + 1) * 128], lhsT=Kt[:, h * 128:(h + 1) * 128],
                             rhs=_fap(Qtz, h * 512, [[160, 4], [1, 32]]), start=True, stop=True)
            return r
        S.op("pe", att, reads=[Ktk, Qk], writes=[pattk])
        attm, attmk = self.attm.next()
        mask = K.mask_f if d == 0 else K.mask_b
        S.op("dve", lambda e: e.tensor_tensor(out=v3(attm), in0=v3(patt),
                                              in1=bass.AP(tensor=mask.tensor, offset=mask.offset,
                                                          ap=[list(mask.ap[0]), [0, 4], [1, 128]]), op=ALU.mult),
             reads=[pattk], writes=[attmk])
        order = range(4) if d == 0 else range(3, -1, -1)
        snaps = {}
        vt = self.v
        for c in order:
            snaps[c] = (st["sbf"], st["sbk"])
            pu, puk = self.pu.next()

            def um(e, c=c, pu=pu):
                r = None
                for h in range(4):
                    r = e.matmul(pu[:, h * 128:(h + 1) * 128], lhsT=Khz[:, h * 4 + c, :], rhs=vt[:, t, h * 128:(h + 1) * 128],
                                 start=True, stop=True)
                return r
            S.op("pe", um, reads=[Khzk, "hgv"], writes=[puk])
            col = (32 * c + 31) if d == 0 else (32 * c)
            s32, sk = st["s32"], st["sk"]
            dbc = _fap(Eb, col, [[128, 4], [0, 128]])
            S.op("dve", lambda e, s32=s32, dbc=dbc: e.tensor_tensor(out=v3(s32), in0=v3(s32), in1=dbc, op=ALU.mult),
                 reads=[sk, Ebk], writes=[sk])
            S.op("dve", lambda e, s32=s32, pu=pu: e.tensor_tensor(out=s32[:], in0=s32[:], in1=pu[:], op=ALU.add),
                 reads=[sk, puk], writes=[sk])
            nb, nbk = self.Sb.next()
            S.op("act", lambda e, nb=nb, s32=s32: e.copy(out=nb[:], in_=s32[:]), reads=[sk], writes=[nbk])
            st["sbf"], st["sbk"] = nb, nbk
        po, pok = self.po.next()

        def om(e):
            r = None
            for h in range(4):
                e.matmul(po[:, h * 128:(h + 1) * 128], lhsT=attm[:, h * 128:(h + 1) * 128], rhs=vt[:, t, h * 128:(h + 1) * 128],
                         start=True, stop=False)
                for c in range(4):
                    r = e.matmul(po[:, h * 128:(h + 1) * 128], lhsT=Qtz[:, h * 4 + c, :], rhs=snaps[c][0][:, h * 128:(h + 1) * 128],
                                 start=False, stop=(c == 3))
            return r
        S.op("pe", om, reads=[attmk, "hgv", Qk] + [snaps[c][1] for c in range(4)], writes=[pok])
        dst = self.ostore[:, t, :]
        ok = ("ostore", t)
        if not accumulate:
            S.op("act", lambda e: e.copy(out=dst, in_=po[:]), reads=[pok], writes=[ok])
        else:
            S.op("dve", lambda e: e.tensor_tensor(out=dst, in0=po[:], in1=dst, op=ALU.add), reads=[pok, ok], writes=[ok])


def phase_hg1(S, nc, K, T, l, with_ctx):
    from contextlib import ExitStack
    with ExitStack() as es:
        E = HgEnv(S, nc, K, T, l, es)
        E.load(0)
        st = E.chain_init(None)
        for t in range(NOWN):
            E.tile(0, t, st, accumulate=False)
        s32 = st["s32"]
        S.dma("sp", lambda e: e.dma_start(out=T.hsend[:, :].rearrange("(h p) n -> p h n", p=128),
                                          in_=s32[:].rearrange("p (h n) -> p h n", h=4)), "hsend", reads=[st["sk"]],
              writes=["hsend"])
        if with_ctx:
            E.load(1)
            st = E.chain_init(None)
            for t in (1, 0):
                E.tile(1, t, st, accumulate=True)
        for t in range(NOWN):
            S.dma("sp", lambda e, t=t: e.dma_start(out=T.hostore[t * 128:(t + 1) * 128, :], in_=E.ostore[:, t, :]),
                  ("hos", t % 4), reads=[("ostore", t)], writes=[("hostore", t)])
        S.flush("hg1_%d" % l)


def phase_hg2(S, nc, K, T, l, with_ctx, state_src):
    from contextlib import ExitStack
    with ExitStack() as es:
        sb = lambda name, shape, dt: es.enter_context(nc.sbuf_tensor(_u(name), shape, dt))
        pst = lambda name, shape, dt: es.enter_context(nc.psum_tensor(_u(name), shape, dt))
        E = HgEnv(S, nc, K, T, l, es)
        for t in range(NOWN):
            S.dma("sp", lambda e, t=t: e.dma_start(out=E.ostore[:, t, :], in_=T.hostore[t * 128:(t + 1) * 128, :]),
                  ("hos", t % 4), writes=[("ostore", t)])
        E.load(1)
        st = E.chain_init(None if state_src is None else state_src)
        for t in range(NOWN - 1, 1, -1):
            E.tile(1, t, st, accumulate=True)
        for t in range(NOWN):
            S.dma("sp", lambda e, t=t: e.dma_start(out=T.hostore[t * 128:(t + 1) * 128, :], in_=E.ostore[:, t, :]),
                  ("hos2", t % 4), reads=[("ostore", t)], writes=[("hostore", t)])
        S.flush("hg2_%d" % l)
    with ExitStack() as es:
        sb = lambda name, shape, dt: es.enter_context(nc.sbuf_tensor(_u(name), shape, dt))
        pst = lambda name, shape, dt: es.enter_context(nc.psum_tensor(_u(name), shape, dt))
        orr = Ring("hor", [sb("hor%d" % i, [128, 512], F32) for i in range(3)])
        gt = sb("hgt", [128, NOWN, 512], BF16)
        ngb = sb("hngb", [128, 512], F32)
        stH = sb("hstH", [128, 4, NOWN * 128], BF16)
        S.dma("sp", lambda e: e.dma_start(out=gt[:], in_=T.utok[0:NOWN * 128, 1536:2048].rearrange("(t p) c -> p t c", p=128)),
              "hgt", writes=["hgt"])
        S.dma("sp", lambda e: e.dma_start(out=ngb[:], in_=bcast_row(T.hnorm_g[l:l + 1, :], 512)), "hngb", writes=["hngb"])
        ssb = Ring("hss", [sb("hss%d" % i, [128, 4], F32) for i in range(2)])
        junk = sb("hjunk", [128, 128], F32)
        sgb = Ring("hsgt", [sb("hsgt%d" % i, [128, 512], F32) for i in range(2)])
        onb = Ring("hon", [sb("hon%d" % i, [128, 512], F32) for i in range(2)])
        hgb = Ring("hhg", [sb("hhg%d" % i, [128, 512], BF16) for i in range(2)])
        pT = Ring("hpT", [pst("hpT%d" % i, [128, 512], BF16) for i in range(1)])
        t0 = 0 if with_ctx else 2
        for t in range(t0, NOWN):
            ot, otk = orr.next()
            S.dma("sp", lambda e, ot=ot, t=t: e.dma_start(out=ot[:], in_=T.hostore[t * 128:(t + 1) * 128, :]), ("hor", otk[1]),
                  writes=[otk])
            ss, ssk = ssb.next()
            for h in range(4):
                S.op("act", lambda e, h=h, ss=ss, ot=ot: e.activation(out=junk[:], in_=ot[:, h * 128:(h + 1) * 128],
                                                                    func=AF.Square, accum_out=ss[:, h:h + 1]),
                     reads=[otk], writes=[(ssk, h), "hjunk"])
            allss = [(ssk, h) for h in range(4)]
            S.op("dve", lambda e, ss=ss: e.tensor_scalar(out=ss[:], in0=ss[:], scalar1=1.0 / 128, scalar2=EPS, op0=ALU.mult,
                                                         op1=ALU.add), reads=allss, writes=allss)
            S.op("act", lambda e, ss=ss: e.activation(out=ss[:], in_=ss[:], func=AF.Sqrt), reads=allss, writes=allss)
            S.op("dve", lambda e, ss=ss: e.reciprocal(out=ss[:], in_=ss[:]), reads=allss, writes=allss)
            sgt, sgk = sgb.next()
            S.op("act", lambda e, sgt=sgt, t=t: e.activation(out=sgt[:], in_=gt[:, t, :], func=AF.Silu), reads=["hgt"],
                 writes=[sgk])
            on, onk = onb.next()
            S.op("dve", lambda e, on=on, ss=ss, ot=ot: e.tensor_tensor(
                out=on[:].rearrange("p (h n) -> p h n", h=4), in0=ot[:].rearrange("p (h n) -> p h n", h=4),
                in1=_bc_last(ss[:].rearrange("p (h o) -> p h o", o=1), 128), op=ALU.mult),
                reads=allss + [otk], writes=[onk])
            S.op("dve", lambda e, on=on: e.tensor_tensor(out=on[:], in0=on[:], in1=ngb[:], op=ALU.mult), reads=[onk, "hngb"],
                 writes=[onk])
            hg, hgk = hgb.next()
            S.op("dve", lambda e, on=on, sgt=sgt, hg=hg: e.tensor_tensor(out=hg[:], in0=on[:], in1=sgt[:], op=ALU.mult),
                 reads=[onk, sgk], writes=[hgk])
            p, pk = pT.next()

            def tr(e, p=p, hg=hg):
                r = None
                for c in range(4):
                    r = e.transpose(out=p[:, c * 128:(c + 1) * 128], in_=hg[:, c * 128:(c + 1) * 128], identity=K.ident_bf)
                return r
            S.op("pe", tr, reads=[hgk], writes=[pk])
            S.op("act", lambda e, p=p, t=t: e.copy(out=stH[:, :, t * 128:(t + 1) * 128],
                                                   in_=p[:].rearrange("p (c n) -> p c n", c=4)), reads=[pk],
                 writes=[("stH", t)])
        c0 = t0 * 128
        for c in range(4):
            S.dma("sp", lambda e, c=c: e.dma_start(out=T.mixT[(12 + c) * 128:(13 + c) * 128, c0:NOWN * 128],
                                                   in_=stH[:, c, c0:NOWN * 128]), ("hst", c),
                  reads=[("stH", t) for t in range(t0, NOWN)], writes=[("mixT", 12 + c)])
        S.flush("hg3_%d" % l)


def prep_hgrn(hgrn_lb, hgrn_norm_g, odd):
    lb = hgrn_lb[:, ::-1] if odd else hgrn_lb
    hlb = lb.reshape(DEPTH, 2, 4, 128).transpose(3, 0, 1, 2).reshape(128, DEPTH * 8)
    return np.ascontiguousarray(hlb).astype(np.float32), np.ascontiguousarray(hgrn_norm_g).astype(np.float32)


def load_bc(S, nc, T, l, idx, es, tag):
    sb = lambda name, shape, dt: es.enter_context(nc.sbuf_tensor(_u(name), shape, dt))
    out = {}
    for kind in range(2):
        g = sb(tag + "%d" % kind, [128, D], F32)
        src = T.mod[l * 2 + kind:l * 2 + kind + 1, idx * D:(idx + 1) * D]
        S.dma("sp", lambda e, g=g, src=src: e.dma_start(out=g[:], in_=bcast_row(src, D)), (tag, kind), writes=[(tag, kind)])
        out[kind] = g
    return out


def phase_C(S, nc, K, T, l, with_ctx, xs):
    from contextlib import ExitStack
    with ExitStack() as es:
        sb = lambda name, shape, dt: es.enter_context(nc.sbuf_tensor(_u(name), shape, dt))
        pst = lambda name, shape, dt: es.enter_context(nc.psum_tensor(_u(name), shape, dt))
        mix = sb("mixall", [128, 16, NOWN * 128], BF16)
        for c in range(16):
            S.dma("sp", lambda e, c=c: e.dma_start(out=mix[:, c, :], in_=T.mixT[c * 128:(c + 1) * 128, :]), ("mixl", c % 4),
                  writes=[("mix", c)])
        ga = load_bc(S, nc, T, l, MOD_GA1, es, "ga1")
        wo = Ring("wo", [sb("wo%d" % i, [128, 16, 512], BF16) for i in range(2)])
        xi = Ring("cxi", [sb("cxi%d" % i, [128, 512], F32) for i in range(3)])
        yt = Ring("cyt", [sb("cyt%d" % i, [128, 512], F32) for i in range(3)])
        pp = Ring("cpp", [pst("cpp%d" % i, [128, 512], F32) for i in range(4)])
        t0 = 0 if with_ctx else 2
        for ng in range(4):
            w, wk = wo.next()
            src = T.w_outm[(l * 4 + ng) * 128:(l * 4 + ng + 1) * 128, :]
            S.dma("pool", lambda e, w=w, src=src: e.dma_start(out=w[:].rearrange("p k n -> p (k n)"), in_=src), ("wo", wk[1]),
                  writes=[wk])
            for t in range(t0, NOWN):
                p, pk = pp.next()

                def mm(e, p=p, w=w, t=t):
                    r = None
                    for k in range(16):
                        r = e.matmul(p[:], lhsT=mix[:, k, t * 128:(t + 1) * 128], rhs=w[:, k, :], start=(k == 0), stop=(k == 15))
                    return r
                S.op("pe", mm, reads=[wk] + [("mix", c) for c in range(16)], writes=[pk])
                x, xk = xi.next()
                S.dma("sp", lambda e, x=x, t=t, ng=ng: e.dma_start(out=x[:], in_=xs[t * 128:(t + 1) * 128, ng * 512:(ng + 1) * 512]),
                      ("cxi", xk[1]), writes=[xk])
                y, yk = yt.next()
                g = ga[tile_kind(t)]
                S.op("dve", lambda e, y=y, p=p, g=g, ng=ng: e.tensor_tensor(out=y[:], in0=p[:], in1=g[:, ng * 512:(ng + 1) * 512],
                                                                           op=ALU.mult),
                     reads=[pk, ("ga1", tile_kind(t))], writes=[yk])
                S.op("pool", lambda e, y=y, x=x: e.tensor_tensor(out=y[:], in0=y[:], in1=x[:], op=ALU.add), reads=[yk, xk],
                     writes=[yk])
                S.dma("sp", lambda e, y=y, t=t, ng=ng: e.dma_start(out=T.x1[t * 128:(t + 1) * 128, ng * 512:(ng + 1) * 512], in_=y[:]),
                      ("cyo", yk[1]), reads=[yk], writes=[("x1", t, ng)])
        S.flush("C_%d" % l)


BIG = 1.0e30


def phase_D1(S, nc, K, T, l, with_ctx):
    from contextlib import ExitStack
    with ExitStack() as es:
        sb = lambda name, shape, dt: es.enter_context(nc.sbuf_tensor(_u(name), shape, dt))
        pst = lambda name, shape, dt: es.enter_context(nc.psum_tensor(_u(name), shape, dt))
        h2T = sb("h2T", [128, 16, NOWN * 128], BF16)
        wtall = sb("wtall", [128, NOWN, 32], F32)
        wr = sb("wr", [128, 16, 36], F32)
        br = sb("brb", [128, 36], F32)
        S.dma("sp", lambda e: e.dma_start(out=wr[:].rearrange("p k n -> p (k n)"), in_=T.w_r[l * 128:(l + 1) * 128, :]), "wr",
              writes=["wr"])
        S.dma("sp", lambda e: e.dma_start(out=br[:], in_=bcast_row(T.b_r[l:l + 1, :], 36)), "brb", writes=["brb"])
        AB = load_mod_AB(S, nc, T, l, (MOD_SH2, MOD_S2), T.g_ffn[l:l + 1, :], es, "m2")
        t0 = 0 if with_ctx else 2
        tl = list(range(t0, NOWN))
        tiles = [(t * 128, tile_kind(t)) for t in tl]
        ptr = Ring("rptr", [pst("rptr%d" % i, [128, 512], F32) for i in range(2)])
        pr = Ring("rpr", [pst("rpr%d" % i, [128, 36], F32) for i in range(1)])
        hT32 = Ring("rhT32", [sb("rhT32_%d" % i, [128, 16, 128], F32) for i in range(2)])
        sm = lambda nm, w: Ring(nm, [sb("%s%d" % (nm, i), [128, w], F32) for i in range(2)])
        lgb, st1, gmb, egb, pen, lem, o1b, lem2, o2b = (sm("rlg", 36), sm("rst", 8), sm("rgm", 4), sm("reg", 4), sm("rpen", 4),
                                                        sm("rlem", 32), sm("ro1", 32), sm("rlem2", 32), sm("ro2", 32))

        def router(i, h32, h32k):
            t = tl[i]
            hT, hTk = hT32.next()
            for g in range(4):
                p, pk = ptr.next()

                def tr(e, p=p, g=g):
                    r = None
                    for j in range(4):
                        kk = g * 4 + j
                        r = e.transpose(out=p[:, j * 128:(j + 1) * 128], in_=h32[:, kk * 128:(kk + 1) * 128], identity=K.ident32)
                    return r
                S.op("pe", tr, reads=[h32k], writes=[pk])
                S.op("act", lambda e, p=p, g=g: e.copy(out=hT[:, g * 4:(g + 1) * 4, :], in_=p[:].rearrange("p (j n) -> p j n", j=4)),
                     reads=[pk], writes=[(hTk, g)])
            p, pk = pr.next()

            def mm(e, p=p):
                r = None
                for k in range(16):
                    r = e.matmul(p[:], lhsT=hT[:, k, :], rhs=wr[:, k, :], start=(k == 0), stop=(k == 15))
                return r
            S.op("pe", mm, reads=[(hTk, g) for g in range(4)] + ["wr"], writes=[pk])
            lg, lgk = lgb.next()
            S.op("dve", lambda e: e.tensor_tensor(out=lg[:], in0=p[:], in1=br[:], op=ALU.add), reads=[pk, "brb"], writes=[lgk])
            s, sk = st1.next()
            S.op("dve", lambda e: e.reduce_max(out=s[:, 0:1], in_=lg[:, 0:4], axis=AX.X), reads=[lgk], writes=[sk])
            S.op("dve", lambda e: e.tensor_scalar(out=s[:, 1:2], in0=s[:, 0:1], scalar1=-1.0, scalar2=None, op0=ALU.mult),
                 reads=[sk], writes=[sk])
            gm, gmk = gmb.next()
            S.op("dve", lambda e: e.tensor_scalar(out=gm[:], in0=lg[:, 0:4], scalar1=s[:, 0:1], scalar2=None, op0=ALU.is_equal),
                 reads=[lgk, sk], writes=[gmk])
            eg, egk = egb.next()
            S.op("act", lambda e: e.activation(out=eg[:], in_=lg[:, 0:4], func=AF.Exp, bias=s[:, 1:2], accum_out=s[:, 2:3]),
                 reads=[lgk, sk], writes=[egk, sk])
            pn, pnk = pen.next()
            S.op("dve", lambda e: e.tensor_scalar(out=pn[:], in0=gm[:], scalar1=BIG, scalar2=-BIG, op0=ALU.mult, op1=ALU.add),
                 reads=[gmk], writes=[pnk])
            le, lek = lem.next()
            S.op("dve", lambda e: e.tensor_tensor(out=le[:].rearrange("p (g n) -> p g n", g=4),
                                                  in0=lg[:, 4:36].rearrange("p (g n) -> p g n", g=4),
                                                  in1=_bc_last(pn[:].rearrange("p (g o) -> p g o", o=1), 8), op=ALU.add),
                 reads=[lgk, pnk], writes=[lek])
            S.op("dve", lambda e: e.reduce_max(out=s[:, 3:4], in_=le[:], axis=AX.X), reads=[lek, sk], writes=[sk])
            o1, o1k = o1b.next()
            S.op("dve", lambda e: e.tensor_scalar(out=o1[:], in0=le[:], scalar1=s[:, 3:4], scalar2=None, op0=ALU.is_equal),
                 reads=[lek, sk], writes=[o1k])
            l2, l2k = lem2.next()
            S.op("dve", lambda e: e.scalar_tensor_tensor(out=l2[:], in0=o1[:], scalar=-BIG, in1=le[:], op0=ALU.mult, op1=ALU.add),
                 reads=[o1k, lek], writes=[l2k])
            S.op("dve", lambda e: e.reduce_max(out=s[:, 4:5], in_=l2[:], axis=AX.X), reads=[l2k, sk], writes=[sk])
            o2, o2k = o2b.next()
            S.op("dve", lambda e: e.tensor_scalar(out=o2[:], in0=l2[:], scalar1=s[:, 4:5], scalar2=None, op0=ALU.is_equal),
                 reads=[l2k, sk], writes=[o2k])
            S.op("dve", lambda e: e.tensor_tensor(out=s[:, 5:6], in0=s[:, 4:5], in1=s[:, 3:4], op=ALU.subtract), reads=[sk],
                 writes=[sk])
            S.op("act", lambda e: e.activation(out=s[:, 6:7], in_=s[:, 5:6], func=AF.Exp), reads=[sk], writes=[sk])
            S.op("dve", lambda e: e.scalar_tensor_tensor(out=s[:, 7:8], in0=s[:, 6:7], scalar=1.0, in1=s[:, 2:3], op0=ALU.add,
                                                         op1=ALU.mult), reads=[sk], writes=[sk])
            S.op("dve", lambda e: e.reciprocal(out=s[:, 7:8], in_=s[:, 7:8]), reads=[sk], writes=[sk])
            S.op("dve", lambda e: e.tensor_tensor(out=s[:, 6:7], in0=s[:, 6:7], in1=s[:, 7:8], op=ALU.mult), reads=[sk], writes=[sk])
            S.op("dve", lambda e: e.tensor_scalar(out=wtall[:, t, :], in0=o1[:], scalar1=s[:, 7:8], scalar2=None, op0=ALU.mult),
                 reads=[o1k, sk], writes=[("wt", t)])
            S.op("dve", lambda e: e.scalar_tensor_tensor(out=wtall[:, t, :], in0=o2[:], scalar=s[:, 6:7], in1=wtall[:, t, :],
                                                         op0=ALU.mult, op1=ALU.add), reads=[o2k, sk, ("wt", t)], writes=[("wt", t)])

        emit_norm(S, nc, K, tiles, lambda i: T.x1[tl[i] * 128:(tl[i] + 1) * 128, :], AB, h2T, es, want32=router)
        for c in range(16):
            S.dma("sp", lambda e, c=c: e.dma_start(out=T.h2T[c * 128:(c + 1) * 128, :], in_=h2T[:, c, :]), ("h2o", c % 4),
                  reads=[("hT", t, half) for t in tl for half in range(2)], writes=[("h2Td", c)])
        S.dma("sp", lambda e: e.dma_start(out=T.wt[:, :], in_=wtall[:].rearrange("p t n -> p (t n)")), "wto",
              reads=[("wt", t) for t in tl], writes=["wtd"])
        S.flush("D1_%d" % l)


def prep_misc(w_out, w_rg, b_rg, w_re, b_re):
    wo = np.empty((DEPTH, 4, 128, 16 * 512), np.float32)
    for l in range(DEPTH):
        for g in range(4):
            wo[l, g] = w_out[l][:, g * 512:(g + 1) * 512].reshape(16, 128, 512).transpose(1, 0, 2).reshape(128, -1)
    wr = np.concatenate([w_rg, w_re], axis=-1)
    wr = wr.reshape(DEPTH, 16, 128, 36).transpose(0, 2, 1, 3).reshape(DEPTH * 128, 16 * 36)
    br = np.concatenate([b_rg, b_re], axis=-1)
    return wo.reshape(-1, 16 * 512), np.ascontiguousarray(wr).astype(np.float32), np.ascontiguousarray(br).astype(np.float32)


def phase_D2(S, nc, K, T, l, with_ctx, nexp=NEXP):
    from contextlib import ExitStack
    t0 = 0 if with_ctx else 2
    tl = list(range(t0, NOWN))
    parts = [tl[i:i + 6] for i in range(0, len(tl), 6)]
    with ExitStack() as es:
        sb = lambda name, shape, dt: es.enter_context(nc.sbuf_tensor(_u(name), shape, dt))
        pst = lambda name, shape, dt: es.enter_context(nc.psum_tensor(_u(name), shape, dt))
        wt = sb("wtl", [128, NOWN, 32], F32)
        S.dma("sp", lambda e: e.dma_start(out=wt[:].rearrange("p t n -> p (t n)"), in_=T.wt[:, :]), "wtl", writes=["wtl"])
        h2 = sb("h2p", [128, 16, 768], BF16)
        acc = sb("facc", [128, 6, D], F32)
        w1r = Ring("w1", [sb("w1_%d" % i, [128, 16, 512], BF16) for i in range(2)])
        w3r = Ring("w3", [sb("w3_%d" % i, [128, 16, 512], BF16) for i in range(2)])
        w2r = Ring("w2", [sb("w2_%d" % i, [128, 4, D], BF16) for i in range(2)])
        aTr = Ring("aT", [sb("aT_%d" % i, [128, 4, 512], BF16) for i in range(2)])
        sgr = Ring("sgm", [sb("sgm_%d" % i, [128, 512], F32) for i in range(2)])
        pg = Ring("pg", [pst("pg%d" % i, [128, 512], F32) for i in range(2)])
        pu = Ring("pu", [pst("pu%d" % i, [128, 512], F32) for i in range(2)])
        po = Ring("po", [pst("po%d" % i, [128, 512], F32) for i in range(3)])
        for pi, part in enumerate(parts):
            ntp = len(part)
            c0 = part[0] * 128
            S.dma("sp", lambda e, c0=c0, ntp=ntp: e.dma_start(
                out=h2[:, :, 0:ntp * 128], in_=T.h2T[:, c0:c0 + ntp * 128].rearrange("(c p) n -> p c n", p=128)),
                "h2p", writes=["h2p"])
            S.op("dve", lambda e: e.memset(acc[:], 0.0), writes=[("acc", i) for i in range(6)])
            groups = [(0, min(4, ntp))] + ([(4, ntp - 4)] if ntp > 4 else [])
            for ex in range(nexp):
                w1, w1k = w1r.next()
                w3, w3k = w3r.next()
                w2, w2k = w2r.next()
                r0 = (l * nexp + ex) * 128
                S.dma("pool", lambda e, w1=w1, r0=r0: e.dma_start(out=w1[:].rearrange("p k n -> p (k n)"),
                                                                in_=T.w1m[r0:r0 + 128, :]), ("w1", w1k[1]), writes=[w1k])
                S.dma("pool", lambda e, w3=w3, r0=r0: e.dma_start(out=w3[:].rearrange("p k n -> p (k n)"),
                                                                in_=T.w3m[r0:r0 + 128, :]), ("w3", w3k[1]), writes=[w3k])
                S.dma("pool", lambda e, w2=w2, r0=r0: e.dma_start(out=w2[:].rearrange("p k n -> p (k n)"),
                                                                in_=T.w2m[r0:r0 + 128, :]), ("w2", w2k[1]), writes=[w2k])
                for (g0, gn) in groups:
                    n = gn * 128
                    cs = g0 * 128
                    aT, aTk = aTr.next()
                    for j in range(4):
                        p1, p1k = pg.next()
                        p3, p3k = pu.next()

                        def mm(e, p=p1, w=w1, j=j, cs=cs, n=n):
                            r = None
                            for k in range(16):
                                r = e.matmul(p[:, 0:n], lhsT=w[:, k, j * 128:(j + 1) * 128], rhs=h2[:, k, cs:cs + n],
                                             start=(k == 0), stop=(k == 15))
                            return r
                        S.op("pe", mm, reads=[w1k, "h2p"], writes=[p1k])

                        def mm3(e, p=p3, w=w3, j=j, cs=cs, n=n):
                            r = None
                            for k in range(16):
                                r = e.matmul(p[:, 0:n], lhsT=w[:, k, j * 128:(j + 1) * 128], rhs=h2[:, k, cs:cs + n],
                                             start=(k == 0), stop=(k == 15))
                            return r
                        S.op("pe", mm3, reads=[w3k, "h2p"], writes=[p3k])
                        sg, sgk = sgr.next()
                        S.op("act", lambda e, sg=sg, p1=p1, n=n: e.activation(out=sg[:, 0:n], in_=p1[:, 0:n], func=AF.Silu),
                             reads=[p1k], writes=[sgk])
                        S.op("dve", lambda e, aT=aT, sg=sg, p3=p3, j=j, n=n: e.tensor_tensor(
                            out=aT[:, j, 0:n], in0=p3[:, 0:n], in1=sg[:, 0:n], op=ALU.mult), reads=[p3k, sgk], writes=[(aTk, j)])
                    for ti in range(gn):
                        tix = g0 + ti
                        t = part[tix]
                        for nn in range(4):
                            p, pk = po.next()

                            def mmo(e, p=p, aT=aT, w2=w2, ti=ti, nn=nn):
                                r = None
                                for j in range(4):
                                    r = e.matmul(p[:], lhsT=aT[:, j, ti * 128:(ti + 1) * 128], rhs=w2[:, j, nn * 512:(nn + 1) * 512],
                                                 start=(j == 0), stop=(j == 3))
                                return r
                            S.op("pe", mmo, reads=[(aTk, j) for j in range(4)] + [w2k], writes=[pk])
                            dst = acc[:, tix, nn * 512:(nn + 1) * 512]
                            S.op("dve", lambda e, p=p, dst=dst, t=t, ex=ex: e.scalar_tensor_tensor(
                                out=dst, in0=p[:], scalar=wt[:, t, ex:ex + 1], in1=dst, op0=ALU.mult, op1=ALU.add),
                                reads=[pk, "wtl", ("acc", tix)], writes=[("acc", tix)])
            for tix, t in enumerate(part):
                S.dma("sp", lambda e, tix=tix, t=t: e.dma_start(out=T.facc[t * 128:(t + 1) * 128, :], in_=acc[:, tix, :]),
                      ("fao", tix), reads=[("acc", tix)], writes=[("faccd", t)])
        S.flush("D2_%d" % l)


def phase_D3(S, nc, K, T, l, with_ctx, last, sparse=False):
    from contextlib import ExitStack
    t0 = 0 if with_ctx else 2
    with ExitStack() as es:
        sb = lambda name, shape, dt: es.enter_context(nc.sbuf_tensor(_u(name), shape, dt))
        ga = load_bc(S, nc, T, l, MOD_GA2, es, "ga2")
        if last:
            gf = sb("gfin", [128, D], F32)
            S.dma("sp", lambda e: e.dma_start(out=gf[:], in_=bcast_row(T.g_final[0:1, :], D)), "gfin", writes=["gfin"])
            jb = sb("d3junk", [128, D], BF16)
            st = Ring("d3st", [sb("d3st%d" % i, [128, 2], F32) for i in range(2)])
        xb = Ring("d3x", [sb("d3x%d" % i, [128, D], F32) for i in range(2)])
        fb = Ring("d3f", [sb("d3f%d" % i, [128, D], F32) for i in range(2)])
        if sparse:
            y2r = Ring("d3y2", [sb("d3y2_%d" % i, [128, D], F32) for i in range(2)])
            slu = sb("slul", [128, NOWN, 2], U32)
            aa = sb("aal", [128, NOWN, 2], F32)
            S.dma("sp", lambda e: e.dma_start(out=slu[:].rearrange("p t k -> p (t k)"), in_=T.slotsu[:, :]), "slul", writes=["slul"])
            S.dma("sp", lambda e: e.dma_start(out=aa[:].rearrange("p t k -> p (t k)"), in_=T.aall[:, :]), "aal", writes=["aal"])
        for t in range(t0, NOWN):
            x, xk = xb.next()
            f, fk = fb.next()
            S.dma("sp", lambda e, x=x, t=t: e.dma_start(out=x[:], in_=T.x1[t * 128:(t + 1) * 128, :]), ("d3x", xk[1]), writes=[xk])
            if not sparse:
                S.dma("sp", lambda e, f=f, t=t: e.dma_start(out=f[:], in_=T.facc[t * 128:(t + 1) * 128, :]), ("d3f", fk[1]),
                      writes=[fk])
            else:
                y2, y2k = y2r.next()
                S.dma("pool", lambda e, f=f, t=t: e.indirect_dma_start(out=f[:], out_offset=None, in_=T.ysd[:, :],
                                                                      in_offset=bass.IndirectOffsetOnAxis(ap=slu[:, t, 0:1], axis=0)),
                      ("d3f", fk[1]), reads=["slul"], writes=[fk])
                S.dma("pool", lambda e, y2=y2, t=t: e.indirect_dma_start(out=y2[:], out_offset=None, in_=T.ysd[:, :],
                                                                        in_offset=bass.IndirectOffsetOnAxis(ap=slu[:, t, 1:2], axis=0)),
                      ("d3y2", y2k[1]), reads=["slul"], writes=[y2k])
                S.op("dve", lambda e, f=f, t=t: e.tensor_scalar(out=f[:], in0=f[:], scalar1=aa[:, t, 0:1], scalar2=None, op0=ALU.mult),
                     reads=[fk, "aal"], writes=[fk])
                S.op("dve", lambda e, f=f, y2=y2, t=t: e.scalar_tensor_tensor(out=f[:], in0=y2[:], scalar=aa[:, t, 1:2], in1=f[:],
                                                                              op0=ALU.mult, op1=ALU.add),
                     reads=[fk, y2k, "aal"], writes=[fk])
            g = ga[tile_kind(t)]
            S.op("dve", lambda e, f=f, g=g: e.tensor_tensor(out=f[:], in0=f[:], in1=g[:], op=ALU.mult),
                 reads=[fk, ("ga2", tile_kind(t))], writes=[fk])
            S.op("pool", lambda e, f=f, x=x: e.tensor_tensor(out=f[:], in0=f[:], in1=x[:], op=ALU.add), reads=[fk, xk], writes=[fk])
            if not last:
                S.dma("sp", lambda e, f=f, t=t: e.dma_start(out=T.xs2[t * 128:(t + 1) * 128, :], in_=f[:]), ("d3o", fk[1]),
                      reads=[fk], writes=[("xs2", t)])
                if t >= 16:
                    S.dma("sp", lambda e, f=f, t=t: e.dma_start(out=T.xsend[(t - 16) * 128:(t - 15) * 128, :], in_=f[:]),
                          ("d3o2", fk[1]), reads=[fk], writes=[("xsend", t)])
            else:
                s, sk = st.next()
                S.op("act", lambda e, f=f, s=s: e.activation(out=jb[:], in_=f[:], func=AF.Square, accum_out=s[:, 0:1]),
                     reads=[fk], writes=[sk, "d3junk"])
                S.op("dve", lambda e, s=s: e.tensor_scalar(out=s[:, 1:2], in0=s[:, 0:1], scalar1=1.0 / D, scalar2=EPS,
                                                           op0=ALU.mult, op1=ALU.add), reads=[sk], writes=[sk])
                S.op("act", lambda e, s=s: e.activation(out=s[:, 1:2], in_=s[:, 1:2], func=AF.Sqrt), reads=[sk], writes=[sk])
                S.op("dve", lambda e, s=s: e.reciprocal(out=s[:, 1:2], in_=s[:, 1:2]), reads=[sk], writes=[sk])
                S.op("dve", lambda e, f=f, s=s: e.scalar_tensor_tensor(out=f[:], in0=f[:], scalar=s[:, 1:2], in1=gf[:],
                                                                       op0=ALU.mult, op1=ALU.mult),
                     reads=[fk, sk, "gfin"], writes=[fk])
                S.dma("sp", lambda e, f=f, t=t: e.dma_start(out=T.out[(t - 2) * 128:(t - 1) * 128, :], in_=f[:]), ("d3o", fk[1]),
                      reads=[fk], writes=[("out", t)])
        S.flush("D3_%d" % l)


PAIRS = [[0, 1], [2, 3], [4, 5], [6, 7]]


def coll(S, fn, key, reads, writes):
    return S._emit("pool", fn, S.dsem(key), 1, reads, writes)


def phase_xchg_x(S, nc, K, T):
    from contextlib import ExitStack
    with ExitStack() as es:
        sb = lambda name, shape, dt: es.enter_context(nc.sbuf_tensor(_u(name), shape, dt))
        pst = lambda name, shape, dt: es.enter_context(nc.psum_tensor(_u(name), shape, dt))
        coll(S, lambda e: e.collective_compute("AllGather", ALU.bypass, replica_groups=PAIRS, ins=[T.xsend.opt()],
                                               outs=[T.xrecv.opt()]), "ccx", [], ["xrecv"])
        pf = sb("pflag", [128, 2], F32)
        S.dma("sp", lambda e: e.dma_start(out=pf[:], in_=T.pflag[:, :]), "pflag", writes=["pflag"])
        s0r = Ring("xs0", [sb("xs0_%d" % i, [128, D], F32) for i in range(2)])
        s1r = Ring("xs1", [sb("xs1_%d" % i, [128, D], F32) for i in range(2)])
        pp = Ring("xpp", [pst("xpp%d" % i, [128, 512], F32) for i in range(4)])
        for (ht, pr0) in ((18, 128), (19, 0)):
            a, ak = s0r.next()
            b, bk = s1r.next()
            S.dma("sp", lambda e, a=a, pr0=pr0: e.dma_start(out=a[:], in_=T.xrecv[pr0:pr0 + 128, :]), ("xs0", ak[1]),
                  reads=["xrecv"], writes=[ak])
            S.dma("sp", lambda e, b=b, pr0=pr0: e.dma_start(out=b[:], in_=T.xrecv[256 + pr0:256 + pr0 + 128, :]), ("xs1", bk[1]),
                  reads=["xrecv"], writes=[bk])
            S.op("dve", lambda e, a=a: e.tensor_scalar(out=a[:], in0=a[:], scalar1=pf[:, 0:1], scalar2=None, op0=ALU.mult),
                 reads=[ak, "pflag"], writes=[ak])
            S.op("dve", lambda e, a=a, b=b: e.scalar_tensor_tensor(out=a[:], in0=b[:], scalar=pf[:, 1:2], in1=a[:], op0=ALU.mult,
                                                                   op1=ALU.add), reads=[ak, bk, "pflag"], writes=[ak])
            for nn in range(4):
                p, pk = pp.next()
                S.op("pe", lambda e, p=p, a=a, nn=nn: e.matmul(p[:], lhsT=K.anti32, rhs=a[:, nn * 512:(nn + 1) * 512], start=True,
                                                              stop=True), reads=[ak], writes=[pk])
                S.op("act", lambda e, p=p, b=b, nn=nn: e.copy(out=b[:, nn * 512:(nn + 1) * 512], in_=p[:]), reads=[pk, bk],
                     writes=[(bk, nn)])
            S.dma("sp", lambda e, b=b, ht=ht: e.dma_start(out=T.xs2[ht * 128:(ht + 1) * 128, :], in_=b[:]), ("xso", bk[1]),
                  reads=[(bk, nn) for nn in range(4)], writes=[("xs2", ht)])
        S.flush("xchg_x")


def phase_xchg_h(S, nc, K, T):
    from contextlib import ExitStack
    with ExitStack() as es:
        sb = lambda name, shape, dt: es.enter_context(nc.sbuf_tensor(_u(name), shape, dt))
        coll(S, lambda e: e.collective_compute("AllGather", ALU.bypass, replica_groups=PAIRS, ins=[T.hsend.opt()],
                                               outs=[T.hrecv2.opt()]), "cch", [], ["hrecv2"])
        pf = sb("pflagh", [128, 2], F32)
        S.dma("sp", lambda e: e.dma_start(out=pf[:], in_=T.pflag[:, :]), "pflagh", writes=["pflagh"])
        a = sb("hxa", [128, 4, 128], F32)
        b = sb("hxb", [128, 4, 128], F32)
        S.dma("sp", lambda e: e.dma_start(out=a[:], in_=T.hrecv2[0:512, :].rearrange("(h p) n -> p h n", p=128)), "hxa",
              reads=["hrecv2"], writes=["hxa"])
        S.dma("sp", lambda e: e.dma_start(out=b[:], in_=T.hrecv2[512:1024, :].rearrange("(h p) n -> p h n", p=128)), "hxb",
              reads=["hrecv2"], writes=["hxb"])
        S.op("dve", lambda e: e.tensor_scalar(out=a[:], in0=a[:], scalar1=pf[:, 0:1], scalar2=None, op0=ALU.mult),
             reads=["hxa", "pflagh"], writes=["hxa"])
        S.op("dve", lambda e: e.scalar_tensor_tensor(out=a[:], in0=b[:], scalar=pf[:, 1:2], in1=a[:], op0=ALU.mult, op1=ALU.add),
             reads=["hxa", "hxb", "pflagh"], writes=["hxa"])
        S.dma("sp", lambda e: e.dma_start(out=T.hrecv[:, :].rearrange("(h p) n -> p h n", p=128), in_=a[:]), "hxo",
              reads=["hxa"], writes=["hrecv"])
        S.flush("xchg_h")


def phase_mod(S, nc, K, T):
    from contextlib import ExitStack
    with ExitStack() as es:
        sb = lambda name, shape, dt: es.enter_context(nc.sbuf_tensor(_u(name), shape, dt))
        pst = lambda name, shape, dt: es.enter_context(nc.psum_tensor(_u(name), shape, dt))
        cT = sb("cT", [128, 16, 5], F32)
        S.dma("sp", lambda e: e.dma_start(out=cT[:].rearrange("p k r -> p (k r)"), in_=T.cT[:, :]), "cT", writes=["cT"])
        S.op("act", lambda e: e.activation(out=cT[:], in_=cT[:], func=AF.Silu), reads=["cT"], writes=["cT"])
        ba = sb("bada", [1, DEPTH * 1536], F32)
        S.dma("sp", lambda e: e.dma_start(out=ba[:], in_=T.b_ada[0:1, :]), "bada", writes=["bada"])
        wa = sb("wada", [128, 16, 1536], F32)
        ml = sb("modloc", [5, DEPTH * 1536], F32)
        pp = Ring("mpp", [pst("mpp%d" % i, [128, 512], F32) for i in range(3)])
        for l in range(DEPTH):
            S.dma("sp", lambda e, l=l: e.dma_start(out=wa[:].rearrange("p k n -> p (k n)"), in_=T.w_adam[l * 128:(l + 1) * 128, :]),
                  "wada", writes=["wada"])
            for nn in range(3):
                p, pk = pp.next()

                def mm(e, p=p, nn=nn, l=l):
                    for k in range(16):
                        e.matmul(p[0:5, :], lhsT=cT[:, k, :], rhs=wa[:, k, nn * 512:(nn + 1) * 512], start=(k == 0), stop=False)
                    return e.matmul(p[0:5, :], lhsT=K.ones32[0:1, 0:5], rhs=ba[0:1, l * 1536 + nn * 512:l * 1536 + (nn + 1) * 512],
                                    start=False, stop=True)
                S.op("pe", mm, reads=["cT", "wada", "bada"], writes=[pk])
                S.op("act", lambda e, p=p, nn=nn, l=l: e.copy(out=ml[:, l * 1536 + nn * 512:l * 1536 + (nn + 1) * 512], in_=p[0:5, :]),
                     reads=[pk], writes=[("ml", l, nn)])
        allml = [("ml", l, nn) for l in range(DEPTH) for nn in range(3)]
        S.dma("sp", lambda e: e.dma_start(out=T.msend[:, :], in_=ml[:]), "mso", reads=allml, writes=["msend"])
        coll(S, lambda e: e.collective_compute("AllGather", ALU.bypass, replica_groups=[list(range(8))], ins=[T.msend.opt()],
                                               outs=[T.mrecv.opt()]), "ccm", ["msend"], ["mrecv"])
        S.flush("mod1")
    with ExitStack() as es:
        sb = lambda name, shape, dt: es.enter_context(nc.sbuf_tensor(_u(name), shape, dt))
        pst = lambda name, shape, dt: es.enter_context(nc.psum_tensor(_u(name), shape, dt))
        pp = Ring("mpp2", [pst("mpp2_%d" % i, [128, 512], F32) for i in range(3)])
        G = sb("modG", [5, 8, DEPTH * 1536], F32)
        S.dma("sp", lambda e: e.dma_start(out=G[:], in_=T.mrecv[:, :].rearrange("(r f) n -> f r n", f=5)), "modG",
              reads=["mrecv"], writes=["modG"])
        bs = sb("bsel", [5, 2], F32)
        S.dma("sp", lambda e: e.dma_start(out=bs[:], in_=T.bsel[:, :]), "bsel", writes=["bsel"])
        ms = sb("modsel", [2, DEPTH, 8 * 1536], F32)
        for l in range(DEPTH):
            for r in range(8):
                for nn in range(3):
                    p, pk = pp.next()
                    S.op("pe", lambda e, p=p, l=l, r=r, nn=nn: e.matmul(
                        p[0:2, :], lhsT=bs[:, :], rhs=G[:, r, l * 1536 + nn * 512:l * 1536 + (nn + 1) * 512], start=True, stop=True),
                        reads=["modG", "bsel"], writes=[pk])
                    dst = ms[:, l, r * 1536 + nn * 512:r * 1536 + (nn + 1) * 512]
                    if (r + nn) % 2 == 0:
                        S.op("act", lambda e, p=p, dst=dst: e.copy(out=dst, in_=p[0:2, :]), reads=[pk], writes=[("ms", l, r, nn)])
                    else:
                        S.op("dve", lambda e, p=p, dst=dst: e.tensor_copy(out=dst, in_=p[0:2, :]), reads=[pk],
                             writes=[("ms", l, r, nn)])
        allms = [("ms", l, r, nn) for l in range(DEPTH) for r in range(8) for nn in range(3)]
        S.dma("sp", lambda e: e.dma_start(out=T.mod[:, :].rearrange("(l k) n -> k l n", k=2), in_=ms[:]), "mso2", reads=allms,
              writes=["mod"])
        S.flush("mod")


def prep_experts(wg, wu, wd):
    ne = wg.shape[1]
    w1 = np.ascontiguousarray(wg.reshape(DEPTH * ne, 16, 128, 512).transpose(0, 2, 1, 3)).reshape(-1, 16 * 512)
    w3 = np.ascontiguousarray(wu.reshape(DEPTH * ne, 16, 128, 512).transpose(0, 2, 1, 3)).reshape(-1, 16 * 512)
    w2 = np.ascontiguousarray(wd.reshape(DEPTH * ne, 4, 128, D).transpose(0, 2, 1, 3)).reshape(-1, 4 * D)
    return w1, w3, w2


_NC_CACHE = {}


def make_in_maps(x, c, ctx, c_ctx, w_ada, b_ada, g_mix, g_ffn, w_in, conv_w, conv_b, conv_ln_g, conv_ln_b, na_rpb,
                 hgrn_lb, hgrn_norm_g, w_out, w_router_group, b_router_group, w_router_expert, b_router_expert,
                 w_exp_gate, w_exp_up, w_exp_down, g_final):
    f = lambda a: np.ascontiguousarray(np.asarray(a, dtype=np.float32))
    x, c, ctx, c_ctx, w_ada, b_ada = f(x), f(c), f(ctx), f(c_ctx), f(w_ada), f(b_ada)
    w_in = f(w_in)
    consts = host_consts()
    shared = {}
    for odd in (False, True):
        wf, wt = prep_w_in(w_in, odd)
        cwT, cvec = prep_conv(f(conv_w), f(conv_b), f(conv_ln_g), f(conv_ln_b), odd)
        nab = prep_na_bias(f(na_rpb), odd)
        hlb, hng = prep_hgrn(f(hgrn_lb), f(hgrn_norm_g), odd)
        shared[odd] = dict(w_feat=wf, w_tokm=wt, conv_wT=cwT, conv_vec=cvec, na_bias=nab, hlb=hlb, hnorm_g=hng)
    wom, wrm, brm = prep_misc(f(w_out), f(w_router_group), f(b_router_group), f(w_router_expert), f(b_router_expert))
    w1m, w3m, w2m = prep_experts_sparse(f(w_exp_gate), f(w_exp_up), f(w_exp_down))
    C = np.concatenate([c, c_ctx[None, :]], axis=0)
    cT = np.ascontiguousarray(C.reshape(5, 16, 128).transpose(2, 1, 0)).reshape(128, 80)
    in_maps = []
    for core in range(8):
        b, s = core // 2, core % 2
        odd = s == 1
        m = dict(shared[odd])
        m["x_in"] = core_tokens(x[b], ctx[b], odd)
        m["consts"] = consts
        m["g_mix"] = f(g_mix)
        m["g_ffn"] = f(g_ffn)
        m["g_final"] = f(g_final).reshape(1, D)
        m["w_outm"] = wom
        m["w_r"] = wrm
        m["b_r"] = brm
        m["w1m"], m["w3m"], m["w2m"] = w1m, w3m, w2m
        m["cT"] = cT
        sl = slice(core * 1536, (core + 1) * 1536)
        wa = np.empty((DEPTH, 128, 16 * 1536), np.float32)
        for l in range(DEPTH):
            wa[l] = w_ada[l][:, sl].reshape(16, 128, 1536).transpose(1, 0, 2).reshape(128, -1)
        m["w_adam"] = wa.reshape(DEPTH * 128, 16 * 1536)
        m["b_ada"] = np.ascontiguousarray(np.concatenate([b_ada[l][sl] for l in range(DEPTH)])[None, :])
        bs = np.zeros((5, 2), np.float32)
        bs[b, 0] = 1.0
        bs[4, 1] = 1.0
        m["bsel"] = bs
        pf = np.zeros((128, 2), np.float32)
        pf[:, 0 if odd else 1] = 1.0
        m["pflag"] = pf
        in_maps.append(m)
    return in_maps


def kernel(**inputs):
    in_maps = make_in_maps(**inputs)
    if "nc" not in _NC_CACHE:
        _NC_CACHE["nc"] = build({})
    nc = _NC_CACHE["nc"]
    res = run_bass_kernel_spmd(nc, in_maps, core_ids=list(range(8)))
    out = np.empty((4, 4096, D), np.float32)
    for core in range(8):
        b, s = core // 2, core % 2
        o = np.asarray(res.results[core]["out"], dtype=np.float32)
        if s == 0:
            out[b, 0:2048] = o
        else:
            out[b, 2048:4096] = o[::-1]
    return out


U32 = mybir.dt.uint32
_NE = [NEXP]


def RWv():
    return DEPTH * _NE[0] * 128
OOB = 1.0e6


BLK = 256


def nblocks(with_ctx):
    ntok = (NOWN if with_ctx else 16) * 128
    return (2 * ntok) // BLK + NEXP


def phase_D1s(S, nc, K, T, l, with_ctx):
    from contextlib import ExitStack
    NB = nblocks(with_ctx)
    with ExitStack() as es:
        sb = lambda name, shape, dt: es.enter_context(nc.sbuf_tensor(_u(name), shape, dt))
        pst = lambda name, shape, dt: es.enter_context(nc.psum_tensor(_u(name), shape, dt))
        h2 = sb("h2tok", [128, NOWN, D], BF16)
        o1all = sb("o1all", [128, NOWN, 32], F32)
        o2all = sb("o2all", [128, NOWN, 32], F32)
        aall = sb("aall", [128, NOWN, 2], F32)
        wr = sb("wr", [128, 16, 36], F32)
        br = sb("brb", [128, 36], F32)
        S.dma("sp", lambda e: e.dma_start(out=wr[:].rearrange("p k n -> p (k n)"), in_=T.w_r[l * 128:(l + 1) * 128, :]), "wr",
              writes=["wr"])
        S.dma("sp", lambda e: e.dma_start(out=br[:], in_=bcast_row(T.b_r[l:l + 1, :], 36)), "brb", writes=["brb"])
        t0 = 0 if with_ctx else 2
        tl = list(range(t0, NOWN))
        nt = len(tl)
        with ExitStack() as es2:
            sb2 = lambda name, shape, dt: es2.enter_context(nc.sbuf_tensor(_u(name), shape, dt))
            pst2 = lambda name, shape, dt: es2.enter_context(nc.psum_tensor(_u(name), shape, dt))
            AB = load_mod_AB(S, nc, T, l, (MOD_SH2, MOD_S2), T.g_ffn[l:l + 1, :], es2, "m2")
            ptr = Ring("rptr", [pst2("rptr%d" % i, [128, 512], F32) for i in range(2)])
            pr = Ring("rpr", [pst2("rpr%d" % i, [128, 36], F32) for i in range(1)])
            hT32 = Ring("rhT32", [sb2("rhT32_%d" % i, [128, 16, 128], F32) for i in range(2)])
            sm = lambda nm, w: Ring(nm, [sb2("%s%d" % (nm, i), [128, w], F32) for i in range(2)])
            lgb, st1, gmb, egb, pen, lem, lem2 = (sm("rlg", 36), sm("rst", 8), sm("rgm", 4), sm("reg", 4), sm("rpen", 4),
                                                  sm("rlem", 32), sm("rlem2", 32))

            def router(i, h32, h32k):
                t = tl[i]
                hT, hTk = hT32.next()
                for g in range(4):
                    p, pk = ptr.next()

                    def tr(e, p=p, g=g):
                        r = None
                        for j in range(4):
                            kk = g * 4 + j
                            r = e.transpose(out=p[:, j * 128:(j + 1) * 128], in_=h32[:, kk * 128:(kk + 1) * 128],
                                            identity=K.ident32)
                        return r
                    S.op("pe", tr, reads=[h32k], writes=[pk])
                    S.op("act", lambda e, p=p, g=g: e.copy(out=hT[:, g * 4:(g + 1) * 4, :],
                                                           in_=p[:].rearrange("p (j n) -> p j n", j=4)),
                         reads=[pk], writes=[(hTk, g)])
                p, pk = pr.next()

                def mm(e, p=p):
                    r = None
                    for k in range(16):
                        r = e.matmul(p[:], lhsT=hT[:, k, :], rhs=wr[:, k, :], start=(k == 0), stop=(k == 15))
                    return r
                S.op("pe", mm, reads=[(hTk, g) for g in range(4)] + ["wr"], writes=[pk])
                lg, lgk = lgb.next()
                S.op("dve", lambda e: e.tensor_tensor(out=lg[:], in0=p[:], in1=br[:], op=ALU.add), reads=[pk, "brb"], writes=[lgk])
                s, sk = st1.next()
                S.op("dve", lambda e: e.reduce_max(out=s[:, 0:1], in_=lg[:, 0:4], axis=AX.X), reads=[lgk], writes=[sk])
                S.op("dve", lambda e: e.tensor_scalar(out=s[:, 1:2], in0=s[:, 0:1], scalar1=-1.0, scalar2=None, op0=ALU.mult),
                     reads=[sk], writes=[sk])
                gm, gmk = gmb.next()
                S.op("dve", lambda e: e.tensor_scalar(out=gm[:], in0=lg[:, 0:4], scalar1=s[:, 0:1], scalar2=None,
                                                      op0=ALU.is_equal), reads=[lgk, sk], writes=[gmk])
                eg, egk = egb.next()
                S.op("act", lambda e: e.activation(out=eg[:], in_=lg[:, 0:4], func=AF.Exp, bias=s[:, 1:2], accum_out=s[:, 2:3]),
                     reads=[lgk, sk], writes=[egk, sk])
                pn, pnk = pen.next()
                S.op("dve", lambda e: e.tensor_scalar(out=pn[:], in0=gm[:], scalar1=BIG, scalar2=-BIG, op0=ALU.mult, op1=ALU.add),
                     reads=[gmk], writes=[pnk])
                le, lek = lem.next()
                S.op("dve", lambda e: e.tensor_tensor(out=le[:].rearrange("p (g n) -> p g n", g=4),
                                                      in0=lg[:, 4:36].rearrange("p (g n) -> p g n", g=4),
                                                      in1=_bc_last(pn[:].rearrange("p (g o) -> p g o", o=1), 8), op=ALU.add),
                     reads=[lgk, pnk], writes=[lek])
                S.op("dve", lambda e: e.reduce_max(out=s[:, 3:4], in_=le[:], axis=AX.X), reads=[lek, sk], writes=[sk])
                o1 = o1all[:, t, :]
                o2 = o2all[:, t, :]
                S.op("dve", lambda e: e.tensor_scalar(out=o1, in0=le[:], scalar1=s[:, 3:4], scalar2=None, op0=ALU.is_equal),
                     reads=[lek, sk], writes=[("o1", t)])
                l2, l2k = lem2.next()
                S.op("dve", lambda e: e.scalar_tensor_tensor(out=l2[:], in0=o1, scalar=-BIG, in1=le[:], op0=ALU.mult, op1=ALU.add),
                     reads=[("o1", t), lek], writes=[l2k])
                S.op("dve", lambda e: e.reduce_max(out=s[:, 4:5], in_=l2[:], axis=AX.X), reads=[l2k, sk], writes=[sk])
                S.op("dve", lambda e: e.tensor_scalar(out=o2, in0=l2[:], scalar1=s[:, 4:5], scalar2=None, op0=ALU.is_equal),
                     reads=[l2k, sk], writes=[("o2", t)])
                S.op("dve", lambda e: e.tensor_tensor(out=s[:, 5:6], in0=s[:, 4:5], in1=s[:, 3:4], op=ALU.subtract), reads=[sk],
                     writes=[sk])
                S.op("act", lambda e: e.activation(out=s[:, 6:7], in_=s[:, 5:6], func=AF.Exp), reads=[sk], writes=[sk])
                S.op("dve", lambda e: e.scalar_tensor_tensor(out=s[:, 7:8], in0=s[:, 6:7], scalar=1.0, in1=s[:, 2:3], op0=ALU.add,
                                                             op1=ALU.mult), reads=[sk], writes=[sk])
                S.op("dve", lambda e: e.reciprocal(out=aall[:, t, 0:1], in_=s[:, 7:8]), reads=[sk], writes=[("aa", t)])
                S.op("dve", lambda e: e.tensor_tensor(out=aall[:, t, 1:2], in0=s[:, 6:7], in1=aall[:, t, 0:1], op=ALU.mult),
                     reads=[sk, ("aa", t)], writes=[("aa", t)])

            tiles = [(t * D, tile_kind(t)) for t in tl]
            emit_norm(S, nc, K, tiles, lambda i: T.x1[tl[i] * 128:(tl[i] + 1) * 128, :], AB, None, es2, want32=router,
                      htok=lambda i: (h2[:, tl[i], :], ("h2tok", tl[i])))
            S.flush("D1s_a%d" % l)
        oe = sb("oeall", [128, NOWN, 32], BF16)
        allo = [("o1", t) for t in tl] + [("o2", t) for t in tl]
        lo, hi = tl[0], tl[-1] + 1
        S.op("dve", lambda e: e.tensor_tensor(out=oe[:, lo:hi, :], in0=o1all[:, lo:hi, :], in1=o2all[:, lo:hi, :], op=ALU.add),
             writes=["oe"])
        rall = sb("rall", [128, NOWN, 32], F32)
        pR = Ring("pR", [pst("pR%d" % i, [128, 32], F32) for i in range(3)])
        Lbf = K.cbf[:, 8, :]
        for i, t in enumerate(tl):
            p, pk = pR.next()

            def mm(e, p=p, i=i, t=t):
                r = e.matmul(p[:], lhsT=Lbf, rhs=oe[:, t, :], start=True, stop=(i == 0))
                for jj in range(i):
                    r = e.matmul(p[:], lhsT=K.ones_bf, rhs=oe[:, tl[jj], :], start=False, stop=(jj == i - 1))
                return r
            S.op("pe", mm, reads=["oe"], writes=[pk])
            S.op("act", lambda e, p=p, t=t: e.copy(out=rall[:, t, :], in_=p[:]), reads=[pk], writes=[("rall", t)])
        p, pk = pR.next()

        def mmc(e, p=p):
            r = None
            for jj in range(nt):
                r = e.matmul(p[:], lhsT=K.ones_bf, rhs=oe[:, tl[jj], :], start=(jj == 0), stop=(jj == nt - 1))
            return r
        S.op("pe", mmc, reads=["oe"], writes=[pk])
        cnt = sb("cnt", [128, 32], F32)
        S.op("act", lambda e: e.copy(out=cnt[:], in_=p[:]), reads=[pk], writes=["cnt"])
        cmp1 = sb("cmp1", [128, 32, 18], F32)
        thr = K.c32[:, 9, 0:18]
        thr_bc = bass.AP(tensor=thr.tensor, offset=thr.offset, ap=[list(thr.ap[0]), [0, 32], [1, 18]])
        S.op("dve", lambda e: e.tensor_scalar(out=cnt[:], in0=cnt[:], scalar1=128.0 / BLK, scalar2=None, op0=ALU.mult),
             reads=["cnt"], writes=["cnt"])
        S.op("dve", lambda e: e.tensor_tensor(out=cmp1[:], in0=_bc_last(cnt[:].rearrange("p (a o) -> p a o", o=1), 18), in1=thr_bc,
                                              op=ALU.is_gt), reads=["cnt"], writes=["cmp1"])
        nb = sb("nbk", [128, 32], F32)
        S.op("dve", lambda e: e.reduce_sum(out=nb[:], in_=cmp1[:], axis=AX.X), reads=["cmp1"], writes=["nbk"])
        pend = sb("pend", [128, 32], F32)
        S.op("dve", lambda e: e.tensor_tensor_scan(out=pend[:], data0=K.ones32[:, 0:32], data1=nb[:], initial=0.0, op0=ALU.mult,
                                                   op1=ALU.add), reads=["nbk"], writes=["pend"])
        pst128 = sb("pst128", [128, 32], F32)
        S.op("dve", lambda e: e.tensor_tensor(out=pst128[:], in0=pend[:], in1=nb[:], op=ALU.subtract), reads=["pend", "nbk"],
             writes=["pst128"])
        S.op("dve", lambda e: e.tensor_scalar(out=pst128[:], in0=pst128[:], scalar1=float(BLK), scalar2=None, op0=ALU.mult),
             reads=["pst128"], writes=["pst128"])
        slots_f = sb("slotsf", [128, NOWN, 2], F32)
        slots_u = sb("slotsu", [128, NOWN, 2], U32)
        S.op("dve", lambda e: e.memset(slots_f[:], 0.0), writes=[("slf", t) for t in range(NOWN)])
        basr = Ring("basr", [sb("basr%d" % i, [128, 32], F32) for i in range(2)])
        junkr = Ring("sjunk", [sb("sjunk%d" % i, [128, 32], F32) for i in range(2)])
        for t in tl:
            ba, bak = basr.next()
            S.op("dve", lambda e, ba=ba, t=t: e.tensor_tensor(out=ba[:], in0=rall[:, t, :], in1=pst128[:], op=ALU.add),
                 reads=[("rall", t), "pst128"], writes=[bak])
            for kk, oall in enumerate((o1all, o2all)):
                jk, jkk = junkr.next()
                S.op("dve", lambda e, ba=ba, t=t, oall=oall, jk=jk: e.tensor_tensor(out=jk[:], in0=oall[:, t, :], in1=ba[:], op=ALU.mult),
                     reads=[bak], writes=[jkk])
                S.op("dve", lambda e, t=t, kk=kk, jk=jk: e.reduce_sum(out=slots_f[:, t, kk:kk + 1], in_=jk[:], axis=AX.X),
                     reads=[jkk, ("slf", t)], writes=[("slf", t)])
        S.op("dve", lambda e: e.tensor_copy(out=slots_u[:], in_=slots_f[:]), reads=[("slf", t) for t in range(NOWN)],
             writes=["slu"])
        for t in tl:
            for kk in range(2):
                S.dma("pool", lambda e, t=t, kk=kk: e.indirect_dma_start(
                    out=T.xsort[:, :], out_offset=bass.IndirectOffsetOnAxis(ap=slots_u[:, t, kk:kk + 1], axis=0),
                    in_=h2[:, t, :], in_offset=None), ("xsc", (2 * t + kk) % 4), reads=["slu", ("h2tok", t)], writes=[("xsort", t, kk)])
        cmp2 = sb("cmp2", [128, NB, 32], F32)
        jv = K.c32[:, 9, 32:32 + NB]
        S.op("dve", lambda e: e.tensor_tensor(
            out=cmp2[:], in0=bass.AP(tensor=pend[:].tensor, offset=pend[:].offset, ap=[list(pend[:].ap[0]), [0, NB], [1, 32]]),
            in1=_bc_last(jv.rearrange("p (a o) -> p a o", o=1), 32), op=ALU.is_le), reads=["pend"], writes=["cmp2"])
        blk = sb("blk", [128, NB], F32)
        S.op("dve", lambda e: e.reduce_sum(out=blk[:], in_=cmp2[:], axis=AX.X), reads=["cmp2"], writes=["blk"])
        oobf = sb("oobf", [128, NB], F32)
        S.op("dve", lambda e: e.tensor_scalar(out=oobf[:], in0=blk[:], scalar1=float(_NE[0]) - 0.5, scalar2=OOB, op0=ALU.is_gt,
                                              op1=ALU.mult), reads=["blk"], writes=["oobf"])
        S.op("dve", lambda e: e.tensor_scalar(out=blk[:], in0=blk[:], scalar1=float(_NE[0] - 1), scalar2=128.0, op0=ALU.min, op1=ALU.mult),
             reads=["blk"], writes=["blk"])
        S.op("dve", lambda e: e.tensor_tensor(out=blk[:], in0=blk[:], in1=oobf[:], op=ALU.add), reads=["blk", "oobf"], writes=["blk"])
        S.op("dve", lambda e: e.tensor_scalar(out=blk[:], in0=blk[:], scalar1=K.c32[:, 9, 127:128], scalar2=float(l * _NE[0] * 128),
                                              op0=ALU.add, op1=ALU.add), reads=["blk"], writes=["blk"])
        blk4 = sb("blk4", [128, NB, 4], F32)
        for c in range(4):
            S.op("dve", lambda e, c=c: e.tensor_scalar(out=blk4[:, :, c], in0=blk[:], scalar1=float(c * RWv()), scalar2=None,
                                                       op0=ALU.add), reads=["blk"], writes=[("blk4", c)])
        blku = sb("blku", [128, NB, 4], U32)
        S.op("dve", lambda e: e.tensor_copy(out=blku[:], in_=blk4[:]), reads=[("blk4", c) for c in range(4)], writes=["blku"])
        S.dma("sp", lambda e: e.dma_start(out=T.blku[:, 0:NB * 4], in_=blku[:].rearrange("p j c -> p (j c)")), "blkuo",
              reads=["blku"], writes=["blkud"])
        S.dma("sp", lambda e: e.dma_start(out=T.slotsu[:, :], in_=slots_u[:].rearrange("p t k -> p (t k)")), "sluo", reads=["slu"],
              writes=["slud"])
        S.dma("sp", lambda e: e.dma_start(out=T.aall[:, :], in_=aall[:].rearrange("p t k -> p (t k)")), "aao",
              reads=[("aa", t) for t in tl], writes=["aad"])
        S.flush("D1s_b%d" % l)


def phase_D2s(S, nc, K, T, l, with_ctx):
    from contextlib import ExitStack
    NB = nblocks(with_ctx)
    NS = BLK // 128
    t0 = 0 if with_ctx else 2
    tl = list(range(t0, NOWN))
    with ExitStack() as es:
        sb = lambda name, shape, dt: es.enter_context(nc.sbuf_tensor(_u(name), shape, dt))
        pst = lambda name, shape, dt: es.enter_context(nc.psum_tensor(_u(name), shape, dt))
        blku = sb("blkul", [128, NB, 4], U32)
        S.dma("sp", lambda e: e.dma_start(out=blku[:].rearrange("p j c -> p (j c)"), in_=T.blku[:, 0:NB * 4]), "blkul",
              writes=["blkul"])
        w1r = Ring("sw1", [sb("sw1_%d" % i, [128, 16, 512], BF16) for i in range(2)])
        w3r = Ring("sw3", [sb("sw3_%d" % i, [128, 16, 512], BF16) for i in range(2)])
        w2r = Ring("sw2", [sb("sw2_%d" % i, [128, 4, D], BF16) for i in range(3)])
        for r_ in (w1r, w3r, w2r):
            for i in range(len(r_.bufs)):
                S.op("dve", lambda e, r_=r_, i=i: e.memset(r_.bufs[i][:], 0.0), writes=[(r_.name, i)])
        xtr = Ring("sxt", [sb("sxt%d" % i, [128, NS, D], BF16) for i in range(2)])
        xTr = Ring("sxT", [sb("sxT%d" % i, [128, 16, BLK], BF16) for i in range(2)])
        sgr = Ring("ssg", [sb("ssg%d" % i, [128, 4 * BLK], F32) for i in range(2)])
        aTr = Ring("saT", [sb("saT%d" % i, [128, 4, BLK], BF16) for i in range(3)])
        ysr = Ring("sys", [sb("sys%d" % i, [128, D], F32) for i in range(3)])
        pT = Ring("spT", [pst("spT%d" % i, [128, 1024], BF16) for i in range(2)])
        pg = Ring("spg", [pst("spg%d" % i, [128, 2 * BLK], F32) for i in range(2)])
        pu = Ring("spu", [pst("spu%d" % i, [128, 2 * BLK], F32) for i in range(2)])
        po = Ring("spo", [pst("spo%d" % i, [128, 512], F32) for i in range(2)])
        breg = {}
        HB = 2 * BLK
        state = {}

        def stage_a(j):
            w1, w1k = w1r.next()
            w3, w3k = w3r.next()
            w2, w2k = w2r.next()
            for (w, wk, src, nm) in ((w1, w1k, T.w1m, "sw1"), (w3, w3k, T.w3m, "sw3"), (w2, w2k, T.w2m, "sw2")):
                wf = w[:].rearrange("p k n -> p (k n)")

                def ld(e, wf=wf, src=src, j=j):
                    r = []
                    if "reg" not in breg:
                        breg["reg"] = e.to_reg(0 if os.environ.get("MOE_NOLOAD") else 4 * RWv() - 1)
                    for c in range(4):
                        r.append(e.indirect_dma_start(out=wf[:, c * 2048:(c + 1) * 2048], out_offset=None, in_=src[:, :],
                                                      in_offset=bass.IndirectOffsetOnAxis(ap=blku[:, j, c:c + 1], axis=0),
                                                      bounds_check=breg["reg"], oob_is_err=False))
                    return r
                S.dma("pool", ld, (nm, wk[1]), reads=["blkul"], writes=[wk], n=4)
            xt, xtk = xtr.next()
            S.dma("sp", lambda e, xt=xt, j=j: e.dma_start(out=xt[:], in_=T.xsort[j * BLK:(j + 1) * BLK, :].rearrange(
                "(s p) n -> p s n", p=128)), ("sxt", xtk[1]), writes=[xtk])
            xT, xTk = xTr.next()
            for si in range(NS):
                for half in range(2):
                    p, pk = pT.next()

                    def tr(e, p=p, xt=xt, half=half, si=si):
                        r = None
                        for k in range(8):
                            kk = half * 8 + k
                            r = e.transpose(out=p[:, k * 128:(k + 1) * 128], in_=xt[:, si, kk * 128:(kk + 1) * 128],
                                            identity=K.ident_bf)
                        return r
                    S.op("pe", tr, reads=[xtk], writes=[pk])
                    dst = xT[:, half * 8:(half + 1) * 8, si * 128:(si + 1) * 128]
                    if half == 0:
                        S.op("act", lambda e, p=p, dst=dst: e.copy(out=dst, in_=p[:].rearrange("p (k n) -> p k n", k=8)),
                             reads=[pk], writes=[(xTk, si, 0)])
                    else:
                        S.op("dve", lambda e, p=p, dst=dst: e.tensor_copy(out=dst, in_=p[:].rearrange("p (k n) -> p k n", k=8)),
                             reads=[pk], writes=[(xTk, si, 1)])
            xTkeys = [(xTk, si, hh) for si in range(NS) for hh in range(2)]
            aT, aTk = aTr.next()
            for hf in range(2):
                p1, p1k = pg.next()
                p3, p3k = pu.next()

                def mg(e, p=p1, w=w1, xT=xT, hf=hf):
                    r = None
                    for jj in range(2):
                        jd = hf * 2 + jj
                        for k in range(16):
                            r = e.matmul(p[:, jj * BLK:(jj + 1) * BLK], lhsT=w[:, k, jd * 128:(jd + 1) * 128], rhs=xT[:, k, :],
                                         start=(k == 0), stop=(k == 15))
                    return r
                S.op("pe", mg, reads=[w1k] + xTkeys, writes=[p1k])

                def mu(e, p=p3, w=w3, xT=xT, hf=hf):
                    r = None
                    for jj in range(2):
                        jd = hf * 2 + jj
                        for k in range(16):
                            r = e.matmul(p[:, jj * BLK:(jj + 1) * BLK], lhsT=w[:, k, jd * 128:(jd + 1) * 128], rhs=xT[:, k, :],
                                         start=(k == 0), stop=(k == 15))
                    return r
                S.op("pe", mu, reads=[w3k] + xTkeys, writes=[p3k])
                sg, sgk = sgr.next()
                S.op("act", lambda e, sg=sg, p1=p1: e.activation(out=sg[:, 0:HB], in_=p1[:], func=AF.Silu), reads=[p1k], writes=[sgk])
                S.op("dve", lambda e, aT=aT, sg=sg, p3=p3, hf=hf: e.tensor_tensor(
                    out=aT[:, hf * 2:hf * 2 + 2, :].rearrange("p j n -> p (j n)"), in0=p3[:], in1=sg[:, 0:HB], op=ALU.mult),
                    reads=[p3k, sgk], writes=[(aTk, hf)])
            state[j] = (aT, aTk, w2, w2k)

        def stage_b(j):
            aT, aTk, w2, w2k = state.pop(j)
            for si in range(NS):
                ys, ysk = ysr.next()
                for nn in range(4):
                    p, pk = po.next()

                    def mo(e, p=p, aT=aT, w2=w2, nn=nn, si=si):
                        r = None
                        for jd in range(4):
                            r = e.matmul(p[:], lhsT=aT[:, jd, si * 128:(si + 1) * 128], rhs=w2[:, jd, nn * 512:(nn + 1) * 512],
                                         start=(jd == 0), stop=(jd == 3))
                        return r
                    S.op("pe", mo, reads=[(aTk, 0), (aTk, 1), w2k], writes=[pk])
                    if nn % 2 == 0:
                        S.op("act", lambda e, p=p, ys=ys, nn=nn: e.copy(out=ys[:, nn * 512:(nn + 1) * 512], in_=p[:]), reads=[pk],
                             writes=[(ysk, nn)])
                    else:
                        S.op("dve", lambda e, p=p, ys=ys, nn=nn: e.tensor_copy(out=ys[:, nn * 512:(nn + 1) * 512], in_=p[:]),
                             reads=[pk], writes=[(ysk, nn)])
                r0 = j * BLK + si * 128
                S.dma("sp", lambda e, ys=ys, r0=r0: e.dma_start(out=T.ysd[r0:r0 + 128, :], in_=ys[:]), ("syo", ysk[1]),
                      reads=[(ysk, nn) for nn in range(4)], writes=[("ysd", j, si)])

        for j in range(NB + 1):
            if j < NB:
                stage_a(j)
            if j >= 1:
                stage_b(j - 1)
        S.flush("D2s_%d" % l)


def prep_experts_sparse(wg, wu, wd):
    ne = wg.shape[1]
    rw = DEPTH * ne * 128
    w1 = np.ascontiguousarray(wg.reshape(DEPTH * ne, 4, 4, 128, 512).transpose(1, 0, 3, 2, 4)).reshape(4 * rw, 2048)
    w3 = np.ascontiguousarray(wu.reshape(DEPTH * ne, 4, 4, 128, 512).transpose(1, 0, 3, 2, 4)).reshape(4 * rw, 2048)
    w2 = np.ascontiguousarray(wd.reshape(DEPTH * ne, 4, 128, D).transpose(1, 0, 2, 3)).reshape(4 * rw, 2048)
    return w1, w3, w2
```
